# Optimizing a Trainium2 kernel written in Bass

```python
import jax, jax.numpy as jnp
from jax import lax
import numpy as np

D_MODEL = 1024
BATCH = 8
SEQ = 4096
DEPTH = 1

N_HEADS = 8
HEAD_DIM = 128
ATTN_WIDTH = N_HEADS * HEAD_DIM
ROPE_THETA = 10000.0
IDX_HEADS = 8
IDX_DIM = 64
IDX_TOPK_MAX = 256
Q_BLOCK = 32
SGU_GROUPS = 8
SGU_WIDTH = 1024
SGU_GROUP_DIM = SGU_WIDTH // SGU_GROUPS
CHUNK = 128
N_GROUPS = 4
EXPERTS_PER_GROUP = 8
N_EXPERTS = N_GROUPS * EXPERTS_PER_GROUP
TOP_K_INNER = 2
D_EXPERT = 256
NORM_EPS = 1e-6

SPLIT_SIZES = (ATTN_WIDTH, ATTN_WIDTH, ATTN_WIDTH,
               IDX_HEADS * IDX_DIM, IDX_DIM, IDX_HEADS,
               SGU_WIDTH, SGU_WIDTH,
               D_MODEL, D_MODEL)
D_IN_PROJ = sum(SPLIT_SIZES)

kernel_name = "hybrid_gmlp_dsa_hmoe_block"


def rms_norm(x, g):
    xf = x.astype(jnp.float32)
    xf = xf * lax.rsqrt(jnp.mean(xf * xf, axis=-1, keepdims=True) + NORM_EPS)
    return (xf * g.astype(jnp.float32)).astype(x.dtype)


def layer_norm(x, g):
    xf = x.astype(jnp.float32)
    mu = jnp.mean(xf, axis=-1, keepdims=True)
    var = jnp.mean(jnp.square(xf - mu), axis=-1, keepdims=True)
    return ((xf - mu) * lax.rsqrt(var + NORM_EPS) * g.astype(jnp.float32)).astype(x.dtype)


def rope(x, positions):
    d = x.shape[-1]
    inv = ROPE_THETA ** (-jnp.arange(0, d, 2, dtype=jnp.float32) / d)
    ang = positions.astype(jnp.float32)[..., None] * inv
    cos = jnp.cos(ang)[:, :, None, :]
    sin = jnp.sin(ang)[:, :, None, :]
    x1, x2 = jnp.split(x.astype(jnp.float32), 2, axis=-1)
    out = jnp.concatenate([x1 * cos - x2 * sin, x2 * cos + x1 * sin], axis=-1)
    return out.astype(x.dtype)


def split_columns(proj):
    parts, start = [], 0
    for size in SPLIT_SIZES:
        parts.append(proj[..., start:start + size])
        start += size
    return parts


def dsa_attention(q, k, v, qi, ki, wi):
    B, L = q.shape[0], q.shape[1]
    top_k = min(IDX_TOPK_MAX, L // 4)
    nb = L // Q_BLOCK

    def to_blocks(a):
        return jnp.moveaxis(a.reshape(B, nb, Q_BLOCK, *a.shape[2:]), 1, 0)

    starts = jnp.arange(nb, dtype=jnp.int32) * Q_BLOCK
    key_pos = jnp.arange(L, dtype=jnp.int32)
    gather = jax.vmap(lambda src, idx: src[idx])

    def block(args):
        qb, qib, wb, start = args
        qpos = start + jnp.arange(Q_BLOCK, dtype=jnp.int32)
        rel = jax.nn.relu(jnp.einsum('bqhd,bsd->bqhs', qib, ki))
        score = jnp.einsum('bqh,bqhs->bqs', wb, rel).astype(jnp.float32)
        causal = key_pos[None, :] <= qpos[:, None]
        score = jnp.where(causal[None], score, -jnp.inf)
        _, sel = lax.top_k(score, top_k)
        valid = sel <= qpos[None, :, None]
        kg = gather(k, sel)
        vg = gather(v, sel)
        logits = jnp.einsum('bqhd,bqkhd->bhqk', qb, kg).astype(jnp.float32) * (HEAD_DIM ** -0.5)
        logits = jnp.where(valid[:, None], logits, -jnp.inf)
        p = jax.nn.softmax(logits, axis=-1).astype(v.dtype)
        return jnp.einsum('bhqk,bqkhd->bqhd', p, vg)

    out = lax.map(block, (to_blocks(q), to_blocks(qi), to_blocks(wi), starts))
    return jnp.moveaxis(out, 0, 1).reshape(B, L, ATTN_WIDTH)


def spatial_gating(u, v, norm_g, w_s, b_s):
    B, L, _ = u.shape
    nc = L // CHUNK
    v = layer_norm(v, norm_g)
    vc = v.reshape(B, nc, CHUNK, SGU_GROUPS, SGU_GROUP_DIM)
    mask = jnp.tril(jnp.ones((CHUNK, CHUNK), dtype=bool))
    w = jnp.where(mask[None], w_s, jnp.zeros_like(w_s))
    mixed = jnp.einsum('gts,bcsgd->bctgd', w, vc) + b_s.T[None, None, :, :, None]
    return u * mixed.reshape(B, L, SGU_WIDTH)


def hier_moe(xn, w_group, b_group, w_er, b_er, w_e_in, w_e_out):
    B, L, D = xn.shape
    T = B * L
    xt = xn.reshape(T, D)
    g_logits = (xt @ w_group).astype(jnp.float32) + b_group.astype(jnp.float32)
    g_sel = jnp.argmax(g_logits, axis=-1)
    g_prob = jnp.take_along_axis(jax.nn.softmax(g_logits, axis=-1), g_sel[:, None], axis=1)
    e_logits = ((xt @ w_er).astype(jnp.float32) + b_er.astype(jnp.float32)).reshape(T, N_GROUPS, EXPERTS_PER_GROUP)
    e_in_group = jnp.take_along_axis(e_logits, g_sel[:, None, None], axis=1)[:, 0]
    top_v, top_i = lax.top_k(e_in_group, TOP_K_INNER)
    gates = g_prob * jax.nn.softmax(top_v, axis=-1)
    expert_id = (g_sel[:, None] * EXPERTS_PER_GROUP + top_i).reshape(-1)
    token_id = jnp.repeat(jnp.arange(T, dtype=jnp.int32), TOP_K_INNER)
    order = jnp.argsort(expert_id)
    tok_sorted = token_id[order]
    group_sizes = jnp.bincount(expert_id, length=N_EXPERTS).astype(jnp.int32)
    xs = xt[tok_sorted]
    hdn = lax.ragged_dot(xs, w_e_in, group_sizes)
    gate_h, up_h = jnp.split(hdn, 2, axis=-1)
    act = jax.nn.silu(gate_h) * up_h
    ys = lax.ragged_dot(act, w_e_out, group_sizes)
    ys = ys * gates.reshape(-1)[order][:, None].astype(ys.dtype)
    return jnp.zeros_like(xt).at[tok_sorted].add(ys).reshape(B, L, D)


def setup_inputs(seed: int = 0) -> dict:
    key = jax.random.key(seed)
    ks = jax.random.split(key, 20)
    f32 = jnp.float32
    nrm = lambda k, shape, scale: jax.random.normal(k, shape, f32) * scale
    x = jax.random.normal(ks[0], (BATCH, SEQ, D_MODEL), f32)
    offset = jax.random.randint(ks[1], (BATCH, 1), 0, 1024, dtype=jnp.int32)
    positions = offset + jnp.arange(SEQ, dtype=jnp.int32)[None, :]
    return {
        "x": x,
        "positions": positions,
        "norm1_g": 1.0 + nrm(ks[2], (DEPTH, D_MODEL), 0.02),
        "w_in": nrm(ks[3], (DEPTH, D_MODEL, D_IN_PROJ), D_MODEL ** -0.5),
        "sgu_norm_g": 1.0 + nrm(ks[4], (DEPTH, SGU_WIDTH), 0.02),
        "sgu_w": nrm(ks[5], (DEPTH, SGU_GROUPS, CHUNK, CHUNK), CHUNK ** -0.5),
        "sgu_b": 1.0 + nrm(ks[6], (DEPTH, SGU_GROUPS, CHUNK), 0.1),
        "w_branch_a": nrm(ks[7], (DEPTH, SGU_WIDTH, D_MODEL), SGU_WIDTH ** -0.5),
        "w_branch_b": nrm(ks[8], (DEPTH, ATTN_WIDTH, D_MODEL), ATTN_WIDTH ** -0.5),
        "w_out": nrm(ks[9], (DEPTH, D_MODEL, D_MODEL), D_MODEL ** -0.5),
        "norm2_g": 1.0 + nrm(ks[10], (DEPTH, D_MODEL), 0.02),
        "w_router_group": nrm(ks[11], (DEPTH, D_MODEL, N_GROUPS), D_MODEL ** -0.5),
        "b_router_group": nrm(ks[12], (DEPTH, N_GROUPS), 0.01),
        "w_router_expert": nrm(ks[13], (DEPTH, D_MODEL, N_EXPERTS), D_MODEL ** -0.5),
        "b_router_expert": nrm(ks[14], (DEPTH, N_EXPERTS), 0.01),
        "w_expert_in": nrm(ks[15], (DEPTH, N_EXPERTS, D_MODEL, 2 * D_EXPERT), D_MODEL ** -0.5),
        "w_expert_out": nrm(ks[16], (DEPTH, N_EXPERTS, D_EXPERT, D_MODEL), D_EXPERT ** -0.5),
        "norm_f_g": 1.0 + nrm(ks[17], (D_MODEL,), 0.02),
    }


def reference(x, positions, norm1_g, w_in, sgu_norm_g, sgu_w, sgu_b, w_branch_a, w_branch_b,
              w_out, norm2_g, w_router_group, b_router_group, w_router_expert, b_router_expert,
              w_expert_in, w_expert_out, norm_f_g):
    B, L, _ = x.shape
    h = x
    for l in range(DEPTH):
        xn = rms_norm(h, norm1_g[l])
        proj = xn @ w_in[l]
        q, k, v, qi, ki, wi, su, sv, ga, gb = split_columns(proj)
        q = rope(q.reshape(B, L, N_HEADS, HEAD_DIM), positions)
        k = rope(k.reshape(B, L, N_HEADS, HEAD_DIM), positions)
        v = v.reshape(B, L, N_HEADS, HEAD_DIM)
        qi = rope(qi.reshape(B, L, IDX_HEADS, IDX_DIM), positions)
        ki = rope(ki[:, :, None, :], positions)[:, :, 0]
        y_attn = dsa_attention(q, k, v, qi, ki, wi)
        y_sgu = spatial_gating(jax.nn.gelu(su), jax.nn.gelu(sv), sgu_norm_g[l], sgu_w[l], sgu_b[l])
        merged = (jax.nn.sigmoid(ga) * (y_sgu @ w_branch_a[l])
                  + jax.nn.sigmoid(gb) * (y_attn @ w_branch_b[l]))
        h = h + merged @ w_out[l]
        h = h + hier_moe(rms_norm(h, norm2_g[l]), w_router_group[l], b_router_group[l],
                         w_router_expert[l], b_router_expert[l], w_expert_in[l], w_expert_out[l])
    return rms_norm(h, norm_f_g)
```

```python
from contextlib import ExitStack
import numpy as np
import concourse.bass as bass
import concourse.mybir as mybir

F32 = mybir.dt.float32
BF16 = mybir.dt.bfloat16
I32 = mybir.dt.int32
U32 = mybir.dt.uint32
AF = mybir.ActivationFunctionType
ALU = mybir.AluOpType
AX = mybir.AxisListType


class Tok:
    __slots__ = ("w", "r", "name")

    def __init__(self, name=""):
        self.w = None
        self.r = []
        self.name = name


class Op:
    __slots__ = ("eng", "fn", "deps", "dma", "sig", "sem", "val", "gidx", "slotwait")

    def __init__(self, eng, fn, dma, gidx):
        self.eng = eng
        self.fn = fn
        self.dma = dma
        self.deps = []
        self.sig = False
        self.sem = None
        self.val = 0
        self.gidx = gidx
        self.slotwait = None


class Kern:
    ENGS = ("pe", "act", "dve", "pool", "sp")
    NSLOT = {"sp": 20, "act": 6, "pool": 12}
    ROLL = 30000

    def __init__(self, nc):
        self.nc = nc
        self.ops = {e: [] for e in self.ENGS}
        self.n = 0
        self.toks = []
        self.es = ExitStack()
        self.nsem = 0

    def tok(self, name=""):
        t = Tok(name)
        self.toks.append(t)
        return t

    def toks_n(self, n, name=""):
        return [self.tok(f"{name}{i}") for i in range(n)]

    def op(self, eng, fn, r=(), w=(), dma=False):
        o = Op(eng, fn, dma, self.n)
        self.n += 1
        deps = {}
        for t in r:
            if t.w is not None:
                deps[id(t.w)] = t.w
        for t in w:
            if t.w is not None:
                deps[id(t.w)] = t.w
            for q in t.r:
                deps[id(q)] = q
        for d in deps.values():
            if d is o:
                continue
            if d.eng == eng and not d.dma and not dma:
                if eng == "pe":
                    continue
            o.deps.append(d)
        for t in r:
            t.r.append(o)
        for t in w:
            t.w = o
            t.r = []
        self.ops[eng].append(o)
        return o

    def barrier(self):
        deps = {}
        for t in self.toks:
            if t.w is not None:
                deps[id(t.w)] = t.w
            for q in t.r:
                deps[id(q)] = q
        dl = list(deps.values())
        for e in self.ENGS:
            o = Op(e, None, False, self.n)
            self.n += 1
            o.deps = [d for d in dl if d.fn is not None]
            self.ops[e].append(o)
        for t in self.toks:
            t.r = []
            t.w = None

    def _newsem(self, name):
        self.nsem += 1
        return self.es.enter_context(self.nc.semaphore(f"{name}_{self.nsem}"))

    def emit(self):
        nc = self.nc
        for e in self.ENGS:
            for o in self.ops[e]:
                for d in o.deps:
                    d.sig = True
        for e in self.ENGS:
            cur = None
            cnt = 0
            slots = None
            slot_uses = None
            slot_last = None
            k = 0
            for o in self.ops[e]:
                if o.fn is None:
                    continue
                if o.dma:
                    if slots is None:
                        ns = self.NSLOT[e]
                        slots = [self._newsem(f"d{e}") for _ in range(ns)]
                        slot_uses = [0] * ns
                        slot_last = [None] * ns
                    s = k % len(slots)
                    k += 1
                    o.slotwait = slot_last[s]
                    slot_uses[s] += 1
                    o.sem = slots[s]
                    o.val = 16 * slot_uses[s]
                    o.sig = True
                    slot_last[s] = o
                elif o.sig:
                    if cur is None or cnt >= self.ROLL:
                        cur = self._newsem(f"c{e}")
                        cnt = 0
                    cnt += 1
                    o.sem = cur
                    o.val = cnt
        with nc.Block() as block:
            def run(e, eng):
                waited = {}
                for o in self.ops[e]:
                    need = {}
                    dl = list(o.deps)
                    if o.slotwait is not None:
                        dl.append(o.slotwait)
                    for d in dl:
                        key = id(d.sem)
                        if waited.get(key, 0) >= d.val:
                            continue
                        if key not in need or need[key][1] < d.val:
                            need[key] = (d.sem, d.val)
                    for key, (sem, val) in need.items():
                        eng.wait_ge(sem, val)
                        waited[key] = val
                    if o.fn is None:
                        continue
                    ins = o.fn(eng)
                    if o.sig:
                        ins.then_inc(o.sem, 16 if o.dma else 1)

            @block.tensor
            def _(eng):
                run("pe", eng)

            @block.scalar
            def _(eng):
                run("act", eng)

            @block.vector
            def _(eng):
                run("dve", eng)

            @block.gpsimd
            def _(eng):
                run("pool", eng)

            @block.sync
            def _(eng):
                run("sp", eng)
        self.es.close()


U8 = mybir.dt.uint8
DTSZ = {F32: 4, BF16: 2, I32: 4, U32: 4}


class Arena:
    def __init__(self, nc, nbytes):
        self.t = nc.alloc_sbuf_tensor("arena", [128, nbytes], U8)
        self.n = nbytes
        self.off = 0
        self.peak = 0

    def alloc(self, shape, dt):
        n = int(np.prod(shape)) * DTSZ[dt]
        n = (n + 63) // 64 * 64
        assert self.off + n <= self.n, f"arena overflow {self.off}+{n}>{self.n}"
        v = self.t[:, self.off:self.off + n].bitcast(dt)
        self.off += n
        self.peak = max(self.peak, self.off)
        tot = int(np.prod(shape))
        v = v[:, 0:tot]
        if len(shape) == 2:
            v = v.rearrange("p (a b) -> p a b", a=shape[0])
        elif len(shape) == 3:
            v = v.rearrange("p (a b c) -> p a b c", a=shape[0], b=shape[1])
        return v

    def mark(self):
        return self.off

    def release(self, m):
        self.off = m

from concourse.bass_utils import run_bass_kernel_spmd

L = 4096
D = 1024
NT = 32
NCH = 8
DIN = 7752
CQ, CK, CV, CQI, CKI, CSU, CSV, CGA, CGB = 0, 1024, 2048, 3072, 3584, 3656, 4680, 5704, 6728
NE = 32
CAP = 512
DUMMY = NE * CAP
KI = 20
EPS = 1e-6
PI = float(np.pi)
MAGIC = 12582912.0
C1 = 6.28125
C2 = 2 * PI - C1
NEG = -1.0e30
ARENA = 196 * 1024

C_INV, C_SGN, C_INVI, C_NH, C_HPI, C_NTHR, C_EPS, C_ONE, C_DUM, C_PW = 0, 1, 2, 34, 35, 36, 37, 38, 39, 40
NCST = 72


def host_consts():
    c = np.zeros((128, NCST), np.float32)
    inv128 = (np.float32(10000.0) ** (-np.arange(0, 128, 2, dtype=np.float32) / np.float32(128))).astype(np.float32)
    inv64 = (np.float32(10000.0) ** (-np.arange(0, 64, 2, dtype=np.float32) / np.float32(64))).astype(np.float32)
    p = np.arange(128)
    c[:, C_INV] = inv128[p % 64]
    c[:, C_SGN] = np.where(p < 64, -1.0, 1.0)
    c[:, C_INVI:C_INVI + 32] = inv64[None, :]
    c[:, C_NH] = -0.5
    c[:, C_HPI] = PI / 2
    c[:, C_NTHR] = -1.0e29
    c[:, C_EPS] = EPS
    c[:, C_ONE] = 1.0
    c[:, C_DUM] = NE * CAP + p
    c[:, C_PW:C_PW + KI + 2] = (2.0 ** -(np.arange(KI + 2) + 1.0))[None, :]
    m = np.zeros((128, 128 * 3 + 64), np.float32)
    t = np.arange(128)[:, None]
    s = np.arange(128)[None, :]
    m[:, 0:128] = np.where(s <= t, 0.0, NEG)
    m[:, 128:256] = np.where(s >= t, 1.0, 0.0)
    m[:, 256:384] = np.where(s <= t, 1.0, 0.0)
    m[:, 384:416] = (np.arange(32) * CAP)[None, :]
    m[:, 416:448] = 1.0
    return c, m


class Prog:
    pass


def build_program(stop_after=None, dbg=False):
    nc = bass.Bass("TRN2", target_bir_lowering=False)
    K = Kern(nc)
    AR = Arena(nc, ARENA)
    P = Prog()
    P.nc = nc

    def din(name, shape, dt=F32):
        return nc.dram_tensor(name, shape, dt, kind="ExternalInput").ap()

    def dscr(name, shape, dt, out=False):
        return nc.dram_tensor(name, shape, dt, kind="ExternalOutput" if (out and dbg) else "Internal").ap()

    x_d = din("x", [L, D])
    posr_d = din("pos_row", [1, L], I32)
    posc_d = din("pos_col", [128, NT], I32)
    g1_d = din("norm1_g", [1, D])
    win_d = din("w_in", [D, DIN])
    gs_d = din("sgu_norm_g", [1, D])
    sw_d = din("sgu_w", [8, 128, 128])
    sb_d = din("sgu_b", [8, 128])
    wa_d = din("w_branch_a", [D, D])
    wb_d = din("w_branch_b", [D, D])
    wo_d = din("w_out", [D, D])
    g2_d = din("norm2_g", [1, D])
    wrg_d = din("w_router_group", [D, 4])
    brg_d = din("b_router_group", [1, 4])
    wre_d = din("w_router_expert", [D, 32])
    bre_d = din("b_router_expert", [1, 32])
    wei_d = din("w_expert_in", [NE, D, 512])
    weo_d = din("w_expert_out", [NE, 256, D])
    gf_d = din("norm_f_g", [1, D])
    cst_d = din("cst", [128, NCST])
    cm_d = din("cmat", [128, 448])
    out_d = nc.dram_tensor("out", [L, D], F32, kind="ExternalOutput").ap()

    qT_s = dscr("qT_s", [8, 128, L], BF16, True)
    kT_s = dscr("kT_s", [8, 128, L], BF16, True)
    v_s = dscr("v_s", [NT, 128, 8 * 129], BF16, True)
    qiT_s = dscr("qiT_s", [5, 128, L], BF16, True)
    suT_s = dscr("suT_s", [D, L], BF16)
    ysgT_s = dscr("ysgT_s", [D, L], BF16, True)
    sgT_s = dscr("sgT_s", [2 * D, L], BF16, True)
    yatT_s = dscr("yatT_s", [D, L], BF16, True)
    h2_s = dscr("h2_s", [L, D], F32, True)
    xs_s = dscr("xs_s", [NE * CAP + 128, D], BF16)
    ys_s = dscr("ys_s", [NE * CAP + 128, D], F32)

    ps = [nc.alloc_psum_tensor(f"ps{i}", [128, 512], F32) for i in range(8)]
    pst = [K.tok(f"ps{i}") for i in range(8)]

    def psb(i):
        return ps[i][:, :].bitcast(BF16)

    def dma(eng, out, in_, r=(), w=()):
        return K.op(eng, lambda e: e.dma_start(out=out, in_=in_), r=r, w=w, dma=True)

    def mm(out, lhsT, rhs, start, stop, r=(), w=(), sgc=False):
        return K.op("pe", lambda e: e.matmul(out, lhsT=lhsT, rhs=rhs, start=start, stop=stop, skip_group_check=sgc), r=r, w=w)

    def tr(out, in_, ident, r=(), w=()):
        return K.op("pe", lambda e: e.transpose(out, in_, ident), r=r, w=w)

    def act(out, in_, func, r=(), w=(), bias=None, scale=1.0, accum=None):
        def f(e):
            kw = {}
            if bias is not None:
                kw["bias"] = bias
            if accum is not None:
                kw["accum_out"] = accum
            return e.activation(out=out, in_=in_, func=func, scale=scale, **kw)
        return K.op("act", f, r=r, w=w)

    def ts(eng, out, in0, s1, op0, s2=None, op1=None, r=(), w=(), accum=None):
        def f(e):
            kw = {}
            if op1 is not None:
                kw["op1"] = op1
            if accum is not None:
                kw["accum_out"] = accum
            return e.tensor_scalar(out=out, in0=in0, scalar1=s1, scalar2=s2, op0=op0, **kw)
        return K.op(eng, f, r=r, w=w)

    def tt(eng, out, in0, in1, op, r=(), w=()):
        return K.op(eng, lambda e: e.tensor_tensor(out=out, in0=in0, in1=in1, op=op), r=r, w=w)

    def stt(out, in0, scalar, in1, op0, op1, r=(), w=(), accum=None):
        def f(e):
            kw = {}
            if accum is not None:
                kw["accum_out"] = accum
            return e.scalar_tensor_tensor(out=out, in0=in0, scalar=scalar, in1=in1, op0=op0, op1=op1, **kw)
        return K.op("dve", f, r=r, w=w)

    def cp(eng, out, in_, r=(), w=()):
        if eng == "act":
            return K.op("act", lambda e: e.activation(out=out, in_=in_, func=AF.Copy), r=r, w=w)
        return K.op(eng, lambda e: e.tensor_copy(out, in_), r=r, w=w)

    def mset(eng, out, val, r=(), w=()):
        return K.op(eng, lambda e: e.memset(out, val), r=r, w=w)

    class B:
        def __init__(self, shape, dt, name=""):
            self.ap = AR.alloc(shape, dt)
            self.t = K.tok(name)

    def ring(n, shape, dt, name=""):
        return [B(shape, dt, f"{name}{i}") for i in range(n)]

    cst = B([NCST], F32, "cst")
    cm = B([448], F32, "cm")
    ident = B([128], BF16, "ident")
    identf = B([128], F32, "identf")
    wi_sb = B([NT, 8], F32, "wi")
    rt_gate = B([NT * 2], F32, "gate")
    rt_pos = B([NT * 2], I32, "pos")
    dma("sp", cst.ap, cst_d, w=[cst.t])
    dma("sp", cm.ap, cm_d, w=[cm.t])
    mset("pool", identf.ap, 0.0, w=[identf.t])
    K.op("pool", lambda e: e.affine_select(out=identf.ap, in_=identf.ap, pattern=[[-1, 128]], compare_op=ALU.not_equal,
                                           fill=1.0, base=0, channel_multiplier=1), r=[identf.t], w=[identf.t])
    cp("pool", ident.ap, identf.ap, r=[identf.t], w=[ident.t])

    zt = B([D], BF16, "zt")
    mset("pool", zt.ap, 0.0, w=[zt.t])
    nrow_t = (NE * CAP + 128) // 128
    for z0 in range(0, nrow_t, 16):
        zn = min(16, nrow_t - z0)
        dma("act", xs_s[z0 * 128:(z0 + zn) * 128, :].rearrange("(a p) d -> p a d", p=128),
            zt.ap.unsqueeze(1).broadcast_to([128, zn, D]), r=[zt.t])

    def col(b, j, n=1):
        return b.ap[:, j:j + n]

    def sincos(ang, n, kk, sin_out, cos_out, r, w):
        tk = K.tok()
        ts("dve", kk, ang, 1.0 / (2 * PI), ALU.mult, MAGIC, ALU.add, r=r, w=[tk])
        ts("dve", kk, kk, MAGIC, ALU.subtract, r=[tk], w=[tk])
        stt(ang, kk, -C1, ang, ALU.mult, ALU.add, r=r + [tk], w=r)
        stt(ang, kk, -C2, ang, ALU.mult, ALU.add, r=r + [tk], w=r)
        ts("dve", ang, ang, PI, ALU.min, -PI, ALU.max, r=r, w=r)
        act(sin_out, ang, AF.Sin, r=r, w=w)
        ts("dve", kk, ang, -1.0, ALU.mult, r=r, w=[tk])
        tt("dve", kk, kk, ang, ALU.max, r=r + [tk], w=[tk])
        act(cos_out, kk, AF.Sin, bias=col(cst, C_HPI), scale=-1.0, r=[tk, cst.t], w=w)


    def finish():
        K.barrier()
        K.emit()
        P.__dict__.update(dict(K=K, AR=AR))
        return P

    m_0 = AR.mark()
    xnT = AR.alloc([8, L], BF16)
    xnT_t = [K.tok(f"xnT{i}") for i in range(NT)]
    m_p1 = AR.mark()

    xbuf = ring(2, [D], F32, "xb")
    junk = B([D], F32, "junk")
    g1b = B([D], F32, "g1b")
    xnb = ring(2, [D], BF16, "xnb")
    ms1 = B([NT], F32, "ms1")
    rs1 = B([NT], F32, "rs1")
    dma("sp", g1b.ap, g1_d.partition_broadcast(128), w=[g1b.t])
    for i in range(NT):
        xb = xbuf[i % 2]
        nb = xnb[i % 2]
        dma("sp", xb.ap, x_d[i * 128:(i + 1) * 128, :], w=[xb.t])
        stt(junk.ap, xb.ap, 1.0 / D, xb.ap, ALU.mult, ALU.mult, r=[xb.t], w=[junk.t, ms1.t], accum=ms1.ap[:, i:i + 1])
        ts("pool", rs1.ap[:, i:i + 1], ms1.ap[:, i:i + 1], EPS, ALU.add, r=[ms1.t], w=[rs1.t])
        tt("pool", rs1.ap[:, i:i + 1], rs1.ap[:, i:i + 1], col(cst, C_NH), ALU.pow, r=[rs1.t, cst.t], w=[rs1.t])
        stt(nb.ap, xb.ap, rs1.ap[:, i:i + 1], g1b.ap, ALU.mult, ALU.mult, r=[xb.t, rs1.t, g1b.t], w=[nb.t])
        bk = 4 + (i % 2)
        for kc in range(8):
            tr(psb(bk)[:, kc * 128:(kc + 1) * 128], nb.ap[:, kc * 128:(kc + 1) * 128], ident.ap, r=[nb.t, ident.t], w=[pst[bk]])
        cp("act", xnT[:, :, i * 128:(i + 1) * 128], psb(bk).rearrange("p (a b) -> p a b", a=8), r=[pst[bk]], w=[xnT_t[i]])
    K.barrier()
    AR.release(m_p1)

    wbuf = ring(3, [8, 512], BF16, "wb")
    wstate = {"n": 0}

    def load_w(c0, ncols):
        b = wbuf[wstate["n"] % 3]
        wstate["n"] += 1
        dma("pool", b.ap[:, :, 0:ncols], win_d[:, c0:c0 + ncols].rearrange("(kc p) c -> p kc c", p=128), w=[b.t])
        return b

    bank_rr = {"n": 0}

    def next_bank():
        bk = bank_rr["n"] % 4
        bank_rr["n"] += 1
        return bk

    def fm_block(wb, j, c, bk):
        for kc in range(8):
            mm(ps[bk][:, :], wb.ap[:, kc, j * 128:(j + 1) * 128], xnT[:, kc, c * 512:(c + 1) * 512], kc == 0, kc == 7,
               r=[wb.t] + xnT_t[4 * c:4 * c + 4], w=[pst[bk]])

    def tm_block(wb, ncols, i, bk, col0=0):
        for kc in range(8):
            mm(ps[bk][:, 0:ncols], xnT[:, kc, i * 128:(i + 1) * 128], wb.ap[:, kc, col0:col0 + ncols], kc == 0, kc == 7,
               r=[wb.t, xnT_t[i]], w=[pst[bk]])

    m_b1 = AR.mark()
    cosT = B([L], F32, "cosT")
    sinT = B([L], F32, "sinT")
    posi = B([1024], I32, "posi")
    angw = B([1024], F32, "angw")
    kkw = B([1024], F32, "kkw")
    for cc in range(4):
        sl = slice(cc * 1024, (cc + 1) * 1024)
        dma("sp", posi.ap, posr_d[:, sl].partition_broadcast(128), w=[posi.t])
        cp("dve", angw.ap, posi.ap, r=[posi.t], w=[angw.t])
        ts("dve", angw.ap, angw.ap, col(cst, C_INV), ALU.mult, r=[angw.t, cst.t], w=[angw.t])
        sincos(angw.ap, 1024, kkw.ap, sinT.ap[:, sl], cosT.ap[:, sl], r=[angw.t], w=[sinT.t, cosT.t])
        ts("dve", sinT.ap[:, sl], sinT.ap[:, sl], col(cst, C_SGN), ALU.mult, r=[sinT.t, cst.t], w=[sinT.t])
    posc = B([NT], I32, "posc")
    poscf = B([NT], F32, "poscf")
    sinI = B([NT, 32], F32, "sinI")
    cosI = B([NT, 32], F32, "cosI")
    dma("sp", posc.ap, posc_d, w=[posc.t])
    cp("dve", poscf.ap, posc.ap, r=[posc.t], w=[poscf.t])
    angI = angw.ap.rearrange("p (a b) -> p a b", a=NT)
    tt("dve", angI, poscf.ap.unsqueeze(2).broadcast_to([128, NT, 32]),
       cst.ap[:, C_INVI:C_INVI + 32].unsqueeze(1).broadcast_to([128, NT, 32]), ALU.mult, r=[poscf.t, cst.t, angw.t], w=[angw.t])
    sincos(angw.ap, 1024, kkw.ap, sinI.ap.rearrange("p a b -> p (a b)"), cosI.ap.rearrange("p a b -> p (a b)"),
           r=[angw.t], w=[sinI.t, cosI.t])

    t1r = ring(2, [512], F32, "t1")
    t2r = ring(2, [512], F32, "t2")
    qor = ring(3, [512], BF16, "qo")
    n_rope = {"n": 0}

    def rope_fm(bk, c, dst):
        k_ = n_rope["n"]
        n_rope["n"] += 1
        t1 = t1r[k_ % 2]
        t2 = t2r[k_ % 2]
        qo = qor[k_ % 3]
        sl = slice(c * 512, (c + 1) * 512)
        tt("dve", t1.ap, ps[bk][:, :], cosT.ap[:, sl], ALU.mult, r=[pst[bk], cosT.t], w=[t1.t])
        tt("dve", t2.ap[0:64, :], ps[bk][64:128, :], sinT.ap[0:64, sl], ALU.mult, r=[pst[bk], sinT.t], w=[t2.t])
        tt("dve", t2.ap[64:128, :], ps[bk][0:64, :], sinT.ap[64:128, sl], ALU.mult, r=[pst[bk], sinT.t], w=[t2.t])
        tt("pool", qo.ap, t1.ap, t2.ap, ALU.add, r=[t1.t, t2.t], w=[qo.t])
        dma("sp", dst, qo.ap, r=[qo.t])

    qk_blocks = [(CQ, qT_s, 0), (CQ + 512, qT_s, 4), (CK, kT_s, 0), (CK + 512, kT_s, 4)]
    wnext = load_w(qk_blocks[0][0], 512)
    for bi, (c0, dst_s, h0) in enumerate(qk_blocks):
        wb = wnext
        wnext = load_w(qk_blocks[bi + 1][0], 512) if bi + 1 < 4 else load_w(CV, 512)
        for c in range(NCH):
            for j in range(4):
                bk = next_bank()
                fm_block(wb, j, c, bk)
                rope_fm(bk, c, dst_s[h0 + j, :, c * 512:(c + 1) * 512])
    wv0 = wnext
    wv1 = load_w(CV + 512, 512)
    wqi = load_w(CQI, 512)
    vt = ring(2, [8, 129], BF16, "vt")
    for b_ in vt:
        mset("pool", b_.ap[:, :, 128:129], 1.0, w=[b_.t])
    for i in range(NT):
        v_ = vt[i % 2]
        for half, wb in enumerate((wv0, wv1)):
            bk = next_bank()
            tm_block(wb, 512, i, bk)
            cp("act", v_.ap[:, half * 4:(half + 1) * 4, 0:128], ps[bk][:, :].rearrange("p (a b) -> p a b", a=4), r=[pst[bk]], w=[v_.t])
        dma("sp", v_s[i].rearrange("p (h d) -> p h d", h=8), v_.ap, r=[v_.t])
    wkw = load_w(CKI, 72)
    ra = ring(2, [8, 32], F32, "ra")
    rb = ring(2, [8, 32], F32, "rb")
    qr = ring(2, [640], BF16, "qr")
    qiT_c = ring(2, [5, 512], BF16, "qiTc")
    for i in range(NT):
        c, tau = i // 4, i % 4
        bq = next_bank()
        tm_block(wqi, 512, i, bq)
        bkw = next_bank()
        tm_block(wkw, 72, i, bkw)
        q_ = qr[i % 2]
        a_, b2_ = ra[i % 2], rb[i % 2]
        cosb = cosI.ap[:, i, :].unsqueeze(1).broadcast_to([128, 8, 32])
        sinb = sinI.ap[:, i, :].unsqueeze(1).broadcast_to([128, 8, 32])
        qv = ps[bq][:, :].rearrange("p (h d) -> p h d", h=8)
        qo_ = q_.ap[:, 0:512].rearrange("p (h d) -> p h d", h=8)
        rd = [pst[bq], cosI.t, sinI.t]
        tt("dve", a_.ap, qv[:, :, 0:32], cosb, ALU.mult, r=rd, w=[a_.t])
        tt("dve", b2_.ap, qv[:, :, 32:64], sinb, ALU.mult, r=rd, w=[b2_.t])
        tt("pool", qo_[:, :, 0:32], a_.ap, b2_.ap, ALU.subtract, r=[a_.t, b2_.t], w=[q_.t])
        tt("dve", a_.ap, qv[:, :, 32:64], cosb, ALU.mult, r=rd, w=[a_.t])
        tt("dve", b2_.ap, qv[:, :, 0:32], sinb, ALU.mult, r=rd, w=[b2_.t])
        tt("pool", qo_[:, :, 32:64], a_.ap, b2_.ap, ALU.add, r=[a_.t, b2_.t], w=[q_.t])
        kv = ps[bkw][:, 0:64]
        rdk = [pst[bkw], cosI.t, sinI.t]
        tt("dve", a_.ap[:, 0, :], kv[:, 0:32], cosI.ap[:, i, :], ALU.mult, r=rdk, w=[a_.t])
        tt("dve", b2_.ap[:, 0, :], kv[:, 32:64], sinI.ap[:, i, :], ALU.mult, r=rdk, w=[b2_.t])
        tt("pool", q_.ap[:, 512:544], a_.ap[:, 0, :], b2_.ap[:, 0, :], ALU.subtract, r=[a_.t, b2_.t], w=[q_.t])
        tt("dve", a_.ap[:, 1, :], kv[:, 32:64], cosI.ap[:, i, :], ALU.mult, r=rdk, w=[a_.t])
        tt("dve", b2_.ap[:, 1, :], kv[:, 0:32], sinI.ap[:, i, :], ALU.mult, r=rdk, w=[b2_.t])
        tt("pool", q_.ap[:, 544:576], a_.ap[:, 1, :], b2_.ap[:, 1, :], ALU.add, r=[a_.t, b2_.t], w=[q_.t])
        cp("pool", q_.ap[:, 576:640], q_.ap[:, 512:576], r=[q_.t], w=[q_.t])
        cp("dve", wi_sb.ap[:, i, :], ps[bkw][:, 64:72], r=[pst[bkw]], w=[wi_sb.t])
        bt = 4 + (i % 2)
        for jj in range(5):
            tr(psb(bt)[:, jj * 128:(jj + 1) * 128], q_.ap[:, jj * 128:(jj + 1) * 128], ident.ap, r=[q_.t, ident.t], w=[pst[bt]])
        qc = qiT_c[c % 2]
        cp("act", qc.ap[:, :, tau * 128:(tau + 1) * 128], psb(bt)[:, 0:640].rearrange("p (a b) -> p a b", a=5), r=[pst[bt]], w=[qc.t])
        if tau == 3:
            dma("sp", qiT_s[:, :, c * 512:(c + 1) * 512].rearrange("a p t -> p a t"), qc.ap, r=[qc.t])
    K.barrier()
    AR.release(m_b1)
    if stop_after == "1b":
        return finish()
    m_b2 = AR.mark()
    sub_r = ring(3, [512], BF16, "sub")
    su_t = [K.tok(f"suT{c}") for c in range(NCH)]
    w0 = load_w(CSU, 512)
    w1 = load_w(CSU + 512, 512)
    w2 = load_w(CSV, 512)
    nsub = {"n": 0}

    def act_block_out(bk, func, dst, wtok=()):
        o = sub_r[nsub["n"] % 3]
        nsub["n"] += 1
        act(o.ap, ps[bk][:, :], func, r=[pst[bk]], w=[o.t])
        dma("sp", dst, o.ap, r=[o.t], w=list(wtok))

    for blk, wb in enumerate((w0, w1)):
        for c in range(NCH):
            for j in range(4):
                bk = next_bank()
                fm_block(wb, j, c, bk)
                f0 = (blk * 4 + j) * 128
                act_block_out(bk, AF.Gelu_apprx_tanh, suT_s[f0:f0 + 128, c * 512:(c + 1) * 512], wtok=[su_t[c]])
    w3 = load_w(CSV + 512, 512)
    swf = B([8, 128], F32, "swf")
    swb = B([8, 128], BF16, "swb")
    WtT = B([8, 128], BF16, "WtT")
    dma("sp", swf.ap, sw_d.rearrange("g t s -> t g s"), w=[swf.t])
    tt("dve", swf.ap, swf.ap, cm.ap[:, 256:384].unsqueeze(1).broadcast_to([128, 8, 128]), ALU.mult, r=[swf.t, cm.t], w=[swf.t])
    cp("dve", swb.ap, swf.ap, r=[swf.t], w=[swb.t])
    for g in range(8):
        tr(psb(4)[:, g * 128:(g + 1) * 128], swb.ap[:, g, :], ident.ap, r=[swb.t, ident.t], w=[pst[4]])
    cp("act", WtT.ap, psb(4).rearrange("p (a b) -> p a b", a=8), r=[pst[4]], w=[WtT.t])
    bf_ = B([8, 128], F32, "bf")
    bhl = B([8, 128], BF16, "bhl")
    bhf = B([8, 128], F32, "bhf")
    ones2 = B([128], BF16, "ones2")
    mset("pool", ones2.ap, 1.0, w=[ones2.t])
    mset("pool", bf_.ap, 0.0, w=[bf_.t])
    dma("sp", bf_.ap[0:1, :, :], sb_d.rearrange("(o g) t -> o g t", o=1), r=[bf_.t], w=[bf_.t])
    dma("sp", bf_.ap[1:2, :, :], sb_d.rearrange("(o g) t -> o g t", o=1), r=[bf_.t], w=[bf_.t])
    cp("dve", bhl.ap, bf_.ap, r=[bf_.t], w=[bhl.t])
    cp("dve", bhf.ap, bhl.ap, r=[bhl.t], w=[bhf.t])
    tt("dve", bhf.ap, bf_.ap, bhf.ap, ALU.subtract, r=[bf_.t, bhf.t], w=[bhf.t])
    cp("dve", bf_.ap, bhl.ap, r=[bhl.t, bf_.t], w=[bf_.t])
    sel = B([1], F32, "sel")
    mset("pool", sel.ap, 1.0, w=[sel.t])
    K.op("pool", lambda e: e.affine_select(out=sel.ap, in_=sel.ap, pattern=[[0, 1]], compare_op=ALU.is_equal,
                                           fill=0.0, base=0, channel_multiplier=1), r=[sel.t], w=[sel.t])
    tt("dve", bf_.ap, bf_.ap, bhf.ap, ALU.subtract, r=[bf_.t, bhf.t], w=[bf_.t])
    stt(bhf.ap, bf_.ap, sel.ap[:, 0:1], bhf.ap, ALU.mult, ALU.add, r=[bf_.t, sel.t, bhf.t], w=[bhf.t])
    cp("dve", bhl.ap, bhf.ap, r=[bhf.t], w=[bhl.t])

    gsb = B([D], F32, "gsb")
    dma("sp", gsb.ap, gs_d.partition_broadcast(128), w=[gsb.t])
    gv = ring(2, [D], F32, "gv")
    vln = ring(2, [D], BF16, "vln")
    bst = B([NT, 12], F32, "bst")
    mv = B([NT, 2], F32, "mv")
    rsd = B([NT], F32, "rsd")
    suc = ring(2, [8, 512], BF16, "suc")
    ysg = ring(2, [8, 512], BF16, "ysg")
    for c in range(NCH):
        su_c = suc[c % 2]
        ys_c = ysg[c % 2]
        dma("sp", su_c.ap, suT_s[:, c * 512:(c + 1) * 512].rearrange("(g p) t -> p g t", p=128), r=[su_t[c]], w=[su_c.t])
        for tau in range(4):
            i = 4 * c + tau
            g_ = gv[i % 2]
            vl = vln[i % 2]
            for half, wb in enumerate((w2, w3)):
                bk = next_bank()
                tm_block(wb, 512, i, bk)
                act(g_.ap[:, half * 512:(half + 1) * 512], ps[bk][:, :], AF.Gelu_apprx_tanh, r=[pst[bk]], w=[g_.t])
            K.op("dve", lambda e, i=i, g_=g_: e.bn_stats(out=bst.ap[:, i, 0:6], in_=g_.ap[:, 0:512]), r=[g_.t], w=[bst.t])
            K.op("dve", lambda e, i=i, g_=g_: e.bn_stats(out=bst.ap[:, i, 6:12], in_=g_.ap[:, 512:1024]), r=[g_.t], w=[bst.t])
            K.op("dve", lambda e, i=i: e.bn_aggr(out=mv.ap[:, i, :], in_=bst.ap[:, i, :]), r=[bst.t], w=[mv.t])
            ts("pool", rsd.ap[:, i:i + 1], mv.ap[:, i, 1:2], EPS, ALU.add, r=[mv.t], w=[rsd.t])
            tt("pool", rsd.ap[:, i:i + 1], rsd.ap[:, i:i + 1], col(cst, C_NH), ALU.pow, r=[rsd.t, cst.t], w=[rsd.t])
            ts("dve", g_.ap, g_.ap, mv.ap[:, i, 0:1], ALU.subtract, rsd.ap[:, i:i + 1], ALU.mult, r=[g_.t, mv.t, rsd.t], w=[g_.t])
            tt("pool", vl.ap, g_.ap, gsb.ap, ALU.mult, r=[g_.t, gsb.t], w=[vl.t])
            for g in range(8):
                bk = 6 + g // 4
                o_ = ps[bk][:, (g % 4) * 128:(g % 4 + 1) * 128]
                mm(o_, vl.ap[:, g * 128:(g + 1) * 128], WtT.ap[:, g, :], True, False, r=[vl.t, WtT.t], w=[pst[bk]])
                mm(o_, ones2.ap[0:2, :], bhl.ap[0:2, g, :], False, True, r=[ones2.t, bhl.t], w=[pst[bk]])
            for b_ in range(2):
                tt("dve", ys_c.ap[:, 4 * b_:4 * b_ + 4, tau * 128:(tau + 1) * 128],
                   ps[6 + b_][:, :].rearrange("p (a b) -> p a b", a=4),
                   su_c.ap[:, 4 * b_:4 * b_ + 4, tau * 128:(tau + 1) * 128], ALU.mult, r=[pst[6 + b_], su_c.t], w=[ys_c.t])
        dma("sp", ysgT_s[:, c * 512:(c + 1) * 512].rearrange("(g p) t -> p g t", p=128), ys_c.ap, r=[ys_c.t])
    wg = load_w(CGA, 512)
    for blk in range(4):
        wb = wg
        if blk < 3:
            wg = load_w(CGA + (blk + 1) * 512, 512)
        for c in range(NCH):
            for j in range(4):
                bk = next_bank()
                fm_block(wb, j, c, bk)
                f0 = (blk * 4 + j) * 128
                act_block_out(bk, AF.Sigmoid, sgT_s[f0:f0 + 128, c * 512:(c + 1) * 512])
    K.barrier()
    AR.release(m_0)
    if stop_after == "1":
        return finish()
    SCALE = float(128 ** -0.5)
    vres = AR.alloc([NT, 8, 129], BF16)
    v_t = [K.tok(f"v{i}") for i in range(NT)]
    kiT2 = AR.alloc([L], BF16)
    ki_t = [K.tok(f"ki{i}") for i in range(4)]
    for i in range(NT):
        dma("sp", vres[:, i, :, :], v_s[i].rearrange("p (h d) -> p h d", h=8), w=[v_t[i]])
        if i % 8 == 0:
            q4 = i // 8
            dma("sp", kiT2[:, q4 * 1024:(q4 + 1) * 1024], qiT_s[4, :, q4 * 1024:(q4 + 1) * 1024], w=[ki_t[q4]])
    kTr = ring(2, [L], BF16, "kTr")
    qTc = B([8, 512], BF16, "qTc")
    qiTc = B([4, 512], BF16, "qiTc")
    S = B([L], F32, "S")
    maskb = B([L], BF16, "maskb")
    maskT = B([NT, 512], BF16, "maskT")
    rbuf = ring(3, [512], F32, "rb")
    ebuf = ring(3, [512], BF16, "eb")
    pbuf = ring(3, [512], BF16, "pb")
    ytile = B([4, D], BF16, "ytile")
    yT = B([8, 512], BF16, "yT")
    vmax = B([1], F32, "vmax")
    vmin = B([1], F32, "vmin")
    rngv = B([1], F32, "rngv")
    hw = B([KI + 2], F32, "hw")
    cand = B([1], F32, "cand")
    cnt = B([1], F32, "cnt")
    msg = B([1], F32, "msg")
    thr = B([1], F32, "thr")
    recr = ring(2, [4], F32, "rec")
    ctr = {"sc": 0, "lg": 0, "tr": 0, "e": 0}

    for c in range(NCH):
        csl = slice(c * 512, (c + 1) * 512)
        dma("sp", qTc.ap, qT_s[:, :, csl].rearrange("h p t -> p h t"), w=[qTc.t])
        dma("sp", qiTc.ap, qiT_s[0:4, :, csl].rearrange("a p t -> p a t"), w=[qiTc.t])
        for tau in range(4):
            i = 4 * c + tau
            n = 128 * (i + 1)
            nv = 128 * i
            for sc in range((n + 511) // 512):
                w_ = min(512, n - 512 * sc)
                sl = slice(sc * 512, sc * 512 + w_)
                for h in range(8):
                    bk = ctr["sc"] % 2
                    ctr["sc"] += 1
                    pr = slice(64 * (h % 2), 64 * (h % 2) + 64)
                    mm(ps[bk][:, 0:w_], qiTc.ap[pr, h // 2, tau * 128:(tau + 1) * 128], kiT2[pr, sl], True, True,
                       r=[qiTc.t, ki_t[sc // 2]], w=[pst[bk]])
                    rb = rbuf[ctr["sc"] % 3]
                    act(rb.ap[:, 0:w_], ps[bk][:, 0:w_], AF.Relu, r=[pst[bk]], w=[rb.t])
                    if h == 0:
                        ts("dve", S.ap[:, sl], rb.ap[:, 0:w_], wi_sb.ap[:, i, 0:1], ALU.mult, r=[rb.t, wi_sb.t], w=[S.t])
                    else:
                        stt(S.ap[:, sl], rb.ap[:, 0:w_], wi_sb.ap[:, i, h:h + 1], S.ap[:, sl], ALU.mult, ALU.add,
                            r=[rb.t, wi_sb.t, S.t], w=[S.t])
            tt("pool", S.ap[:, nv:n], S.ap[:, nv:n], cm.ap[:, 0:128], ALU.add, r=[S.t, cm.t], w=[S.t])
            if i >= 2:
                K.op("dve", lambda e, n=n: e.tensor_reduce(out=vmax.ap, in_=S.ap[:, 0:n], axis=AX.X, op=ALU.max), r=[S.t], w=[vmax.t])
                K.op("dve", lambda e, nv=nv: e.tensor_reduce(out=vmin.ap, in_=S.ap[:, 0:nv], axis=AX.X, op=ALU.min), r=[S.t], w=[vmin.t])
                tt("dve", rngv.ap, vmax.ap, vmin.ap, ALU.subtract, r=[vmax.t, vmin.t], w=[rngv.t])
                ts("dve", hw.ap, cst.ap[:, C_PW:C_PW + KI + 2], rngv.ap[:, 0:1], ALU.mult, r=[cst.t, rngv.t], w=[hw.t])
                tt("dve", cand.ap, vmin.ap, hw.ap[:, 0:1], ALU.add, r=[vmin.t, hw.t], w=[cand.t])
                for k in range(KI):
                    ts("dve", maskb.ap[:, 0:n], S.ap[:, 0:n], cand.ap[:, 0:1], ALU.is_ge, None, ALU.add,
                       r=[S.t, cand.t], w=[maskb.t, cnt.t], accum=cnt.ap)
                    ts("dve", msg.ap, cnt.ap, 255.5, ALU.is_ge, 0.5, ALU.subtract, r=[cnt.t], w=[msg.t])
                    stt(cand.ap, msg.ap, hw.ap[:, k:k + 1], cand.ap, ALU.mult, ALU.add, r=[msg.t, hw.t, cand.t], w=[cand.t])
                tt("dve", thr.ap, cand.ap, hw.ap[:, KI:KI + 1], ALU.subtract, r=[cand.t, hw.t], w=[thr.t])
                thr_ap, thr_t = thr.ap[:, 0:1], thr.t
            else:
                thr_ap, thr_t = col(cst, C_NTHR), cst.t
            ts("dve", maskb.ap[:, 0:n], S.ap[:, 0:n], thr_ap, ALU.is_ge, r=[S.t, thr_t], w=[maskb.t])
            for j0 in range(0, i + 1, 8):
                nb = min(8, i + 1 - j0)
                bk = 2 + ctr["tr"] % 2
                ctr["tr"] += 1
                for jj in range(nb):
                    tr(psb(bk)[:, jj * 128:(jj + 1) * 128], maskb.ap[:, (j0 + jj) * 128:(j0 + jj + 1) * 128], ident.ap,
                       r=[maskb.t, ident.t], w=[pst[bk]])
                cp("act", maskT.ap[:, j0:j0 + nb, tau * 128:(tau + 1) * 128],
                   psb(bk)[:, 0:nb * 128].rearrange("p (a b) -> p a b", a=nb), r=[pst[bk]], w=[maskT.t])
        nj = 4 * c + 4
        for h in range(8):
            kt = kTr[h % 2]
            dma("sp", kt.ap[:, 0:nj * 128], kT_s[h, :, 0:nj * 128], w=[kt.t])
            accA = 4 + 2 * (h % 2)
            accB = accA + 1
            for j in range(nj):
                r0 = max(0, j - 4 * c)
                N = 512 - 128 * r0
                bk = 2 + ctr["lg"] % 2
                ctr["lg"] += 1
                mm(ps[bk][:, 0:N], kt.ap[:, j * 128:(j + 1) * 128], qTc.ap[:, h, r0 * 128:512], True, True,
                   r=[kt.t, qTc.t], w=[pst[bk]])
                e_ = ebuf[ctr["e"] % 3]
                p_ = pbuf[ctr["e"] % 3]
                ctr["e"] += 1
                act(e_.ap[:, 0:N], ps[bk][:, 0:N], AF.Exp, scale=SCALE, r=[pst[bk]], w=[e_.t])
                tt("dve", p_.ap[:, 0:N], e_.ap[:, 0:N], maskT.ap[:, j, r0 * 128:512], ALU.mult, r=[e_.t, maskT.t], w=[p_.t])
                for tau in range(r0, 4):
                    if tau < 3:
                        o_, ob = ps[accA][:, tau * 129:(tau + 1) * 129], accA
                    else:
                        o_, ob = ps[accB][:, 0:129], accB
                    first = (j == 0 and tau in (0, 3))
                    mm(o_, p_.ap[:, (tau - r0) * 128:(tau - r0 + 1) * 128], vres[:, j, h, :], first, j == nj - 1,
                       r=[p_.t, v_t[j]], w=[pst[ob]], sgc=True)
            rc = recr[h % 2]
            K.op("dve", lambda e, rc=rc, accA=accA: e.reciprocal(
                out=rc.ap[:, 0:3], in_=ps[accA][:, 0:387].rearrange("p (a b) -> p a b", b=129)[:, :, 128]), r=[pst[accA]], w=[rc.t])
            K.op("dve", lambda e, rc=rc, accB=accB: e.reciprocal(out=rc.ap[:, 3:4], in_=ps[accB][:, 128:129]), r=[pst[accB]], w=[rc.t])
            for tau in range(4):
                src, sb_ = (ps[accA][:, tau * 129:tau * 129 + 128], accA) if tau < 3 else (ps[accB][:, 0:128], accB)
                ts("dve", ytile.ap[:, tau, h * 128:(h + 1) * 128], src, rc.ap[:, tau:tau + 1], ALU.mult, r=[pst[sb_], rc.t], w=[ytile.t])
        for tau in range(4):
            bk = 2 + ctr["tr"] % 2
            ctr["tr"] += 1
            for kc in range(8):
                tr(psb(bk)[:, kc * 128:(kc + 1) * 128], ytile.ap[:, tau, kc * 128:(kc + 1) * 128], ident.ap, r=[ytile.t, ident.t], w=[pst[bk]])
            cp("act", yT.ap[:, :, tau * 128:(tau + 1) * 128], psb(bk).rearrange("p (a b) -> p a b", a=8), r=[pst[bk]], w=[yT.t])
        dma("sp", yatT_s[:, csl].rearrange("(g p) t -> p g t", p=128), yT.ap, r=[yT.t])
    K.barrier()
    AR.release(m_0)
    if stop_after == "2":
        return finish()
    m_3 = AR.mark()
    Wa_sb = B([8, D], BF16, "Wa")
    Wb_sb = B([8, D], BF16, "Wb")
    Wo_sb = B([8, D], BF16, "Wo")
    for wsb, wd in ((Wa_sb, wa_d), (Wb_sb, wb_d), (Wo_sb, wo_d)):
        dma("pool", wsb.ap, wd.rearrange("(kc p) n -> p kc n", p=128), w=[wsb.t])
    g2b = B([D], F32, "g2b")
    dma("sp", g2b.ap, g2_d.partition_broadcast(128), w=[g2b.t])
    wr = B([8, 36], F32, "wr")
    br = B([36], F32, "br")
    dma("sp", wr.ap[:, :, 0:4], wrg_d.rearrange("(kc p) n -> p kc n", p=128), w=[wr.t])
    dma("sp", wr.ap[:, :, 4:36], wre_d.rearrange("(kc p) n -> p kc n", p=128), w=[wr.t])
    dma("sp", br.ap[:, 0:4], brg_d.partition_broadcast(128), w=[br.t])
    dma("sp", br.ap[:, 4:36], bre_d.partition_broadcast(128), w=[br.t])
    trib = B([128], BF16, "trib")
    onesb = B([128], BF16, "onesb")
    cp("pool", trib.ap, cm.ap[:, 128:256], r=[cm.t], w=[trib.t])
    mset("pool", onesb.ap, 1.0, w=[onesb.t])
    base = B([32], F32, "base")
    capb = B([32], F32, "capb")
    ts("pool", base.ap, cm.ap[:, 384:416], -1.0, ALU.add, r=[cm.t], w=[base.t])
    ts("pool", capb.ap, cm.ap[:, 384:416], float(CAP) - 0.5, ALU.add, r=[cm.t], w=[capb.t])
    inr = [ring(2, [8, 512], BF16, nm) for nm in ("ysgc", "yatc", "sgac", "sgbc")]
    mT = B([8, 512], BF16, "mT")
    t1p = ring(2, [512], F32, "t1p")
    t2p = ring(2, [512], F32, "t2p")
    xtr = ring(2, [D], F32, "xt")
    h2r = ring(2, [D], F32, "h2t")
    junk3 = B([D], F32, "junk3")
    ms2 = B([NT], F32, "ms2")
    rs2 = B([NT], F32, "rs2")
    xn2f = B([D], F32, "xn2f")
    xn2b = ring(2, [D], BF16, "xn2b")
    xn2T = B([8, 128], F32, "xn2T")
    lgt = B([36], F32, "lgt")
    sm = {nm: B([sz], F32, nm) for nm, sz in (
        ("gmax", 1), ("negg", 1), ("ohg", 4), ("j4", 4), ("se", 1), ("gp", 1), ("esel", 8), ("m1", 1), ("oh1", 8), ("es2", 8),
        ("m2", 1), ("oh2", 8), ("dlt", 1), ("ex", 1), ("den", 1), ("p1", 1), ("p2", 1), ("M1", 32), ("M2", 32), ("posf", 32),
        ("okf", 32), ("j32", 32), ("pk", 2), ("ok", 2), ("gk", 2))}
    Mb = B([32], BF16, "Mb")

    def S_(nm):
        return sm[nm].ap

    def T_(nm):
        return sm[nm].t

    for c in range(NCH):
        csl = slice(c * 512, (c + 1) * 512)
        ysg_c, yat_c, sga_c, sgb_c = [rg[c % 2] for rg in inr]
        dma("sp", ysg_c.ap, ysgT_s[:, csl].rearrange("(g p) t -> p g t", p=128), w=[ysg_c.t])
        dma("sp", yat_c.ap, yatT_s[:, csl].rearrange("(g p) t -> p g t", p=128), w=[yat_c.t])
        dma("sp", sga_c.ap, sgT_s[0:D, csl].rearrange("(g p) t -> p g t", p=128), w=[sga_c.t])
        dma("sp", sgb_c.ap, sgT_s[D:2 * D, csl].rearrange("(g p) t -> p g t", p=128), w=[sgb_c.t])
        for nb in range(8):
            bA = next_bank()
            for kc in range(8):
                mm(ps[bA][:, :], Wa_sb.ap[:, kc, nb * 128:(nb + 1) * 128], ysg_c.ap[:, kc, :], kc == 0, kc == 7, r=[Wa_sb.t, ysg_c.t], w=[pst[bA]])
            bB = next_bank()
            for kc in range(8):
                mm(ps[bB][:, :], Wb_sb.ap[:, kc, nb * 128:(nb + 1) * 128], yat_c.ap[:, kc, :], kc == 0, kc == 7, r=[Wb_sb.t, yat_c.t], w=[pst[bB]])
            t1, t2 = t1p[nb % 2], t2p[nb % 2]
            tt("dve", t1.ap, ps[bA][:, :], sga_c.ap[:, nb, :], ALU.mult, r=[pst[bA], sga_c.t], w=[t1.t])
            tt("dve", t2.ap, ps[bB][:, :], sgb_c.ap[:, nb, :], ALU.mult, r=[pst[bB], sgb_c.t], w=[t2.t])
            tt("pool", mT.ap[:, nb, :], t1.ap, t2.ap, ALU.add, r=[t1.t, t2.t], w=[mT.t])
        for tau in range(4):
            i = 4 * c + tau
            xt, h2t, xb2 = xtr[i % 2], h2r[i % 2], xn2b[i % 2]
            dma("sp", xt.ap, x_d[i * 128:(i + 1) * 128, :], w=[xt.t])
            for half in range(2):
                hs = slice(half * 512, (half + 1) * 512)
                bk = next_bank()
                for kc in range(8):
                    mm(ps[bk][:, :], mT.ap[:, kc, tau * 128:(tau + 1) * 128], Wo_sb.ap[:, kc, hs], kc == 0, kc == 7, r=[mT.t, Wo_sb.t], w=[pst[bk]])
                tt("dve", h2t.ap[:, hs], ps[bk][:, :], xt.ap[:, hs], ALU.add, r=[pst[bk], xt.t], w=[h2t.t])
            dma("sp", h2_s[i * 128:(i + 1) * 128, :], h2t.ap, r=[h2t.t])
            stt(junk3.ap, h2t.ap, 1.0 / D, h2t.ap, ALU.mult, ALU.mult, r=[h2t.t], w=[junk3.t, ms2.t], accum=ms2.ap[:, i:i + 1])
            ts("pool", rs2.ap[:, i:i + 1], ms2.ap[:, i:i + 1], EPS, ALU.add, r=[ms2.t], w=[rs2.t])
            tt("pool", rs2.ap[:, i:i + 1], rs2.ap[:, i:i + 1], col(cst, C_NH), ALU.pow, r=[rs2.t, cst.t], w=[rs2.t])
            stt(xn2f.ap, h2t.ap, rs2.ap[:, i:i + 1], g2b.ap, ALU.mult, ALU.mult, r=[h2t.t, rs2.t, g2b.t], w=[xn2f.t])
            cp("pool", xb2.ap, xn2f.ap, r=[xn2f.t], w=[xb2.t])
            for q4 in range(2):
                bt = 4 + q4
                for jj in range(4):
                    kc = q4 * 4 + jj
                    tr(ps[bt][:, jj * 128:(jj + 1) * 128], xn2f.ap[:, kc * 128:(kc + 1) * 128], identf.ap, r=[xn2f.t, identf.t], w=[pst[bt]])
                cp("act", xn2T.ap[:, q4 * 4:(q4 + 1) * 4, :], ps[bt][:, :].rearrange("p (a b) -> p a b", a=4), r=[pst[bt]], w=[xn2T.t])
            for kc in range(8):
                mm(ps[6][:, 0:36], xn2T.ap[:, kc, :], wr.ap[:, kc, :], kc == 0, kc == 7, r=[xn2T.t, wr.t], w=[pst[6]])
            tt("dve", lgt.ap, ps[6][:, 0:36], br.ap, ALU.add, r=[pst[6], br.t], w=[lgt.t])
            gl = lgt.ap[:, 0:4]
            el = lgt.ap[:, 4:36].rearrange("p (g j) -> p g j", g=4)
            K.op("dve", lambda e: e.tensor_reduce(out=S_("gmax"), in_=gl, axis=AX.X, op=ALU.max), r=[lgt.t], w=[T_("gmax")])
            ts("dve", S_("ohg"), gl, S_("gmax")[:, 0:1], ALU.is_ge, r=[lgt.t, T_("gmax")], w=[T_("ohg")])
            ts("dve", S_("negg"), S_("gmax"), -1.0, ALU.mult, r=[T_("gmax")], w=[T_("negg")])
            act(S_("j4"), gl, AF.Exp, bias=S_("negg")[:, 0:1], r=[lgt.t, T_("negg")], w=[T_("j4"), T_("se")], accum=S_("se"))
            K.op("dve", lambda e: e.reciprocal(out=S_("gp"), in_=S_("se")), r=[T_("se")], w=[T_("gp")])
            ts("dve", S_("esel"), el[:, 0, :], S_("ohg")[:, 0:1], ALU.mult, r=[lgt.t, T_("ohg")], w=[T_("esel")])
            for g in range(1, 4):
                stt(S_("esel"), el[:, g, :], S_("ohg")[:, g:g + 1], S_("esel"), ALU.mult, ALU.add, r=[lgt.t, T_("ohg"), T_("esel")], w=[T_("esel")])
            K.op("dve", lambda e: e.tensor_reduce(out=S_("m1"), in_=S_("esel"), axis=AX.X, op=ALU.max), r=[T_("esel")], w=[T_("m1")])
            ts("dve", S_("oh1"), S_("esel"), S_("m1")[:, 0:1], ALU.is_ge, r=[T_("esel"), T_("m1")], w=[T_("oh1")])
            stt(S_("es2"), S_("oh1"), NEG, S_("esel"), ALU.mult, ALU.add, r=[T_("oh1"), T_("esel")], w=[T_("es2")])
            K.op("dve", lambda e: e.tensor_reduce(out=S_("m2"), in_=S_("es2"), axis=AX.X, op=ALU.max), r=[T_("es2")], w=[T_("m2")])
            ts("dve", S_("oh2"), S_("es2"), S_("m2")[:, 0:1], ALU.is_ge, r=[T_("es2"), T_("m2")], w=[T_("oh2")])
            tt("dve", S_("dlt"), S_("m2"), S_("m1"), ALU.subtract, r=[T_("m1"), T_("m2")], w=[T_("dlt")])
            act(S_("ex"), S_("dlt"), AF.Exp, r=[T_("dlt")], w=[T_("ex")])
            ts("dve", S_("den"), S_("ex"), 1.0, ALU.add, r=[T_("ex")], w=[T_("den")])
            K.op("dve", lambda e: e.reciprocal(out=S_("p1"), in_=S_("den")), r=[T_("den")], w=[T_("p1")])
            tt("dve", S_("p2"), S_("ex"), S_("p1"), ALU.mult, r=[T_("ex"), T_("p1")], w=[T_("p2")])
            tt("dve", S_("gk")[:, 0:1], S_("p1"), S_("gp"), ALU.mult, r=[T_("p1"), T_("gp")], w=[T_("gk")])
            tt("dve", S_("gk")[:, 1:2], S_("p2"), S_("gp"), ALU.mult, r=[T_("p2"), T_("gp")], w=[T_("gk")])
            ohg_b = S_("ohg").unsqueeze(2).broadcast_to([128, 4, 8])
            for nm, oh in (("M1", "oh1"), ("M2", "oh2")):
                tt("dve", S_(nm).rearrange("p (g j) -> p g j", g=4), ohg_b, S_(oh).unsqueeze(1).broadcast_to([128, 4, 8]), ALU.mult,
                   r=[T_("ohg"), T_(oh)], w=[T_(nm)])
            tt("dve", Mb.ap, S_("M1"), S_("M2"), ALU.add, r=[T_("M1"), T_("M2")], w=[Mb.t])
            mm(ps[7][:, 0:32], trib.ap, Mb.ap, True, True, r=[trib.t, Mb.t], w=[pst[7]])
            mm(ps[7][:, 32:64], onesb.ap, Mb.ap, False, True, r=[onesb.t, Mb.t], w=[pst[7]], sgc=True)
            tt("dve", S_("posf"), ps[7][:, 0:32], base.ap, ALU.add, r=[pst[7], base.t], w=[T_("posf")])
            tt("dve", base.ap, ps[7][:, 32:64], base.ap, ALU.add, r=[pst[7], base.t], w=[base.t])
            tt("dve", S_("okf"), S_("posf"), capb.ap, ALU.is_lt, r=[T_("posf"), capb.t], w=[T_("okf")])
            for k_, nm in enumerate(("M1", "M2")):
                stt(S_("j32"), S_(nm), 1.0, S_("posf"), ALU.mult, ALU.mult, r=[T_(nm), T_("posf")], w=[T_("j32"), T_("pk")], accum=S_("pk")[:, k_:k_ + 1])
                stt(S_("j32"), S_(nm), 1.0, S_("okf"), ALU.mult, ALU.mult, r=[T_(nm), T_("okf")], w=[T_("j32"), T_("ok")], accum=S_("ok")[:, k_:k_ + 1])
            ts("dve", S_("pk"), S_("pk"), col(cst, C_DUM), ALU.subtract, r=[T_("pk"), cst.t], w=[T_("pk")])
            tt("dve", S_("pk"), S_("pk"), S_("ok"), ALU.mult, r=[T_("pk"), T_("ok")], w=[T_("pk")])
            ts("dve", S_("pk"), S_("pk"), col(cst, C_DUM), ALU.add, r=[T_("pk"), cst.t], w=[T_("pk")])
            cp("dve", rt_pos.ap[:, 2 * i:2 * i + 2], S_("pk"), r=[T_("pk")], w=[rt_pos.t])
            tt("dve", rt_gate.ap[:, 2 * i:2 * i + 2], S_("gk"), S_("ok"), ALU.mult, r=[T_("gk"), T_("ok")], w=[rt_gate.t])
            for k_ in range(2):
                K.op("pool", lambda e, i=i, k_=k_, xb2=xb2: e.indirect_dma_start(
                    out=xs_s[:, :], out_offset=bass.IndirectOffsetOnAxis(ap=rt_pos.ap[:, 2 * i + k_:2 * i + k_ + 1], axis=0),
                    in_=xb2.ap, in_offset=None), r=[xb2.t, rt_pos.t], dma=True)
    K.barrier()
    AR.release(m_3)
    if stop_after == "3":
        return finish()
    m_4 = AR.mark()
    wi_r = ring(2, [8, 512], BF16, "wie")
    wo_r = ring(2, [2, D], BF16, "woe")
    xs_r = ring(2, [4, D], BF16, "xs")
    xsT_r = ring(2, [8, 512], BF16, "xsT")
    sg_r = ring(2, [512], F32, "sg")
    aT_r = ring(2, [2, 512], BF16, "aT")
    ys_r = ring(3, [D], F32, "ysb")
    nys = {"n": 0}
    mset("pool", ys_r[2].ap, 0.0, w=[ys_r[2].t])
    dma("sp", ys_s[NE * CAP:NE * CAP + 128, :], ys_r[2].ap, r=[ys_r[2].t])
    for e_i in range(NE):
        wi_, wo_, xs_, xsT_, aT_ = wi_r[e_i % 2], wo_r[e_i % 2], xs_r[e_i % 2], xsT_r[e_i % 2], aT_r[e_i % 2]
        dma("pool", wi_.ap, wei_d[e_i].rearrange("(kc p) f -> p kc f", p=128), w=[wi_.t])
        dma("pool", wo_.ap, weo_d[e_i].rearrange("(fc p) n -> p fc n", p=128), w=[wo_.t])
        dma("sp", xs_.ap, xs_s[e_i * CAP:(e_i + 1) * CAP, :].rearrange("(st p) d -> p st d", p=128), w=[xs_.t])
        for st in range(4):
            bk = 4 + st % 2
            for kc in range(8):
                tr(psb(bk)[:, kc * 128:(kc + 1) * 128], xs_.ap[:, st, kc * 128:(kc + 1) * 128], ident.ap, r=[xs_.t, ident.t], w=[pst[bk]])
            cp("act" if st % 2 else "dve", xsT_.ap[:, :, st * 128:(st + 1) * 128], psb(bk).rearrange("p (a b) -> p a b", a=8), r=[pst[bk]], w=[xsT_.t])
        for p_ in range(2):
            bG = next_bank()
            for kc in range(8):
                mm(ps[bG][:, :], wi_.ap[:, kc, p_ * 128:(p_ + 1) * 128], xsT_.ap[:, kc, :], kc == 0, kc == 7, r=[wi_.t, xsT_.t], w=[pst[bG]])
            bU = next_bank()
            for kc in range(8):
                mm(ps[bU][:, :], wi_.ap[:, kc, 256 + p_ * 128:256 + (p_ + 1) * 128], xsT_.ap[:, kc, :], kc == 0, kc == 7, r=[wi_.t, xsT_.t], w=[pst[bU]])
            sg_ = sg_r[p_]
            act(sg_.ap, ps[bG][:, :], AF.Silu, r=[pst[bG]], w=[sg_.t])
            tt("dve", aT_.ap[:, p_, :], sg_.ap, ps[bU][:, :], ALU.mult, r=[sg_.t, pst[bU]], w=[aT_.t])
        for st in range(4):
            yb = ys_r[nys["n"] % 3]
            nys["n"] += 1
            for half in range(2):
                hs = slice(half * 512, (half + 1) * 512)
                bk = next_bank()
                for fc in range(2):
                    mm(ps[bk][:, :], aT_.ap[:, fc, st * 128:(st + 1) * 128], wo_.ap[:, fc, hs], fc == 0, fc == 1, r=[aT_.t, wo_.t], w=[pst[bk]])
                cp("act" if half else "dve", yb.ap[:, hs], ps[bk][:, :], r=[pst[bk]], w=[yb.t])
            r0_ = e_i * CAP + st * 128
            dma("sp", ys_s[r0_:r0_ + 128, :], yb.ap, r=[yb.t])
    K.barrier()
    AR.release(m_4)
    gfb = B([D], F32, "gfb")
    dma("sp", gfb.ap, gf_d.partition_broadcast(128), w=[gfb.t])
    Y0r = ring(2, [D], F32, "Y0")
    Y1r = ring(2, [D], F32, "Y1")
    h2l = ring(2, [D], F32, "h2l")
    h3r = ring(2, [D], F32, "h3")
    outr = ring(2, [D], F32, "outb")
    junk4 = B([D], F32, "junk4")
    ms3 = B([NT], F32, "ms3")
    rs3 = B([NT], F32, "rs3")
    for b_ in Y0r + Y1r:
        mset("pool", b_.ap, 0.0, w=[b_.t])
    for i in range(NT):
        Y0, Y1, h2_, h3, ob = Y0r[i % 2], Y1r[i % 2], h2l[i % 2], h3r[i % 2], outr[i % 2]
        for k_, Y in enumerate((Y0, Y1)):
            K.op("pool", lambda e, i=i, k_=k_, Y=Y: e.indirect_dma_start(
                out=Y.ap, out_offset=None, in_=ys_s[:, :], in_offset=bass.IndirectOffsetOnAxis(ap=rt_pos.ap[:, 2 * i + k_:2 * i + k_ + 1], axis=0)),
                r=[rt_pos.t], w=[Y.t], dma=True)
        dma("sp", h2_.ap, h2_s[i * 128:(i + 1) * 128, :], w=[h2_.t])
        stt(h3.ap, Y0.ap, rt_gate.ap[:, 2 * i:2 * i + 1], h2_.ap, ALU.mult, ALU.add, r=[Y0.t, rt_gate.t, h2_.t], w=[h3.t])
        stt(h3.ap, Y1.ap, rt_gate.ap[:, 2 * i + 1:2 * i + 2], h3.ap, ALU.mult, ALU.add, r=[Y1.t, rt_gate.t, h3.t], w=[h3.t])
        stt(junk4.ap, h3.ap, 1.0 / D, h3.ap, ALU.mult, ALU.mult, r=[h3.t], w=[junk4.t, ms3.t], accum=ms3.ap[:, i:i + 1])
        ts("pool", rs3.ap[:, i:i + 1], ms3.ap[:, i:i + 1], EPS, ALU.add, r=[ms3.t], w=[rs3.t])
        tt("pool", rs3.ap[:, i:i + 1], rs3.ap[:, i:i + 1], col(cst, C_NH), ALU.pow, r=[rs3.t, cst.t], w=[rs3.t])
        stt(ob.ap, h3.ap, rs3.ap[:, i:i + 1], gfb.ap, ALU.mult, ALU.mult, r=[h3.t, rs3.t, gfb.t], w=[ob.t])
        dma("sp", out_d[i * 128:(i + 1) * 128, :], ob.ap, r=[ob.t])
    return finish()


def make_in_maps(inp, cores):
    c, m = host_consts()
    maps = []
    for b in cores:
        pos = np.ascontiguousarray(inp["positions"][b]).astype(np.int32)
        maps.append({
            "x": np.ascontiguousarray(inp["x"][b], dtype=np.float32),
            "pos_row": pos.reshape(1, L),
            "pos_col": np.ascontiguousarray(pos.reshape(NT, 128).T),
            "norm1_g": np.ascontiguousarray(inp["norm1_g"][0]).reshape(1, D),
            "w_in": np.ascontiguousarray(inp["w_in"][0]),
            "sgu_norm_g": np.ascontiguousarray(inp["sgu_norm_g"][0]).reshape(1, D),
            "sgu_w": np.ascontiguousarray(inp["sgu_w"][0]),
            "sgu_b": np.ascontiguousarray(inp["sgu_b"][0]),
            "w_branch_a": np.ascontiguousarray(inp["w_branch_a"][0]),
            "w_branch_b": np.ascontiguousarray(inp["w_branch_b"][0]),
            "w_out": np.ascontiguousarray(inp["w_out"][0]),
            "norm2_g": np.ascontiguousarray(inp["norm2_g"][0]).reshape(1, D),
            "w_router_group": np.ascontiguousarray(inp["w_router_group"][0]),
            "b_router_group": np.ascontiguousarray(inp["b_router_group"][0]).reshape(1, 4),
            "w_router_expert": np.ascontiguousarray(inp["w_router_expert"][0]),
            "b_router_expert": np.ascontiguousarray(inp["b_router_expert"][0]).reshape(1, 32),
            "w_expert_in": np.ascontiguousarray(inp["w_expert_in"][0]),
            "w_expert_out": np.ascontiguousarray(inp["w_expert_out"][0]),
            "norm_f_g": np.ascontiguousarray(inp["norm_f_g"]).reshape(1, D),
            "cst": c,
            "cmat": m,
        })
    return maps


def kernel(**inputs):
    P = build_program()
    maps = make_in_maps(inputs, list(range(8)))
    res = run_bass_kernel_spmd(P.nc, maps, core_ids=list(range(8)))
    return np.stack([np.asarray(r["out"], dtype=np.float32) for r in res.results], axis=0)
```

```python
from contextlib import ExitStack
import numpy as np
import concourse.bass as bass
import concourse.mybir as mybir

F32 = mybir.dt.float32
BF16 = mybir.dt.bfloat16
I32 = mybir.dt.int32
U32 = mybir.dt.uint32
AF = mybir.ActivationFunctionType
ALU = mybir.AluOpType
AX = mybir.AxisListType


class Tok:
    __slots__ = ("w", "r", "name")

    def __init__(self, name=""):
        self.w = None
        self.r = []
        self.name = name


class Op:
    __slots__ = ("eng", "fn", "deps", "dma", "sig", "sem", "val", "gidx", "slotwait")

    def __init__(self, eng, fn, dma, gidx):
        self.eng = eng
        self.fn = fn
        self.dma = dma
        self.deps = []
        self.sig = False
        self.sem = None
        self.val = 0
        self.gidx = gidx
        self.slotwait = None


class Kern:
    ENGS = ("pe", "act", "dve", "pool", "sp")
    NSLOT = {"sp": 20, "act": 6, "pool": 12}
    ROLL = 30000

    def __init__(self, nc):
        self.nc = nc
        self.ops = {e: [] for e in self.ENGS}
        self.n = 0
        self.toks = []
        self.es = ExitStack()
        self.nsem = 0

    def tok(self, name=""):
        t = Tok(name)
        self.toks.append(t)
        return t

    def toks_n(self, n, name=""):
        return [self.tok(f"{name}{i}") for i in range(n)]

    def op(self, eng, fn, r=(), w=(), dma=False):
        o = Op(eng, fn, dma, self.n)
        self.n += 1
        deps = {}
        for t in r:
            if t.w is not None:
                deps[id(t.w)] = t.w
        for t in w:
            if t.w is not None:
                deps[id(t.w)] = t.w
            for q in t.r:
                deps[id(q)] = q
        for d in deps.values():
            if d is o:
                continue
            if d.eng == eng and not d.dma and not dma:
                if eng == "pe":
                    continue
            o.deps.append(d)
        for t in r:
            t.r.append(o)
        for t in w:
            t.w = o
            t.r = []
        self.ops[eng].append(o)
        return o

    def barrier(self):
        deps = {}
        for t in self.toks:
            if t.w is not None:
                deps[id(t.w)] = t.w
            for q in t.r:
                deps[id(q)] = q
        dl = list(deps.values())
        for e in self.ENGS:
            o = Op(e, None, False, self.n)
            self.n += 1
            o.deps = [d for d in dl if d.fn is not None]
            self.ops[e].append(o)
        for t in self.toks:
            t.r = []
            t.w = None

    def _newsem(self, name):
        self.nsem += 1
        return self.es.enter_context(self.nc.semaphore(f"{name}_{self.nsem}"))

    def emit(self):
        nc = self.nc
        for e in self.ENGS:
            for o in self.ops[e]:
                for d in o.deps:
                    d.sig = True
        for e in self.ENGS:
            cur = None
            cnt = 0
            slots = None
            slot_uses = None
            slot_last = None
            k = 0
            for o in self.ops[e]:
                if o.fn is None:
                    continue
                if o.dma:
                    if slots is None:
                        ns = self.NSLOT[e]
                        slots = [self._newsem(f"d{e}") for _ in range(ns)]
                        slot_uses = [0] * ns
                        slot_last = [None] * ns
                    s = k % len(slots)
                    k += 1
                    o.slotwait = slot_last[s]
                    slot_uses[s] += 1
                    o.sem = slots[s]
                    o.val = 16 * slot_uses[s]
                    o.sig = True
                    slot_last[s] = o
                elif o.sig:
                    if cur is None or cnt >= self.ROLL:
                        cur = self._newsem(f"c{e}")
                        cnt = 0
                    cnt += 1
                    o.sem = cur
                    o.val = cnt
        with nc.Block() as block:
            def run(e, eng):
                waited = {}
                for o in self.ops[e]:
                    need = {}
                    dl = list(o.deps)
                    if o.slotwait is not None:
                        dl.append(o.slotwait)
                    for d in dl:
                        key = id(d.sem)
                        if waited.get(key, 0) >= d.val:
                            continue
                        if key not in need or need[key][1] < d.val:
                            need[key] = (d.sem, d.val)
                    for key, (sem, val) in need.items():
                        eng.wait_ge(sem, val)
                        waited[key] = val
                    if o.fn is None:
                        continue
                    ins = o.fn(eng)
                    if o.sig:
                        ins.then_inc(o.sem, 16 if o.dma else 1)

            @block.tensor
            def _(eng):
                run("pe", eng)

            @block.scalar
            def _(eng):
                run("act", eng)

            @block.vector
            def _(eng):
                run("dve", eng)

            @block.gpsimd
            def _(eng):
                run("pool", eng)

            @block.sync
            def _(eng):
                run("sp", eng)
        self.es.close()


U8 = mybir.dt.uint8
DTSZ = {F32: 4, BF16: 2, I32: 4, U32: 4}


class Arena:
    def __init__(self, nc, nbytes):
        self.t = nc.alloc_sbuf_tensor("arena", [128, nbytes], U8)
        self.n = nbytes
        self.off = 0
        self.peak = 0

    def alloc(self, shape, dt):
        n = int(np.prod(shape)) * DTSZ[dt]
        n = (n + 63) // 64 * 64
        assert self.off + n <= self.n, f"arena overflow {self.off}+{n}>{self.n}"
        v = self.t[:, self.off:self.off + n].bitcast(dt)
        self.off += n
        self.peak = max(self.peak, self.off)
        tot = int(np.prod(shape))
        v = v[:, 0:tot]
        if len(shape) == 2:
            v = v.rearrange("p (a b) -> p a b", a=shape[0])
        elif len(shape) == 3:
            v = v.rearrange("p (a b c) -> p a b c", a=shape[0], b=shape[1])
        return v

    def mark(self):
        return self.off

    def release(self, m):
        self.off = m

from concourse.bass_utils import run_bass_kernel_spmd

L = 4096
D = 1024
NT = 32
NCH = 8
DIN = 7752
CQ, CK, CV, CQI, CKI, CSU, CSV, CGA, CGB = 0, 1024, 2048, 3072, 3584, 3656, 4680, 5704, 6728
NE = 32
CAP = 512
DUMMY = NE * CAP
KI = 20
EPS = 1e-6
PI = float(np.pi)
MAGIC = 12582912.0
C1 = 6.28125
C2 = 2 * PI - C1
NEG = -1.0e30
ARENA = 206 * 1024

C_INV, C_SGN, C_INVI, C_NH, C_HPI, C_NTHR, C_EPS, C_ONE, C_DUM, C_PW = 0, 1, 2, 34, 35, 36, 37, 38, 39, 40
NCST = 72


def host_consts():
    c = np.zeros((128, NCST), np.float32)
    inv128 = (np.float32(10000.0) ** (-np.arange(0, 128, 2, dtype=np.float32) / np.float32(128))).astype(np.float32)
    inv64 = (np.float32(10000.0) ** (-np.arange(0, 64, 2, dtype=np.float32) / np.float32(64))).astype(np.float32)
    p = np.arange(128)
    c[:, C_INV] = inv128[p % 64]
    c[:, C_SGN] = np.where(p < 64, -1.0, 1.0)
    c[:, C_INVI:C_INVI + 32] = inv64[None, :]
    c[:, C_NH] = -0.5
    c[:, C_HPI] = PI / 2
    c[:, C_NTHR] = -1.0e29
    c[:, C_EPS] = EPS
    c[:, C_ONE] = 1.0
    c[:, C_DUM] = NE * CAP + p
    c[:, C_PW:C_PW + KI + 2] = (2.0 ** -(np.arange(KI + 2) + 1.0))[None, :]
    m = np.zeros((128, 128 * 3 + 64), np.float32)
    t = np.arange(128)[:, None]
    s = np.arange(128)[None, :]
    m[:, 0:128] = np.where(s <= t, 0.0, NEG)
    m[:, 128:256] = np.where(s >= t, 1.0, 0.0)
    m[:, 256:384] = np.where(s <= t, 1.0, 0.0)
    m[:, 384:416] = (np.arange(32) * CAP)[None, :]
    m[:, 416:448] = 1.0
    return c, m


class Prog:
    pass


def build_program(stop_after=None, dbg=False):
    nc = bass.Bass("TRN2", target_bir_lowering=False)
    K = Kern(nc)
    AR = Arena(nc, ARENA)
    P = Prog()
    P.nc = nc

    def din(name, shape, dt=F32):
        return nc.dram_tensor(name, shape, dt, kind="ExternalInput").ap()

    def dscr(name, shape, dt, out=False):
        return nc.dram_tensor(name, shape, dt, kind="ExternalOutput" if (out and dbg) else "Internal").ap()

    x_d = din("x", [L, D])
    posr_d = din("pos_row", [1, L], I32)
    posc_d = din("pos_col", [128, NT], I32)
    g1_d = din("norm1_g", [1, D])
    win_d = din("w_in", [D, DIN])
    gs_d = din("sgu_norm_g", [1, D])
    sw_d = din("sgu_w", [8, 128, 128])
    sb_d = din("sgu_b", [8, 128])
    wa_d = din("w_branch_a", [D, D])
    wb_d = din("w_branch_b", [D, D])
    wo_d = din("w_out", [D, D])
    g2_d = din("norm2_g", [1, D])
    wrg_d = din("w_router_group", [D, 4])
    brg_d = din("b_router_group", [1, 4])
    wre_d = din("w_router_expert", [D, 32])
    bre_d = din("b_router_expert", [1, 32])
    wei_d = din("w_expert_in", [NE, D, 512])
    weo_d = din("w_expert_out", [NE, 256, D])
    gf_d = din("norm_f_g", [1, D])
    cst_d = din("cst", [128, NCST])
    cm_d = din("cmat", [128, 448])
    out_d = nc.dram_tensor("out", [L, D], F32, kind="ExternalOutput").ap()

    qT_s = dscr("qT_s", [8, 128, L], BF16, True)
    kT_s = dscr("kT_s", [8, 128, L], BF16, True)
    v_s = dscr("v_s", [NT, 128, 8 * 129], BF16, True)
    qiT_s = dscr("qiT_s", [5, 128, L], BF16, True)
    suT_s = dscr("suT_s", [D, L], BF16)
    ysgT_s = dscr("ysgT_s", [D, L], BF16, True)
    sgT_s = dscr("sgT_s", [2 * D, L], BF16, True)
    yatT_s = dscr("yatT_s", [D, L], BF16, True)
    h2_s = dscr("h2_s", [L, D], F32, True)
    xs_s = dscr("xs_s", [NE * CAP + 128, D], BF16)
    ys_s = dscr("ys_s", [NE * CAP + 128, D], F32)

    ps = [nc.alloc_psum_tensor(f"ps{i}", [128, 512], F32) for i in range(8)]
    pst = [K.tok(f"ps{i}") for i in range(8)]

    def psb(i):
        return ps[i][:, :].bitcast(BF16)

    def dma(eng, out, in_, r=(), w=()):
        return K.op(eng, lambda e: e.dma_start(out=out, in_=in_), r=r, w=w, dma=True)

    def mm(out, lhsT, rhs, start, stop, r=(), w=(), sgc=False):
        return K.op("pe", lambda e: e.matmul(out, lhsT=lhsT, rhs=rhs, start=start, stop=stop, skip_group_check=sgc), r=r, w=w)

    def tr(out, in_, ident, r=(), w=()):
        return K.op("pe", lambda e: e.transpose(out, in_, ident), r=r, w=w)

    def act(out, in_, func, r=(), w=(), bias=None, scale=1.0, accum=None):
        def f(e):
            kw = {}
            if bias is not None:
                kw["bias"] = bias
            if accum is not None:
                kw["accum_out"] = accum
            return e.activation(out=out, in_=in_, func=func, scale=scale, **kw)
        return K.op("act", f, r=r, w=w)

    def ts(eng, out, in0, s1, op0, s2=None, op1=None, r=(), w=(), accum=None):
        def f(e):
            kw = {}
            if op1 is not None:
                kw["op1"] = op1
            if accum is not None:
                kw["accum_out"] = accum
            return e.tensor_scalar(out=out, in0=in0, scalar1=s1, scalar2=s2, op0=op0, **kw)
        return K.op(eng, f, r=r, w=w)

    def tt(eng, out, in0, in1, op, r=(), w=()):
        return K.op(eng, lambda e: e.tensor_tensor(out=out, in0=in0, in1=in1, op=op), r=r, w=w)

    def stt(out, in0, scalar, in1, op0, op1, r=(), w=(), accum=None):
        def f(e):
            kw = {}
            if accum is not None:
                kw["accum_out"] = accum
            return e.scalar_tensor_tensor(out=out, in0=in0, scalar=scalar, in1=in1, op0=op0, op1=op1, **kw)
        return K.op("dve", f, r=r, w=w)

    def cp(eng, out, in_, r=(), w=()):
        if eng == "act":
            return K.op("act", lambda e: e.activation(out=out, in_=in_, func=AF.Copy), r=r, w=w)
        return K.op(eng, lambda e: e.tensor_copy(out, in_), r=r, w=w)

    def mset(eng, out, val, r=(), w=()):
        return K.op(eng, lambda e: e.memset(out, val), r=r, w=w)

    class B:
        def __init__(self, shape, dt, name=""):
            self.ap = AR.alloc(shape, dt)
            self.t = K.tok(name)

    def ring(n, shape, dt, name=""):
        return [B(shape, dt, f"{name}{i}") for i in range(n)]

    cst = B([NCST], F32, "cst")
    cm = B([448], F32, "cm")
    ident = B([128], BF16, "ident")
    identf = B([128], F32, "identf")
    wi_sb = B([NT, 8], F32, "wi")
    rt_gate = B([NT * 2], F32, "gate")
    rt_pos = B([NT * 2], I32, "pos")
    dma("sp", cst.ap, cst_d, w=[cst.t])
    dma("sp", cm.ap, cm_d, w=[cm.t])
    mset("pool", identf.ap, 0.0, w=[identf.t])
    K.op("pool", lambda e: e.affine_select(out=identf.ap, in_=identf.ap, pattern=[[-1, 128]], compare_op=ALU.not_equal,
                                           fill=1.0, base=0, channel_multiplier=1), r=[identf.t], w=[identf.t])
    cp("pool", ident.ap, identf.ap, r=[identf.t], w=[ident.t])

    zt = B([D], BF16, "zt")
    mset("pool", zt.ap, 0.0, w=[zt.t])
    nrow_t = (NE * CAP + 128) // 128
    for z0 in range(0, nrow_t, 16):
        zn = min(16, nrow_t - z0)
        dma("act", xs_s[z0 * 128:(z0 + zn) * 128, :].rearrange("(a p) d -> p a d", p=128),
            zt.ap.unsqueeze(1).broadcast_to([128, zn, D]), r=[zt.t])

    def col(b, j, n=1):
        return b.ap[:, j:j + n]

    def sincos(ang, n, kk, sin_out, cos_out, r, w):
        tk = K.tok()
        ts("dve", kk, ang, 1.0 / (2 * PI), ALU.mult, MAGIC, ALU.add, r=r, w=[tk])
        ts("dve", kk, kk, MAGIC, ALU.subtract, r=[tk], w=[tk])
        stt(ang, kk, -C1, ang, ALU.mult, ALU.add, r=r + [tk], w=r)
        stt(ang, kk, -C2, ang, ALU.mult, ALU.add, r=r + [tk], w=r)
        ts("dve", ang, ang, PI, ALU.min, -PI, ALU.max, r=r, w=r)
        act(sin_out, ang, AF.Sin, r=r, w=w)
        ts("dve", kk, ang, -1.0, ALU.mult, r=r, w=[tk])
        tt("dve", kk, kk, ang, ALU.max, r=r + [tk], w=[tk])
        act(cos_out, kk, AF.Sin, bias=col(cst, C_HPI), scale=-1.0, r=[tk, cst.t], w=w)


    def finish():
        K.barrier()
        K.emit()
        P.__dict__.update(dict(K=K, AR=AR))
        return P

    m_0 = AR.mark()
    xnT = AR.alloc([8, L], BF16)
    xnT_t = [K.tok(f"xnT{i}") for i in range(NT)]
    m_p1 = AR.mark()

    xbuf = ring(3, [D], F32, "xb")
    junk = B([D], F32, "junk")
    g1b = B([D], F32, "g1b")
    xnb = ring(2, [D], BF16, "xnb")
    ms1 = B([NT], F32, "ms1")
    rs1 = B([NT], F32, "rs1")
    ms1_t = [K.tok() for _ in range(NT)]
    rs1_t = [K.tok() for _ in range(NT)]
    dma("sp", g1b.ap, g1_d.partition_broadcast(128), w=[g1b.t])

    def a_front(i):
        xb = xbuf[i % 3]
        dma("sp", xb.ap, x_d[i * 128:(i + 1) * 128, :], w=[xb.t])
        stt(junk.ap, xb.ap, 1.0 / D, xb.ap, ALU.mult, ALU.mult, r=[xb.t], w=[junk.t, ms1_t[i]], accum=ms1.ap[:, i:i + 1])
        ts("pool", rs1.ap[:, i:i + 1], ms1.ap[:, i:i + 1], EPS, ALU.add, r=[ms1_t[i]], w=[rs1_t[i]])
        tt("pool", rs1.ap[:, i:i + 1], rs1.ap[:, i:i + 1], col(cst, C_NH), ALU.pow, r=[rs1_t[i], cst.t], w=[rs1_t[i]])

    def a_back(i):
        xb = xbuf[i % 3]
        nb = xnb[i % 2]
        stt(nb.ap, xb.ap, rs1.ap[:, i:i + 1], g1b.ap, ALU.mult, ALU.mult, r=[xb.t, rs1_t[i], g1b.t], w=[nb.t])
        bk = 4 + (i % 2)
        for kc in range(8):
            tr(psb(bk)[:, kc * 128:(kc + 1) * 128], nb.ap[:, kc * 128:(kc + 1) * 128], ident.ap, r=[nb.t, ident.t], w=[pst[bk]])
        cp("act", xnT[:, :, i * 128:(i + 1) * 128], psb(bk).rearrange("p (a b) -> p a b", a=8), r=[pst[bk]], w=[xnT_t[i]])

    for i in range(NT + 1):
        if i < NT:
            a_front(i)
        if i >= 1:
            a_back(i - 1)
    K.barrier()
    AR.release(m_p1)

    wbuf = ring(3, [8, 512], BF16, "wb")
    wstg = ring(2, [4, 512], F32, "wstg")
    wstate = {"n": 0}

    def load_w(c0, ncols):
        b = wbuf[wstate["n"] % 3]
        wstate["n"] += 1
        for hf in range(2):
            sg = wstg[hf]
            dma("sp", sg.ap[:, :, 0:ncols], win_d[hf * 512:(hf + 1) * 512, c0:c0 + ncols].rearrange("(kc p) c -> p kc c", p=128), w=[sg.t])
            cp("pool", b.ap[:, 4 * hf:4 * hf + 4, 0:ncols], sg.ap[:, :, 0:ncols], r=[sg.t], w=[b.t])
        return b

    bank_rr = {"n": 0}

    def next_bank():
        bk = bank_rr["n"] % 4
        bank_rr["n"] += 1
        return bk

    def fm_block(wb, j, c, bk):
        for kc in range(8):
            mm(ps[bk][:, :], wb.ap[:, kc, j * 128:(j + 1) * 128], xnT[:, kc, c * 512:(c + 1) * 512], kc == 0, kc == 7,
               r=[wb.t] + xnT_t[4 * c:4 * c + 4], w=[pst[bk]])

    def tm_block(wb, ncols, i, bk, col0=0):
        for kc in range(8):
            mm(ps[bk][:, 0:ncols], xnT[:, kc, i * 128:(i + 1) * 128], wb.ap[:, kc, col0:col0 + ncols], kc == 0, kc == 7,
               r=[wb.t, xnT_t[i]], w=[pst[bk]])

    m_b1 = AR.mark()
    cosT = B([L], F32, "cosT")
    sinT = B([L], F32, "sinT")
    posi = B([1024], I32, "posi")
    angw = B([1024], F32, "angw")
    kkw = B([1024], F32, "kkw")
    for cc in range(4):
        sl = slice(cc * 1024, (cc + 1) * 1024)
        dma("sp", posi.ap, posr_d[:, sl].partition_broadcast(128), w=[posi.t])
        cp("dve", angw.ap, posi.ap, r=[posi.t], w=[angw.t])
        ts("dve", angw.ap, angw.ap, col(cst, C_INV), ALU.mult, r=[angw.t, cst.t], w=[angw.t])
        sincos(angw.ap, 1024, kkw.ap, sinT.ap[:, sl], cosT.ap[:, sl], r=[angw.t], w=[sinT.t, cosT.t])
        ts("dve", sinT.ap[:, sl], sinT.ap[:, sl], col(cst, C_SGN), ALU.mult, r=[sinT.t, cst.t], w=[sinT.t])
    posc = B([NT], I32, "posc")
    poscf = B([NT], F32, "poscf")
    sinI = B([NT, 32], F32, "sinI")
    cosI = B([NT, 32], F32, "cosI")
    dma("sp", posc.ap, posc_d, w=[posc.t])
    cp("dve", poscf.ap, posc.ap, r=[posc.t], w=[poscf.t])
    angI = angw.ap.rearrange("p (a b) -> p a b", a=NT)
    tt("dve", angI, poscf.ap.unsqueeze(2).broadcast_to([128, NT, 32]),
       cst.ap[:, C_INVI:C_INVI + 32].unsqueeze(1).broadcast_to([128, NT, 32]), ALU.mult, r=[poscf.t, cst.t, angw.t], w=[angw.t])
    sincos(angw.ap, 1024, kkw.ap, sinI.ap.rearrange("p a b -> p (a b)"), cosI.ap.rearrange("p a b -> p (a b)"),
           r=[angw.t], w=[sinI.t, cosI.t])

    t1r = ring(2, [512], F32, "t1")
    t2r = ring(2, [512], F32, "t2")
    qor = ring(3, [512], BF16, "qo")
    n_rope = {"n": 0}

    def rope_fm(bk, c, dst):
        k_ = n_rope["n"]
        n_rope["n"] += 1
        t1 = t1r[k_ % 2]
        t2 = t2r[k_ % 2]
        qo = qor[k_ % 3]
        sl = slice(c * 512, (c + 1) * 512)
        tt("dve", t1.ap, ps[bk][:, :], cosT.ap[:, sl], ALU.mult, r=[pst[bk], cosT.t], w=[t1.t])
        tt("dve", t2.ap[0:64, :], ps[bk][64:128, :], sinT.ap[0:64, sl], ALU.mult, r=[pst[bk], sinT.t], w=[t2.t])
        tt("dve", t2.ap[64:128, :], ps[bk][0:64, :], sinT.ap[64:128, sl], ALU.mult, r=[pst[bk], sinT.t], w=[t2.t])
        tt("pool", qo.ap, t1.ap, t2.ap, ALU.add, r=[t1.t, t2.t], w=[qo.t])
        dma("sp", dst, qo.ap, r=[qo.t])

    qk_blocks = [(CQ, qT_s, 0), (CQ + 512, qT_s, 4), (CK, kT_s, 0), (CK + 512, kT_s, 4)]
    wnext = load_w(qk_blocks[0][0], 512)
    for bi, (c0, dst_s, h0) in enumerate(qk_blocks):
        wb = wnext
        wnext = load_w(qk_blocks[bi + 1][0], 512) if bi + 1 < 4 else load_w(CV, 512)
        for c in range(NCH):
            for j in range(4):
                bk = next_bank()
                fm_block(wb, j, c, bk)
                rope_fm(bk, c, dst_s[h0 + j, :, c * 512:(c + 1) * 512])
    wv0 = wnext
    wv1 = load_w(CV + 512, 512)
    wqi = load_w(CQI, 512)
    vt = ring(2, [8, 129], BF16, "vt")
    for b_ in vt:
        mset("pool", b_.ap[:, :, 128:129], 1.0, w=[b_.t])
    for i in range(NT):
        v_ = vt[i % 2]
        for half, wb in enumerate((wv0, wv1)):
            bk = next_bank()
            tm_block(wb, 512, i, bk)
            cp("act", v_.ap[:, half * 4:(half + 1) * 4, 0:128], ps[bk][:, :].rearrange("p (a b) -> p a b", a=4), r=[pst[bk]], w=[v_.t])
        dma("sp", v_s[i].rearrange("p (h d) -> p h d", h=8), v_.ap, r=[v_.t])
    wkw = load_w(CKI, 72)
    ra = ring(3, [9, 32], F32, "ra")
    rb = ring(3, [9, 32], F32, "rb")
    qst = ring(3, [9, 64], F32, "qst")
    qr = ring(3, [640], BF16, "qr")
    qiT_c = ring(2, [5, 512], BF16, "qiTc")
    def ip_front(i):
        bq = next_bank()
        tm_block(wqi, 512, i, bq)
        bkw = next_bank()
        tm_block(wkw, 72, i, bkw)
        q_ = qr[i % 3]
        a_, b2_, st_ = ra[i % 3], rb[i % 3], qst[i % 3]
        cp("act", st_.ap[:, 0:8, :], ps[bq][:, :].rearrange("p (h d) -> p h d", h=8), r=[pst[bq]], w=[st_.t])
        cp("act", st_.ap[:, 8, :], ps[bkw][:, 0:64], r=[pst[bkw]], w=[st_.t])
        cosb = cosI.ap[:, i, :].unsqueeze(1).broadcast_to([128, 9, 32])
        sinb = sinI.ap[:, i, :].unsqueeze(1).broadcast_to([128, 9, 32])
        qo_ = q_.ap[:, 0:576].rearrange("p (h d) -> p h d", h=9)
        rd = [st_.t, cosI.t, sinI.t]
        tt("dve", a_.ap, st_.ap[:, :, 0:32], cosb, ALU.mult, r=rd, w=[a_.t])
        tt("dve", b2_.ap, st_.ap[:, :, 32:64], sinb, ALU.mult, r=rd, w=[b2_.t])
        tt("pool", qo_[:, :, 0:32], a_.ap, b2_.ap, ALU.subtract, r=[a_.t, b2_.t], w=[q_.t])
        tt("dve", a_.ap, st_.ap[:, :, 32:64], cosb, ALU.mult, r=rd, w=[a_.t])
        tt("dve", b2_.ap, st_.ap[:, :, 0:32], sinb, ALU.mult, r=rd, w=[b2_.t])
        tt("pool", qo_[:, :, 32:64], a_.ap, b2_.ap, ALU.add, r=[a_.t, b2_.t], w=[q_.t])
        cp("pool", q_.ap[:, 576:640], q_.ap[:, 512:576], r=[q_.t], w=[q_.t])
        cp("act", wi_sb.ap[:, i, :], ps[bkw][:, 64:72], r=[pst[bkw]], w=[wi_sb.t])

    def ip_back(i):
        c, tau = i // 4, i % 4
        q_ = qr[i % 3]
        bt = 4 + (i % 2)
        for jj in range(5):
            tr(psb(bt)[:, jj * 128:(jj + 1) * 128], q_.ap[:, jj * 128:(jj + 1) * 128], ident.ap, r=[q_.t, ident.t], w=[pst[bt]])
        qc = qiT_c[c % 2]
        cp("act", qc.ap[:, :, tau * 128:(tau + 1) * 128], psb(bt)[:, 0:640].rearrange("p (a b) -> p a b", a=5), r=[pst[bt]], w=[qc.t])
        if tau == 3:
            dma("sp", qiT_s[:, :, c * 512:(c + 1) * 512].rearrange("a p t -> p a t"), qc.ap, r=[qc.t])
    for i in range(NT + 1):
        if i < NT:
            ip_front(i)
        if i >= 1:
            ip_back(i - 1)
    K.barrier()
    AR.release(m_b1)
    if stop_after == "1b":
        return finish()
    m_b2 = AR.mark()
    sub_r = ring(3, [512], BF16, "sub")
    su_t = [K.tok(f"suT{c}") for c in range(NCH)]
    w0 = load_w(CSU, 512)
    w1 = load_w(CSU + 512, 512)
    w2 = load_w(CSV, 512)
    nsub = {"n": 0}

    def act_block_out(bk, func, dst, wtok=()):
        o = sub_r[nsub["n"] % 3]
        nsub["n"] += 1
        act(o.ap, ps[bk][:, :], func, r=[pst[bk]], w=[o.t])
        dma("sp", dst, o.ap, r=[o.t], w=list(wtok))

    for blk, wb in enumerate((w0, w1)):
        for c in range(NCH):
            for j in range(4):
                bk = next_bank()
                fm_block(wb, j, c, bk)
                f0 = (blk * 4 + j) * 128
                act_block_out(bk, AF.Gelu_apprx_tanh, suT_s[f0:f0 + 128, c * 512:(c + 1) * 512], wtok=[su_t[c]])
    w3 = load_w(CSV + 512, 512)
    swf = B([8, 128], F32, "swf")
    swb = B([8, 128], BF16, "swb")
    WtT = B([8, 128], BF16, "WtT")
    dma("sp", swf.ap, sw_d.rearrange("g t s -> t g s"), w=[swf.t])
    tt("dve", swf.ap, swf.ap, cm.ap[:, 256:384].unsqueeze(1).broadcast_to([128, 8, 128]), ALU.mult, r=[swf.t, cm.t], w=[swf.t])
    cp("dve", swb.ap, swf.ap, r=[swf.t], w=[swb.t])
    for g in range(8):
        tr(psb(4)[:, g * 128:(g + 1) * 128], swb.ap[:, g, :], ident.ap, r=[swb.t, ident.t], w=[pst[4]])
    cp("act", WtT.ap, psb(4).rearrange("p (a b) -> p a b", a=8), r=[pst[4]], w=[WtT.t])
    bf_ = B([8, 128], F32, "bf")
    bhl = B([8, 128], BF16, "bhl")
    bhf = B([8, 128], F32, "bhf")
    ones2 = B([128], BF16, "ones2")
    mset("pool", ones2.ap, 1.0, w=[ones2.t])
    mset("pool", bf_.ap, 0.0, w=[bf_.t])
    dma("sp", bf_.ap[0:1, :, :], sb_d.rearrange("(o g) t -> o g t", o=1), r=[bf_.t], w=[bf_.t])
    dma("sp", bf_.ap[1:2, :, :], sb_d.rearrange("(o g) t -> o g t", o=1), r=[bf_.t], w=[bf_.t])
    cp("dve", bhl.ap, bf_.ap, r=[bf_.t], w=[bhl.t])
    cp("dve", bhf.ap, bhl.ap, r=[bhl.t], w=[bhf.t])
    tt("dve", bhf.ap, bf_.ap, bhf.ap, ALU.subtract, r=[bf_.t, bhf.t], w=[bhf.t])
    cp("dve", bf_.ap, bhl.ap, r=[bhl.t, bf_.t], w=[bf_.t])
    sel = B([1], F32, "sel")
    mset("pool", sel.ap, 1.0, w=[sel.t])
    K.op("pool", lambda e: e.affine_select(out=sel.ap, in_=sel.ap, pattern=[[0, 1]], compare_op=ALU.is_equal,
                                           fill=0.0, base=0, channel_multiplier=1), r=[sel.t], w=[sel.t])
    tt("dve", bf_.ap, bf_.ap, bhf.ap, ALU.subtract, r=[bf_.t, bhf.t], w=[bf_.t])
    stt(bhf.ap, bf_.ap, sel.ap[:, 0:1], bhf.ap, ALU.mult, ALU.add, r=[bf_.t, sel.t, bhf.t], w=[bhf.t])
    cp("dve", bhl.ap, bhf.ap, r=[bhf.t], w=[bhl.t])

    gsb = B([D], F32, "gsb")
    dma("sp", gsb.ap, gs_d.partition_broadcast(128), w=[gsb.t])
    gv = ring(3, [D], F32, "gv")
    vln = ring(3, [D], BF16, "vln")
    bst = B([NT, 12], F32, "bst")
    mv = B([NT, 2], F32, "mv")
    rsd = B([NT], F32, "rsd")
    bst_t = [K.tok() for _ in range(NT)]
    mv_t = [K.tok() for _ in range(NT)]
    rsd_t = [K.tok() for _ in range(NT)]
    suc = ring(2, [8, 512], BF16, "suc")
    ysg = ring(2, [8, 512], BF16, "ysg")
    for c in range(NCH):
        su_c = suc[c % 2]
        ys_c = ysg[c % 2]
        dma("sp", su_c.ap, suT_s[:, c * 512:(c + 1) * 512].rearrange("(g p) t -> p g t", p=128), r=[su_t[c]], w=[su_c.t])
        def sv_front(tau, c=c):
            i = 4 * c + tau
            g_ = gv[i % 3]
            vl = vln[i % 3]
            sb0 = 6
            for half, wb in enumerate((w2, w3)):
                bk = next_bank()
                tm_block(wb, 512, i, bk)
                act(g_.ap[:, half * 512:(half + 1) * 512], ps[bk][:, :], AF.Gelu_apprx_tanh, r=[pst[bk]], w=[g_.t])
            K.op("dve", lambda e, i=i, g_=g_: e.bn_stats(out=bst.ap[:, i, 0:6], in_=g_.ap[:, 0:512]), r=[g_.t], w=[bst_t[i]])
            K.op("dve", lambda e, i=i, g_=g_: e.bn_stats(out=bst.ap[:, i, 6:12], in_=g_.ap[:, 512:1024]), r=[g_.t], w=[bst_t[i]])
            K.op("dve", lambda e, i=i: e.bn_aggr(out=mv.ap[:, i, :], in_=bst.ap[:, i, :]), r=[bst_t[i]], w=[mv_t[i]])
            ts("pool", rsd.ap[:, i:i + 1], mv.ap[:, i, 1:2], EPS, ALU.add, r=[mv_t[i]], w=[rsd_t[i]])
            tt("pool", rsd.ap[:, i:i + 1], rsd.ap[:, i:i + 1], col(cst, C_NH), ALU.pow, r=[rsd_t[i], cst.t], w=[rsd_t[i]])
            ts("dve", g_.ap, g_.ap, mv.ap[:, i, 0:1], ALU.subtract, rsd.ap[:, i:i + 1], ALU.mult, r=[g_.t, mv_t[i], rsd_t[i]], w=[g_.t])
            tt("pool", vl.ap, g_.ap, gsb.ap, ALU.mult, r=[g_.t, gsb.t], w=[vl.t])

        def sv_back(tau, c=c, su_c=su_c, ys_c=ys_c):
            i = 4 * c + tau
            vl = vln[i % 3]
            sb0 = 6
            for g in range(8):
                bk = sb0 + g // 4
                o_ = ps[bk][:, (g % 4) * 128:(g % 4 + 1) * 128]
                mm(o_, vl.ap[:, g * 128:(g + 1) * 128], WtT.ap[:, g, :], True, False, r=[vl.t, WtT.t], w=[pst[bk]])
                mm(o_, ones2.ap[0:2, :], bhl.ap[0:2, g, :], False, True, r=[ones2.t, bhl.t], w=[pst[bk]])
            for b_ in range(2):
                tt("dve", ys_c.ap[:, 4 * b_:4 * b_ + 4, tau * 128:(tau + 1) * 128],
                   ps[sb0 + b_][:, :].rearrange("p (a b) -> p a b", a=4),
                   su_c.ap[:, 4 * b_:4 * b_ + 4, tau * 128:(tau + 1) * 128], ALU.mult, r=[pst[sb0 + b_], su_c.t], w=[ys_c.t])
        for tau in range(5):
            if tau < 4:
                sv_front(tau)
            if tau >= 1:
                sv_back(tau - 1)
        dma("sp", ysgT_s[:, c * 512:(c + 1) * 512].rearrange("(g p) t -> p g t", p=128), ys_c.ap, r=[ys_c.t])
    wg = load_w(CGA, 512)
    for blk in range(4):
        wb = wg
        if blk < 3:
            wg = load_w(CGA + (blk + 1) * 512, 512)
        for c in range(NCH):
            for j in range(4):
                bk = next_bank()
                fm_block(wb, j, c, bk)
                f0 = (blk * 4 + j) * 128
                act_block_out(bk, AF.Sigmoid, sgT_s[f0:f0 + 128, c * 512:(c + 1) * 512])
    K.barrier()
    AR.release(m_0)
    if stop_after == "1":
        return finish()
    SCALE = float(128 ** -0.5)
    vres = AR.alloc([NT, 8, 129], BF16)
    v_t = [K.tok(f"v{i}") for i in range(NT)]
    kiT2 = AR.alloc([L], BF16)
    ki_t = [K.tok(f"ki{i}") for i in range(4)]
    for i in range(NT):
        dma("sp", vres[:, i, :, :], v_s[i].rearrange("p (h d) -> p h d", h=8), w=[v_t[i]])
        if i % 8 == 0:
            q4 = i // 8
            dma("sp", kiT2[:, q4 * 1024:(q4 + 1) * 1024], qiT_s[4, :, q4 * 1024:(q4 + 1) * 1024], w=[ki_t[q4]])
    kTr = ring(2, [L], BF16, "kTr")
    qTc = B([8, 512], BF16, "qTc")
    qiTc = B([4, 512], BF16, "qiTc")
    S = B([L], F32, "S")
    maskb = B([L], BF16, "maskb")
    maskT = B([NT, 512], BF16, "maskT")
    rbuf = ring(3, [512], F32, "rb")
    ebuf = ring(3, [512], BF16, "eb")
    pbuf = ring(3, [512], BF16, "pb")
    ytile = B([4, D], BF16, "ytile")
    yT = B([8, 512], BF16, "yT")
    vmax = B([1], F32, "vmax")
    vmin = B([1], F32, "vmin")
    rngv = B([1], F32, "rngv")
    hw = B([KI + 2], F32, "hw")
    cand = B([1], F32, "cand")
    cnt = B([1], F32, "cnt")
    msg = B([1], F32, "msg")
    thr = B([1], F32, "thr")
    recr = ring(2, [4], F32, "rec")
    ctr = {"sc": 0, "lg": 0, "tr": 0, "e": 0}

    for c in range(NCH):
        csl = slice(c * 512, (c + 1) * 512)
        dma("sp", qTc.ap, qT_s[:, :, csl].rearrange("h p t -> p h t"), w=[qTc.t])
        dma("sp", qiTc.ap, qiT_s[0:4, :, csl].rearrange("a p t -> p a t"), w=[qiTc.t])
        for tau in range(4):
            i = 4 * c + tau
            n = 128 * (i + 1)
            nv = 128 * i
            for sc in range((n + 511) // 512):
                w_ = min(512, n - 512 * sc)
                sl = slice(sc * 512, sc * 512 + w_)
                for h in range(8):
                    bk = ctr["sc"] % 2
                    ctr["sc"] += 1
                    pr = slice(64 * (h % 2), 64 * (h % 2) + 64)
                    mm(ps[bk][:, 0:w_], qiTc.ap[pr, h // 2, tau * 128:(tau + 1) * 128], kiT2[pr, sl], True, True,
                       r=[qiTc.t, ki_t[sc // 2]], w=[pst[bk]])
                    rb = rbuf[ctr["sc"] % 3]
                    act(rb.ap[:, 0:w_], ps[bk][:, 0:w_], AF.Relu, r=[pst[bk]], w=[rb.t])
                    if h == 0:
                        ts("dve", S.ap[:, sl], rb.ap[:, 0:w_], wi_sb.ap[:, i, 0:1], ALU.mult, r=[rb.t, wi_sb.t], w=[S.t])
                    else:
                        stt(S.ap[:, sl], rb.ap[:, 0:w_], wi_sb.ap[:, i, h:h + 1], S.ap[:, sl], ALU.mult, ALU.add,
                            r=[rb.t, wi_sb.t, S.t], w=[S.t])
            tt("pool", S.ap[:, nv:n], S.ap[:, nv:n], cm.ap[:, 0:128], ALU.add, r=[S.t, cm.t], w=[S.t])
            if i >= 2:
                K.op("dve", lambda e, n=n: e.tensor_reduce(out=vmax.ap, in_=S.ap[:, 0:n], axis=AX.X, op=ALU.max), r=[S.t], w=[vmax.t])
                K.op("dve", lambda e, nv=nv: e.tensor_reduce(out=vmin.ap, in_=S.ap[:, 0:nv], axis=AX.X, op=ALU.min), r=[S.t], w=[vmin.t])
                tt("dve", rngv.ap, vmax.ap, vmin.ap, ALU.subtract, r=[vmax.t, vmin.t], w=[rngv.t])
                ts("dve", hw.ap, cst.ap[:, C_PW:C_PW + KI + 2], rngv.ap[:, 0:1], ALU.mult, r=[cst.t, rngv.t], w=[hw.t])
                tt("dve", cand.ap, vmin.ap, hw.ap[:, 0:1], ALU.add, r=[vmin.t, hw.t], w=[cand.t])
                for k in range(KI):
                    ts("dve", maskb.ap[:, 0:n], S.ap[:, 0:n], cand.ap[:, 0:1], ALU.is_ge, None, ALU.add,
                       r=[S.t, cand.t], w=[maskb.t, cnt.t], accum=cnt.ap)
                    ts("dve", msg.ap, cnt.ap, 255.5, ALU.is_ge, 0.5, ALU.subtract, r=[cnt.t], w=[msg.t])
                    stt(cand.ap, msg.ap, hw.ap[:, k:k + 1], cand.ap, ALU.mult, ALU.add, r=[msg.t, hw.t, cand.t], w=[cand.t])
                tt("dve", thr.ap, cand.ap, hw.ap[:, KI:KI + 1], ALU.subtract, r=[cand.t, hw.t], w=[thr.t])
                thr_ap, thr_t = thr.ap[:, 0:1], thr.t
            else:
                thr_ap, thr_t = col(cst, C_NTHR), cst.t
            ts("dve", maskb.ap[:, 0:n], S.ap[:, 0:n], thr_ap, ALU.is_ge, r=[S.t, thr_t], w=[maskb.t])
            for j0 in range(0, i + 1, 8):
                nb = min(8, i + 1 - j0)
                bk = 2 + ctr["tr"] % 2
                ctr["tr"] += 1
                for jj in range(nb):
                    tr(psb(bk)[:, jj * 128:(jj + 1) * 128], maskb.ap[:, (j0 + jj) * 128:(j0 + jj + 1) * 128], ident.ap,
                       r=[maskb.t, ident.t], w=[pst[bk]])
                cp("act", maskT.ap[:, j0:j0 + nb, tau * 128:(tau + 1) * 128],
                   psb(bk)[:, 0:nb * 128].rearrange("p (a b) -> p a b", a=nb), r=[pst[bk]], w=[maskT.t])
        nj = 4 * c + 4
        steps = [(h, j) for h in range(8) for j in range(nj)]
        LB = (1, 2, 3)

        def emit_qk(k):
            h, j = steps[k]
            kt = kTr[h % 2]
            if j == 0:
                dma("sp", kt.ap[:, 0:nj * 128], kT_s[h, :, 0:nj * 128], w=[kt.t])
            r0 = max(0, j - 4 * c)
            N = 512 - 128 * r0
            bk = LB[k % 3]
            mm(ps[bk][:, 0:N], kt.ap[:, j * 128:(j + 1) * 128], qTc.ap[:, h, r0 * 128:512], True, True,
               r=[kt.t, qTc.t], w=[pst[bk]])

        emit_qk(0)
        emit_qk(1)
        for k, (h, j) in enumerate(steps):
            if k + 2 < len(steps):
                emit_qk(k + 2)
            accA = 4 + 2 * (h % 2)
            accB = accA + 1
            r0 = max(0, j - 4 * c)
            N = 512 - 128 * r0
            bk = LB[k % 3]
            e_ = ebuf[k % 3]
            p_ = pbuf[k % 3]
            act(e_.ap[:, 0:N], ps[bk][:, 0:N], AF.Exp, scale=SCALE, r=[pst[bk]], w=[e_.t])
            tt("dve", p_.ap[:, 0:N], e_.ap[:, 0:N], maskT.ap[:, j, r0 * 128:512], ALU.mult, r=[e_.t, maskT.t], w=[p_.t])
            for tau in range(r0, 4):
                if tau < 3:
                    o_, ob = ps[accA][:, tau * 129:(tau + 1) * 129], accA
                else:
                    o_, ob = ps[accB][:, 0:129], accB
                first = (j == 0 and tau in (0, 3))
                mm(o_, p_.ap[:, (tau - r0) * 128:(tau - r0 + 1) * 128], vres[:, j, h, :], first, j == nj - 1,
                   r=[p_.t, v_t[j]], w=[pst[ob]], sgc=True)
            if j == nj - 1:
                rc = recr[h % 2]
                K.op("dve", lambda e, rc=rc, accA=accA: e.reciprocal(
                    out=rc.ap[:, 0:3], in_=ps[accA][:, 0:387].rearrange("p (a b) -> p a b", b=129)[:, :, 128]), r=[pst[accA]], w=[rc.t])
                K.op("dve", lambda e, rc=rc, accB=accB: e.reciprocal(out=rc.ap[:, 3:4], in_=ps[accB][:, 128:129]), r=[pst[accB]], w=[rc.t])
                for tau in range(4):
                    src, sb_ = (ps[accA][:, tau * 129:tau * 129 + 128], accA) if tau < 3 else (ps[accB][:, 0:128], accB)
                    ts("dve", ytile.ap[:, tau, h * 128:(h + 1) * 128], src, rc.ap[:, tau:tau + 1], ALU.mult, r=[pst[sb_], rc.t], w=[ytile.t])
        for tau in range(4):
            bk = 2 + ctr["tr"] % 2
            ctr["tr"] += 1
            for kc in range(8):
                tr(psb(bk)[:, kc * 128:(kc + 1) * 128], ytile.ap[:, tau, kc * 128:(kc + 1) * 128], ident.ap, r=[ytile.t, ident.t], w=[pst[bk]])
            cp("act", yT.ap[:, :, tau * 128:(tau + 1) * 128], psb(bk).rearrange("p (a b) -> p a b", a=8), r=[pst[bk]], w=[yT.t])
        dma("sp", yatT_s[:, csl].rearrange("(g p) t -> p g t", p=128), yT.ap, r=[yT.t])
    K.barrier()
    AR.release(m_0)
    if stop_after == "2":
        return finish()
    m_3 = AR.mark()
    Wa_sb = B([8, D], BF16, "Wa")
    Wb_sb = B([8, D], BF16, "Wb")
    Wo_sb = B([8, D], BF16, "Wo")
    w3stg = ring(2, [2, D], F32, "w3stg")
    for wi3, (wsb, wd) in enumerate(((Wa_sb, wa_d), (Wb_sb, wb_d), (Wo_sb, wo_d))):
        for q4 in range(4):
            sg = w3stg[(wi3 * 4 + q4) % 2]
            dma("sp", sg.ap, wd[q4 * 256:(q4 + 1) * 256, :].rearrange("(kc p) n -> p kc n", p=128), w=[sg.t])
            cp("dve" if q4 % 2 else "act", wsb.ap[:, 2 * q4:2 * q4 + 2, :], sg.ap, r=[sg.t], w=[wsb.t])
    g2b = B([D], F32, "g2b")
    dma("sp", g2b.ap, g2_d.partition_broadcast(128), w=[g2b.t])
    wr = B([8, 36], F32, "wr")
    br = B([36], F32, "br")
    dma("sp", wr.ap[:, :, 0:4], wrg_d.rearrange("(kc p) n -> p kc n", p=128), w=[wr.t])
    dma("sp", wr.ap[:, :, 4:36], wre_d.rearrange("(kc p) n -> p kc n", p=128), w=[wr.t])
    dma("sp", br.ap[:, 0:4], brg_d.partition_broadcast(128), w=[br.t])
    dma("sp", br.ap[:, 4:36], bre_d.partition_broadcast(128), w=[br.t])
    trib = B([128], BF16, "trib")
    onesb = B([128], BF16, "onesb")
    cp("pool", trib.ap, cm.ap[:, 128:256], r=[cm.t], w=[trib.t])
    mset("pool", onesb.ap, 1.0, w=[onesb.t])
    base = B([32], F32, "base")
    capb = B([32], F32, "capb")
    ts("pool", base.ap, cm.ap[:, 384:416], -1.0, ALU.add, r=[cm.t], w=[base.t])
    ts("pool", capb.ap, cm.ap[:, 384:416], float(CAP) - 0.5, ALU.add, r=[cm.t], w=[capb.t])
    inr = [ring(2, [8, 512], BF16, nm) for nm in ("ysgc", "yatc", "sgac", "sgbc")]
    mT = B([8, 512], BF16, "mT")
    t1p = ring(2, [512], F32, "t1p")
    t2p = ring(2, [512], F32, "t2p")
    xtr = ring(2, [D], F32, "xt")
    h2r = ring(2, [D], F32, "h2t")
    ms2 = B([NT], F32, "ms2")
    rs2 = B([NT], F32, "rs2")
    xn2f = B([D], F32, "xn2f")
    xn2b = ring(2, [D], BF16, "xn2b")
    xn2T = B([8, 128], F32, "xn2T")
    lgt = B([36], F32, "lgt")
    sm = {nm: B([sz], F32, nm) for nm, sz in (
        ("gmax", 1), ("negg", 1), ("ohg", 4), ("j4", 4), ("se", 1), ("gp", 1), ("esel", 8), ("m1", 1), ("oh1", 8), ("es2", 8),
        ("m2", 1), ("oh2", 8), ("dlt", 1), ("ex", 1), ("den", 1), ("p1", 1), ("p2", 1), ("M1", 32), ("M2", 32), ("posf", 32),
        ("okf", 32), ("j32", 32), ("pk", 2), ("ok", 2), ("gk", 2))}
    Mb = B([32], BF16, "Mb")

    def S_(nm):
        return sm[nm].ap

    def T_(nm):
        return sm[nm].t

    def load_chunk3(c):
        csl = slice(c * 512, (c + 1) * 512)
        ysg_c, yat_c, sga_c, sgb_c = [rg[c % 2] for rg in inr]
        dma("sp", ysg_c.ap, ysgT_s[:, csl].rearrange("(g p) t -> p g t", p=128), w=[ysg_c.t])
        dma("sp", yat_c.ap, yatT_s[:, csl].rearrange("(g p) t -> p g t", p=128), w=[yat_c.t])
        dma("sp", sga_c.ap, sgT_s[0:D, csl].rearrange("(g p) t -> p g t", p=128), w=[sga_c.t])
        dma("sp", sgb_c.ap, sgT_s[D:2 * D, csl].rearrange("(g p) t -> p g t", p=128), w=[sgb_c.t])

    load_chunk3(0)
    for c in range(NCH):
        csl = slice(c * 512, (c + 1) * 512)
        ysg_c, yat_c, sga_c, sgb_c = [rg[c % 2] for rg in inr]
        if c + 1 < NCH:
            load_chunk3(c + 1)
        for nb in range(8):
            bA = next_bank()
            for kc in range(8):
                mm(ps[bA][:, :], Wa_sb.ap[:, kc, nb * 128:(nb + 1) * 128], ysg_c.ap[:, kc, :], kc == 0, kc == 7, r=[Wa_sb.t, ysg_c.t], w=[pst[bA]])
            bB = next_bank()
            for kc in range(8):
                mm(ps[bB][:, :], Wb_sb.ap[:, kc, nb * 128:(nb + 1) * 128], yat_c.ap[:, kc, :], kc == 0, kc == 7, r=[Wb_sb.t, yat_c.t], w=[pst[bB]])
            t1, t2 = t1p[nb % 2], t2p[nb % 2]
            tt("dve", t1.ap, ps[bA][:, :], sga_c.ap[:, nb, :], ALU.mult, r=[pst[bA], sga_c.t], w=[t1.t])
            tt("dve", t2.ap, ps[bB][:, :], sgb_c.ap[:, nb, :], ALU.mult, r=[pst[bB], sgb_c.t], w=[t2.t])
            tt("pool", mT.ap[:, nb, :], t1.ap, t2.ap, ALU.add, r=[t1.t, t2.t], w=[mT.t])
        for tau in range(4):
            i = 4 * c + tau
            xt, h2t, xb2 = xtr[i % 2], h2r[i % 2], xn2b[i % 2]
            dma("sp", xt.ap, x_d[i * 128:(i + 1) * 128, :], w=[xt.t])
            for half in range(2):
                hs = slice(half * 512, (half + 1) * 512)
                bk = next_bank()
                for kc in range(8):
                    mm(ps[bk][:, :], mT.ap[:, kc, tau * 128:(tau + 1) * 128], Wo_sb.ap[:, kc, hs], kc == 0, kc == 7, r=[mT.t, Wo_sb.t], w=[pst[bk]])
                tt("dve", h2t.ap[:, hs], ps[bk][:, :], xt.ap[:, hs], ALU.add, r=[pst[bk], xt.t], w=[h2t.t])
            dma("sp", h2_s[i * 128:(i + 1) * 128, :], h2t.ap, r=[h2t.t])
            stt(xn2f.ap, h2t.ap, 1.0 / D, h2t.ap, ALU.mult, ALU.mult, r=[h2t.t], w=[xn2f.t, ms2.t], accum=ms2.ap[:, i:i + 1])
            ts("pool", rs2.ap[:, i:i + 1], ms2.ap[:, i:i + 1], EPS, ALU.add, r=[ms2.t], w=[rs2.t])
            tt("pool", rs2.ap[:, i:i + 1], rs2.ap[:, i:i + 1], col(cst, C_NH), ALU.pow, r=[rs2.t, cst.t], w=[rs2.t])
            stt(xn2f.ap, h2t.ap, rs2.ap[:, i:i + 1], g2b.ap, ALU.mult, ALU.mult, r=[h2t.t, rs2.t, g2b.t], w=[xn2f.t])
            cp("pool", xb2.ap, xn2f.ap, r=[xn2f.t], w=[xb2.t])
            for q4 in range(2):
                bt = 4 + q4
                for jj in range(4):
                    kc = q4 * 4 + jj
                    tr(ps[bt][:, jj * 128:(jj + 1) * 128], xn2f.ap[:, kc * 128:(kc + 1) * 128], identf.ap, r=[xn2f.t, identf.t], w=[pst[bt]])
                cp("act", xn2T.ap[:, q4 * 4:(q4 + 1) * 4, :], ps[bt][:, :].rearrange("p (a b) -> p a b", a=4), r=[pst[bt]], w=[xn2T.t])
            for kc in range(8):
                mm(ps[6][:, 0:36], xn2T.ap[:, kc, :], wr.ap[:, kc, :], kc == 0, kc == 7, r=[xn2T.t, wr.t], w=[pst[6]])
            tt("dve", lgt.ap, ps[6][:, 0:36], br.ap, ALU.add, r=[pst[6], br.t], w=[lgt.t])
            gl = lgt.ap[:, 0:4]
            el = lgt.ap[:, 4:36].rearrange("p (g j) -> p g j", g=4)
            K.op("dve", lambda e: e.tensor_reduce(out=S_("gmax"), in_=gl, axis=AX.X, op=ALU.max), r=[lgt.t], w=[T_("gmax")])
            ts("dve", S_("ohg"), gl, S_("gmax")[:, 0:1], ALU.is_ge, r=[lgt.t, T_("gmax")], w=[T_("ohg")])
            ts("dve", S_("negg"), S_("gmax"), -1.0, ALU.mult, r=[T_("gmax")], w=[T_("negg")])
            act(S_("j4"), gl, AF.Exp, bias=S_("negg")[:, 0:1], r=[lgt.t, T_("negg")], w=[T_("j4"), T_("se")], accum=S_("se"))
            K.op("dve", lambda e: e.reciprocal(out=S_("gp"), in_=S_("se")), r=[T_("se")], w=[T_("gp")])
            ts("dve", S_("esel"), el[:, 0, :], S_("ohg")[:, 0:1], ALU.mult, r=[lgt.t, T_("ohg")], w=[T_("esel")])
            for g in range(1, 4):
                stt(S_("esel"), el[:, g, :], S_("ohg")[:, g:g + 1], S_("esel"), ALU.mult, ALU.add, r=[lgt.t, T_("ohg"), T_("esel")], w=[T_("esel")])
            K.op("dve", lambda e: e.tensor_reduce(out=S_("m1"), in_=S_("esel"), axis=AX.X, op=ALU.max), r=[T_("esel")], w=[T_("m1")])
            ts("dve", S_("oh1"), S_("esel"), S_("m1")[:, 0:1], ALU.is_ge, r=[T_("esel"), T_("m1")], w=[T_("oh1")])
            stt(S_("es2"), S_("oh1"), NEG, S_("esel"), ALU.mult, ALU.add, r=[T_("oh1"), T_("esel")], w=[T_("es2")])
            K.op("dve", lambda e: e.tensor_reduce(out=S_("m2"), in_=S_("es2"), axis=AX.X, op=ALU.max), r=[T_("es2")], w=[T_("m2")])
            ts("dve", S_("oh2"), S_("es2"), S_("m2")[:, 0:1], ALU.is_ge, r=[T_("es2"), T_("m2")], w=[T_("oh2")])
            tt("dve", S_("dlt"), S_("m2"), S_("m1"), ALU.subtract, r=[T_("m1"), T_("m2")], w=[T_("dlt")])
            act(S_("ex"), S_("dlt"), AF.Exp, r=[T_("dlt")], w=[T_("ex")])
            ts("dve", S_("den"), S_("ex"), 1.0, ALU.add, r=[T_("ex")], w=[T_("den")])
            K.op("dve", lambda e: e.reciprocal(out=S_("p1"), in_=S_("den")), r=[T_("den")], w=[T_("p1")])
            tt("dve", S_("p2"), S_("ex"), S_("p1"), ALU.mult, r=[T_("ex"), T_("p1")], w=[T_("p2")])
            tt("dve", S_("gk")[:, 0:1], S_("p1"), S_("gp"), ALU.mult, r=[T_("p1"), T_("gp")], w=[T_("gk")])
            tt("dve", S_("gk")[:, 1:2], S_("p2"), S_("gp"), ALU.mult, r=[T_("p2"), T_("gp")], w=[T_("gk")])
            ohg_b = S_("ohg").unsqueeze(2).broadcast_to([128, 4, 8])
            for nm, oh in (("M1", "oh1"), ("M2", "oh2")):
                tt("dve", S_(nm).rearrange("p (g j) -> p g j", g=4), ohg_b, S_(oh).unsqueeze(1).broadcast_to([128, 4, 8]), ALU.mult,
                   r=[T_("ohg"), T_(oh)], w=[T_(nm)])
            tt("dve", Mb.ap, S_("M1"), S_("M2"), ALU.add, r=[T_("M1"), T_("M2")], w=[Mb.t])
            mm(ps[7][:, 0:32], trib.ap, Mb.ap, True, True, r=[trib.t, Mb.t], w=[pst[7]])
            mm(ps[7][:, 32:64], onesb.ap, Mb.ap, False, True, r=[onesb.t, Mb.t], w=[pst[7]], sgc=True)
            tt("dve", S_("posf"), ps[7][:, 0:32], base.ap, ALU.add, r=[pst[7], base.t], w=[T_("posf")])
            tt("dve", base.ap, ps[7][:, 32:64], base.ap, ALU.add, r=[pst[7], base.t], w=[base.t])
            tt("dve", S_("okf"), S_("posf"), capb.ap, ALU.is_lt, r=[T_("posf"), capb.t], w=[T_("okf")])
            for k_, nm in enumerate(("M1", "M2")):
                stt(S_("j32"), S_(nm), 1.0, S_("posf"), ALU.mult, ALU.mult, r=[T_(nm), T_("posf")], w=[T_("j32"), T_("pk")], accum=S_("pk")[:, k_:k_ + 1])
                stt(S_("j32"), S_(nm), 1.0, S_("okf"), ALU.mult, ALU.mult, r=[T_(nm), T_("okf")], w=[T_("j32"), T_("ok")], accum=S_("ok")[:, k_:k_ + 1])
            ts("dve", S_("pk"), S_("pk"), col(cst, C_DUM), ALU.subtract, r=[T_("pk"), cst.t], w=[T_("pk")])
            tt("dve", S_("pk"), S_("pk"), S_("ok"), ALU.mult, r=[T_("pk"), T_("ok")], w=[T_("pk")])
            ts("dve", S_("pk"), S_("pk"), col(cst, C_DUM), ALU.add, r=[T_("pk"), cst.t], w=[T_("pk")])
            cp("dve", rt_pos.ap[:, 2 * i:2 * i + 2], S_("pk"), r=[T_("pk")], w=[rt_pos.t])
            tt("dve", rt_gate.ap[:, 2 * i:2 * i + 2], S_("gk"), S_("ok"), ALU.mult, r=[T_("gk"), T_("ok")], w=[rt_gate.t])
            for k_ in range(2):
                K.op("pool", lambda e, i=i, k_=k_, xb2=xb2: e.indirect_dma_start(
                    out=xs_s[:, :], out_offset=bass.IndirectOffsetOnAxis(ap=rt_pos.ap[:, 2 * i + k_:2 * i + k_ + 1], axis=0),
                    in_=xb2.ap, in_offset=None), r=[xb2.t, rt_pos.t], dma=True)
    K.barrier()
    AR.release(m_3)
    if stop_after == "3":
        return finish()
    m_4 = AR.mark()
    wi_r = ring(2, [8, 512], BF16, "wie")
    wo_r = ring(2, [2, D], BF16, "woe")
    wis_r = ring(2, [8, 512], F32, "wis")
    wos_r = ring(2, [2, D], F32, "wos")
    xs_r = ring(2, [4, D], BF16, "xs")
    xsT_r = ring(2, [8, 512], BF16, "xsT")
    sg_r = ring(2, [512], F32, "sg")
    aT_r = ring(2, [2, 512], BF16, "aT")
    ys_r = ring(3, [D], F32, "ysb")
    nys = {"n": 0}
    mset("pool", ys_r[2].ap, 0.0, w=[ys_r[2].t])
    dma("sp", ys_s[NE * CAP:NE * CAP + 128, :], ys_r[2].ap, r=[ys_r[2].t])
    def load_expert(e_i):
        wi_, wo_, xs_ = wi_r[e_i % 2], wo_r[e_i % 2], xs_r[e_i % 2]
        sgi, sgo = wis_r[e_i % 2], wos_r[e_i % 2]
        dma("sp", xs_.ap, xs_s[e_i * CAP:(e_i + 1) * CAP, :].rearrange("(st p) d -> p st d", p=128), w=[xs_.t])
        dma("sp", sgi.ap, wei_d[e_i].rearrange("(kc p) f -> p kc f", p=128), w=[sgi.t])
        dma("sp", sgo.ap, weo_d[e_i].rearrange("(fc p) n -> p fc n", p=128), w=[sgo.t])
        cp("pool", wi_.ap, sgi.ap, r=[sgi.t], w=[wi_.t])
        cp("pool", wo_.ap, sgo.ap, r=[sgo.t], w=[wo_.t])

    load_expert(0)
    for e_i in range(NE):
        wi_, wo_, xs_, xsT_, aT_ = wi_r[e_i % 2], wo_r[e_i % 2], xs_r[e_i % 2], xsT_r[e_i % 2], aT_r[e_i % 2]
        if e_i + 1 < NE:
            load_expert(e_i + 1)
        for st in range(4):
            bk = 4 + st % 2
            for kc in range(8):
                tr(psb(bk)[:, kc * 128:(kc + 1) * 128], xs_.ap[:, st, kc * 128:(kc + 1) * 128], ident.ap, r=[xs_.t, ident.t], w=[pst[bk]])
            cp("act" if st % 2 else "dve", xsT_.ap[:, :, st * 128:(st + 1) * 128], psb(bk).rearrange("p (a b) -> p a b", a=8), r=[pst[bk]], w=[xsT_.t])
        for p_ in range(2):
            bG = next_bank()
            for kc in range(8):
                mm(ps[bG][:, :], wi_.ap[:, kc, p_ * 128:(p_ + 1) * 128], xsT_.ap[:, kc, :], kc == 0, kc == 7, r=[wi_.t, xsT_.t], w=[pst[bG]])
            bU = next_bank()
            for kc in range(8):
                mm(ps[bU][:, :], wi_.ap[:, kc, 256 + p_ * 128:256 + (p_ + 1) * 128], xsT_.ap[:, kc, :], kc == 0, kc == 7, r=[wi_.t, xsT_.t], w=[pst[bU]])
            sg_ = sg_r[p_]
            act(sg_.ap, ps[bG][:, :], AF.Silu, r=[pst[bG]], w=[sg_.t])
            tt("dve", aT_.ap[:, p_, :], sg_.ap, ps[bU][:, :], ALU.mult, r=[sg_.t, pst[bU]], w=[aT_.t])
        for st in range(4):
            yb = ys_r[nys["n"] % 3]
            nys["n"] += 1
            for half in range(2):
                hs = slice(half * 512, (half + 1) * 512)
                bk = next_bank()
                for fc in range(2):
                    mm(ps[bk][:, :], aT_.ap[:, fc, st * 128:(st + 1) * 128], wo_.ap[:, fc, hs], fc == 0, fc == 1, r=[aT_.t, wo_.t], w=[pst[bk]])
                cp("act" if half else "dve", yb.ap[:, hs], ps[bk][:, :], r=[pst[bk]], w=[yb.t])
            r0_ = e_i * CAP + st * 128
            dma("sp", ys_s[r0_:r0_ + 128, :], yb.ap, r=[yb.t])
    K.barrier()
    AR.release(m_4)
    gfb = B([D], F32, "gfb")
    dma("sp", gfb.ap, gf_d.partition_broadcast(128), w=[gfb.t])
    Y0r = ring(2, [D], F32, "Y0")
    Y1r = ring(2, [D], F32, "Y1")
    h2l = ring(2, [D], F32, "h2l")
    h3r = ring(2, [D], F32, "h3")
    outr = ring(2, [D], F32, "outb")
    junk4 = B([D], F32, "junk4")
    ms3 = B([NT], F32, "ms3")
    rs3 = B([NT], F32, "rs3")
    for b_ in Y0r + Y1r:
        mset("pool", b_.ap, 0.0, w=[b_.t])
    for i in range(NT):
        Y0, Y1, h2_, h3, ob = Y0r[i % 2], Y1r[i % 2], h2l[i % 2], h3r[i % 2], outr[i % 2]
        for k_, Y in enumerate((Y0, Y1)):
            K.op("pool", lambda e, i=i, k_=k_, Y=Y: e.indirect_dma_start(
                out=Y.ap, out_offset=None, in_=ys_s[:, :], in_offset=bass.IndirectOffsetOnAxis(ap=rt_pos.ap[:, 2 * i + k_:2 * i + k_ + 1], axis=0)),
                r=[rt_pos.t], w=[Y.t], dma=True)
        dma("sp", h2_.ap, h2_s[i * 128:(i + 1) * 128, :], w=[h2_.t])
        stt(h3.ap, Y0.ap, rt_gate.ap[:, 2 * i:2 * i + 1], h2_.ap, ALU.mult, ALU.add, r=[Y0.t, rt_gate.t, h2_.t], w=[h3.t])
        stt(h3.ap, Y1.ap, rt_gate.ap[:, 2 * i + 1:2 * i + 2], h3.ap, ALU.mult, ALU.add, r=[Y1.t, rt_gate.t, h3.t], w=[h3.t])
        stt(junk4.ap, h3.ap, 1.0 / D, h3.ap, ALU.mult, ALU.mult, r=[h3.t], w=[junk4.t, ms3.t], accum=ms3.ap[:, i:i + 1])
        ts("pool", rs3.ap[:, i:i + 1], ms3.ap[:, i:i + 1], EPS, ALU.add, r=[ms3.t], w=[rs3.t])
        tt("pool", rs3.ap[:, i:i + 1], rs3.ap[:, i:i + 1], col(cst, C_NH), ALU.pow, r=[rs3.t, cst.t], w=[rs3.t])
        stt(ob.ap, h3.ap, rs3.ap[:, i:i + 1], gfb.ap, ALU.mult, ALU.mult, r=[h3.t, rs3.t, gfb.t], w=[ob.t])
        dma("sp", out_d[i * 128:(i + 1) * 128, :], ob.ap, r=[ob.t])
    return finish()


def make_in_maps(inp, cores):
    c, m = host_consts()
    maps = []
    for b in cores:
        pos = np.ascontiguousarray(inp["positions"][b]).astype(np.int32)
        maps.append({
            "x": np.ascontiguousarray(inp["x"][b], dtype=np.float32),
            "pos_row": pos.reshape(1, L),
            "pos_col": np.ascontiguousarray(pos.reshape(NT, 128).T),
            "norm1_g": np.ascontiguousarray(inp["norm1_g"][0]).reshape(1, D),
            "w_in": np.ascontiguousarray(inp["w_in"][0]),
            "sgu_norm_g": np.ascontiguousarray(inp["sgu_norm_g"][0]).reshape(1, D),
            "sgu_w": np.ascontiguousarray(inp["sgu_w"][0]),
            "sgu_b": np.ascontiguousarray(inp["sgu_b"][0]),
            "w_branch_a": np.ascontiguousarray(inp["w_branch_a"][0]),
            "w_branch_b": np.ascontiguousarray(inp["w_branch_b"][0]),
            "w_out": np.ascontiguousarray(inp["w_out"][0]),
            "norm2_g": np.ascontiguousarray(inp["norm2_g"][0]).reshape(1, D),
            "w_router_group": np.ascontiguousarray(inp["w_router_group"][0]),
            "b_router_group": np.ascontiguousarray(inp["b_router_group"][0]).reshape(1, 4),
            "w_router_expert": np.ascontiguousarray(inp["w_router_expert"][0]),
            "b_router_expert": np.ascontiguousarray(inp["b_router_expert"][0]).reshape(1, 32),
            "w_expert_in": np.ascontiguousarray(inp["w_expert_in"][0]),
            "w_expert_out": np.ascontiguousarray(inp["w_expert_out"][0]),
            "norm_f_g": np.ascontiguousarray(inp["norm_f_g"]).reshape(1, D),
            "cst": c,
            "cmat": m,
        })
    return maps


def kernel(**inputs):
    P = build_program()
    maps = make_in_maps(inputs, list(range(8)))
    res = run_bass_kernel_spmd(P.nc, maps, core_ids=list(range(8)))
    return np.stack([np.asarray(r["out"], dtype=np.float32) for r in res.results], axis=0)
```

```python
from contextlib import ExitStack
import numpy as np
import concourse.bass as bass
import concourse.mybir as mybir

F32 = mybir.dt.float32
BF16 = mybir.dt.bfloat16
I32 = mybir.dt.int32
U32 = mybir.dt.uint32
AF = mybir.ActivationFunctionType
ALU = mybir.AluOpType
AX = mybir.AxisListType


class Tok:
    __slots__ = ("w", "r", "name")

    def __init__(self, name=""):
        self.w = None
        self.r = []
        self.name = name


class Op:
    __slots__ = ("eng", "fn", "deps", "dma", "sig", "sem", "val", "gidx", "slotwait")

    def __init__(self, eng, fn, dma, gidx):
        self.eng = eng
        self.fn = fn
        self.dma = dma
        self.deps = []
        self.sig = False
        self.sem = None
        self.val = 0
        self.gidx = gidx
        self.slotwait = None


class Kern:
    ENGS = ("pe", "act", "dve", "pool", "sp")
    NSLOT = {"sp": 20, "act": 6, "pool": 12}
    ROLL = 30000

    def __init__(self, nc):
        self.nc = nc
        self.ops = {e: [] for e in self.ENGS}
        self.n = 0
        self.toks = []
        self.es = ExitStack()
        self.nsem = 0

    def tok(self, name=""):
        t = Tok(name)
        self.toks.append(t)
        return t

    def toks_n(self, n, name=""):
        return [self.tok(f"{name}{i}") for i in range(n)]

    def op(self, eng, fn, r=(), w=(), dma=False):
        o = Op(eng, fn, dma, self.n)
        self.n += 1
        deps = {}
        for t in r:
            if t.w is not None:
                deps[id(t.w)] = t.w
        for t in w:
            if t.w is not None:
                deps[id(t.w)] = t.w
            for q in t.r:
                deps[id(q)] = q
        for d in deps.values():
            if d is o:
                continue
            if d.eng == eng and not d.dma and not dma:
                if eng == "pe":
                    continue
            o.deps.append(d)
        for t in r:
            t.r.append(o)
        for t in w:
            t.w = o
            t.r = []
        self.ops[eng].append(o)
        return o

    def barrier(self):
        deps = {}
        for t in self.toks:
            if t.w is not None:
                deps[id(t.w)] = t.w
            for q in t.r:
                deps[id(q)] = q
        dl = list(deps.values())
        for e in self.ENGS:
            o = Op(e, None, False, self.n)
            self.n += 1
            o.deps = [d for d in dl if d.fn is not None]
            self.ops[e].append(o)
        for t in self.toks:
            t.r = []
            t.w = None

    def _newsem(self, name):
        self.nsem += 1
        return self.es.enter_context(self.nc.semaphore(f"{name}_{self.nsem}"))

    def emit(self):
        nc = self.nc
        for e in self.ENGS:
            for o in self.ops[e]:
                for d in o.deps:
                    d.sig = True
        for e in self.ENGS:
            cur = None
            cnt = 0
            slots = None
            slot_uses = None
            slot_last = None
            k = 0
            for o in self.ops[e]:
                if o.fn is None:
                    continue
                if o.dma:
                    if slots is None:
                        ns = self.NSLOT[e]
                        slots = [self._newsem(f"d{e}") for _ in range(ns)]
                        slot_uses = [0] * ns
                        slot_last = [None] * ns
                    s = k % len(slots)
                    k += 1
                    o.slotwait = slot_last[s]
                    slot_uses[s] += 1
                    o.sem = slots[s]
                    o.val = 16 * slot_uses[s]
                    o.sig = True
                    slot_last[s] = o
                elif o.sig:
                    if cur is None or cnt >= self.ROLL:
                        cur = self._newsem(f"c{e}")
                        cnt = 0
                    cnt += 1
                    o.sem = cur
                    o.val = cnt
        with nc.Block() as block:
            def run(e, eng):
                waited = {}
                for o in self.ops[e]:
                    need = {}
                    dl = list(o.deps)
                    if o.slotwait is not None:
                        dl.append(o.slotwait)
                    for d in dl:
                        key = id(d.sem)
                        if waited.get(key, 0) >= d.val:
                            continue
                        if key not in need or need[key][1] < d.val:
                            need[key] = (d.sem, d.val)
                    for key, (sem, val) in need.items():
                        eng.wait_ge(sem, val)
                        waited[key] = val
                    if o.fn is None:
                        continue
                    ins = o.fn(eng)
                    if o.sig:
                        ins.then_inc(o.sem, 16 if o.dma else 1)

            @block.tensor
            def _(eng):
                run("pe", eng)

            @block.scalar
            def _(eng):
                run("act", eng)

            @block.vector
            def _(eng):
                run("dve", eng)

            @block.gpsimd
            def _(eng):
                run("pool", eng)

            @block.sync
            def _(eng):
                run("sp", eng)
        self.es.close()


U8 = mybir.dt.uint8
DTSZ = {F32: 4, BF16: 2, I32: 4, U32: 4}


class Arena:
    def __init__(self, nc, nbytes):
        self.t = nc.alloc_sbuf_tensor("arena", [128, nbytes], U8)
        self.n = nbytes
        self.off = 0
        self.peak = 0

    def alloc(self, shape, dt):
        n = int(np.prod(shape)) * DTSZ[dt]
        n = (n + 63) // 64 * 64
        assert self.off + n <= self.n, f"arena overflow {self.off}+{n}>{self.n}"
        v = self.t[:, self.off:self.off + n].bitcast(dt)
        self.off += n
        self.peak = max(self.peak, self.off)
        tot = int(np.prod(shape))
        v = v[:, 0:tot]
        if len(shape) == 2:
            v = v.rearrange("p (a b) -> p a b", a=shape[0])
        elif len(shape) == 3:
            v = v.rearrange("p (a b c) -> p a b c", a=shape[0], b=shape[1])
        return v

    def mark(self):
        return self.off

    def release(self, m):
        self.off = m

from concourse.bass_utils import run_bass_kernel_spmd

L = 4096
D = 1024
NT = 32
NCH = 8
DIN = 7752
CQ, CK, CV, CQI, CKI, CSU, CSV, CGA, CGB = 0, 1024, 2048, 3072, 3584, 3656, 4680, 5704, 6728
NE = 32
CAP = 512
DUMMY = NE * CAP
KI = 20
EPS = 1e-6
PI = float(np.pi)
MAGIC = 12582912.0
C1 = 6.28125
C2 = 2 * PI - C1
NEG = -1.0e30
ARENA = 206 * 1024

C_INV, C_SGN, C_INVI, C_NH, C_HPI, C_NTHR, C_EPS, C_ONE, C_DUM, C_PW, C_NB = 0, 1, 2, 34, 35, 36, 37, 38, 39, 40, 64
NCST = 96


def host_consts():
    c = np.zeros((128, NCST), np.float32)
    inv128 = (np.float32(10000.0) ** (-np.arange(0, 128, 2, dtype=np.float32) / np.float32(128))).astype(np.float32)
    inv64 = (np.float32(10000.0) ** (-np.arange(0, 64, 2, dtype=np.float32) / np.float32(64))).astype(np.float32)
    p = np.arange(128)
    c[:, C_INV] = inv128[p % 64]
    c[:, C_SGN] = np.where(p < 64, -1.0, 1.0)
    c[:, C_INVI:C_INVI + 32] = inv64[None, :]
    c[:, C_NH] = -0.5
    c[:, C_HPI] = PI / 2
    c[:, C_NTHR] = -1.0e29
    c[:, C_EPS] = EPS
    c[:, C_ONE] = 1.0
    c[:, C_DUM] = NE * CAP + p
    c[:, C_PW:C_PW + KI + 2] = (2.0 ** -(np.arange(KI + 2) + 1.0))[None, :]
    c[:, C_NB:C_NB + NT] = (128.0 * (np.arange(NT) + 1.0) - 511.0)[None, :]
    m = np.zeros((128, 128 * 3 + 64), np.float32)
    t = np.arange(128)[:, None]
    s = np.arange(128)[None, :]
    m[:, 0:128] = np.where(s <= t, 0.0, NEG)
    m[:, 128:256] = np.where(s >= t, 1.0, 0.0)
    m[:, 256:384] = np.where(s <= t, 1.0, 0.0)
    m[:, 384:416] = (np.arange(32) * CAP)[None, :]
    m[:, 416:448] = 1.0
    return c, m


class Prog:
    pass


def build_program(stop_after=None, dbg=False):
    nc = bass.Bass("TRN2", target_bir_lowering=False)
    K = Kern(nc)
    AR = Arena(nc, ARENA)
    P = Prog()
    P.nc = nc

    def din(name, shape, dt=F32):
        return nc.dram_tensor(name, shape, dt, kind="ExternalInput").ap()

    def dscr(name, shape, dt, out=False):
        return nc.dram_tensor(name, shape, dt, kind="ExternalOutput" if (out and dbg) else "Internal").ap()

    x_d = din("x", [L, D])
    posr_d = din("pos_row", [1, L], I32)
    posc_d = din("pos_col", [128, NT], I32)
    g1_d = din("norm1_g", [1, D])
    win_d = din("w_in", [D, DIN])
    gs_d = din("sgu_norm_g", [1, D])
    sw_d = din("sgu_w", [8, 128, 128])
    sb_d = din("sgu_b", [8, 128])
    wa_d = din("w_branch_a", [D, D])
    wb_d = din("w_branch_b", [D, D])
    wo_d = din("w_out", [D, D])
    g2_d = din("norm2_g", [1, D])
    wrg_d = din("w_router_group", [D, 4])
    brg_d = din("b_router_group", [1, 4])
    wre_d = din("w_router_expert", [D, 32])
    bre_d = din("b_router_expert", [1, 32])
    wei_d = din("w_expert_in", [NE, D, 512])
    weo_d = din("w_expert_out", [NE, 256, D])
    gf_d = din("norm_f_g", [1, D])
    cst_d = din("cst", [128, NCST])
    cm_d = din("cmat", [128, 448])
    out_d = nc.dram_tensor("out", [L, D], F32, kind="ExternalOutput").ap()

    qT_s = dscr("qT_s", [8, 128, L], BF16, True)
    kT_s = dscr("kT_s", [8, 128, L], BF16, True)
    v_s = dscr("v_s", [NT, 128, 8 * 129], BF16, True)
    qiT_s = dscr("qiT_s", [5, 128, L], BF16, True)
    suT_s = dscr("suT_s", [D, L], BF16)
    ysgT_s = dscr("ysgT_s", [D, L], BF16, True)
    sgT_s = dscr("sgT_s", [2 * D, L], BF16, True)
    yatT_s = dscr("yatT_s", [D, L], BF16, True)
    h2_s = dscr("h2_s", [L, D], F32, True)
    xs_s = dscr("xs_s", [NE * CAP + 128, D], BF16)
    ys_s = dscr("ys_s", [NE * CAP + 128, D], F32)

    ps = [nc.alloc_psum_tensor(f"ps{i}", [128, 512], F32) for i in range(8)]
    pst = [K.tok(f"ps{i}") for i in range(8)]

    def psb(i):
        return ps[i][:, :].bitcast(BF16)

    def dma(eng, out, in_, r=(), w=()):
        return K.op(eng, lambda e: e.dma_start(out=out, in_=in_), r=r, w=w, dma=True)

    def mm(out, lhsT, rhs, start, stop, r=(), w=(), sgc=False):
        return K.op("pe", lambda e: e.matmul(out, lhsT=lhsT, rhs=rhs, start=start, stop=stop, skip_group_check=sgc), r=r, w=w)

    def tr(out, in_, ident, r=(), w=()):
        return K.op("pe", lambda e: e.transpose(out, in_, ident), r=r, w=w)

    def act(out, in_, func, r=(), w=(), bias=None, scale=1.0, accum=None):
        def f(e):
            kw = {}
            if bias is not None:
                kw["bias"] = bias
            if accum is not None:
                kw["accum_out"] = accum
            return e.activation(out=out, in_=in_, func=func, scale=scale, **kw)
        return K.op("act", f, r=r, w=w)

    def ts(eng, out, in0, s1, op0, s2=None, op1=None, r=(), w=(), accum=None):
        def f(e):
            kw = {}
            if op1 is not None:
                kw["op1"] = op1
            if accum is not None:
                kw["accum_out"] = accum
            return e.tensor_scalar(out=out, in0=in0, scalar1=s1, scalar2=s2, op0=op0, **kw)
        return K.op(eng, f, r=r, w=w)

    def tt(eng, out, in0, in1, op, r=(), w=()):
        return K.op(eng, lambda e: e.tensor_tensor(out=out, in0=in0, in1=in1, op=op), r=r, w=w)

    def stt(out, in0, scalar, in1, op0, op1, r=(), w=(), accum=None):
        def f(e):
            kw = {}
            if accum is not None:
                kw["accum_out"] = accum
            return e.scalar_tensor_tensor(out=out, in0=in0, scalar=scalar, in1=in1, op0=op0, op1=op1, **kw)
        return K.op("dve", f, r=r, w=w)

    def cp(eng, out, in_, r=(), w=()):
        if eng == "act":
            return K.op("act", lambda e: e.activation(out=out, in_=in_, func=AF.Copy), r=r, w=w)
        return K.op(eng, lambda e: e.tensor_copy(out, in_), r=r, w=w)

    def mset(eng, out, val, r=(), w=()):
        return K.op(eng, lambda e: e.memset(out, val), r=r, w=w)

    class B:
        def __init__(self, shape, dt, name=""):
            self.ap = AR.alloc(shape, dt)
            self.t = K.tok(name)

    def ring(n, shape, dt, name=""):
        return [B(shape, dt, f"{name}{i}") for i in range(n)]

    cst = B([NCST], F32, "cst")
    cm = B([448], F32, "cm")
    ident = B([128], BF16, "ident")
    identf = B([128], F32, "identf")
    wi_sb = B([NT, 8], F32, "wi")
    rt_gate = B([NT * 2], F32, "gate")
    rt_pos = B([NT * 2], I32, "pos")
    dma("sp", cst.ap, cst_d, w=[cst.t])
    dma("sp", cm.ap, cm_d, w=[cm.t])
    mset("pool", identf.ap, 0.0, w=[identf.t])
    K.op("pool", lambda e: e.affine_select(out=identf.ap, in_=identf.ap, pattern=[[-1, 128]], compare_op=ALU.not_equal,
                                           fill=1.0, base=0, channel_multiplier=1), r=[identf.t], w=[identf.t])
    cp("pool", ident.ap, identf.ap, r=[identf.t], w=[ident.t])

    zt = B([D], BF16, "zt")
    mset("pool", zt.ap, 0.0, w=[zt.t])

    def col(b, j, n=1):
        return b.ap[:, j:j + n]

    def sincos(ang, n, kk, sin_out, cos_out, r, w):
        tk = K.tok()
        ts("dve", kk, ang, 1.0 / (2 * PI), ALU.mult, MAGIC, ALU.add, r=r, w=[tk])
        ts("dve", kk, kk, MAGIC, ALU.subtract, r=[tk], w=[tk])
        stt(ang, kk, -C1, ang, ALU.mult, ALU.add, r=r + [tk], w=r)
        stt(ang, kk, -C2, ang, ALU.mult, ALU.add, r=r + [tk], w=r)
        ts("dve", ang, ang, PI, ALU.min, -PI, ALU.max, r=r, w=r)
        act(sin_out, ang, AF.Sin, r=r, w=w)
        ts("dve", kk, ang, -1.0, ALU.mult, r=r, w=[tk])
        tt("dve", kk, kk, ang, ALU.max, r=r + [tk], w=[tk])
        act(cos_out, kk, AF.Sin, bias=col(cst, C_HPI), scale=-1.0, r=[tk, cst.t], w=w)


    def finish():
        K.barrier()
        K.emit()
        P.__dict__.update(dict(K=K, AR=AR))
        return P

    m_0 = AR.mark()
    xnT = AR.alloc([8, L], BF16)
    xnT_t = [K.tok(f"xnT{i}") for i in range(NT)]
    m_p1 = AR.mark()

    xbuf = ring(3, [D], F32, "xb")
    junk = B([D], F32, "junk")
    g1b = B([D], F32, "g1b")
    xnb = ring(2, [D], BF16, "xnb")
    ms1 = B([NT], F32, "ms1")
    rs1 = B([NT], F32, "rs1")
    ms1_t = [K.tok() for _ in range(NT)]
    rs1_t = [K.tok() for _ in range(NT)]
    dma("sp", g1b.ap, g1_d.partition_broadcast(128), w=[g1b.t])

    def a_front(i):
        xb = xbuf[i % 3]
        dma("sp", xb.ap, x_d[i * 128:(i + 1) * 128, :], w=[xb.t])
        stt(junk.ap, xb.ap, 1.0 / D, xb.ap, ALU.mult, ALU.mult, r=[xb.t], w=[junk.t, ms1_t[i]], accum=ms1.ap[:, i:i + 1])
        ts("pool", rs1.ap[:, i:i + 1], ms1.ap[:, i:i + 1], EPS, ALU.add, r=[ms1_t[i]], w=[rs1_t[i]])
        tt("pool", rs1.ap[:, i:i + 1], rs1.ap[:, i:i + 1], col(cst, C_NH), ALU.pow, r=[rs1_t[i], cst.t], w=[rs1_t[i]])

    def a_back(i):
        xb = xbuf[i % 3]
        nb = xnb[i % 2]
        stt(nb.ap, xb.ap, rs1.ap[:, i:i + 1], g1b.ap, ALU.mult, ALU.mult, r=[xb.t, rs1_t[i], g1b.t], w=[nb.t])
        bk = 4 + (i % 2)
        for kc in range(8):
            tr(psb(bk)[:, kc * 128:(kc + 1) * 128], nb.ap[:, kc * 128:(kc + 1) * 128], ident.ap, r=[nb.t, ident.t], w=[pst[bk]])
        cp("act", xnT[:, :, i * 128:(i + 1) * 128], psb(bk).rearrange("p (a b) -> p a b", a=8), r=[pst[bk]], w=[xnT_t[i]])

    for i in range(NT + 1):
        if i < NT:
            a_front(i)
        if i >= 1:
            a_back(i - 1)
    K.barrier()
    AR.release(m_p1)

    wbuf = ring(3, [8, 512], BF16, "wb")
    wstg = ring(2, [4, 512], F32, "wstg")
    wstate = {"n": 0}

    def load_w(c0, ncols):
        b = wbuf[wstate["n"] % 3]
        wstate["n"] += 1
        for hf in range(2):
            sg = wstg[hf]
            dma("sp", sg.ap[:, :, 0:ncols], win_d[hf * 512:(hf + 1) * 512, c0:c0 + ncols].rearrange("(kc p) c -> p kc c", p=128), w=[sg.t])
            cp("pool", b.ap[:, 4 * hf:4 * hf + 4, 0:ncols], sg.ap[:, :, 0:ncols], r=[sg.t], w=[b.t])
        return b

    bank_rr = {"n": 0}

    def next_bank():
        bk = bank_rr["n"] % 4
        bank_rr["n"] += 1
        return bk

    def fm_block(wb, j, c, bk):
        for kc in range(8):
            mm(ps[bk][:, :], wb.ap[:, kc, j * 128:(j + 1) * 128], xnT[:, kc, c * 512:(c + 1) * 512], kc == 0, kc == 7,
               r=[wb.t] + xnT_t[4 * c:4 * c + 4], w=[pst[bk]])

    def tm_block(wb, ncols, i, bk, col0=0):
        for kc in range(8):
            mm(ps[bk][:, 0:ncols], xnT[:, kc, i * 128:(i + 1) * 128], wb.ap[:, kc, col0:col0 + ncols], kc == 0, kc == 7,
               r=[wb.t, xnT_t[i]], w=[pst[bk]])

    m_b1 = AR.mark()
    cosT = B([L], F32, "cosT")
    sinT = B([L], F32, "sinT")
    posi = B([1024], I32, "posi")
    angw = B([1024], F32, "angw")
    kkw = B([1024], F32, "kkw")
    for cc in range(4):
        sl = slice(cc * 1024, (cc + 1) * 1024)
        dma("sp", posi.ap, posr_d[:, sl].partition_broadcast(128), w=[posi.t])
        cp("dve", angw.ap, posi.ap, r=[posi.t], w=[angw.t])
        ts("dve", angw.ap, angw.ap, col(cst, C_INV), ALU.mult, r=[angw.t, cst.t], w=[angw.t])
        sincos(angw.ap, 1024, kkw.ap, sinT.ap[:, sl], cosT.ap[:, sl], r=[angw.t], w=[sinT.t, cosT.t])
        ts("dve", sinT.ap[:, sl], sinT.ap[:, sl], col(cst, C_SGN), ALU.mult, r=[sinT.t, cst.t], w=[sinT.t])
    posc = B([NT], I32, "posc")
    poscf = B([NT], F32, "poscf")
    sinI = B([NT, 32], F32, "sinI")
    cosI = B([NT, 32], F32, "cosI")
    dma("sp", posc.ap, posc_d, w=[posc.t])
    cp("dve", poscf.ap, posc.ap, r=[posc.t], w=[poscf.t])
    angI = angw.ap.rearrange("p (a b) -> p a b", a=NT)
    tt("dve", angI, poscf.ap.unsqueeze(2).broadcast_to([128, NT, 32]),
       cst.ap[:, C_INVI:C_INVI + 32].unsqueeze(1).broadcast_to([128, NT, 32]), ALU.mult, r=[poscf.t, cst.t, angw.t], w=[angw.t])
    sincos(angw.ap, 1024, kkw.ap, sinI.ap.rearrange("p a b -> p (a b)"), cosI.ap.rearrange("p a b -> p (a b)"),
           r=[angw.t], w=[sinI.t, cosI.t])

    t1r = ring(2, [512], F32, "t1")
    t2r = ring(2, [512], F32, "t2")
    qor = ring(3, [512], BF16, "qo")
    n_rope = {"n": 0}

    def rope_fm(bk, c, dst):
        k_ = n_rope["n"]
        n_rope["n"] += 1
        t1 = t1r[k_ % 2]
        t2 = t2r[k_ % 2]
        qo = qor[k_ % 3]
        sl = slice(c * 512, (c + 1) * 512)
        tt("dve", t1.ap, ps[bk][:, :], cosT.ap[:, sl], ALU.mult, r=[pst[bk], cosT.t], w=[t1.t])
        tt("dve", t2.ap[0:64, :], ps[bk][64:128, :], sinT.ap[0:64, sl], ALU.mult, r=[pst[bk], sinT.t], w=[t2.t])
        tt("dve", t2.ap[64:128, :], ps[bk][0:64, :], sinT.ap[64:128, sl], ALU.mult, r=[pst[bk], sinT.t], w=[t2.t])
        tt("pool", qo.ap, t1.ap, t2.ap, ALU.add, r=[t1.t, t2.t], w=[qo.t])
        dma("sp", dst, qo.ap, r=[qo.t])

    qk_blocks = [(CQ, qT_s, 0), (CQ + 512, qT_s, 4), (CK, kT_s, 0), (CK + 512, kT_s, 4)]
    wnext = load_w(qk_blocks[0][0], 512)
    for bi, (c0, dst_s, h0) in enumerate(qk_blocks):
        wb = wnext
        wnext = load_w(qk_blocks[bi + 1][0], 512) if bi + 1 < 4 else load_w(CV, 512)
        for c in range(NCH):
            for j in range(4):
                bk = next_bank()
                fm_block(wb, j, c, bk)
                rope_fm(bk, c, dst_s[h0 + j, :, c * 512:(c + 1) * 512])
    wv0 = wnext
    wv1 = load_w(CV + 512, 512)
    wqi = load_w(CQI, 512)
    vt = ring(2, [8, 129], BF16, "vt")
    for b_ in vt:
        mset("pool", b_.ap[:, :, 128:129], 1.0, w=[b_.t])
    for i in range(NT):
        v_ = vt[i % 2]
        for half, wb in enumerate((wv0, wv1)):
            bk = next_bank()
            tm_block(wb, 512, i, bk)
            cp("act", v_.ap[:, half * 4:(half + 1) * 4, 0:128], ps[bk][:, :].rearrange("p (a b) -> p a b", a=4), r=[pst[bk]], w=[v_.t])
        dma("sp", v_s[i].rearrange("p (h d) -> p h d", h=8), v_.ap, r=[v_.t])
    wkw = load_w(CKI, 72)
    ra = ring(3, [9, 32], F32, "ra")
    rb = ring(3, [9, 32], F32, "rb")
    qst = ring(3, [9, 64], F32, "qst")
    qr = ring(3, [640], BF16, "qr")
    qiT_c = ring(2, [5, 512], BF16, "qiTc")
    def ip_front(i):
        bq = next_bank()
        tm_block(wqi, 512, i, bq)
        bkw = next_bank()
        tm_block(wkw, 72, i, bkw)
        q_ = qr[i % 3]
        a_, b2_, st_ = ra[i % 3], rb[i % 3], qst[i % 3]
        cp("act", st_.ap[:, 0:8, :], ps[bq][:, :].rearrange("p (h d) -> p h d", h=8), r=[pst[bq]], w=[st_.t])
        cp("act", st_.ap[:, 8, :], ps[bkw][:, 0:64], r=[pst[bkw]], w=[st_.t])
        cosb = cosI.ap[:, i, :].unsqueeze(1).broadcast_to([128, 9, 32])
        sinb = sinI.ap[:, i, :].unsqueeze(1).broadcast_to([128, 9, 32])
        qo_ = q_.ap[:, 0:576].rearrange("p (h d) -> p h d", h=9)
        rd = [st_.t, cosI.t, sinI.t]
        tt("dve", a_.ap, st_.ap[:, :, 0:32], cosb, ALU.mult, r=rd, w=[a_.t])
        tt("dve", b2_.ap, st_.ap[:, :, 32:64], sinb, ALU.mult, r=rd, w=[b2_.t])
        tt("pool", qo_[:, :, 0:32], a_.ap, b2_.ap, ALU.subtract, r=[a_.t, b2_.t], w=[q_.t])
        tt("dve", a_.ap, st_.ap[:, :, 32:64], cosb, ALU.mult, r=rd, w=[a_.t])
        tt("dve", b2_.ap, st_.ap[:, :, 0:32], sinb, ALU.mult, r=rd, w=[b2_.t])
        tt("pool", qo_[:, :, 32:64], a_.ap, b2_.ap, ALU.add, r=[a_.t, b2_.t], w=[q_.t])
        cp("pool", q_.ap[:, 576:640], q_.ap[:, 512:576], r=[q_.t], w=[q_.t])
        cp("act", wi_sb.ap[:, i, :], ps[bkw][:, 64:72], r=[pst[bkw]], w=[wi_sb.t])

    def ip_back(i):
        c, tau = i // 4, i % 4
        q_ = qr[i % 3]
        bt = 4 + (i % 2)
        for jj in range(5):
            tr(psb(bt)[:, jj * 128:(jj + 1) * 128], q_.ap[:, jj * 128:(jj + 1) * 128], ident.ap, r=[q_.t, ident.t], w=[pst[bt]])
        qc = qiT_c[c % 2]
        cp("act", qc.ap[:, :, tau * 128:(tau + 1) * 128], psb(bt)[:, 0:640].rearrange("p (a b) -> p a b", a=5), r=[pst[bt]], w=[qc.t])
        if tau == 3:
            dma("sp", qiT_s[:, :, c * 512:(c + 1) * 512].rearrange("a p t -> p a t"), qc.ap, r=[qc.t])
    for i in range(NT + 1):
        if i < NT:
            ip_front(i)
        if i >= 1:
            ip_back(i - 1)
    K.barrier()
    AR.release(m_b1)
    if stop_after == "1b":
        return finish()
    m_b2 = AR.mark()
    sub_r = ring(3, [512], BF16, "sub")
    su_t = [K.tok(f"suT{c}") for c in range(NCH)]
    w0 = load_w(CSU, 512)
    w1 = load_w(CSU + 512, 512)
    w2 = load_w(CSV, 512)
    nsub = {"n": 0}

    def act_block_out(bk, func, dst, wtok=()):
        o = sub_r[nsub["n"] % 3]
        nsub["n"] += 1
        act(o.ap, ps[bk][:, :], func, r=[pst[bk]], w=[o.t])
        dma("sp", dst, o.ap, r=[o.t], w=list(wtok))

    for blk, wb in enumerate((w0, w1)):
        for c in range(NCH):
            for j in range(4):
                bk = next_bank()
                fm_block(wb, j, c, bk)
                f0 = (blk * 4 + j) * 128
                act_block_out(bk, AF.Gelu_apprx_tanh, suT_s[f0:f0 + 128, c * 512:(c + 1) * 512], wtok=[su_t[c]])
    w3 = load_w(CSV + 512, 512)
    swf = B([8, 128], F32, "swf")
    swb = B([8, 128], BF16, "swb")
    WtT = B([8, 128], BF16, "WtT")
    dma("sp", swf.ap, sw_d.rearrange("g t s -> t g s"), w=[swf.t])
    tt("dve", swf.ap, swf.ap, cm.ap[:, 256:384].unsqueeze(1).broadcast_to([128, 8, 128]), ALU.mult, r=[swf.t, cm.t], w=[swf.t])
    cp("dve", swb.ap, swf.ap, r=[swf.t], w=[swb.t])
    for g in range(8):
        tr(psb(4)[:, g * 128:(g + 1) * 128], swb.ap[:, g, :], ident.ap, r=[swb.t, ident.t], w=[pst[4]])
    cp("act", WtT.ap, psb(4).rearrange("p (a b) -> p a b", a=8), r=[pst[4]], w=[WtT.t])
    bf_ = B([8, 128], F32, "bf")
    bhl = B([8, 128], BF16, "bhl")
    bhf = B([8, 128], F32, "bhf")
    ones2 = B([128], BF16, "ones2")
    mset("pool", ones2.ap, 1.0, w=[ones2.t])
    mset("pool", bf_.ap, 0.0, w=[bf_.t])
    dma("sp", bf_.ap[0:1, :, :], sb_d.rearrange("(o g) t -> o g t", o=1), r=[bf_.t], w=[bf_.t])
    dma("sp", bf_.ap[1:2, :, :], sb_d.rearrange("(o g) t -> o g t", o=1), r=[bf_.t], w=[bf_.t])
    cp("dve", bhl.ap, bf_.ap, r=[bf_.t], w=[bhl.t])
    cp("dve", bhf.ap, bhl.ap, r=[bhl.t], w=[bhf.t])
    tt("dve", bhf.ap, bf_.ap, bhf.ap, ALU.subtract, r=[bf_.t, bhf.t], w=[bhf.t])
    cp("dve", bf_.ap, bhl.ap, r=[bhl.t, bf_.t], w=[bf_.t])
    sel = B([1], F32, "sel")
    mset("pool", sel.ap, 1.0, w=[sel.t])
    K.op("pool", lambda e: e.affine_select(out=sel.ap, in_=sel.ap, pattern=[[0, 1]], compare_op=ALU.is_equal,
                                           fill=0.0, base=0, channel_multiplier=1), r=[sel.t], w=[sel.t])
    tt("dve", bf_.ap, bf_.ap, bhf.ap, ALU.subtract, r=[bf_.t, bhf.t], w=[bf_.t])
    stt(bhf.ap, bf_.ap, sel.ap[:, 0:1], bhf.ap, ALU.mult, ALU.add, r=[bf_.t, sel.t, bhf.t], w=[bhf.t])
    cp("dve", bhl.ap, bhf.ap, r=[bhf.t], w=[bhl.t])

    gsb = B([D], F32, "gsb")
    dma("sp", gsb.ap, gs_d.partition_broadcast(128), w=[gsb.t])
    gv = ring(3, [D], F32, "gv")
    vln = ring(3, [D], BF16, "vln")
    bst = B([NT, 12], F32, "bst")
    mv = B([NT, 2], F32, "mv")
    rsd = B([NT], F32, "rsd")
    bst_t = [K.tok() for _ in range(NT)]
    mv_t = [K.tok() for _ in range(NT)]
    rsd_t = [K.tok() for _ in range(NT)]
    suc = ring(2, [8, 512], BF16, "suc")
    ysg = ring(2, [8, 512], BF16, "ysg")
    for c in range(NCH):
        su_c = suc[c % 2]
        ys_c = ysg[c % 2]
        dma("sp", su_c.ap, suT_s[:, c * 512:(c + 1) * 512].rearrange("(g p) t -> p g t", p=128), r=[su_t[c]], w=[su_c.t])
        def sv_front(tau, c=c):
            i = 4 * c + tau
            g_ = gv[i % 3]
            vl = vln[i % 3]
            sb0 = 6
            for half, wb in enumerate((w2, w3)):
                bk = next_bank()
                tm_block(wb, 512, i, bk)
                act(g_.ap[:, half * 512:(half + 1) * 512], ps[bk][:, :], AF.Gelu_apprx_tanh, r=[pst[bk]], w=[g_.t])
            K.op("dve", lambda e, i=i, g_=g_: e.bn_stats(out=bst.ap[:, i, 0:6], in_=g_.ap[:, 0:512]), r=[g_.t], w=[bst_t[i]])
            K.op("dve", lambda e, i=i, g_=g_: e.bn_stats(out=bst.ap[:, i, 6:12], in_=g_.ap[:, 512:1024]), r=[g_.t], w=[bst_t[i]])
            K.op("dve", lambda e, i=i: e.bn_aggr(out=mv.ap[:, i, :], in_=bst.ap[:, i, :]), r=[bst_t[i]], w=[mv_t[i]])
            ts("pool", rsd.ap[:, i:i + 1], mv.ap[:, i, 1:2], EPS, ALU.add, r=[mv_t[i]], w=[rsd_t[i]])
            tt("pool", rsd.ap[:, i:i + 1], rsd.ap[:, i:i + 1], col(cst, C_NH), ALU.pow, r=[rsd_t[i], cst.t], w=[rsd_t[i]])
            ts("dve", g_.ap, g_.ap, mv.ap[:, i, 0:1], ALU.subtract, rsd.ap[:, i:i + 1], ALU.mult, r=[g_.t, mv_t[i], rsd_t[i]], w=[g_.t])
            tt("pool", vl.ap, g_.ap, gsb.ap, ALU.mult, r=[g_.t, gsb.t], w=[vl.t])

        def sv_back(tau, c=c, su_c=su_c, ys_c=ys_c):
            i = 4 * c + tau
            vl = vln[i % 3]
            sb0 = 6
            for g in range(8):
                bk = sb0 + g // 4
                o_ = ps[bk][:, (g % 4) * 128:(g % 4 + 1) * 128]
                mm(o_, vl.ap[:, g * 128:(g + 1) * 128], WtT.ap[:, g, :], True, False, r=[vl.t, WtT.t], w=[pst[bk]])
                mm(o_, ones2.ap[0:2, :], bhl.ap[0:2, g, :], False, True, r=[ones2.t, bhl.t], w=[pst[bk]])
            for b_ in range(2):
                tt("dve", ys_c.ap[:, 4 * b_:4 * b_ + 4, tau * 128:(tau + 1) * 128],
                   ps[sb0 + b_][:, :].rearrange("p (a b) -> p a b", a=4),
                   su_c.ap[:, 4 * b_:4 * b_ + 4, tau * 128:(tau + 1) * 128], ALU.mult, r=[pst[sb0 + b_], su_c.t], w=[ys_c.t])
        for tau in range(5):
            if tau < 4:
                sv_front(tau)
            if tau >= 1:
                sv_back(tau - 1)
        dma("sp", ysgT_s[:, c * 512:(c + 1) * 512].rearrange("(g p) t -> p g t", p=128), ys_c.ap, r=[ys_c.t])
    wg = load_w(CGA, 512)
    for blk in range(4):
        wb = wg
        if blk < 3:
            wg = load_w(CGA + (blk + 1) * 512, 512)
        for c in range(NCH):
            for j in range(4):
                bk = next_bank()
                fm_block(wb, j, c, bk)
                f0 = (blk * 4 + j) * 128
                act_block_out(bk, AF.Sigmoid, sgT_s[f0:f0 + 128, c * 512:(c + 1) * 512])
    K.barrier()
    AR.release(m_0)
    if stop_after == "1":
        return finish()
    SCALE = float(128 ** -0.5)
    vres = AR.alloc([NT, 8, 129], BF16)
    v_t = [K.tok(f"v{i}") for i in range(NT)]
    kiT2 = AR.alloc([L], BF16)
    ki_t = [K.tok(f"ki{i}") for i in range(4)]
    for i in range(NT):
        dma("sp", vres[:, i, :, :], v_s[i].rearrange("p (h d) -> p h d", h=8), w=[v_t[i]])
        if i % 8 == 0:
            q4 = i // 8
            dma("sp", kiT2[:, q4 * 1024:(q4 + 1) * 1024], qiT_s[4, :, q4 * 1024:(q4 + 1) * 1024], w=[ki_t[q4]])
    nrow_t = (NE * CAP + 128) // 128
    for z0 in range(0, nrow_t, 16):
        zn = min(16, nrow_t - z0)
        dma("sp", xs_s[z0 * 128:(z0 + zn) * 128, :].rearrange("(a p) d -> p a d", p=128),
            zt.ap.unsqueeze(1).broadcast_to([128, zn, D]), r=[zt.t])

    kTr = ring(2, [L], BF16, "kTr")
    qTc = B([8, 512], BF16, "qTc")
    qiTc = B([4, 512], BF16, "qiTc")
    S = B([L], F32, "S")
    maskb = B([L], BF16, "maskb")
    maskb2 = B([L], BF16, "maskb2")
    maskT = B([NT, 512], BF16, "maskT")
    rbuf = ring(3, [512], F32, "rb")
    ebuf = ring(3, [512], BF16, "eb")
    pbuf = ring(3, [512], BF16, "pb")
    S2 = B([L], F32, "S2")

    class _V:
        pass
    ytile = _V()
    ytile.ap = S2.ap[:, 0:2048].bitcast(BF16).rearrange("p (a b) -> p a b", a=4)
    ytile.t = S2.t
    yT = _V()
    yT.ap = S2.ap[:, 2048:4096].bitcast(BF16).rearrange("p (a b) -> p a b", a=8)
    yT.t = S2.t
    Sb = (S, S2)
    Mb2 = (maskb, maskb2)
    sst = []
    for q_ in range(2):
        sst.append({nm: B([sz], F32, f"{nm}{q_}") for nm, sz in (
            ("vmax", 1), ("vmin", 1), ("rngv", 1), ("hw", KI + 2), ("nhw", KI + 2), ("cand", 1), ("cnt", 1), ("msg", 1), ("thr", 1))})
    recr = ring(2, [4], F32, "rec")
    ctr = {"sc": 0, "lg": 0, "tr": 0, "e": 0}

    for c in range(NCH):
        csl = slice(c * 512, (c + 1) * 512)
        dma("sp", qTc.ap, qT_s[:, :, csl].rearrange("h p t -> p h t"), w=[qTc.t])
        dma("sp", qiTc.ap, qiT_s[0:4, :, csl].rearrange("a p t -> p a t"), w=[qiTc.t])
        def idx_scores(tau, Sx):
            i = 4 * c + tau
            n = 128 * (i + 1)
            nv = 128 * i
            for sc in range((n + 511) // 512):
                w_ = min(512, n - 512 * sc)
                sl = slice(sc * 512, sc * 512 + w_)
                for h in range(8):
                    bk = ctr["sc"] % 2
                    ctr["sc"] += 1
                    pr = slice(64 * (h % 2), 64 * (h % 2) + 64)
                    mm(ps[bk][:, 0:w_], qiTc.ap[pr, h // 2, tau * 128:(tau + 1) * 128], kiT2[pr, sl], True, True,
                       r=[qiTc.t, ki_t[sc // 2]], w=[pst[bk]])
                    rb = rbuf[ctr["sc"] % 3]
                    act(rb.ap[:, 0:w_], ps[bk][:, 0:w_], AF.Relu, r=[pst[bk]], w=[rb.t])
                    if h == 0:
                        ts("dve", Sx.ap[:, sl], rb.ap[:, 0:w_], wi_sb.ap[:, i, 0:1], ALU.mult, r=[rb.t, wi_sb.t], w=[Sx.t])
                    else:
                        stt(Sx.ap[:, sl], rb.ap[:, 0:w_], wi_sb.ap[:, i, h:h + 1], Sx.ap[:, sl], ALU.mult, ALU.add,
                            r=[rb.t, wi_sb.t, Sx.t], w=[Sx.t])
            tt("pool", Sx.ap[:, nv:n], Sx.ap[:, nv:n], cm.ap[:, 0:128], ALU.add, r=[Sx.t, cm.t], w=[Sx.t])

        def search_pre(tau, Sx, mb, st, on_act):
            i = 4 * c + tau
            n = 128 * (i + 1)
            nv = 128 * i
            if i < 2:
                return
            A_ = lambda nm: st[nm].ap
            T2 = lambda nm: st[nm].t
            K.op("dve", lambda e: e.tensor_reduce(out=A_("vmax"), in_=Sx.ap[:, 0:n], axis=AX.X, op=ALU.max), r=[Sx.t], w=[T2("vmax")])
            K.op("dve", lambda e: e.tensor_reduce(out=A_("vmin"), in_=Sx.ap[:, 0:nv], axis=AX.X, op=ALU.min), r=[Sx.t], w=[T2("vmin")])
            tt("dve", A_("rngv"), A_("vmax"), A_("vmin"), ALU.subtract, r=[T2("vmax"), T2("vmin")], w=[T2("rngv")])
            ts("dve", A_("hw"), cst.ap[:, C_PW:C_PW + KI + 2], A_("rngv")[:, 0:1], ALU.mult, r=[cst.t, T2("rngv")], w=[T2("hw")])
            if not on_act:
                tt("dve", A_("cand"), A_("vmin"), A_("hw")[:, 0:1], ALU.add, r=[T2("vmin"), T2("hw")], w=[T2("cand")])
            else:
                ts("dve", A_("nhw"), A_("hw"), -1.0, ALU.mult, r=[T2("hw")], w=[T2("nhw")])
                stt(A_("cand"), A_("vmin"), -1.0, A_("hw")[:, 0:1], ALU.mult, ALU.subtract, r=[T2("vmin"), T2("hw")], w=[T2("cand")])

        def search(tau, Sx, mb, st, on_act):
            i = 4 * c + tau
            n = 128 * (i + 1)
            if i < 2:
                return col(cst, C_NTHR), cst.t
            A_ = lambda nm: st[nm].ap
            T2 = lambda nm: st[nm].t
            if not on_act:
                for k in range(KI):
                    ts("dve", mb.ap[:, 0:n], Sx.ap[:, 0:n], A_("cand")[:, 0:1], ALU.is_ge, None, ALU.add,
                       r=[Sx.t, T2("cand")], w=[mb.t, T2("cnt")], accum=A_("cnt"))
                    ts("dve", A_("msg"), A_("cnt"), 255.5, ALU.is_ge, 0.5, ALU.subtract, r=[T2("cnt")], w=[T2("msg")])
                    stt(A_("cand"), A_("msg"), A_("hw")[:, k:k + 1], A_("cand"), ALU.mult, ALU.add, r=[T2("msg"), T2("hw"), T2("cand")], w=[T2("cand")])
                tt("dve", A_("thr"), A_("cand"), A_("hw")[:, KI:KI + 1], ALU.subtract, r=[T2("cand"), T2("hw")], w=[T2("thr")])
            else:
                for k in range(KI):
                    act(mb.ap[:, 0:n], Sx.ap[:, 0:n], AF.Sign, bias=A_("cand")[:, 0:1], r=[Sx.t, T2("cand")], w=[mb.t, T2("cnt")], accum=A_("cnt"))
                    act(A_("msg"), A_("cnt"), AF.Sign, bias=col(cst, C_NB + i), r=[T2("cnt"), cst.t], w=[T2("msg")])
                    act(A_("cand"), A_("msg"), AF.Identity, bias=A_("cand")[:, 0:1], scale=A_("nhw")[:, k + 1:k + 2],
                        r=[T2("msg"), T2("nhw"), T2("cand")], w=[T2("cand")])
                act(A_("thr"), A_("cand"), AF.Identity, bias=A_("nhw")[:, KI:KI + 1], scale=-1.0, r=[T2("cand"), T2("nhw")], w=[T2("thr")])
            return A_("thr")[:, 0:1], T2("thr")

        def make_mask(tau, Sx, mb, thr_ap, thr_t):
            i = 4 * c + tau
            n = 128 * (i + 1)
            ts("dve", mb.ap[:, 0:n], Sx.ap[:, 0:n], thr_ap, ALU.is_ge, r=[Sx.t, thr_t], w=[mb.t])
            for j0 in range(0, i + 1, 8):
                nb = min(8, i + 1 - j0)
                bk = 2 + ctr["tr"] % 2
                ctr["tr"] += 1
                for jj in range(nb):
                    tr(psb(bk)[:, jj * 128:(jj + 1) * 128], mb.ap[:, (j0 + jj) * 128:(j0 + jj + 1) * 128], ident.ap,
                       r=[mb.t, ident.t], w=[pst[bk]])
                cp("act", maskT.ap[:, j0:j0 + nb, tau * 128:(tau + 1) * 128],
                   psb(bk)[:, 0:nb * 128].rearrange("p (a b) -> p a b", a=nb), r=[pst[bk]], w=[maskT.t])

        for pair in range(2):
            t0, t1_ = 2 * pair, 2 * pair + 1
            idx_scores(t0, Sb[0])
            idx_scores(t1_, Sb[1])
            search_pre(t0, Sb[0], Mb2[0], sst[0], False)
            search_pre(t1_, Sb[1], Mb2[1], sst[1], True)
            th1 = search(t1_, Sb[1], Mb2[1], sst[1], True)
            th0 = search(t0, Sb[0], Mb2[0], sst[0], False)
            make_mask(t0, Sb[0], Mb2[0], *th0)
            make_mask(t1_, Sb[1], Mb2[1], *th1)
        nj = 4 * c + 4
        steps = [(h, j) for h in range(8) for j in range(nj)]
        LB = (1, 2, 3)

        def emit_qk(k):
            h, j = steps[k]
            kt = kTr[h % 2]
            if j == 0:
                dma("sp", kt.ap[:, 0:nj * 128], kT_s[h, :, 0:nj * 128], w=[kt.t])
            r0 = max(0, j - 4 * c)
            N = 512 - 128 * r0
            bk = LB[k % 3]
            mm(ps[bk][:, 0:N], kt.ap[:, j * 128:(j + 1) * 128], qTc.ap[:, h, r0 * 128:512], True, True,
               r=[kt.t, qTc.t], w=[pst[bk]])

        emit_qk(0)
        emit_qk(1)
        for k, (h, j) in enumerate(steps):
            if k + 2 < len(steps):
                emit_qk(k + 2)
            accA = 4 + 2 * (h % 2)
            accB = accA + 1
            r0 = max(0, j - 4 * c)
            N = 512 - 128 * r0
            bk = LB[k % 3]
            e_ = ebuf[k % 3]
            p_ = pbuf[k % 3]
            act(e_.ap[:, 0:N], ps[bk][:, 0:N], AF.Exp, scale=SCALE, r=[pst[bk]], w=[e_.t])
            tt("dve", p_.ap[:, 0:N], e_.ap[:, 0:N], maskT.ap[:, j, r0 * 128:512], ALU.mult, r=[e_.t, maskT.t], w=[p_.t])
            for tau in range(r0, 4):
                if tau < 3:
                    o_, ob = ps[accA][:, tau * 129:(tau + 1) * 129], accA
                else:
                    o_, ob = ps[accB][:, 0:129], accB
                first = (j == 0 and tau in (0, 3))
                mm(o_, p_.ap[:, (tau - r0) * 128:(tau - r0 + 1) * 128], vres[:, j, h, :], first, j == nj - 1,
                   r=[p_.t, v_t[j]], w=[pst[ob]], sgc=True)
            if j == nj - 1:
                rc = recr[h % 2]
                K.op("dve", lambda e, rc=rc, accA=accA: e.reciprocal(
                    out=rc.ap[:, 0:3], in_=ps[accA][:, 0:387].rearrange("p (a b) -> p a b", b=129)[:, :, 128]), r=[pst[accA]], w=[rc.t])
                K.op("dve", lambda e, rc=rc, accB=accB: e.reciprocal(out=rc.ap[:, 3:4], in_=ps[accB][:, 128:129]), r=[pst[accB]], w=[rc.t])
                for tau in range(4):
                    src, sb_ = (ps[accA][:, tau * 129:tau * 129 + 128], accA) if tau < 3 else (ps[accB][:, 0:128], accB)
                    ts("dve", ytile.ap[:, tau, h * 128:(h + 1) * 128], src, rc.ap[:, tau:tau + 1], ALU.mult, r=[pst[sb_], rc.t], w=[ytile.t])
        for tau in range(4):
            bk = 2 + ctr["tr"] % 2
            ctr["tr"] += 1
            for kc in range(8):
                tr(psb(bk)[:, kc * 128:(kc + 1) * 128], ytile.ap[:, tau, kc * 128:(kc + 1) * 128], ident.ap, r=[ytile.t, ident.t], w=[pst[bk]])
            cp("act", yT.ap[:, :, tau * 128:(tau + 1) * 128], psb(bk).rearrange("p (a b) -> p a b", a=8), r=[pst[bk]], w=[yT.t])
        dma("sp", yatT_s[:, csl].rearrange("(g p) t -> p g t", p=128), yT.ap, r=[yT.t])
    K.barrier()
    AR.release(m_0)
    if stop_after == "2":
        return finish()
    m_3 = AR.mark()
    Wa_sb = B([8, D], BF16, "Wa")
    Wb_sb = B([8, D], BF16, "Wb")
    Wo_sb = B([8, D], BF16, "Wo")
    w3stg = ring(2, [2, D], F32, "w3stg")
    for wi3, (wsb, wd) in enumerate(((Wa_sb, wa_d), (Wb_sb, wb_d), (Wo_sb, wo_d))):
        for q4 in range(4):
            sg = w3stg[(wi3 * 4 + q4) % 2]
            dma("sp", sg.ap, wd[q4 * 256:(q4 + 1) * 256, :].rearrange("(kc p) n -> p kc n", p=128), w=[sg.t])
            cp("dve" if q4 % 2 else "act", wsb.ap[:, 2 * q4:2 * q4 + 2, :], sg.ap, r=[sg.t], w=[wsb.t])
    g2b = B([D], F32, "g2b")
    dma("sp", g2b.ap, g2_d.partition_broadcast(128), w=[g2b.t])
    wr = B([8, 36], F32, "wr")
    br = B([36], F32, "br")
    dma("sp", wr.ap[:, :, 0:4], wrg_d.rearrange("(kc p) n -> p kc n", p=128), w=[wr.t])
    dma("sp", wr.ap[:, :, 4:36], wre_d.rearrange("(kc p) n -> p kc n", p=128), w=[wr.t])
    dma("sp", br.ap[:, 0:4], brg_d.partition_broadcast(128), w=[br.t])
    dma("sp", br.ap[:, 4:36], bre_d.partition_broadcast(128), w=[br.t])
    trib = B([128], BF16, "trib")
    onesb = B([128], BF16, "onesb")
    cp("pool", trib.ap, cm.ap[:, 128:256], r=[cm.t], w=[trib.t])
    mset("pool", onesb.ap, 1.0, w=[onesb.t])
    base = B([32], F32, "base")
    capb = B([32], F32, "capb")
    ts("pool", base.ap, cm.ap[:, 384:416], -1.0, ALU.add, r=[cm.t], w=[base.t])
    ts("pool", capb.ap, cm.ap[:, 384:416], float(CAP) - 0.5, ALU.add, r=[cm.t], w=[capb.t])
    inr = [ring(2, [8, 512], BF16, nm) for nm in ("ysgc", "yatc", "sgac", "sgbc")]
    mT = B([8, 512], BF16, "mT")
    t1p = ring(2, [512], F32, "t1p")
    t2p = ring(2, [512], F32, "t2p")
    xtr = ring(2, [D], F32, "xt")
    h2r = ring(2, [D], F32, "h2t")
    ms2 = B([NT], F32, "ms2")
    rs2 = B([NT], F32, "rs2")
    xn2f = B([D], F32, "xn2f")
    xn2b = ring(2, [D], BF16, "xn2b")
    xn2T = B([8, 128], F32, "xn2T")
    lgt = B([36], F32, "lgt")
    sm = {nm: B([sz], F32, nm) for nm, sz in (
        ("gmax", 1), ("negg", 1), ("ohg", 4), ("j4", 4), ("se", 1), ("gp", 1), ("esel", 8), ("m1", 1), ("oh1", 8), ("es2", 8),
        ("m2", 1), ("oh2", 8), ("dlt", 1), ("ex", 1), ("den", 1), ("p1", 1), ("p2", 1), ("M1", 32), ("M2", 32), ("posf", 32),
        ("okf", 32), ("j32", 32), ("pk", 2), ("ok", 2), ("gk", 2))}
    Mb = B([32], BF16, "Mb")

    def S_(nm):
        return sm[nm].ap

    def T_(nm):
        return sm[nm].t

    def load_chunk3(c):
        csl = slice(c * 512, (c + 1) * 512)
        ysg_c, yat_c, sga_c, sgb_c = [rg[c % 2] for rg in inr]
        dma("sp", ysg_c.ap, ysgT_s[:, csl].rearrange("(g p) t -> p g t", p=128), w=[ysg_c.t])
        dma("sp", yat_c.ap, yatT_s[:, csl].rearrange("(g p) t -> p g t", p=128), w=[yat_c.t])
        dma("sp", sga_c.ap, sgT_s[0:D, csl].rearrange("(g p) t -> p g t", p=128), w=[sga_c.t])
        dma("sp", sgb_c.ap, sgT_s[D:2 * D, csl].rearrange("(g p) t -> p g t", p=128), w=[sgb_c.t])

    load_chunk3(0)
    for c in range(NCH):
        csl = slice(c * 512, (c + 1) * 512)
        ysg_c, yat_c, sga_c, sgb_c = [rg[c % 2] for rg in inr]
        if c + 1 < NCH:
            load_chunk3(c + 1)
        for nb in range(8):
            bA = next_bank()
            for kc in range(8):
                mm(ps[bA][:, :], Wa_sb.ap[:, kc, nb * 128:(nb + 1) * 128], ysg_c.ap[:, kc, :], kc == 0, kc == 7, r=[Wa_sb.t, ysg_c.t], w=[pst[bA]])
            bB = next_bank()
            for kc in range(8):
                mm(ps[bB][:, :], Wb_sb.ap[:, kc, nb * 128:(nb + 1) * 128], yat_c.ap[:, kc, :], kc == 0, kc == 7, r=[Wb_sb.t, yat_c.t], w=[pst[bB]])
            t1, t2 = t1p[nb % 2], t2p[nb % 2]
            tt("dve", t1.ap, ps[bA][:, :], sga_c.ap[:, nb, :], ALU.mult, r=[pst[bA], sga_c.t], w=[t1.t])
            tt("dve", t2.ap, ps[bB][:, :], sgb_c.ap[:, nb, :], ALU.mult, r=[pst[bB], sgb_c.t], w=[t2.t])
            tt("pool", mT.ap[:, nb, :], t1.ap, t2.ap, ALU.add, r=[t1.t, t2.t], w=[mT.t])
        for tau in range(4):
            i = 4 * c + tau
            xt, h2t, xb2 = xtr[i % 2], h2r[i % 2], xn2b[i % 2]
            dma("sp", xt.ap, x_d[i * 128:(i + 1) * 128, :], w=[xt.t])
            for half in range(2):
                hs = slice(half * 512, (half + 1) * 512)
                bk = next_bank()
                for kc in range(8):
                    mm(ps[bk][:, :], mT.ap[:, kc, tau * 128:(tau + 1) * 128], Wo_sb.ap[:, kc, hs], kc == 0, kc == 7, r=[mT.t, Wo_sb.t], w=[pst[bk]])
                tt("dve", h2t.ap[:, hs], ps[bk][:, :], xt.ap[:, hs], ALU.add, r=[pst[bk], xt.t], w=[h2t.t])
            dma("sp", h2_s[i * 128:(i + 1) * 128, :], h2t.ap, r=[h2t.t])
            stt(xn2f.ap, h2t.ap, 1.0 / D, h2t.ap, ALU.mult, ALU.mult, r=[h2t.t], w=[xn2f.t, ms2.t], accum=ms2.ap[:, i:i + 1])
            ts("pool", rs2.ap[:, i:i + 1], ms2.ap[:, i:i + 1], EPS, ALU.add, r=[ms2.t], w=[rs2.t])
            tt("pool", rs2.ap[:, i:i + 1], rs2.ap[:, i:i + 1], col(cst, C_NH), ALU.pow, r=[rs2.t, cst.t], w=[rs2.t])
            stt(xn2f.ap, h2t.ap, rs2.ap[:, i:i + 1], g2b.ap, ALU.mult, ALU.mult, r=[h2t.t, rs2.t, g2b.t], w=[xn2f.t])
            cp("pool", xb2.ap, xn2f.ap, r=[xn2f.t], w=[xb2.t])
            for q4 in range(2):
                bt = 4 + q4
                for jj in range(4):
                    kc = q4 * 4 + jj
                    tr(ps[bt][:, jj * 128:(jj + 1) * 128], xn2f.ap[:, kc * 128:(kc + 1) * 128], identf.ap, r=[xn2f.t, identf.t], w=[pst[bt]])
                cp("act", xn2T.ap[:, q4 * 4:(q4 + 1) * 4, :], ps[bt][:, :].rearrange("p (a b) -> p a b", a=4), r=[pst[bt]], w=[xn2T.t])
            for kc in range(8):
                mm(ps[6][:, 0:36], xn2T.ap[:, kc, :], wr.ap[:, kc, :], kc == 0, kc == 7, r=[xn2T.t, wr.t], w=[pst[6]])
            tt("dve", lgt.ap, ps[6][:, 0:36], br.ap, ALU.add, r=[pst[6], br.t], w=[lgt.t])
            gl = lgt.ap[:, 0:4]
            el = lgt.ap[:, 4:36].rearrange("p (g j) -> p g j", g=4)
            K.op("dve", lambda e: e.tensor_reduce(out=S_("gmax"), in_=gl, axis=AX.X, op=ALU.max), r=[lgt.t], w=[T_("gmax")])
            ts("dve", S_("ohg"), gl, S_("gmax")[:, 0:1], ALU.is_ge, r=[lgt.t, T_("gmax")], w=[T_("ohg")])
            ts("dve", S_("negg"), S_("gmax"), -1.0, ALU.mult, r=[T_("gmax")], w=[T_("negg")])
            act(S_("j4"), gl, AF.Exp, bias=S_("negg")[:, 0:1], r=[lgt.t, T_("negg")], w=[T_("j4"), T_("se")], accum=S_("se"))
            K.op("dve", lambda e: e.reciprocal(out=S_("gp"), in_=S_("se")), r=[T_("se")], w=[T_("gp")])
            ts("dve", S_("esel"), el[:, 0, :], S_("ohg")[:, 0:1], ALU.mult, r=[lgt.t, T_("ohg")], w=[T_("esel")])
            for g in range(1, 4):
                stt(S_("esel"), el[:, g, :], S_("ohg")[:, g:g + 1], S_("esel"), ALU.mult, ALU.add, r=[lgt.t, T_("ohg"), T_("esel")], w=[T_("esel")])
            K.op("dve", lambda e: e.tensor_reduce(out=S_("m1"), in_=S_("esel"), axis=AX.X, op=ALU.max), r=[T_("esel")], w=[T_("m1")])
            ts("dve", S_("oh1"), S_("esel"), S_("m1")[:, 0:1], ALU.is_ge, r=[T_("esel"), T_("m1")], w=[T_("oh1")])
            stt(S_("es2"), S_("oh1"), NEG, S_("esel"), ALU.mult, ALU.add, r=[T_("oh1"), T_("esel")], w=[T_("es2")])
            K.op("dve", lambda e: e.tensor_reduce(out=S_("m2"), in_=S_("es2"), axis=AX.X, op=ALU.max), r=[T_("es2")], w=[T_("m2")])
            ts("dve", S_("oh2"), S_("es2"), S_("m2")[:, 0:1], ALU.is_ge, r=[T_("es2"), T_("m2")], w=[T_("oh2")])
            tt("dve", S_("dlt"), S_("m2"), S_("m1"), ALU.subtract, r=[T_("m1"), T_("m2")], w=[T_("dlt")])
            act(S_("ex"), S_("dlt"), AF.Exp, r=[T_("dlt")], w=[T_("ex")])
            ts("dve", S_("den"), S_("ex"), 1.0, ALU.add, r=[T_("ex")], w=[T_("den")])
            K.op("dve", lambda e: e.reciprocal(out=S_("p1"), in_=S_("den")), r=[T_("den")], w=[T_("p1")])
            tt("dve", S_("p2"), S_("ex"), S_("p1"), ALU.mult, r=[T_("ex"), T_("p1")], w=[T_("p2")])
            tt("dve", S_("gk")[:, 0:1], S_("p1"), S_("gp"), ALU.mult, r=[T_("p1"), T_("gp")], w=[T_("gk")])
            tt("dve", S_("gk")[:, 1:2], S_("p2"), S_("gp"), ALU.mult, r=[T_("p2"), T_("gp")], w=[T_("gk")])
            ohg_b = S_("ohg").unsqueeze(2).broadcast_to([128, 4, 8])
            for nm, oh in (("M1", "oh1"), ("M2", "oh2")):
                tt("dve", S_(nm).rearrange("p (g j) -> p g j", g=4), ohg_b, S_(oh).unsqueeze(1).broadcast_to([128, 4, 8]), ALU.mult,
                   r=[T_("ohg"), T_(oh)], w=[T_(nm)])
            tt("dve", Mb.ap, S_("M1"), S_("M2"), ALU.add, r=[T_("M1"), T_("M2")], w=[Mb.t])
            mm(ps[7][:, 0:32], trib.ap, Mb.ap, True, True, r=[trib.t, Mb.t], w=[pst[7]])
            mm(ps[7][:, 32:64], onesb.ap, Mb.ap, False, True, r=[onesb.t, Mb.t], w=[pst[7]], sgc=True)
            tt("dve", S_("posf"), ps[7][:, 0:32], base.ap, ALU.add, r=[pst[7], base.t], w=[T_("posf")])
            tt("dve", base.ap, ps[7][:, 32:64], base.ap, ALU.add, r=[pst[7], base.t], w=[base.t])
            tt("dve", S_("okf"), S_("posf"), capb.ap, ALU.is_lt, r=[T_("posf"), capb.t], w=[T_("okf")])
            for k_, nm in enumerate(("M1", "M2")):
                stt(S_("j32"), S_(nm), 1.0, S_("posf"), ALU.mult, ALU.mult, r=[T_(nm), T_("posf")], w=[T_("j32"), T_("pk")], accum=S_("pk")[:, k_:k_ + 1])
                stt(S_("j32"), S_(nm), 1.0, S_("okf"), ALU.mult, ALU.mult, r=[T_(nm), T_("okf")], w=[T_("j32"), T_("ok")], accum=S_("ok")[:, k_:k_ + 1])
            ts("dve", S_("pk"), S_("pk"), col(cst, C_DUM), ALU.subtract, r=[T_("pk"), cst.t], w=[T_("pk")])
            tt("dve", S_("pk"), S_("pk"), S_("ok"), ALU.mult, r=[T_("pk"), T_("ok")], w=[T_("pk")])
            ts("dve", S_("pk"), S_("pk"), col(cst, C_DUM), ALU.add, r=[T_("pk"), cst.t], w=[T_("pk")])
            cp("dve", rt_pos.ap[:, 2 * i:2 * i + 2], S_("pk"), r=[T_("pk")], w=[rt_pos.t])
            tt("dve", rt_gate.ap[:, 2 * i:2 * i + 2], S_("gk"), S_("ok"), ALU.mult, r=[T_("gk"), T_("ok")], w=[rt_gate.t])
            for k_ in range(2):
                K.op("pool", lambda e, i=i, k_=k_, xb2=xb2: e.indirect_dma_start(
                    out=xs_s[:, :], out_offset=bass.IndirectOffsetOnAxis(ap=rt_pos.ap[:, 2 * i + k_:2 * i + k_ + 1], axis=0),
                    in_=xb2.ap, in_offset=None), r=[xb2.t, rt_pos.t], dma=True)
    K.barrier()
    AR.release(m_3)
    if stop_after == "3":
        return finish()
    m_4 = AR.mark()
    wi_r = ring(2, [8, 512], BF16, "wie")
    wo_r = ring(2, [2, D], BF16, "woe")
    wis_r = ring(2, [8, 512], F32, "wis")
    wos_r = ring(2, [2, D], F32, "wos")
    xs_r = ring(2, [4, D], BF16, "xs")
    xsT_r = ring(2, [8, 512], BF16, "xsT")
    sg_r = ring(2, [512], F32, "sg")
    aT_r = ring(2, [2, 512], BF16, "aT")
    ys_r = ring(3, [D], F32, "ysb")
    nys = {"n": 0}
    mset("pool", ys_r[2].ap, 0.0, w=[ys_r[2].t])
    dma("sp", ys_s[NE * CAP:NE * CAP + 128, :], ys_r[2].ap, r=[ys_r[2].t])
    def load_expert(e_i):
        wi_, wo_, xs_ = wi_r[e_i % 2], wo_r[e_i % 2], xs_r[e_i % 2]
        sgi, sgo = wis_r[e_i % 2], wos_r[e_i % 2]
        dma("sp", xs_.ap, xs_s[e_i * CAP:(e_i + 1) * CAP, :].rearrange("(st p) d -> p st d", p=128), w=[xs_.t])
        dma("sp", sgi.ap, wei_d[e_i].rearrange("(kc p) f -> p kc f", p=128), w=[sgi.t])
        dma("sp", sgo.ap, weo_d[e_i].rearrange("(fc p) n -> p fc n", p=128), w=[sgo.t])
        cp("pool", wi_.ap, sgi.ap, r=[sgi.t], w=[wi_.t])
        cp("pool", wo_.ap, sgo.ap, r=[sgo.t], w=[wo_.t])

    load_expert(0)
    for e_i in range(NE):
        wi_, wo_, xs_, xsT_, aT_ = wi_r[e_i % 2], wo_r[e_i % 2], xs_r[e_i % 2], xsT_r[e_i % 2], aT_r[e_i % 2]
        if e_i + 1 < NE:
            load_expert(e_i + 1)
        for st in range(4):
            bk = 4 + st % 2
            for kc in range(8):
                tr(psb(bk)[:, kc * 128:(kc + 1) * 128], xs_.ap[:, st, kc * 128:(kc + 1) * 128], ident.ap, r=[xs_.t, ident.t], w=[pst[bk]])
            cp("act" if st % 2 else "dve", xsT_.ap[:, :, st * 128:(st + 1) * 128], psb(bk).rearrange("p (a b) -> p a b", a=8), r=[pst[bk]], w=[xsT_.t])
        for p_ in range(2):
            bG = next_bank()
            for kc in range(8):
                mm(ps[bG][:, :], wi_.ap[:, kc, p_ * 128:(p_ + 1) * 128], xsT_.ap[:, kc, :], kc == 0, kc == 7, r=[wi_.t, xsT_.t], w=[pst[bG]])
            bU = next_bank()
            for kc in range(8):
                mm(ps[bU][:, :], wi_.ap[:, kc, 256 + p_ * 128:256 + (p_ + 1) * 128], xsT_.ap[:, kc, :], kc == 0, kc == 7, r=[wi_.t, xsT_.t], w=[pst[bU]])
            sg_ = sg_r[p_]
            act(sg_.ap, ps[bG][:, :], AF.Silu, r=[pst[bG]], w=[sg_.t])
            tt("dve", aT_.ap[:, p_, :], sg_.ap, ps[bU][:, :], ALU.mult, r=[sg_.t, pst[bU]], w=[aT_.t])
        for st in range(4):
            yb = ys_r[nys["n"] % 3]
            nys["n"] += 1
            for half in range(2):
                hs = slice(half * 512, (half + 1) * 512)
                bk = next_bank()
                for fc in range(2):
                    mm(ps[bk][:, :], aT_.ap[:, fc, st * 128:(st + 1) * 128], wo_.ap[:, fc, hs], fc == 0, fc == 1, r=[aT_.t, wo_.t], w=[pst[bk]])
                cp("act" if half else "dve", yb.ap[:, hs], ps[bk][:, :], r=[pst[bk]], w=[yb.t])
            r0_ = e_i * CAP + st * 128
            dma("sp", ys_s[r0_:r0_ + 128, :], yb.ap, r=[yb.t])
    K.barrier()
    AR.release(m_4)
    gfb = B([D], F32, "gfb")
    dma("sp", gfb.ap, gf_d.partition_broadcast(128), w=[gfb.t])
    Y0r = ring(2, [D], F32, "Y0")
    Y1r = ring(2, [D], F32, "Y1")
    h2l = ring(2, [D], F32, "h2l")
    h3r = ring(2, [D], F32, "h3")
    outr = ring(2, [D], F32, "outb")
    junk4 = B([D], F32, "junk4")
    ms3 = B([NT], F32, "ms3")
    rs3 = B([NT], F32, "rs3")
    for b_ in Y0r + Y1r:
        mset("pool", b_.ap, 0.0, w=[b_.t])
    for i in range(NT):
        Y0, Y1, h2_, h3, ob = Y0r[i % 2], Y1r[i % 2], h2l[i % 2], h3r[i % 2], outr[i % 2]
        for k_, Y in enumerate((Y0, Y1)):
            K.op("pool", lambda e, i=i, k_=k_, Y=Y: e.indirect_dma_start(
                out=Y.ap, out_offset=None, in_=ys_s[:, :], in_offset=bass.IndirectOffsetOnAxis(ap=rt_pos.ap[:, 2 * i + k_:2 * i + k_ + 1], axis=0)),
                r=[rt_pos.t], w=[Y.t], dma=True)
        dma("sp", h2_.ap, h2_s[i * 128:(i + 1) * 128, :], w=[h2_.t])
        stt(h3.ap, Y0.ap, rt_gate.ap[:, 2 * i:2 * i + 1], h2_.ap, ALU.mult, ALU.add, r=[Y0.t, rt_gate.t, h2_.t], w=[h3.t])
        stt(h3.ap, Y1.ap, rt_gate.ap[:, 2 * i + 1:2 * i + 2], h3.ap, ALU.mult, ALU.add, r=[Y1.t, rt_gate.t, h3.t], w=[h3.t])
        stt(junk4.ap, h3.ap, 1.0 / D, h3.ap, ALU.mult, ALU.mult, r=[h3.t], w=[junk4.t, ms3.t], accum=ms3.ap[:, i:i + 1])
        ts("pool", rs3.ap[:, i:i + 1], ms3.ap[:, i:i + 1], EPS, ALU.add, r=[ms3.t], w=[rs3.t])
        tt("pool", rs3.ap[:, i:i + 1], rs3.ap[:, i:i + 1], col(cst, C_NH), ALU.pow, r=[rs3.t, cst.t], w=[rs3.t])
        stt(ob.ap, h3.ap, rs3.ap[:, i:i + 1], gfb.ap, ALU.mult, ALU.mult, r=[h3.t, rs3.t, gfb.t], w=[ob.t])
        dma("sp", out_d[i * 128:(i + 1) * 128, :], ob.ap, r=[ob.t])
    return finish()


def make_in_maps(inp, cores):
    c, m = host_consts()
    maps = []
    for b in cores:
        pos = np.ascontiguousarray(inp["positions"][b]).astype(np.int32)
        maps.append({
            "x": np.ascontiguousarray(inp["x"][b], dtype=np.float32),
            "pos_row": pos.reshape(1, L),
            "pos_col": np.ascontiguousarray(pos.reshape(NT, 128).T),
            "norm1_g": np.ascontiguousarray(inp["norm1_g"][0]).reshape(1, D),
            "w_in": np.ascontiguousarray(inp["w_in"][0]),
            "sgu_norm_g": np.ascontiguousarray(inp["sgu_norm_g"][0]).reshape(1, D),
            "sgu_w": np.ascontiguousarray(inp["sgu_w"][0]),
            "sgu_b": np.ascontiguousarray(inp["sgu_b"][0]),
            "w_branch_a": np.ascontiguousarray(inp["w_branch_a"][0]),
            "w_branch_b": np.ascontiguousarray(inp["w_branch_b"][0]),
            "w_out": np.ascontiguousarray(inp["w_out"][0]),
            "norm2_g": np.ascontiguousarray(inp["norm2_g"][0]).reshape(1, D),
            "w_router_group": np.ascontiguousarray(inp["w_router_group"][0]),
            "b_router_group": np.ascontiguousarray(inp["b_router_group"][0]).reshape(1, 4),
            "w_router_expert": np.ascontiguousarray(inp["w_router_expert"][0]),
            "b_router_expert": np.ascontiguousarray(inp["b_router_expert"][0]).reshape(1, 32),
            "w_expert_in": np.ascontiguousarray(inp["w_expert_in"][0]),
            "w_expert_out": np.ascontiguousarray(inp["w_expert_out"][0]),
            "norm_f_g": np.ascontiguousarray(inp["norm_f_g"]).reshape(1, D),
            "cst": c,
            "cmat": m,
        })
    return maps


def kernel(**inputs):
    P = build_program()
    maps = make_in_maps(inputs, list(range(8)))
    res = run_bass_kernel_spmd(P.nc, maps, core_ids=list(range(8)))
    return np.stack([np.asarray(r["out"], dtype=np.float32) for r in res.results], axis=0)
```

```python
from contextlib import ExitStack
import numpy as np
import concourse.bass as bass
import concourse.mybir as mybir

F32 = mybir.dt.float32
BF16 = mybir.dt.bfloat16
I32 = mybir.dt.int32
U32 = mybir.dt.uint32
AF = mybir.ActivationFunctionType
ALU = mybir.AluOpType
AX = mybir.AxisListType


class Tok:
    __slots__ = ("w", "r", "name")

    def __init__(self, name=""):
        self.w = None
        self.r = []
        self.name = name


class Op:
    __slots__ = ("eng", "fn", "deps", "dma", "sig", "sem", "val", "gidx", "slotwait")

    def __init__(self, eng, fn, dma, gidx):
        self.eng = eng
        self.fn = fn
        self.dma = dma
        self.deps = []
        self.sig = False
        self.sem = None
        self.val = 0
        self.gidx = gidx
        self.slotwait = None


class Kern:
    ENGS = ("pe", "act", "dve", "pool", "sp")
    NSLOT = {"sp": 20, "act": 6, "pool": 12}
    ROLL = 30000

    def __init__(self, nc):
        self.nc = nc
        self.ops = {e: [] for e in self.ENGS}
        self.n = 0
        self.toks = []
        self.es = ExitStack()
        self.nsem = 0

    def tok(self, name=""):
        t = Tok(name)
        self.toks.append(t)
        return t

    def toks_n(self, n, name=""):
        return [self.tok(f"{name}{i}") for i in range(n)]

    def op(self, eng, fn, r=(), w=(), dma=False):
        o = Op(eng, fn, dma, self.n)
        self.n += 1
        deps = {}
        for t in r:
            if t.w is not None:
                deps[id(t.w)] = t.w
        for t in w:
            if t.w is not None:
                deps[id(t.w)] = t.w
            for q in t.r:
                deps[id(q)] = q
        for d in deps.values():
            if d is o:
                continue
            if d.eng == eng and not d.dma and not dma:
                if eng == "pe":
                    continue
            o.deps.append(d)
        for t in r:
            t.r.append(o)
        for t in w:
            t.w = o
            t.r = []
        self.ops[eng].append(o)
        return o

    def barrier(self):
        deps = {}
        for t in self.toks:
            if t.w is not None:
                deps[id(t.w)] = t.w
            for q in t.r:
                deps[id(q)] = q
        dl = list(deps.values())
        for e in self.ENGS:
            o = Op(e, None, False, self.n)
            self.n += 1
            o.deps = [d for d in dl if d.fn is not None]
            self.ops[e].append(o)
        for t in self.toks:
            t.r = []
            t.w = None

    def _newsem(self, name):
        self.nsem += 1
        return self.es.enter_context(self.nc.semaphore(f"{name}_{self.nsem}"))

    def emit(self):
        nc = self.nc
        for e in self.ENGS:
            for o in self.ops[e]:
                for d in o.deps:
                    d.sig = True
        for e in self.ENGS:
            cur = None
            cnt = 0
            slots = None
            slot_uses = None
            slot_last = None
            k = 0
            for o in self.ops[e]:
                if o.fn is None:
                    continue
                if o.dma:
                    if slots is None:
                        ns = self.NSLOT[e]
                        slots = [self._newsem(f"d{e}") for _ in range(ns)]
                        slot_uses = [0] * ns
                        slot_last = [None] * ns
                    s = k % len(slots)
                    k += 1
                    o.slotwait = slot_last[s]
                    slot_uses[s] += 1
                    o.sem = slots[s]
                    o.val = 16 * slot_uses[s]
                    o.sig = True
                    slot_last[s] = o
                elif o.sig:
                    if cur is None or cnt >= self.ROLL:
                        cur = self._newsem(f"c{e}")
                        cnt = 0
                    cnt += 1
                    o.sem = cur
                    o.val = cnt
        with nc.Block() as block:
            def run(e, eng):
                waited = {}
                for o in self.ops[e]:
                    need = {}
                    dl = list(o.deps)
                    if o.slotwait is not None:
                        dl.append(o.slotwait)
                    for d in dl:
                        key = id(d.sem)
                        if waited.get(key, 0) >= d.val:
                            continue
                        if key not in need or need[key][1] < d.val:
                            need[key] = (d.sem, d.val)
                    for key, (sem, val) in need.items():
                        eng.wait_ge(sem, val)
                        waited[key] = val
                    if o.fn is None:
                        continue
                    ins = o.fn(eng)
                    if o.sig:
                        ins.then_inc(o.sem, 16 if o.dma else 1)

            @block.tensor
            def _(eng):
                run("pe", eng)

            @block.scalar
            def _(eng):
                run("act", eng)

            @block.vector
            def _(eng):
                run("dve", eng)

            @block.gpsimd
            def _(eng):
                run("pool", eng)

            @block.sync
            def _(eng):
                run("sp", eng)
        self.es.close()


U8 = mybir.dt.uint8
DTSZ = {F32: 4, BF16: 2, I32: 4, U32: 4}


class Arena:
    def __init__(self, nc, nbytes):
        self.t = nc.alloc_sbuf_tensor("arena", [128, nbytes], U8)
        self.n = nbytes
        self.off = 0
        self.peak = 0

    def alloc(self, shape, dt):
        n = int(np.prod(shape)) * DTSZ[dt]
        n = (n + 63) // 64 * 64
        assert self.off + n <= self.n, f"arena overflow {self.off}+{n}>{self.n}"
        v = self.t[:, self.off:self.off + n].bitcast(dt)
        self.off += n
        self.peak = max(self.peak, self.off)
        tot = int(np.prod(shape))
        v = v[:, 0:tot]
        if len(shape) == 2:
            v = v.rearrange("p (a b) -> p a b", a=shape[0])
        elif len(shape) == 3:
            v = v.rearrange("p (a b c) -> p a b c", a=shape[0], b=shape[1])
        return v

    def mark(self):
        return self.off

    def release(self, m):
        self.off = m

from concourse.bass_utils import run_bass_kernel_spmd

L = 4096
D = 1024
NT = 32
NCH = 8
DIN = 7752
CQ, CK, CV, CQI, CKI, CSU, CSV, CGA, CGB = 0, 1024, 2048, 3072, 3584, 3656, 4680, 5704, 6728
NE = 32
CAP = 512
DUMMY = NE * CAP
KI = 20
EPS = 1e-6
PI = float(np.pi)
MAGIC = 12582912.0
C1 = 6.28125
C2 = 2 * PI - C1
NEG = -1.0e30
ARENA = 206 * 1024

C_INV, C_SGN, C_INVI, C_NH, C_HPI, C_NTHR, C_EPS, C_ONE, C_DUM, C_PW, C_NB = 0, 1, 2, 34, 35, 36, 37, 38, 39, 40, 64
NCST = 96


def host_consts():
    c = np.zeros((128, NCST), np.float32)
    inv128 = (np.float32(10000.0) ** (-np.arange(0, 128, 2, dtype=np.float32) / np.float32(128))).astype(np.float32)
    inv64 = (np.float32(10000.0) ** (-np.arange(0, 64, 2, dtype=np.float32) / np.float32(64))).astype(np.float32)
    p = np.arange(128)
    c[:, C_INV] = inv128[p % 64]
    c[:, C_SGN] = np.where(p < 64, -1.0, 1.0)
    c[:, C_INVI:C_INVI + 32] = inv64[None, :]
    c[:, C_NH] = -0.5
    c[:, C_HPI] = PI / 2
    c[:, C_NTHR] = -1.0e29
    c[:, C_EPS] = EPS
    c[:, C_ONE] = 1.0
    c[:, C_DUM] = NE * CAP + p
    c[:, C_PW:C_PW + KI + 2] = (2.0 ** -(np.arange(KI + 2) + 1.0))[None, :]
    c[:, C_NB:C_NB + NT] = (128.0 * (np.arange(NT) + 1.0) - 511.0)[None, :]
    m = np.zeros((128, 128 * 3 + 64), np.float32)
    t = np.arange(128)[:, None]
    s = np.arange(128)[None, :]
    m[:, 0:128] = np.where(s <= t, 0.0, NEG)
    m[:, 128:256] = np.where(s >= t, 1.0, 0.0)
    m[:, 256:384] = np.where(s <= t, 1.0, 0.0)
    m[:, 384:416] = (np.arange(32) * CAP)[None, :]
    m[:, 416:448] = 1.0
    return c, m


class Prog:
    pass


def build_program(stop_after=None, dbg=False):
    nc = bass.Bass("TRN2", target_bir_lowering=False)
    K = Kern(nc)
    AR = Arena(nc, ARENA)
    P = Prog()
    P.nc = nc

    def din(name, shape, dt=F32):
        return nc.dram_tensor(name, shape, dt, kind="ExternalInput").ap()

    def dscr(name, shape, dt, out=False):
        return nc.dram_tensor(name, shape, dt, kind="ExternalOutput" if (out and dbg) else "Internal").ap()

    x_d = din("x", [L, D])
    posr_d = din("pos_row", [1, L], I32)
    posc_d = din("pos_col", [128, NT], I32)
    g1_d = din("norm1_g", [1, D])
    win_d = din("w_in", [D, DIN])
    gs_d = din("sgu_norm_g", [1, D])
    sw_d = din("sgu_w", [8, 128, 128])
    sb_d = din("sgu_b", [8, 128])
    wa_d = din("w_branch_a", [D, D])
    wb_d = din("w_branch_b", [D, D])
    wo_d = din("w_out", [D, D])
    g2_d = din("norm2_g", [1, D])
    wrg_d = din("w_router_group", [D, 4])
    brg_d = din("b_router_group", [1, 4])
    wre_d = din("w_router_expert", [D, 32])
    bre_d = din("b_router_expert", [1, 32])
    wei_d = din("w_expert_in", [NE, D, 512])
    weo_d = din("w_expert_out", [NE, 256, D])
    gf_d = din("norm_f_g", [1, D])
    cst_d = din("cst", [128, NCST])
    cm_d = din("cmat", [128, 448])
    out_d = nc.dram_tensor("out", [L, D], F32, kind="ExternalOutput").ap()

    qT_s = dscr("qT_s", [8, 128, L], BF16, True)
    kT_s = dscr("kT_s", [8, 128, L], BF16, True)
    v_s = dscr("v_s", [NT, 128, 8 * 129], BF16, True)
    qiT_s = dscr("qiT_s", [5, 128, L], BF16, True)
    suT_s = dscr("suT_s", [D, L], BF16)
    ysgT_s = dscr("ysgT_s", [D, L], BF16, True)
    sgT_s = dscr("sgT_s", [2 * D, L], BF16, True)
    yatT_s = dscr("yatT_s", [D, L], BF16, True)
    h2_s = dscr("h2_s", [L, D], F32, True)
    xs_s = dscr("xs_s", [NE * CAP + 128, D], BF16)
    ys_s = dscr("ys_s", [NE * CAP + 128, D], BF16)

    ps = [nc.alloc_psum_tensor(f"ps{i}", [128, 512], F32) for i in range(8)]
    pst = [K.tok(f"ps{i}") for i in range(8)]

    def psb(i):
        return ps[i][:, :].bitcast(BF16)

    def dma(eng, out, in_, r=(), w=()):
        return K.op(eng, lambda e: e.dma_start(out=out, in_=in_), r=r, w=w, dma=True)

    def mm(out, lhsT, rhs, start, stop, r=(), w=(), sgc=False):
        return K.op("pe", lambda e: e.matmul(out, lhsT=lhsT, rhs=rhs, start=start, stop=stop, skip_group_check=sgc), r=r, w=w)

    def tr(out, in_, ident, r=(), w=()):
        return K.op("pe", lambda e: e.transpose(out, in_, ident), r=r, w=w)

    def act(out, in_, func, r=(), w=(), bias=None, scale=1.0, accum=None):
        def f(e):
            kw = {}
            if bias is not None:
                kw["bias"] = bias
            if accum is not None:
                kw["accum_out"] = accum
            return e.activation(out=out, in_=in_, func=func, scale=scale, **kw)
        return K.op("act", f, r=r, w=w)

    def ts(eng, out, in0, s1, op0, s2=None, op1=None, r=(), w=(), accum=None):
        def f(e):
            kw = {}
            if op1 is not None:
                kw["op1"] = op1
            if accum is not None:
                kw["accum_out"] = accum
            return e.tensor_scalar(out=out, in0=in0, scalar1=s1, scalar2=s2, op0=op0, **kw)
        return K.op(eng, f, r=r, w=w)

    def tt(eng, out, in0, in1, op, r=(), w=()):
        return K.op(eng, lambda e: e.tensor_tensor(out=out, in0=in0, in1=in1, op=op), r=r, w=w)

    def stt(out, in0, scalar, in1, op0, op1, r=(), w=(), accum=None):
        def f(e):
            kw = {}
            if accum is not None:
                kw["accum_out"] = accum
            return e.scalar_tensor_tensor(out=out, in0=in0, scalar=scalar, in1=in1, op0=op0, op1=op1, **kw)
        return K.op("dve", f, r=r, w=w)

    def cp(eng, out, in_, r=(), w=()):
        if eng == "act":
            return K.op("act", lambda e: e.activation(out=out, in_=in_, func=AF.Copy), r=r, w=w)
        return K.op(eng, lambda e: e.tensor_copy(out, in_), r=r, w=w)

    def mset(eng, out, val, r=(), w=()):
        return K.op(eng, lambda e: e.memset(out, val), r=r, w=w)

    class B:
        def __init__(self, shape, dt, name=""):
            self.ap = AR.alloc(shape, dt)
            self.t = K.tok(name)

    def ring(n, shape, dt, name=""):
        return [B(shape, dt, f"{name}{i}") for i in range(n)]

    cst = B([NCST], F32, "cst")
    cm = B([448], F32, "cm")
    ident = B([128], BF16, "ident")
    identf = B([128], F32, "identf")
    wi_sb = B([NT, 8], F32, "wi")
    rt_gate = B([NT * 2], F32, "gate")
    rt_pos = B([NT * 2], I32, "pos")
    dma("sp", cst.ap, cst_d, w=[cst.t])
    dma("sp", cm.ap, cm_d, w=[cm.t])
    mset("pool", identf.ap, 0.0, w=[identf.t])
    K.op("pool", lambda e: e.affine_select(out=identf.ap, in_=identf.ap, pattern=[[-1, 128]], compare_op=ALU.not_equal,
                                           fill=1.0, base=0, channel_multiplier=1), r=[identf.t], w=[identf.t])
    cp("pool", ident.ap, identf.ap, r=[identf.t], w=[ident.t])

    zt = B([D], BF16, "zt")
    mset("pool", zt.ap, 0.0, w=[zt.t])

    def col(b, j, n=1):
        return b.ap[:, j:j + n]

    def sincos(ang, n, kk, sin_out, cos_out, r, w):
        tk = K.tok()
        ts("dve", kk, ang, 1.0 / (2 * PI), ALU.mult, MAGIC, ALU.add, r=r, w=[tk])
        ts("dve", kk, kk, MAGIC, ALU.subtract, r=[tk], w=[tk])
        stt(ang, kk, -C1, ang, ALU.mult, ALU.add, r=r + [tk], w=r)
        stt(ang, kk, -C2, ang, ALU.mult, ALU.add, r=r + [tk], w=r)
        ts("dve", ang, ang, PI, ALU.min, -PI, ALU.max, r=r, w=r)
        act(sin_out, ang, AF.Sin, r=r, w=w)
        ts("dve", kk, ang, -1.0, ALU.mult, r=r, w=[tk])
        tt("dve", kk, kk, ang, ALU.max, r=r + [tk], w=[tk])
        act(cos_out, kk, AF.Sin, bias=col(cst, C_HPI), scale=-1.0, r=[tk, cst.t], w=w)


    def finish():
        K.barrier()
        K.emit()
        P.__dict__.update(dict(K=K, AR=AR))
        return P

    m_0 = AR.mark()
    xnT = AR.alloc([8, L], BF16)
    xnT_t = [K.tok(f"xnT{i}") for i in range(NT)]
    m_p1 = AR.mark()

    xbuf = ring(3, [D], F32, "xb")
    junk = B([D], F32, "junk")
    g1b = B([D], F32, "g1b")
    xnb = ring(2, [D], BF16, "xnb")
    ms1 = B([NT], F32, "ms1")
    rs1 = B([NT], F32, "rs1")
    ms1_t = [K.tok() for _ in range(NT)]
    rs1_t = [K.tok() for _ in range(NT)]
    dma("sp", g1b.ap, g1_d.partition_broadcast(128), w=[g1b.t])

    def a_front(i):
        xb = xbuf[i % 3]
        dma("sp", xb.ap, x_d[i * 128:(i + 1) * 128, :], w=[xb.t])
        stt(junk.ap, xb.ap, 1.0 / D, xb.ap, ALU.mult, ALU.mult, r=[xb.t], w=[junk.t, ms1_t[i]], accum=ms1.ap[:, i:i + 1])
        ts("pool", rs1.ap[:, i:i + 1], ms1.ap[:, i:i + 1], EPS, ALU.add, r=[ms1_t[i]], w=[rs1_t[i]])
        tt("pool", rs1.ap[:, i:i + 1], rs1.ap[:, i:i + 1], col(cst, C_NH), ALU.pow, r=[rs1_t[i], cst.t], w=[rs1_t[i]])

    def a_back(i):
        xb = xbuf[i % 3]
        nb = xnb[i % 2]
        stt(nb.ap, xb.ap, rs1.ap[:, i:i + 1], g1b.ap, ALU.mult, ALU.mult, r=[xb.t, rs1_t[i], g1b.t], w=[nb.t])
        bk = 4 + (i % 2)
        for kc in range(8):
            tr(psb(bk)[:, kc * 128:(kc + 1) * 128], nb.ap[:, kc * 128:(kc + 1) * 128], ident.ap, r=[nb.t, ident.t], w=[pst[bk]])
        cp("act", xnT[:, :, i * 128:(i + 1) * 128], psb(bk).rearrange("p (a b) -> p a b", a=8), r=[pst[bk]], w=[xnT_t[i]])

    for i in range(NT + 1):
        if i < NT:
            a_front(i)
        if i >= 1:
            a_back(i - 1)
    K.barrier()
    AR.release(m_p1)

    wbuf = ring(3, [8, 512], BF16, "wb")
    wstg = ring(2, [4, 512], F32, "wstg")
    wstate = {"n": 0}

    def load_w(c0, ncols, ceng="pool"):
        b = wbuf[wstate["n"] % 3]
        wstate["n"] += 1
        for hf in range(2):
            sg = wstg[hf]
            dma("sp", sg.ap[:, :, 0:ncols], win_d[hf * 512:(hf + 1) * 512, c0:c0 + ncols].rearrange("(kc p) c -> p kc c", p=128), w=[sg.t])
            cp(ceng, b.ap[:, 4 * hf:4 * hf + 4, 0:ncols], sg.ap[:, :, 0:ncols], r=[sg.t], w=[b.t])
        return b

    bank_rr = {"n": 0}

    def next_bank():
        bk = bank_rr["n"] % 4
        bank_rr["n"] += 1
        return bk

    def fm_block(wb, j, c, bk):
        for kc in range(8):
            mm(ps[bk][:, :], wb.ap[:, kc, j * 128:(j + 1) * 128], xnT[:, kc, c * 512:(c + 1) * 512], kc == 0, kc == 7,
               r=[wb.t] + xnT_t[4 * c:4 * c + 4], w=[pst[bk]])

    def tm_block(wb, ncols, i, bk, col0=0):
        for kc in range(8):
            mm(ps[bk][:, 0:ncols], xnT[:, kc, i * 128:(i + 1) * 128], wb.ap[:, kc, col0:col0 + ncols], kc == 0, kc == 7,
               r=[wb.t, xnT_t[i]], w=[pst[bk]])

    m_b1 = AR.mark()
    cosT = B([L], F32, "cosT")
    sinT = B([L], F32, "sinT")
    posi = B([1024], I32, "posi")
    angw = B([1024], F32, "angw")
    kkw = B([1024], F32, "kkw")
    for cc in range(4):
        sl = slice(cc * 1024, (cc + 1) * 1024)
        dma("sp", posi.ap, posr_d[:, sl].partition_broadcast(128), w=[posi.t])
        cp("dve", angw.ap, posi.ap, r=[posi.t], w=[angw.t])
        ts("dve", angw.ap, angw.ap, col(cst, C_INV), ALU.mult, r=[angw.t, cst.t], w=[angw.t])
        sincos(angw.ap, 1024, kkw.ap, sinT.ap[:, sl], cosT.ap[:, sl], r=[angw.t], w=[sinT.t, cosT.t])
        ts("dve", sinT.ap[:, sl], sinT.ap[:, sl], col(cst, C_SGN), ALU.mult, r=[sinT.t, cst.t], w=[sinT.t])
    posc = B([NT], I32, "posc")
    poscf = B([NT], F32, "poscf")
    sinI = B([NT, 32], F32, "sinI")
    cosI = B([NT, 32], F32, "cosI")
    dma("sp", posc.ap, posc_d, w=[posc.t])
    cp("dve", poscf.ap, posc.ap, r=[posc.t], w=[poscf.t])
    angI = angw.ap.rearrange("p (a b) -> p a b", a=NT)
    tt("dve", angI, poscf.ap.unsqueeze(2).broadcast_to([128, NT, 32]),
       cst.ap[:, C_INVI:C_INVI + 32].unsqueeze(1).broadcast_to([128, NT, 32]), ALU.mult, r=[poscf.t, cst.t, angw.t], w=[angw.t])
    sincos(angw.ap, 1024, kkw.ap, sinI.ap.rearrange("p a b -> p (a b)"), cosI.ap.rearrange("p a b -> p (a b)"),
           r=[angw.t], w=[sinI.t, cosI.t])

    t1r = ring(2, [512], F32, "t1")
    t2r = ring(2, [512], F32, "t2")
    qor = ring(3, [512], BF16, "qo")
    n_rope = {"n": 0}

    def rope_fm(bk, c, dst):
        k_ = n_rope["n"]
        n_rope["n"] += 1
        t1 = t1r[k_ % 2]
        t2 = t2r[k_ % 2]
        qo = qor[k_ % 3]
        sl = slice(c * 512, (c + 1) * 512)
        tt("dve", t1.ap, ps[bk][:, :], cosT.ap[:, sl], ALU.mult, r=[pst[bk], cosT.t], w=[t1.t])
        tt("dve", t2.ap[0:64, :], ps[bk][64:128, :], sinT.ap[0:64, sl], ALU.mult, r=[pst[bk], sinT.t], w=[t2.t])
        tt("dve", t2.ap[64:128, :], ps[bk][0:64, :], sinT.ap[64:128, sl], ALU.mult, r=[pst[bk], sinT.t], w=[t2.t])
        tt("pool", qo.ap, t1.ap, t2.ap, ALU.add, r=[t1.t, t2.t], w=[qo.t])
        dma("sp", dst, qo.ap, r=[qo.t])

    qk_blocks = [(CQ, qT_s, 0), (CQ + 512, qT_s, 4), (CK, kT_s, 0), (CK + 512, kT_s, 4)]
    wnext = load_w(qk_blocks[0][0], 512)
    for bi, (c0, dst_s, h0) in enumerate(qk_blocks):
        wb = wnext
        wnext = load_w(qk_blocks[bi + 1][0], 512, "act") if bi + 1 < 4 else load_w(CV, 512, "act")
        for c in range(NCH):
            for j in range(4):
                bk = next_bank()
                fm_block(wb, j, c, bk)
                rope_fm(bk, c, dst_s[h0 + j, :, c * 512:(c + 1) * 512])
    wv0 = wnext
    wv1 = load_w(CV + 512, 512)
    wqi = load_w(CQI, 512)
    vt = ring(2, [8, 129], BF16, "vt")
    for b_ in vt:
        mset("pool", b_.ap[:, :, 128:129], 1.0, w=[b_.t])
    for i in range(NT):
        v_ = vt[i % 2]
        for half, wb in enumerate((wv0, wv1)):
            bk = next_bank()
            tm_block(wb, 512, i, bk)
            cp("act", v_.ap[:, half * 4:(half + 1) * 4, 0:128], ps[bk][:, :].rearrange("p (a b) -> p a b", a=4), r=[pst[bk]], w=[v_.t])
        dma("sp", v_s[i].rearrange("p (h d) -> p h d", h=8), v_.ap, r=[v_.t])
    wkw = load_w(CKI, 72)
    w0_pre = load_w(CSU, 512)
    ra = ring(3, [9, 32], F32, "ra")
    rb = ring(3, [9, 32], F32, "rb")
    qst = ring(3, [9, 64], F32, "qst")
    qr = ring(3, [640], BF16, "qr")
    qiT_c = ring(2, [5, 512], BF16, "qiTc")
    def ip_front(i):
        bq = next_bank()
        tm_block(wqi, 512, i, bq)
        bkw = next_bank()
        tm_block(wkw, 72, i, bkw)
        q_ = qr[i % 3]
        a_, b2_, st_ = ra[i % 3], rb[i % 3], qst[i % 3]
        cp("act", st_.ap[:, 0:8, :], ps[bq][:, :].rearrange("p (h d) -> p h d", h=8), r=[pst[bq]], w=[st_.t])
        cp("act", st_.ap[:, 8, :], ps[bkw][:, 0:64], r=[pst[bkw]], w=[st_.t])
        cosb = cosI.ap[:, i, :].unsqueeze(1).broadcast_to([128, 9, 32])
        sinb = sinI.ap[:, i, :].unsqueeze(1).broadcast_to([128, 9, 32])
        qo_ = q_.ap[:, 0:576].rearrange("p (h d) -> p h d", h=9)
        rd = [st_.t, cosI.t, sinI.t]
        tt("dve", a_.ap, st_.ap[:, :, 0:32], cosb, ALU.mult, r=rd, w=[a_.t])
        tt("dve", b2_.ap, st_.ap[:, :, 32:64], sinb, ALU.mult, r=rd, w=[b2_.t])
        tt("pool", qo_[:, :, 0:32], a_.ap, b2_.ap, ALU.subtract, r=[a_.t, b2_.t], w=[q_.t])
        tt("dve", a_.ap, st_.ap[:, :, 32:64], cosb, ALU.mult, r=rd, w=[a_.t])
        tt("dve", b2_.ap, st_.ap[:, :, 0:32], sinb, ALU.mult, r=rd, w=[b2_.t])
        tt("pool", qo_[:, :, 32:64], a_.ap, b2_.ap, ALU.add, r=[a_.t, b2_.t], w=[q_.t])
        cp("pool", q_.ap[:, 576:640], q_.ap[:, 512:576], r=[q_.t], w=[q_.t])
        cp("act", wi_sb.ap[:, i, :], ps[bkw][:, 64:72], r=[pst[bkw]], w=[wi_sb.t])

    def ip_back(i):
        c, tau = i // 4, i % 4
        q_ = qr[i % 3]
        bt = 4 + (i % 2)
        for jj in range(5):
            tr(psb(bt)[:, jj * 128:(jj + 1) * 128], q_.ap[:, jj * 128:(jj + 1) * 128], ident.ap, r=[q_.t, ident.t], w=[pst[bt]])
        qc = qiT_c[c % 2]
        cp("act", qc.ap[:, :, tau * 128:(tau + 1) * 128], psb(bt)[:, 0:640].rearrange("p (a b) -> p a b", a=5), r=[pst[bt]], w=[qc.t])
        if tau == 3:
            dma("sp", qiT_s[:, :, c * 512:(c + 1) * 512].rearrange("a p t -> p a t"), qc.ap, r=[qc.t])
    for i in range(NT + 1):
        if i < NT:
            ip_front(i)
        if i >= 1:
            ip_back(i - 1)
    K.barrier()
    AR.release(m_b1)
    if stop_after == "1b":
        return finish()
    m_b2 = AR.mark()
    sub_r = ring(3, [512], BF16, "sub")
    su_t = [K.tok(f"suT{c}") for c in range(NCH)]
    w0 = w0_pre
    w1 = load_w(CSU + 512, 512)
    w2 = load_w(CSV, 512)
    nsub = {"n": 0}

    def act_block_out(bk, func, dst, wtok=()):
        o = sub_r[nsub["n"] % 3]
        nsub["n"] += 1
        act(o.ap, ps[bk][:, :], func, r=[pst[bk]], w=[o.t])
        dma("sp", dst, o.ap, r=[o.t], w=list(wtok))

    for blk, wb in enumerate((w0, w1)):
        for c in range(NCH):
            for j in range(4):
                bk = next_bank()
                fm_block(wb, j, c, bk)
                f0 = (blk * 4 + j) * 128
                act_block_out(bk, AF.Gelu_apprx_tanh, suT_s[f0:f0 + 128, c * 512:(c + 1) * 512], wtok=[su_t[c]])
    w3 = load_w(CSV + 512, 512)
    swf = B([8, 128], F32, "swf")
    swb = B([8, 128], BF16, "swb")
    WtT = B([8, 128], BF16, "WtT")
    dma("sp", swf.ap, sw_d.rearrange("g t s -> t g s"), w=[swf.t])
    tt("dve", swf.ap, swf.ap, cm.ap[:, 256:384].unsqueeze(1).broadcast_to([128, 8, 128]), ALU.mult, r=[swf.t, cm.t], w=[swf.t])
    cp("dve", swb.ap, swf.ap, r=[swf.t], w=[swb.t])
    for g in range(8):
        tr(psb(4)[:, g * 128:(g + 1) * 128], swb.ap[:, g, :], ident.ap, r=[swb.t, ident.t], w=[pst[4]])
    cp("act", WtT.ap, psb(4).rearrange("p (a b) -> p a b", a=8), r=[pst[4]], w=[WtT.t])
    bf_ = B([8, 128], F32, "bf")
    bhl = B([8, 128], BF16, "bhl")
    bhf = B([8, 128], F32, "bhf")
    ones2 = B([128], BF16, "ones2")
    mset("pool", ones2.ap, 1.0, w=[ones2.t])
    mset("pool", bf_.ap, 0.0, w=[bf_.t])
    dma("sp", bf_.ap[0:1, :, :], sb_d.rearrange("(o g) t -> o g t", o=1), r=[bf_.t], w=[bf_.t])
    dma("sp", bf_.ap[1:2, :, :], sb_d.rearrange("(o g) t -> o g t", o=1), r=[bf_.t], w=[bf_.t])
    cp("dve", bhl.ap, bf_.ap, r=[bf_.t], w=[bhl.t])
    cp("dve", bhf.ap, bhl.ap, r=[bhl.t], w=[bhf.t])
    tt("dve", bhf.ap, bf_.ap, bhf.ap, ALU.subtract, r=[bf_.t, bhf.t], w=[bhf.t])
    cp("dve", bf_.ap, bhl.ap, r=[bhl.t, bf_.t], w=[bf_.t])
    sel = B([1], F32, "sel")
    mset("pool", sel.ap, 1.0, w=[sel.t])
    K.op("pool", lambda e: e.affine_select(out=sel.ap, in_=sel.ap, pattern=[[0, 1]], compare_op=ALU.is_equal,
                                           fill=0.0, base=0, channel_multiplier=1), r=[sel.t], w=[sel.t])
    tt("dve", bf_.ap, bf_.ap, bhf.ap, ALU.subtract, r=[bf_.t, bhf.t], w=[bf_.t])
    stt(bhf.ap, bf_.ap, sel.ap[:, 0:1], bhf.ap, ALU.mult, ALU.add, r=[bf_.t, sel.t, bhf.t], w=[bhf.t])
    cp("dve", bhl.ap, bhf.ap, r=[bhf.t], w=[bhl.t])

    gsb = B([D], F32, "gsb")
    dma("sp", gsb.ap, gs_d.partition_broadcast(128), w=[gsb.t])
    gv = ring(3, [D], F32, "gv")
    vln = ring(3, [D], BF16, "vln")
    bst = B([NT, 12], F32, "bst")
    mv = B([NT, 2], F32, "mv")
    rsd = B([NT], F32, "rsd")
    bst_t = [K.tok() for _ in range(NT)]
    mv_t = [K.tok() for _ in range(NT)]
    rsd_t = [K.tok() for _ in range(NT)]
    suc = ring(2, [8, 512], BF16, "suc")
    ysg = ring(2, [8, 512], BF16, "ysg")
    wg_pre = load_w(CGA, 512)
    for c in range(NCH):
        su_c = suc[c % 2]
        ys_c = ysg[c % 2]
        dma("sp", su_c.ap, suT_s[:, c * 512:(c + 1) * 512].rearrange("(g p) t -> p g t", p=128), r=[su_t[c]], w=[su_c.t])
        def sv_front(tau, c=c):
            i = 4 * c + tau
            g_ = gv[i % 3]
            vl = vln[i % 3]
            sb0 = 6
            for half, wb in enumerate((w2, w3)):
                bk = next_bank()
                tm_block(wb, 512, i, bk)
                act(g_.ap[:, half * 512:(half + 1) * 512], ps[bk][:, :], AF.Gelu_apprx_tanh, r=[pst[bk]], w=[g_.t])
            K.op("dve", lambda e, i=i, g_=g_: e.bn_stats(out=bst.ap[:, i, 0:6], in_=g_.ap[:, 0:512]), r=[g_.t], w=[bst_t[i]])
            K.op("dve", lambda e, i=i, g_=g_: e.bn_stats(out=bst.ap[:, i, 6:12], in_=g_.ap[:, 512:1024]), r=[g_.t], w=[bst_t[i]])
            K.op("dve", lambda e, i=i: e.bn_aggr(out=mv.ap[:, i, :], in_=bst.ap[:, i, :]), r=[bst_t[i]], w=[mv_t[i]])
            ts("pool", rsd.ap[:, i:i + 1], mv.ap[:, i, 1:2], EPS, ALU.add, r=[mv_t[i]], w=[rsd_t[i]])
            tt("pool", rsd.ap[:, i:i + 1], rsd.ap[:, i:i + 1], col(cst, C_NH), ALU.pow, r=[rsd_t[i], cst.t], w=[rsd_t[i]])
            ts("dve", g_.ap, g_.ap, mv.ap[:, i, 0:1], ALU.subtract, rsd.ap[:, i:i + 1], ALU.mult, r=[g_.t, mv_t[i], rsd_t[i]], w=[g_.t])
            tt("pool", vl.ap, g_.ap, gsb.ap, ALU.mult, r=[g_.t, gsb.t], w=[vl.t])

        def sv_back(tau, c=c, su_c=su_c, ys_c=ys_c):
            i = 4 * c + tau
            vl = vln[i % 3]
            sb0 = 6
            for g in range(8):
                bk = sb0 + g // 4
                o_ = ps[bk][:, (g % 4) * 128:(g % 4 + 1) * 128]
                mm(o_, vl.ap[:, g * 128:(g + 1) * 128], WtT.ap[:, g, :], True, False, r=[vl.t, WtT.t], w=[pst[bk]])
                mm(o_, ones2.ap[0:2, :], bhl.ap[0:2, g, :], False, True, r=[ones2.t, bhl.t], w=[pst[bk]])
            for b_ in range(2):
                tt("dve", ys_c.ap[:, 4 * b_:4 * b_ + 4, tau * 128:(tau + 1) * 128],
                   ps[sb0 + b_][:, :].rearrange("p (a b) -> p a b", a=4),
                   su_c.ap[:, 4 * b_:4 * b_ + 4, tau * 128:(tau + 1) * 128], ALU.mult, r=[pst[sb0 + b_], su_c.t], w=[ys_c.t])
        for tau in range(5):
            if tau < 4:
                sv_front(tau)
            if tau >= 1:
                sv_back(tau - 1)
        dma("sp", ysgT_s[:, c * 512:(c + 1) * 512].rearrange("(g p) t -> p g t", p=128), ys_c.ap, r=[ys_c.t])
    wg = wg_pre
    for blk in range(4):
        wb = wg
        if blk < 3:
            wg = load_w(CGA + (blk + 1) * 512, 512)
        for c in range(NCH):
            for j in range(4):
                bk = next_bank()
                fm_block(wb, j, c, bk)
                f0 = (blk * 4 + j) * 128
                act_block_out(bk, AF.Sigmoid, sgT_s[f0:f0 + 128, c * 512:(c + 1) * 512])
    K.barrier()
    AR.release(m_0)
    if stop_after == "1":
        return finish()
    SCALE = float(128 ** -0.5)
    vres = AR.alloc([NT, 8, 129], BF16)
    v_t = [K.tok(f"v{i}") for i in range(NT)]
    kiT2 = AR.alloc([L], BF16)
    ki_t = [K.tok(f"ki{i}") for i in range(4)]
    for i in range(NT):
        dma("sp", vres[:, i, :, :], v_s[i].rearrange("p (h d) -> p h d", h=8), w=[v_t[i]])
        if i % 8 == 0:
            q4 = i // 8
            dma("sp", kiT2[:, q4 * 1024:(q4 + 1) * 1024], qiT_s[4, :, q4 * 1024:(q4 + 1) * 1024], w=[ki_t[q4]])
    nrow_t = (NE * CAP + 128) // 128
    for z0 in range(0, nrow_t, 16):
        zn = min(16, nrow_t - z0)
        dma("sp", xs_s[z0 * 128:(z0 + zn) * 128, :].rearrange("(a p) d -> p a d", p=128),
            zt.ap.unsqueeze(1).broadcast_to([128, zn, D]), r=[zt.t])

    kTr = ring(2, [L], BF16, "kTr")
    qTc = B([8, 512], BF16, "qTc")
    qiTc = B([4, 512], BF16, "qiTc")
    S = B([L], F32, "S")
    maskb = B([L], BF16, "maskb")
    maskb2 = B([L], BF16, "maskb2")
    maskT = B([NT, 512], BF16, "maskT")
    rbuf = ring(3, [512], F32, "rb")
    ebuf = ring(3, [512], BF16, "eb")
    pbuf = ring(3, [512], BF16, "pb")
    S2 = B([L], F32, "S2")

    class _V:
        pass
    ytile = _V()
    ytile.ap = S2.ap[:, 0:2048].bitcast(BF16).rearrange("p (a b) -> p a b", a=4)
    ytile.t = S2.t
    yT = _V()
    yT.ap = S2.ap[:, 2048:4096].bitcast(BF16).rearrange("p (a b) -> p a b", a=8)
    yT.t = S2.t
    Sb = (S, S2)
    Mb2 = (maskb, maskb2)
    sst = []
    for q_ in range(2):
        sst.append({nm: B([sz], F32, f"{nm}{q_}") for nm, sz in (
            ("vmax", 1), ("vmin", 1), ("rngv", 1), ("hw", KI + 2), ("nhw", KI + 2), ("cand", 1), ("cnt", 1), ("msg", 1), ("thr", 1))})
    recr = ring(2, [4], F32, "rec")
    ctr = {"sc": 0, "lg": 0, "tr": 0, "e": 0}

    for c in range(NCH):
        csl = slice(c * 512, (c + 1) * 512)
        dma("sp", qTc.ap, qT_s[:, :, csl].rearrange("h p t -> p h t"), w=[qTc.t])
        dma("sp", qiTc.ap, qiT_s[0:4, :, csl].rearrange("a p t -> p a t"), w=[qiTc.t])
        def idx_scores(tau, Sx):
            i = 4 * c + tau
            n = 128 * (i + 1)
            nv = 128 * i
            for sc in range((n + 511) // 512):
                w_ = min(512, n - 512 * sc)
                sl = slice(sc * 512, sc * 512 + w_)
                for h in range(8):
                    bk = ctr["sc"] % 2
                    ctr["sc"] += 1
                    pr = slice(64 * (h % 2), 64 * (h % 2) + 64)
                    mm(ps[bk][:, 0:w_], qiTc.ap[pr, h // 2, tau * 128:(tau + 1) * 128], kiT2[pr, sl], True, True,
                       r=[qiTc.t, ki_t[sc // 2]], w=[pst[bk]])
                    rb = rbuf[ctr["sc"] % 3]
                    act(rb.ap[:, 0:w_], ps[bk][:, 0:w_], AF.Relu, r=[pst[bk]], w=[rb.t])
                    if h == 0:
                        ts("dve", Sx.ap[:, sl], rb.ap[:, 0:w_], wi_sb.ap[:, i, 0:1], ALU.mult, r=[rb.t, wi_sb.t], w=[Sx.t])
                    else:
                        stt(Sx.ap[:, sl], rb.ap[:, 0:w_], wi_sb.ap[:, i, h:h + 1], Sx.ap[:, sl], ALU.mult, ALU.add,
                            r=[rb.t, wi_sb.t, Sx.t], w=[Sx.t])
            tt("pool", Sx.ap[:, nv:n], Sx.ap[:, nv:n], cm.ap[:, 0:128], ALU.add, r=[Sx.t, cm.t], w=[Sx.t])

        def search_pre(tau, Sx, mb, st, on_act):
            i = 4 * c + tau
            n = 128 * (i + 1)
            nv = 128 * i
            if i < 2:
                return
            A_ = lambda nm: st[nm].ap
            T2 = lambda nm: st[nm].t
            K.op("dve", lambda e: e.tensor_reduce(out=A_("vmax"), in_=Sx.ap[:, 0:n], axis=AX.X, op=ALU.max), r=[Sx.t], w=[T2("vmax")])
            K.op("dve", lambda e: e.tensor_reduce(out=A_("vmin"), in_=Sx.ap[:, 0:nv], axis=AX.X, op=ALU.min), r=[Sx.t], w=[T2("vmin")])
            tt("dve", A_("rngv"), A_("vmax"), A_("vmin"), ALU.subtract, r=[T2("vmax"), T2("vmin")], w=[T2("rngv")])
            ts("dve", A_("hw"), cst.ap[:, C_PW:C_PW + KI + 2], A_("rngv")[:, 0:1], ALU.mult, r=[cst.t, T2("rngv")], w=[T2("hw")])
            if not on_act:
                tt("dve", A_("cand"), A_("vmin"), A_("hw")[:, 0:1], ALU.add, r=[T2("vmin"), T2("hw")], w=[T2("cand")])
            else:
                ts("dve", A_("nhw"), A_("hw"), -1.0, ALU.mult, r=[T2("hw")], w=[T2("nhw")])
                stt(A_("cand"), A_("vmin"), -1.0, A_("hw")[:, 0:1], ALU.mult, ALU.subtract, r=[T2("vmin"), T2("hw")], w=[T2("cand")])

        def search(tau, Sx, mb, st, on_act):
            i = 4 * c + tau
            n = 128 * (i + 1)
            if i < 2:
                return col(cst, C_NTHR), cst.t
            A_ = lambda nm: st[nm].ap
            T2 = lambda nm: st[nm].t
            if not on_act:
                for k in range(KI):
                    ts("dve", mb.ap[:, 0:n], Sx.ap[:, 0:n], A_("cand")[:, 0:1], ALU.is_ge, None, ALU.add,
                       r=[Sx.t, T2("cand")], w=[mb.t, T2("cnt")], accum=A_("cnt"))
                    ts("dve", A_("msg"), A_("cnt"), 255.5, ALU.is_ge, 0.5, ALU.subtract, r=[T2("cnt")], w=[T2("msg")])
                    stt(A_("cand"), A_("msg"), A_("hw")[:, k:k + 1], A_("cand"), ALU.mult, ALU.add, r=[T2("msg"), T2("hw"), T2("cand")], w=[T2("cand")])
                tt("dve", A_("thr"), A_("cand"), A_("hw")[:, KI:KI + 1], ALU.subtract, r=[T2("cand"), T2("hw")], w=[T2("thr")])
            else:
                for k in range(KI):
                    act(mb.ap[:, 0:n], Sx.ap[:, 0:n], AF.Sign, bias=A_("cand")[:, 0:1], r=[Sx.t, T2("cand")], w=[mb.t, T2("cnt")], accum=A_("cnt"))
                    act(A_("msg"), A_("cnt"), AF.Sign, bias=col(cst, C_NB + i), r=[T2("cnt"), cst.t], w=[T2("msg")])
                    act(A_("cand"), A_("msg"), AF.Identity, bias=A_("cand")[:, 0:1], scale=A_("nhw")[:, k + 1:k + 2],
                        r=[T2("msg"), T2("nhw"), T2("cand")], w=[T2("cand")])
                act(A_("thr"), A_("cand"), AF.Identity, bias=A_("nhw")[:, KI:KI + 1], scale=-1.0, r=[T2("cand"), T2("nhw")], w=[T2("thr")])
            return A_("thr")[:, 0:1], T2("thr")

        def make_mask(tau, Sx, mb, thr_ap, thr_t):
            i = 4 * c + tau
            n = 128 * (i + 1)
            ts("dve", mb.ap[:, 0:n], Sx.ap[:, 0:n], thr_ap, ALU.is_ge, r=[Sx.t, thr_t], w=[mb.t])
            for j0 in range(0, i + 1, 8):
                nb = min(8, i + 1 - j0)
                bk = 2 + ctr["tr"] % 2
                ctr["tr"] += 1
                for jj in range(nb):
                    tr(psb(bk)[:, jj * 128:(jj + 1) * 128], mb.ap[:, (j0 + jj) * 128:(j0 + jj + 1) * 128], ident.ap,
                       r=[mb.t, ident.t], w=[pst[bk]])
                cp("act", maskT.ap[:, j0:j0 + nb, tau * 128:(tau + 1) * 128],
                   psb(bk)[:, 0:nb * 128].rearrange("p (a b) -> p a b", a=nb), r=[pst[bk]], w=[maskT.t])

        for pair in range(2):
            t0, t1_ = 2 * pair, 2 * pair + 1
            idx_scores(t0, Sb[0])
            idx_scores(t1_, Sb[1])
            search_pre(t0, Sb[0], Mb2[0], sst[0], False)
            search_pre(t1_, Sb[1], Mb2[1], sst[1], True)
            th1 = search(t1_, Sb[1], Mb2[1], sst[1], True)
            th0 = search(t0, Sb[0], Mb2[0], sst[0], False)
            make_mask(t0, Sb[0], Mb2[0], *th0)
            make_mask(t1_, Sb[1], Mb2[1], *th1)
        nj = 4 * c + 4
        steps = [(h, j) for h in range(8) for j in range(nj)]
        LB = (1, 2, 3)

        def emit_qk(k):
            h, j = steps[k]
            kt = kTr[h % 2]
            if j == 0:
                dma("sp", kt.ap[:, 0:nj * 128], kT_s[h, :, 0:nj * 128], w=[kt.t])
            r0 = max(0, j - 4 * c)
            N = 512 - 128 * r0
            bk = LB[k % 3]
            mm(ps[bk][:, 0:N], kt.ap[:, j * 128:(j + 1) * 128], qTc.ap[:, h, r0 * 128:512], True, True,
               r=[kt.t, qTc.t], w=[pst[bk]])

        emit_qk(0)
        emit_qk(1)
        for k, (h, j) in enumerate(steps):
            if k + 2 < len(steps):
                emit_qk(k + 2)
            accA = 4 + 2 * (h % 2)
            accB = accA + 1
            r0 = max(0, j - 4 * c)
            N = 512 - 128 * r0
            bk = LB[k % 3]
            e_ = ebuf[k % 3]
            p_ = pbuf[k % 3]
            act(e_.ap[:, 0:N], ps[bk][:, 0:N], AF.Exp, scale=SCALE, r=[pst[bk]], w=[e_.t])
            tt("dve", p_.ap[:, 0:N], e_.ap[:, 0:N], maskT.ap[:, j, r0 * 128:512], ALU.mult, r=[e_.t, maskT.t], w=[p_.t])
            for tau in range(r0, 4):
                if tau < 3:
                    o_, ob = ps[accA][:, tau * 129:(tau + 1) * 129], accA
                else:
                    o_, ob = ps[accB][:, 0:129], accB
                first = (j == 0 and tau in (0, 3))
                mm(o_, p_.ap[:, (tau - r0) * 128:(tau - r0 + 1) * 128], vres[:, j, h, :], first, j == nj - 1,
                   r=[p_.t, v_t[j]], w=[pst[ob]], sgc=True)
            if j == nj - 1:
                rc = recr[h % 2]
                K.op("dve", lambda e, rc=rc, accA=accA: e.reciprocal(
                    out=rc.ap[:, 0:3], in_=ps[accA][:, 0:387].rearrange("p (a b) -> p a b", b=129)[:, :, 128]), r=[pst[accA]], w=[rc.t])
                K.op("dve", lambda e, rc=rc, accB=accB: e.reciprocal(out=rc.ap[:, 3:4], in_=ps[accB][:, 128:129]), r=[pst[accB]], w=[rc.t])
                for tau in range(4):
                    src, sb_ = (ps[accA][:, tau * 129:tau * 129 + 128], accA) if tau < 3 else (ps[accB][:, 0:128], accB)
                    ts("dve", ytile.ap[:, tau, h * 128:(h + 1) * 128], src, rc.ap[:, tau:tau + 1], ALU.mult, r=[pst[sb_], rc.t], w=[ytile.t])
        for tau in range(4):
            bk = 2 + ctr["tr"] % 2
            ctr["tr"] += 1
            for kc in range(8):
                tr(psb(bk)[:, kc * 128:(kc + 1) * 128], ytile.ap[:, tau, kc * 128:(kc + 1) * 128], ident.ap, r=[ytile.t, ident.t], w=[pst[bk]])
            cp("act", yT.ap[:, :, tau * 128:(tau + 1) * 128], psb(bk).rearrange("p (a b) -> p a b", a=8), r=[pst[bk]], w=[yT.t])
        dma("sp", yatT_s[:, csl].rearrange("(g p) t -> p g t", p=128), yT.ap, r=[yT.t])
    K.barrier()
    AR.release(m_0)
    if stop_after == "2":
        return finish()
    m_3 = AR.mark()
    Wa_sb = B([8, D], BF16, "Wa")
    Wb_sb = B([8, D], BF16, "Wb")
    Wo_sb = B([8, D], BF16, "Wo")
    w3stg = ring(2, [2, D], F32, "w3stg")
    for wi3, (wsb, wd) in enumerate(((Wa_sb, wa_d), (Wb_sb, wb_d), (Wo_sb, wo_d))):
        for q4 in range(4):
            sg = w3stg[(wi3 * 4 + q4) % 2]
            dma("sp", sg.ap, wd[q4 * 256:(q4 + 1) * 256, :].rearrange("(kc p) n -> p kc n", p=128), w=[sg.t])
            cp("dve" if q4 % 2 else "act", wsb.ap[:, 2 * q4:2 * q4 + 2, :], sg.ap, r=[sg.t], w=[wsb.t])
    g2b = B([D], F32, "g2b")
    dma("sp", g2b.ap, g2_d.partition_broadcast(128), w=[g2b.t])
    wr = B([8, 36], F32, "wr")
    br = B([36], F32, "br")
    dma("sp", wr.ap[:, :, 0:4], wrg_d.rearrange("(kc p) n -> p kc n", p=128), w=[wr.t])
    dma("sp", wr.ap[:, :, 4:36], wre_d.rearrange("(kc p) n -> p kc n", p=128), w=[wr.t])
    dma("sp", br.ap[:, 0:4], brg_d.partition_broadcast(128), w=[br.t])
    dma("sp", br.ap[:, 4:36], bre_d.partition_broadcast(128), w=[br.t])
    trib = B([128], BF16, "trib")
    onesb = B([128], BF16, "onesb")
    cp("pool", trib.ap, cm.ap[:, 128:256], r=[cm.t], w=[trib.t])
    mset("pool", onesb.ap, 1.0, w=[onesb.t])
    base = B([32], F32, "base")
    capb = B([32], F32, "capb")
    ts("pool", base.ap, cm.ap[:, 384:416], -1.0, ALU.add, r=[cm.t], w=[base.t])
    ts("pool", capb.ap, cm.ap[:, 384:416], float(CAP) - 0.5, ALU.add, r=[cm.t], w=[capb.t])
    inr = [ring(2, [8, 512], BF16, nm) for nm in ("ysgc", "yatc", "sgac", "sgbc")]
    mT = B([8, 512], BF16, "mT")
    t1p = ring(2, [512], F32, "t1p")
    t2p = ring(2, [512], F32, "t2p")
    xtr = ring(2, [D], F32, "xt")
    h2r = ring(2, [D], F32, "h2t")
    ms2 = B([NT], F32, "ms2")
    rs2 = B([NT], F32, "rs2")
    xn2f = B([D], F32, "xn2f")
    xn2b = ring(6, [D], BF16, "xn2b")
    xn2T = B([8, 128], F32, "xn2T")
    lgt4 = B([4, 36], F32, "lgt4")
    rb_ = {nm: B(sz, F32, nm) for nm, sz in (
        ("gmax", [4]), ("ohg", [4, 4]), ("dgl", [4, 4]), ("exg", [4, 4]), ("se", [4]), ("gp", [4]), ("prod", [4, 4, 8]), ("esel", [4, 8]),
        ("m1", [4]), ("oh1", [4, 8]), ("es2", [4, 8]), ("m2", [4]), ("oh2", [4, 8]), ("dlt", [4]), ("ex", [4]), ("den", [4]), ("p1", [4]),
        ("p2", [4]), ("gk", [2, 4]), ("M1", [4, 32]), ("M2", [4, 32]), ("posf", [4, 32]), ("okf", [4, 32]), ("pr32", [4, 32]),
        ("pk", [2, 4]), ("ok", [2, 4]))}
    Mb4 = B([4, 32], BF16, "Mb4")
    sm = {nm: B([sz], F32, nm) for nm, sz in (
        ("gmax", 1), ("negg", 1), ("ohg", 4), ("j4", 4), ("se", 1), ("gp", 1), ("esel", 8), ("m1", 1), ("oh1", 8), ("es2", 8),
        ("m2", 1), ("oh2", 8), ("dlt", 1), ("ex", 1), ("den", 1), ("p1", 1), ("p2", 1), ("M1", 32), ("M2", 32), ("posf", 32),
        ("okf", 32), ("j32", 32), ("pk", 2), ("ok", 2), ("gk", 2))}
    Mb = B([32], BF16, "Mb")

    def S_(nm):
        return sm[nm].ap

    def T_(nm):
        return sm[nm].t

    def load_chunk3(c):
        csl = slice(c * 512, (c + 1) * 512)
        ysg_c, yat_c, sga_c, sgb_c = [rg[c % 2] for rg in inr]
        dma("sp", ysg_c.ap, ysgT_s[:, csl].rearrange("(g p) t -> p g t", p=128), w=[ysg_c.t])
        dma("sp", yat_c.ap, yatT_s[:, csl].rearrange("(g p) t -> p g t", p=128), w=[yat_c.t])
        dma("sp", sga_c.ap, sgT_s[0:D, csl].rearrange("(g p) t -> p g t", p=128), w=[sga_c.t])
        dma("sp", sgb_c.ap, sgT_s[D:2 * D, csl].rearrange("(g p) t -> p g t", p=128), w=[sgb_c.t])

    load_chunk3(0)
    for c in range(NCH):
        csl = slice(c * 512, (c + 1) * 512)
        ysg_c, yat_c, sga_c, sgb_c = [rg[c % 2] for rg in inr]
        if c + 1 < NCH:
            load_chunk3(c + 1)
        for nb in range(8):
            bA = next_bank()
            for kc in range(8):
                mm(ps[bA][:, :], Wa_sb.ap[:, kc, nb * 128:(nb + 1) * 128], ysg_c.ap[:, kc, :], kc == 0, kc == 7, r=[Wa_sb.t, ysg_c.t], w=[pst[bA]])
            bB = next_bank()
            for kc in range(8):
                mm(ps[bB][:, :], Wb_sb.ap[:, kc, nb * 128:(nb + 1) * 128], yat_c.ap[:, kc, :], kc == 0, kc == 7, r=[Wb_sb.t, yat_c.t], w=[pst[bB]])
            t1, t2 = t1p[nb % 2], t2p[nb % 2]
            tt("dve", t1.ap, ps[bA][:, :], sga_c.ap[:, nb, :], ALU.mult, r=[pst[bA], sga_c.t], w=[t1.t])
            tt("dve", t2.ap, ps[bB][:, :], sgb_c.ap[:, nb, :], ALU.mult, r=[pst[bB], sgb_c.t], w=[t2.t])
            tt("pool", mT.ap[:, nb, :], t1.ap, t2.ap, ALU.add, r=[t1.t, t2.t], w=[mT.t])
        for tau in range(4):
            i = 4 * c + tau
            xt, h2t, xb2 = xtr[i % 2], h2r[i % 2], xn2b[i % 6]
            dma("sp", xt.ap, x_d[i * 128:(i + 1) * 128, :], w=[xt.t])
            for half in range(2):
                hs = slice(half * 512, (half + 1) * 512)
                bk = next_bank()
                for kc in range(8):
                    mm(ps[bk][:, :], mT.ap[:, kc, tau * 128:(tau + 1) * 128], Wo_sb.ap[:, kc, hs], kc == 0, kc == 7, r=[mT.t, Wo_sb.t], w=[pst[bk]])
                tt("dve", h2t.ap[:, hs], ps[bk][:, :], xt.ap[:, hs], ALU.add, r=[pst[bk], xt.t], w=[h2t.t])
            dma("sp", h2_s[i * 128:(i + 1) * 128, :], h2t.ap, r=[h2t.t])
            stt(xn2f.ap, h2t.ap, 1.0 / D, h2t.ap, ALU.mult, ALU.mult, r=[h2t.t], w=[xn2f.t, ms2.t], accum=ms2.ap[:, i:i + 1])
            ts("pool", rs2.ap[:, i:i + 1], ms2.ap[:, i:i + 1], EPS, ALU.add, r=[ms2.t], w=[rs2.t])
            tt("pool", rs2.ap[:, i:i + 1], rs2.ap[:, i:i + 1], col(cst, C_NH), ALU.pow, r=[rs2.t, cst.t], w=[rs2.t])
            stt(xn2f.ap, h2t.ap, rs2.ap[:, i:i + 1], g2b.ap, ALU.mult, ALU.mult, r=[h2t.t, rs2.t, g2b.t], w=[xn2f.t])
            cp("pool", xb2.ap, xn2f.ap, r=[xn2f.t], w=[xb2.t])
            for q4 in range(2):
                bt = 4 + q4
                for jj in range(4):
                    kc = q4 * 4 + jj
                    tr(ps[bt][:, jj * 128:(jj + 1) * 128], xn2f.ap[:, kc * 128:(kc + 1) * 128], identf.ap, r=[xn2f.t, identf.t], w=[pst[bt]])
                cp("act", xn2T.ap[:, q4 * 4:(q4 + 1) * 4, :], ps[bt][:, :].rearrange("p (a b) -> p a b", a=4), r=[pst[bt]], w=[xn2T.t])
            for kc in range(8):
                mm(ps[6][:, 0:36], xn2T.ap[:, kc, :], wr.ap[:, kc, :], kc == 0, kc == 7, r=[xn2T.t, wr.t], w=[pst[6]])
            tt("dve", lgt4.ap[:, tau, :], ps[6][:, 0:36], br.ap, ALU.add, r=[pst[6], br.t], w=[lgt4.t])

        gl4 = lgt4.ap[:, :, 0:4]
        el4 = lgt4.ap[:, :, 4:36].rearrange("p t (g j) -> p t g j", g=4)
        R_ = lambda nm: rb_[nm].ap
        Q_ = lambda nm: rb_[nm].t
        b3 = lambda ap, shp: ap.unsqueeze(2).broadcast_to(shp)
        K.op("dve", lambda e: e.tensor_reduce(out=R_("gmax"), in_=gl4, axis=AX.X, op=ALU.max), r=[lgt4.t], w=[Q_("gmax")])
        tt("dve", R_("ohg"), gl4, b3(R_("gmax"), [128, 4, 4]), ALU.is_ge, r=[lgt4.t, Q_("gmax")], w=[Q_("ohg")])
        tt("dve", R_("dgl"), gl4, b3(R_("gmax"), [128, 4, 4]), ALU.subtract, r=[lgt4.t, Q_("gmax")], w=[Q_("dgl")])
        act(R_("exg"), R_("dgl"), AF.Exp, r=[Q_("dgl")], w=[Q_("exg")])
        K.op("dve", lambda e: e.tensor_reduce(out=R_("se"), in_=R_("exg"), axis=AX.X, op=ALU.add), r=[Q_("exg")], w=[Q_("se")])
        K.op("dve", lambda e: e.reciprocal(out=R_("gp"), in_=R_("se")), r=[Q_("se")], w=[Q_("gp")])
        tt("dve", R_("prod"), el4, R_("ohg").unsqueeze(3).broadcast_to([128, 4, 4, 8]), ALU.mult, r=[lgt4.t, Q_("ohg")], w=[Q_("prod")])
        tt("dve", R_("esel"), R_("prod")[:, :, 0, :], R_("prod")[:, :, 1, :], ALU.add, r=[Q_("prod")], w=[Q_("esel")])
        tt("dve", R_("esel"), R_("esel"), R_("prod")[:, :, 2, :], ALU.add, r=[Q_("prod"), Q_("esel")], w=[Q_("esel")])
        tt("dve", R_("esel"), R_("esel"), R_("prod")[:, :, 3, :], ALU.add, r=[Q_("prod"), Q_("esel")], w=[Q_("esel")])
        K.op("dve", lambda e: e.tensor_reduce(out=R_("m1"), in_=R_("esel"), axis=AX.X, op=ALU.max), r=[Q_("esel")], w=[Q_("m1")])
        tt("dve", R_("oh1"), R_("esel"), b3(R_("m1"), [128, 4, 8]), ALU.is_ge, r=[Q_("esel"), Q_("m1")], w=[Q_("oh1")])
        stt(R_("es2"), R_("oh1"), NEG, R_("esel"), ALU.mult, ALU.add, r=[Q_("oh1"), Q_("esel")], w=[Q_("es2")])
        K.op("dve", lambda e: e.tensor_reduce(out=R_("m2"), in_=R_("es2"), axis=AX.X, op=ALU.max), r=[Q_("es2")], w=[Q_("m2")])
        tt("dve", R_("oh2"), R_("es2"), b3(R_("m2"), [128, 4, 8]), ALU.is_ge, r=[Q_("es2"), Q_("m2")], w=[Q_("oh2")])
        tt("dve", R_("dlt"), R_("m2"), R_("m1"), ALU.subtract, r=[Q_("m1"), Q_("m2")], w=[Q_("dlt")])
        act(R_("ex"), R_("dlt"), AF.Exp, r=[Q_("dlt")], w=[Q_("ex")])
        ts("dve", R_("den"), R_("ex"), 1.0, ALU.add, r=[Q_("ex")], w=[Q_("den")])
        K.op("dve", lambda e: e.reciprocal(out=R_("p1"), in_=R_("den")), r=[Q_("den")], w=[Q_("p1")])
        tt("dve", R_("p2"), R_("ex"), R_("p1"), ALU.mult, r=[Q_("ex"), Q_("p1")], w=[Q_("p2")])
        tt("dve", R_("gk")[:, 0, :], R_("p1"), R_("gp"), ALU.mult, r=[Q_("p1"), Q_("gp")], w=[Q_("gk")])
        tt("dve", R_("gk")[:, 1, :], R_("p2"), R_("gp"), ALU.mult, r=[Q_("p2"), Q_("gp")], w=[Q_("gk")])
        ohg4 = R_("ohg").unsqueeze(3).broadcast_to([128, 4, 4, 8])
        for nm, oh in (("M1", "oh1"), ("M2", "oh2")):
            tt("dve", R_(nm).rearrange("p t (g j) -> p t g j", g=4), ohg4, R_(oh).unsqueeze(2).broadcast_to([128, 4, 4, 8]), ALU.mult,
               r=[Q_("ohg"), Q_(oh)], w=[Q_(nm)])
        tt("dve", Mb4.ap, R_("M1"), R_("M2"), ALU.add, r=[Q_("M1"), Q_("M2")], w=[Mb4.t])
        for tau in range(4):
            o_ = ps[7][:, tau * 32:(tau + 1) * 32]
            mm(o_, trib.ap, Mb4.ap[:, tau, :], True, tau == 0, r=[trib.t, Mb4.t], w=[pst[7]], sgc=True)
            for tp_ in range(tau):
                mm(o_, onesb.ap, Mb4.ap[:, tp_, :], False, tp_ == tau - 1, r=[onesb.t, Mb4.t], w=[pst[7]], sgc=True)
        for tau in range(4):
            mm(ps[7][:, 128:160], onesb.ap, Mb4.ap[:, tau, :], False, tau == 3, r=[onesb.t, Mb4.t], w=[pst[7]], sgc=True)
        tt("dve", R_("posf"), ps[7][:, 0:128].rearrange("p (t e) -> p t e", t=4), base.ap.unsqueeze(1).broadcast_to([128, 4, 32]), ALU.add,
           r=[pst[7], base.t], w=[Q_("posf")])
        tt("dve", base.ap, ps[7][:, 128:160], base.ap, ALU.add, r=[pst[7], base.t], w=[base.t])
        tt("dve", R_("okf"), R_("posf"), capb.ap.unsqueeze(1).broadcast_to([128, 4, 32]), ALU.is_lt, r=[Q_("posf"), capb.t], w=[Q_("okf")])
        for k_, nm in enumerate(("M1", "M2")):
            tt("dve", R_("pr32"), R_(nm), R_("posf"), ALU.mult, r=[Q_(nm), Q_("posf")], w=[Q_("pr32")])
            K.op("dve", lambda e, k_=k_: e.tensor_reduce(out=R_("pk")[:, k_, :], in_=R_("pr32"), axis=AX.X, op=ALU.add), r=[Q_("pr32")], w=[Q_("pk")])
            tt("dve", R_("pr32"), R_(nm), R_("okf"), ALU.mult, r=[Q_(nm), Q_("okf")], w=[Q_("pr32")])
            K.op("dve", lambda e, k_=k_: e.tensor_reduce(out=R_("ok")[:, k_, :], in_=R_("pr32"), axis=AX.X, op=ALU.add), r=[Q_("pr32")], w=[Q_("ok")])
        ts("dve", R_("pk"), R_("pk"), col(cst, C_DUM), ALU.subtract, r=[Q_("pk"), cst.t], w=[Q_("pk")])
        tt("dve", R_("pk"), R_("pk"), R_("ok"), ALU.mult, r=[Q_("pk"), Q_("ok")], w=[Q_("pk")])
        ts("dve", R_("pk"), R_("pk"), col(cst, C_DUM), ALU.add, r=[Q_("pk"), cst.t], w=[Q_("pk")])
        cp("dve", rt_pos.ap[:, 8 * c:8 * c + 8].rearrange("p (t k) -> p k t", k=2), R_("pk"), r=[Q_("pk")], w=[rt_pos.t])
        tt("dve", rt_gate.ap[:, 8 * c:8 * c + 8].rearrange("p (t k) -> p k t", k=2), R_("gk"), R_("ok"), ALU.mult, r=[Q_("gk"), Q_("ok")], w=[rt_gate.t])
        for tau in range(4):
            i = 4 * c + tau
            xb2 = xn2b[i % 6]
            for k_ in range(2):
                K.op("pool", lambda e, i=i, k_=k_, xb2=xb2: e.indirect_dma_start(
                    out=xs_s[:, :], out_offset=bass.IndirectOffsetOnAxis(ap=rt_pos.ap[:, 2 * i + k_:2 * i + k_ + 1], axis=0),
                    in_=xb2.ap, in_offset=None), r=[xb2.t, rt_pos.t], dma=True)
    K.barrier()
    AR.release(m_3)
    if stop_after == "3":
        return finish()
    m_4 = AR.mark()
    wi_r = ring(2, [8, 512], BF16, "wie")
    wo_r = ring(3, [2, D], BF16, "woe")
    wis_r = ring(2, [8, 512], F32, "wis")
    wos_r = ring(2, [2, D], F32, "wos")
    xs_r = ring(2, [4, D], BF16, "xs")
    xsT_r = ring(2, [8, 512], BF16, "xsT")
    sg_r = ring(2, [512], F32, "sg")
    aT_r = ring(2, [2, 512], BF16, "aT")
    ys_r = ring(3, [D], BF16, "ysb")
    nys = {"n": 0}
    mset("pool", ys_r[2].ap, 0.0, w=[ys_r[2].t])
    dma("sp", ys_s[NE * CAP:NE * CAP + 128, :], ys_r[2].ap, r=[ys_r[2].t])
    def load_expert(e_i):
        wi_, wo_, xs_ = wi_r[e_i % 2], wo_r[e_i % 3], xs_r[e_i % 2]
        sgi, sgo = wis_r[e_i % 2], wos_r[e_i % 2]
        dma("sp", xs_.ap, xs_s[e_i * CAP:(e_i + 1) * CAP, :].rearrange("(st p) d -> p st d", p=128), w=[xs_.t])
        dma("sp", sgi.ap, wei_d[e_i].rearrange("(kc p) f -> p kc f", p=128), w=[sgi.t])
        dma("sp", sgo.ap, weo_d[e_i].rearrange("(fc p) n -> p fc n", p=128), w=[sgo.t])

    def cast_expert(e_i):
        wi_, wo_ = wi_r[e_i % 2], wo_r[e_i % 3]
        sgi, sgo = wis_r[e_i % 2], wos_r[e_i % 2]
        cp("pool", wi_.ap[:, 0:3, :], sgi.ap[:, 0:3, :], r=[sgi.t], w=[wi_.t])
        cp("dve", wi_.ap[:, 3:8, :], sgi.ap[:, 3:8, :], r=[sgi.t], w=[wi_.t])
        cp("act", wo_.ap, sgo.ap, r=[sgo.t], w=[wo_.t])

    def stage_T(e_i):
        xs_, xsT_ = xs_r[e_i % 2], xsT_r[e_i % 2]
        for st in range(4):
            bk = 4 + st % 2
            for kc in range(8):
                tr(psb(bk)[:, kc * 128:(kc + 1) * 128], xs_.ap[:, st, kc * 128:(kc + 1) * 128], ident.ap, r=[xs_.t, ident.t], w=[pst[bk]])
            cp("act" if st % 2 else "dve", xsT_.ap[:, :, st * 128:(st + 1) * 128], psb(bk).rearrange("p (a b) -> p a b", a=8), r=[pst[bk]], w=[xsT_.t])

    def stage_H(e_i):
        wi_, xsT_, aT_ = wi_r[e_i % 2], xsT_r[e_i % 2], aT_r[e_i % 2]
        for p_ in range(2):
            bG = next_bank()
            for kc in range(8):
                mm(ps[bG][:, :], wi_.ap[:, kc, p_ * 128:(p_ + 1) * 128], xsT_.ap[:, kc, :], kc == 0, kc == 7, r=[wi_.t, xsT_.t], w=[pst[bG]])
            bU = next_bank()
            for kc in range(8):
                mm(ps[bU][:, :], wi_.ap[:, kc, 256 + p_ * 128:256 + (p_ + 1) * 128], xsT_.ap[:, kc, :], kc == 0, kc == 7, r=[wi_.t, xsT_.t], w=[pst[bU]])
            sg_ = sg_r[p_]
            act(sg_.ap, ps[bG][:, :], AF.Silu, r=[pst[bG]], w=[sg_.t])
            tt("dve", aT_.ap[:, p_, :], sg_.ap, ps[bU][:, :], ALU.mult, r=[sg_.t, pst[bU]], w=[aT_.t])

    def stage_Y(e_i):
        wo_, aT_ = wo_r[e_i % 3], aT_r[e_i % 2]
        for st in range(4):
            yb = ys_r[nys["n"] % 3]
            nys["n"] += 1
            for half in range(2):
                hs = slice(half * 512, (half + 1) * 512)
                bk = next_bank()
                for fc in range(2):
                    mm(ps[bk][:, :], aT_.ap[:, fc, st * 128:(st + 1) * 128], wo_.ap[:, fc, hs], fc == 0, fc == 1, r=[aT_.t, wo_.t], w=[pst[bk]])
                cp("act" if half else "dve", yb.ap[:, hs], ps[bk][:, :], r=[pst[bk]], w=[yb.t])
            r0_ = e_i * CAP + st * 128
            dma("sp", ys_s[r0_:r0_ + 128, :], yb.ap, r=[yb.t])

    load_expert(0)
    load_expert(1)
    cast_expert(0)
    cast_expert(1)
    stage_T(0)
    stage_T(1)
    stage_H(0)
    for e_i in range(NE):
        if e_i + 2 < NE:
            load_expert(e_i + 2)
            stage_T(e_i + 2)
        if e_i + 1 < NE:
            stage_H(e_i + 1)
        stage_Y(e_i)
        if e_i + 2 < NE:
            cast_expert(e_i + 2)
    K.barrier()
    AR.release(m_4)
    gfb = B([D], F32, "gfb")
    dma("sp", gfb.ap, gf_d.partition_broadcast(128), w=[gfb.t])
    Y0r = ring(3, [D], BF16, "Y0")
    Y1r = ring(3, [D], BF16, "Y1")
    h2l = ring(3, [D], F32, "h2l")
    h3r = ring(3, [D], F32, "h3")
    outr = ring(2, [D], F32, "outb")
    junk4 = B([D], F32, "junk4")
    ms3 = B([NT], F32, "ms3")
    rs3 = B([NT], F32, "rs3")
    for b_ in Y0r + Y1r:
        mset("pool", b_.ap, 0.0, w=[b_.t])
    ms3_t = [K.tok() for _ in range(NT)]
    rs3_t = [K.tok() for _ in range(NT)]

    def fin_front(i):
        Y0, Y1, h2_, h3 = Y0r[i % 3], Y1r[i % 3], h2l[i % 3], h3r[i % 3]
        for k_, Y in enumerate((Y0, Y1)):
            K.op("pool", lambda e, i=i, k_=k_, Y=Y: e.indirect_dma_start(
                out=Y.ap, out_offset=None, in_=ys_s[:, :], in_offset=bass.IndirectOffsetOnAxis(ap=rt_pos.ap[:, 2 * i + k_:2 * i + k_ + 1], axis=0)),
                r=[rt_pos.t], w=[Y.t], dma=True)
        dma("sp", h2_.ap, h2_s[i * 128:(i + 1) * 128, :], w=[h2_.t])
        stt(h3.ap, Y0.ap, rt_gate.ap[:, 2 * i:2 * i + 1], h2_.ap, ALU.mult, ALU.add, r=[Y0.t, rt_gate.t, h2_.t], w=[h3.t])
        stt(h3.ap, Y1.ap, rt_gate.ap[:, 2 * i + 1:2 * i + 2], h3.ap, ALU.mult, ALU.add, r=[Y1.t, rt_gate.t, h3.t], w=[h3.t])
        stt(junk4.ap, h3.ap, 1.0 / D, h3.ap, ALU.mult, ALU.mult, r=[h3.t], w=[junk4.t, ms3_t[i]], accum=ms3.ap[:, i:i + 1])
        ts("pool", rs3.ap[:, i:i + 1], ms3.ap[:, i:i + 1], EPS, ALU.add, r=[ms3_t[i]], w=[rs3_t[i]])
        tt("pool", rs3.ap[:, i:i + 1], rs3.ap[:, i:i + 1], col(cst, C_NH), ALU.pow, r=[rs3_t[i], cst.t], w=[rs3_t[i]])

    def fin_back(i):
        h3, ob = h3r[i % 3], outr[i % 2]
        stt(ob.ap, h3.ap, rs3.ap[:, i:i + 1], gfb.ap, ALU.mult, ALU.mult, r=[h3.t, rs3_t[i], gfb.t], w=[ob.t])
        dma("sp", out_d[i * 128:(i + 1) * 128, :], ob.ap, r=[ob.t])

    for i in range(NT + 1):
        if i < NT:
            fin_front(i)
        if i >= 1:
            fin_back(i - 1)
    return finish()


def make_in_maps(inp, cores):
    c, m = host_consts()
    maps = []
    for b in cores:
        pos = np.ascontiguousarray(inp["positions"][b]).astype(np.int32)
        maps.append({
            "x": np.ascontiguousarray(inp["x"][b], dtype=np.float32),
            "pos_row": pos.reshape(1, L),
            "pos_col": np.ascontiguousarray(pos.reshape(NT, 128).T),
            "norm1_g": np.ascontiguousarray(inp["norm1_g"][0]).reshape(1, D),
            "w_in": np.ascontiguousarray(inp["w_in"][0]),
            "sgu_norm_g": np.ascontiguousarray(inp["sgu_norm_g"][0]).reshape(1, D),
            "sgu_w": np.ascontiguousarray(inp["sgu_w"][0]),
            "sgu_b": np.ascontiguousarray(inp["sgu_b"][0]),
            "w_branch_a": np.ascontiguousarray(inp["w_branch_a"][0]),
            "w_branch_b": np.ascontiguousarray(inp["w_branch_b"][0]),
            "w_out": np.ascontiguousarray(inp["w_out"][0]),
            "norm2_g": np.ascontiguousarray(inp["norm2_g"][0]).reshape(1, D),
            "w_router_group": np.ascontiguousarray(inp["w_router_group"][0]),
            "b_router_group": np.ascontiguousarray(inp["b_router_group"][0]).reshape(1, 4),
            "w_router_expert": np.ascontiguousarray(inp["w_router_expert"][0]),
            "b_router_expert": np.ascontiguousarray(inp["b_router_expert"][0]).reshape(1, 32),
            "w_expert_in": np.ascontiguousarray(inp["w_expert_in"][0]),
            "w_expert_out": np.ascontiguousarray(inp["w_expert_out"][0]),
            "norm_f_g": np.ascontiguousarray(inp["norm_f_g"]).reshape(1, D),
            "cst": c,
            "cmat": m,
        })
    return maps


def kernel(**inputs):
    P = build_program()
    maps = make_in_maps(inputs, list(range(8)))
    res = run_bass_kernel_spmd(P.nc, maps, core_ids=list(range(8)))
    return np.stack([np.asarray(r["out"], dtype=np.float32) for r in res.results], axis=0)
```

```python
from contextlib import ExitStack
import numpy as np
import concourse.bass as bass
import concourse.mybir as mybir

F32 = mybir.dt.float32
BF16 = mybir.dt.bfloat16
I32 = mybir.dt.int32
U32 = mybir.dt.uint32
AF = mybir.ActivationFunctionType
ALU = mybir.AluOpType
AX = mybir.AxisListType


class Tok:
    __slots__ = ("w", "r", "name")

    def __init__(self, name=""):
        self.w = None
        self.r = []
        self.name = name


class Op:
    __slots__ = ("eng", "fn", "deps", "dma", "sig", "sem", "val", "gidx", "slotwait")

    def __init__(self, eng, fn, dma, gidx):
        self.eng = eng
        self.fn = fn
        self.dma = dma
        self.deps = []
        self.sig = False
        self.sem = None
        self.val = 0
        self.gidx = gidx
        self.slotwait = None


class Kern:
    ENGS = ("pe", "act", "dve", "pool", "sp")
    NSLOT = {"sp": 20, "act": 6, "pool": 12}
    ROLL = 30000

    def __init__(self, nc):
        self.nc = nc
        self.ops = {e: [] for e in self.ENGS}
        self.n = 0
        self.toks = []
        self.es = ExitStack()
        self.nsem = 0

    def tok(self, name=""):
        t = Tok(name)
        self.toks.append(t)
        return t

    def toks_n(self, n, name=""):
        return [self.tok(f"{name}{i}") for i in range(n)]

    def op(self, eng, fn, r=(), w=(), dma=False):
        o = Op(eng, fn, dma, self.n)
        self.n += 1
        deps = {}
        for t in r:
            if t.w is not None:
                deps[id(t.w)] = t.w
        for t in w:
            if t.w is not None:
                deps[id(t.w)] = t.w
            for q in t.r:
                deps[id(q)] = q
        for d in deps.values():
            if d is o:
                continue
            if d.eng == eng and not d.dma and not dma:
                if eng == "pe":
                    continue
            o.deps.append(d)
        for t in r:
            t.r.append(o)
        for t in w:
            t.w = o
            t.r = []
        self.ops[eng].append(o)
        return o

    def barrier(self):
        deps = {}
        for t in self.toks:
            if t.w is not None:
                deps[id(t.w)] = t.w
            for q in t.r:
                deps[id(q)] = q
        dl = list(deps.values())
        for e in self.ENGS:
            o = Op(e, None, False, self.n)
            self.n += 1
            o.deps = [d for d in dl if d.fn is not None]
            self.ops[e].append(o)
        for t in self.toks:
            t.r = []
            t.w = None

    def _newsem(self, name):
        self.nsem += 1
        return self.es.enter_context(self.nc.semaphore(f"{name}_{self.nsem}"))

    def emit(self):
        nc = self.nc
        for e in self.ENGS:
            for o in self.ops[e]:
                for d in o.deps:
                    d.sig = True
        for e in self.ENGS:
            cur = None
            cnt = 0
            slots = None
            slot_uses = None
            slot_last = None
            k = 0
            for o in self.ops[e]:
                if o.fn is None:
                    continue
                if o.dma:
                    if slots is None:
                        ns = self.NSLOT[e]
                        slots = [self._newsem(f"d{e}") for _ in range(ns)]
                        slot_uses = [0] * ns
                        slot_last = [None] * ns
                    s = k % len(slots)
                    k += 1
                    o.slotwait = slot_last[s]
                    slot_uses[s] += 1
                    o.sem = slots[s]
                    o.val = 16 * slot_uses[s]
                    o.sig = True
                    slot_last[s] = o
                elif o.sig:
                    if cur is None or cnt >= self.ROLL:
                        cur = self._newsem(f"c{e}")
                        cnt = 0
                    cnt += 1
                    o.sem = cur
                    o.val = cnt
        with nc.Block() as block:
            def run(e, eng):
                waited = {}
                for o in self.ops[e]:
                    need = {}
                    dl = list(o.deps)
                    if o.slotwait is not None:
                        dl.append(o.slotwait)
                    for d in dl:
                        key = id(d.sem)
                        if waited.get(key, 0) >= d.val:
                            continue
                        if key not in need or need[key][1] < d.val:
                            need[key] = (d.sem, d.val)
                    for key, (sem, val) in need.items():
                        eng.wait_ge(sem, val)
                        waited[key] = val
                    if o.fn is None:
                        continue
                    ins = o.fn(eng)
                    if o.sig:
                        ins.then_inc(o.sem, 16 if o.dma else 1)

            @block.tensor
            def _(eng):
                run("pe", eng)

            @block.scalar
            def _(eng):
                run("act", eng)

            @block.vector
            def _(eng):
                run("dve", eng)

            @block.gpsimd
            def _(eng):
                run("pool", eng)

            @block.sync
            def _(eng):
                run("sp", eng)
        self.es.close()


U8 = mybir.dt.uint8
DTSZ = {F32: 4, BF16: 2, I32: 4, U32: 4}


class Arena:
    def __init__(self, nc, nbytes):
        self.t = nc.alloc_sbuf_tensor("arena", [128, nbytes], U8)
        self.n = nbytes
        self.off = 0
        self.peak = 0

    def alloc(self, shape, dt):
        n = int(np.prod(shape)) * DTSZ[dt]
        n = (n + 63) // 64 * 64
        assert self.off + n <= self.n, f"arena overflow {self.off}+{n}>{self.n}"
        v = self.t[:, self.off:self.off + n].bitcast(dt)
        self.off += n
        self.peak = max(self.peak, self.off)
        tot = int(np.prod(shape))
        v = v[:, 0:tot]
        if len(shape) == 2:
            v = v.rearrange("p (a b) -> p a b", a=shape[0])
        elif len(shape) == 3:
            v = v.rearrange("p (a b c) -> p a b c", a=shape[0], b=shape[1])
        return v

    def mark(self):
        return self.off

    def release(self, m):
        self.off = m

from concourse.bass_utils import run_bass_kernel_spmd

L = 4096
D = 1024
NT = 32
NCH = 8
DIN = 7752
CQ, CK, CV, CQI, CKI, CSU, CSV, CGA, CGB = 0, 1024, 2048, 3072, 3584, 3656, 4680, 5704, 6728
NE = 32
CAP = 512
DUMMY = NE * CAP
KI = 20
EPS = 1e-6
PI = float(np.pi)
MAGIC = 12582912.0
C1 = 6.28125
C2 = 2 * PI - C1
NEG = -1.0e30
ARENA = 206 * 1024

C_INV, C_SGN, C_INVI, C_NH, C_HPI, C_NTHR, C_EPS, C_ONE, C_DUM, C_PW, C_NB = 0, 1, 2, 34, 35, 36, 37, 38, 39, 40, 64
NCST = 96


def host_consts():
    c = np.zeros((128, NCST), np.float32)
    inv128 = (np.float32(10000.0) ** (-np.arange(0, 128, 2, dtype=np.float32) / np.float32(128))).astype(np.float32)
    inv64 = (np.float32(10000.0) ** (-np.arange(0, 64, 2, dtype=np.float32) / np.float32(64))).astype(np.float32)
    p = np.arange(128)
    c[:, C_INV] = inv128[p % 64]
    c[:, C_SGN] = np.where(p < 64, -1.0, 1.0)
    c[:, C_INVI:C_INVI + 32] = inv64[None, :]
    c[:, C_NH] = -0.5
    c[:, C_HPI] = PI / 2
    c[:, C_NTHR] = -1.0e29
    c[:, C_EPS] = EPS
    c[:, C_ONE] = 1.0
    c[:, C_DUM] = NE * CAP + p
    c[:, C_PW:C_PW + KI + 2] = (2.0 ** -(np.arange(KI + 2) + 1.0))[None, :]
    c[:, C_NB:C_NB + NT] = (128.0 * (np.arange(NT) + 1.0) - 511.0)[None, :]
    m = np.zeros((128, 128 * 3 + 64), np.float32)
    t = np.arange(128)[:, None]
    s = np.arange(128)[None, :]
    m[:, 0:128] = np.where(s <= t, 0.0, NEG)
    m[:, 128:256] = np.where(s >= t, 1.0, 0.0)
    m[:, 256:384] = np.where(s <= t, 1.0, 0.0)
    m[:, 384:416] = (np.arange(32) * CAP)[None, :]
    m[:, 416:448] = 1.0
    return c, m


class Prog:
    pass


def build_program(stop_after=None, dbg=False):
    nc = bass.Bass("TRN2", target_bir_lowering=False)
    K = Kern(nc)
    AR = Arena(nc, ARENA)
    P = Prog()
    P.nc = nc

    def din(name, shape, dt=F32):
        return nc.dram_tensor(name, shape, dt, kind="ExternalInput").ap()

    def dscr(name, shape, dt, out=False):
        return nc.dram_tensor(name, shape, dt, kind="ExternalOutput" if (out and dbg) else "Internal").ap()

    x_d = din("x", [L, D])
    posr_d = din("pos_row", [1, L], I32)
    posc_d = din("pos_col", [128, NT], I32)
    g1_d = din("norm1_g", [1, D])
    win_d = din("w_in", [D, DIN])
    gs_d = din("sgu_norm_g", [1, D])
    sw_d = din("sgu_w", [8, 128, 128])
    sb_d = din("sgu_b", [8, 128])
    wa_d = din("w_branch_a", [D, D])
    wb_d = din("w_branch_b", [D, D])
    wo_d = din("w_out", [D, D])
    g2_d = din("norm2_g", [1, D])
    wrg_d = din("w_router_group", [D, 4])
    brg_d = din("b_router_group", [1, 4])
    wre_d = din("w_router_expert", [D, 32])
    bre_d = din("b_router_expert", [1, 32])
    wei_d = din("w_expert_in", [NE, D, 512])
    weo_d = din("w_expert_out", [NE, 256, D])
    gf_d = din("norm_f_g", [1, D])
    cst_d = din("cst", [128, NCST])
    cm_d = din("cmat", [128, 448])
    out_d = nc.dram_tensor("out", [L, D], F32, kind="ExternalOutput").ap()

    qT_s = dscr("qT_s", [8, 128, L], BF16, True)
    kT_s = dscr("kT_s", [8, 128, L], BF16, True)
    v_s = dscr("v_s", [NT, 128, 8 * 129], BF16, True)
    qiT_s = dscr("qiT_s", [5, 128, L], BF16, True)
    suT_s = dscr("suT_s", [D, L], BF16)
    ysgT_s = dscr("ysgT_s", [D, L], BF16, True)
    sgT_s = dscr("sgT_s", [2 * D, L], BF16, True)
    yatT_s = dscr("yatT_s", [D, L], BF16, True)
    h2_s = dscr("h2_s", [L, D], F32, True)
    xs_s = dscr("xs_s", [NE * CAP + 128, D], BF16)
    ys_s = dscr("ys_s", [NE * CAP + 128, D], BF16)

    ps = [nc.alloc_psum_tensor(f"ps{i}", [128, 512], F32) for i in range(8)]
    pst = [K.tok(f"ps{i}") for i in range(8)]

    def psb(i):
        return ps[i][:, :].bitcast(BF16)

    def dma(eng, out, in_, r=(), w=()):
        return K.op(eng, lambda e: e.dma_start(out=out, in_=in_), r=r, w=w, dma=True)

    def mm(out, lhsT, rhs, start, stop, r=(), w=(), sgc=False):
        return K.op("pe", lambda e: e.matmul(out, lhsT=lhsT, rhs=rhs, start=start, stop=stop, skip_group_check=sgc), r=r, w=w)

    def tr(out, in_, ident, r=(), w=()):
        return K.op("pe", lambda e: e.transpose(out, in_, ident), r=r, w=w)

    def act(out, in_, func, r=(), w=(), bias=None, scale=1.0, accum=None):
        def f(e):
            kw = {}
            if bias is not None:
                kw["bias"] = bias
            if accum is not None:
                kw["accum_out"] = accum
            return e.activation(out=out, in_=in_, func=func, scale=scale, **kw)
        return K.op("act", f, r=r, w=w)

    def ts(eng, out, in0, s1, op0, s2=None, op1=None, r=(), w=(), accum=None):
        def f(e):
            kw = {}
            if op1 is not None:
                kw["op1"] = op1
            if accum is not None:
                kw["accum_out"] = accum
            return e.tensor_scalar(out=out, in0=in0, scalar1=s1, scalar2=s2, op0=op0, **kw)
        return K.op(eng, f, r=r, w=w)

    def tt(eng, out, in0, in1, op, r=(), w=()):
        return K.op(eng, lambda e: e.tensor_tensor(out=out, in0=in0, in1=in1, op=op), r=r, w=w)

    def stt(out, in0, scalar, in1, op0, op1, r=(), w=(), accum=None):
        def f(e):
            kw = {}
            if accum is not None:
                kw["accum_out"] = accum
            return e.scalar_tensor_tensor(out=out, in0=in0, scalar=scalar, in1=in1, op0=op0, op1=op1, **kw)
        return K.op("dve", f, r=r, w=w)

    def cp(eng, out, in_, r=(), w=()):
        if eng == "act":
            return K.op("act", lambda e: e.activation(out=out, in_=in_, func=AF.Copy), r=r, w=w)
        return K.op(eng, lambda e: e.tensor_copy(out, in_), r=r, w=w)

    def mset(eng, out, val, r=(), w=()):
        return K.op(eng, lambda e: e.memset(out, val), r=r, w=w)

    class B:
        def __init__(self, shape, dt, name=""):
            self.ap = AR.alloc(shape, dt)
            self.t = K.tok(name)

    def ring(n, shape, dt, name=""):
        return [B(shape, dt, f"{name}{i}") for i in range(n)]

    cst = B([NCST], F32, "cst")
    cm = B([448], F32, "cm")
    ident = B([128], BF16, "ident")
    identf = B([128], F32, "identf")
    wi_sb = B([NT, 8], F32, "wi")
    rt_gate = B([NT * 2], F32, "gate")
    rt_pos = B([NT * 2], I32, "pos")
    dma("sp", cst.ap, cst_d, w=[cst.t])
    dma("sp", cm.ap, cm_d, w=[cm.t])
    mset("pool", identf.ap, 0.0, w=[identf.t])
    K.op("pool", lambda e: e.affine_select(out=identf.ap, in_=identf.ap, pattern=[[-1, 128]], compare_op=ALU.not_equal,
                                           fill=1.0, base=0, channel_multiplier=1), r=[identf.t], w=[identf.t])
    cp("pool", ident.ap, identf.ap, r=[identf.t], w=[ident.t])

    zt = B([D], BF16, "zt")
    mset("pool", zt.ap, 0.0, w=[zt.t])

    def col(b, j, n=1):
        return b.ap[:, j:j + n]

    def sincos(ang, n, kk, sin_out, cos_out, r, w):
        tk = K.tok()
        ts("dve", kk, ang, 1.0 / (2 * PI), ALU.mult, MAGIC, ALU.add, r=r, w=[tk])
        ts("dve", kk, kk, MAGIC, ALU.subtract, r=[tk], w=[tk])
        stt(ang, kk, -C1, ang, ALU.mult, ALU.add, r=r + [tk], w=r)
        stt(ang, kk, -C2, ang, ALU.mult, ALU.add, r=r + [tk], w=r)
        ts("dve", ang, ang, PI, ALU.min, -PI, ALU.max, r=r, w=r)
        act(sin_out, ang, AF.Sin, r=r, w=w)
        ts("dve", kk, ang, -1.0, ALU.mult, r=r, w=[tk])
        tt("dve", kk, kk, ang, ALU.max, r=r + [tk], w=[tk])
        act(cos_out, kk, AF.Sin, bias=col(cst, C_HPI), scale=-1.0, r=[tk, cst.t], w=w)


    def finish():
        K.barrier()
        K.emit()
        P.__dict__.update(dict(K=K, AR=AR))
        return P

    m_0 = AR.mark()
    xnT = AR.alloc([8, L], BF16)
    xnT_t = [K.tok(f"xnT{i}") for i in range(NT)]
    m_p1 = AR.mark()

    xbuf = ring(3, [D], F32, "xb")
    junk = B([D], F32, "junk")
    g1b = B([D], F32, "g1b")
    xnb = ring(2, [D], BF16, "xnb")
    ms1 = B([NT], F32, "ms1")
    rs1 = B([NT], F32, "rs1")
    ms1_t = [K.tok() for _ in range(NT)]
    rs1_t = [K.tok() for _ in range(NT)]
    dma("sp", g1b.ap, g1_d.partition_broadcast(128), w=[g1b.t])

    def a_front(i):
        xb = xbuf[i % 3]
        dma("sp", xb.ap, x_d[i * 128:(i + 1) * 128, :], w=[xb.t])
        act(junk.ap, xb.ap, AF.Square, r=[xb.t], w=[junk.t, ms1_t[i]], accum=ms1.ap[:, i:i + 1])
        ts("pool", rs1.ap[:, i:i + 1], ms1.ap[:, i:i + 1], 1.0 / D, ALU.mult, EPS, ALU.add, r=[ms1_t[i]], w=[rs1_t[i]])
        tt("pool", rs1.ap[:, i:i + 1], rs1.ap[:, i:i + 1], col(cst, C_NH), ALU.pow, r=[rs1_t[i], cst.t], w=[rs1_t[i]])

    def a_back(i):
        xb = xbuf[i % 3]
        nb = xnb[i % 2]
        stt(nb.ap, xb.ap, rs1.ap[:, i:i + 1], g1b.ap, ALU.mult, ALU.mult, r=[xb.t, rs1_t[i], g1b.t], w=[nb.t])
        bk = 4 + (i % 2)
        for kc in range(8):
            tr(psb(bk)[:, kc * 128:(kc + 1) * 128], nb.ap[:, kc * 128:(kc + 1) * 128], ident.ap, r=[nb.t, ident.t], w=[pst[bk]])
        cp("act", xnT[:, :, i * 128:(i + 1) * 128], psb(bk).rearrange("p (a b) -> p a b", a=8), r=[pst[bk]], w=[xnT_t[i]])

    for i in range(NT + 1):
        if i < NT:
            a_front(i)
        if i >= 1:
            a_back(i - 1)
    K.barrier()
    AR.release(m_p1)

    wbuf = ring(3, [8, 512], BF16, "wb")
    wstg = ring(2, [4, 512], F32, "wstg")
    wstate = {"n": 0}

    def load_w(c0, ncols, ceng="pool"):
        b = wbuf[wstate["n"] % 3]
        wstate["n"] += 1
        for hf in range(2):
            sg = wstg[hf]
            dma("sp", sg.ap[:, :, 0:ncols], win_d[hf * 512:(hf + 1) * 512, c0:c0 + ncols].rearrange("(kc p) c -> p kc c", p=128), w=[sg.t])
            cp(ceng, b.ap[:, 4 * hf:4 * hf + 4, 0:ncols], sg.ap[:, :, 0:ncols], r=[sg.t], w=[b.t])
        return b

    bank_rr = {"n": 0}

    def next_bank():
        bk = bank_rr["n"] % 4
        bank_rr["n"] += 1
        return bk

    def fm_block(wb, j, c, bk):
        for kc in range(8):
            mm(ps[bk][:, :], wb.ap[:, kc, j * 128:(j + 1) * 128], xnT[:, kc, c * 512:(c + 1) * 512], kc == 0, kc == 7,
               r=[wb.t] + xnT_t[4 * c:4 * c + 4], w=[pst[bk]])

    def tm_block(wb, ncols, i, bk, col0=0):
        for kc in range(8):
            mm(ps[bk][:, 0:ncols], xnT[:, kc, i * 128:(i + 1) * 128], wb.ap[:, kc, col0:col0 + ncols], kc == 0, kc == 7,
               r=[wb.t, xnT_t[i]], w=[pst[bk]])

    m_b1 = AR.mark()
    cosT = B([L], F32, "cosT")
    sinT = B([L], F32, "sinT")
    posi = B([1024], I32, "posi")
    angw = B([1024], F32, "angw")
    kkw = B([1024], F32, "kkw")
    for cc in range(4):
        sl = slice(cc * 1024, (cc + 1) * 1024)
        dma("sp", posi.ap, posr_d[:, sl].partition_broadcast(128), w=[posi.t])
        cp("dve", angw.ap, posi.ap, r=[posi.t], w=[angw.t])
        ts("dve", angw.ap, angw.ap, col(cst, C_INV), ALU.mult, r=[angw.t, cst.t], w=[angw.t])
        sincos(angw.ap, 1024, kkw.ap, sinT.ap[:, sl], cosT.ap[:, sl], r=[angw.t], w=[sinT.t, cosT.t])
        ts("dve", sinT.ap[:, sl], sinT.ap[:, sl], col(cst, C_SGN), ALU.mult, r=[sinT.t, cst.t], w=[sinT.t])
    posc = B([NT], I32, "posc")
    poscf = B([NT], F32, "poscf")
    sinI = B([NT, 32], F32, "sinI")
    cosI = B([NT, 32], F32, "cosI")
    dma("sp", posc.ap, posc_d, w=[posc.t])
    cp("dve", poscf.ap, posc.ap, r=[posc.t], w=[poscf.t])
    angI = angw.ap.rearrange("p (a b) -> p a b", a=NT)
    tt("dve", angI, poscf.ap.unsqueeze(2).broadcast_to([128, NT, 32]),
       cst.ap[:, C_INVI:C_INVI + 32].unsqueeze(1).broadcast_to([128, NT, 32]), ALU.mult, r=[poscf.t, cst.t, angw.t], w=[angw.t])
    sincos(angw.ap, 1024, kkw.ap, sinI.ap.rearrange("p a b -> p (a b)"), cosI.ap.rearrange("p a b -> p (a b)"),
           r=[angw.t], w=[sinI.t, cosI.t])

    t1r = ring(2, [512], F32, "t1")
    t2r = ring(2, [512], F32, "t2")
    qor = ring(3, [512], BF16, "qo")
    n_rope = {"n": 0}

    def rope_fm(bk, c, dst):
        k_ = n_rope["n"]
        n_rope["n"] += 1
        t1 = t1r[k_ % 2]
        t2 = t2r[k_ % 2]
        qo = qor[k_ % 3]
        sl = slice(c * 512, (c + 1) * 512)
        tt("dve", t1.ap, ps[bk][:, :], cosT.ap[:, sl], ALU.mult, r=[pst[bk], cosT.t], w=[t1.t])
        tt("dve", t2.ap[0:64, :], ps[bk][64:128, :], sinT.ap[0:64, sl], ALU.mult, r=[pst[bk], sinT.t], w=[t2.t])
        tt("dve", t2.ap[64:128, :], ps[bk][0:64, :], sinT.ap[64:128, sl], ALU.mult, r=[pst[bk], sinT.t], w=[t2.t])
        tt("pool", qo.ap, t1.ap, t2.ap, ALU.add, r=[t1.t, t2.t], w=[qo.t])
        dma("sp", dst, qo.ap, r=[qo.t])

    qk_blocks = [(CQ, qT_s, 0), (CQ + 512, qT_s, 4), (CK, kT_s, 0), (CK + 512, kT_s, 4)]
    wnext = load_w(qk_blocks[0][0], 512)
    for bi, (c0, dst_s, h0) in enumerate(qk_blocks):
        wb = wnext
        wnext = load_w(qk_blocks[bi + 1][0], 512, "act") if bi + 1 < 4 else load_w(CV, 512, "act")
        for c in range(NCH):
            for j in range(4):
                bk = next_bank()
                fm_block(wb, j, c, bk)
                rope_fm(bk, c, dst_s[h0 + j, :, c * 512:(c + 1) * 512])
    wv0 = wnext
    wv1 = load_w(CV + 512, 512)
    wqi = load_w(CQI, 512)
    vt = ring(2, [8, 129], BF16, "vt")
    for b_ in vt:
        mset("pool", b_.ap[:, :, 128:129], 1.0, w=[b_.t])
    for i in range(NT):
        v_ = vt[i % 2]
        for half, wb in enumerate((wv0, wv1)):
            bk = next_bank()
            tm_block(wb, 512, i, bk)
            cp("act", v_.ap[:, half * 4:(half + 1) * 4, 0:128], ps[bk][:, :].rearrange("p (a b) -> p a b", a=4), r=[pst[bk]], w=[v_.t])
        dma("sp", v_s[i].rearrange("p (h d) -> p h d", h=8), v_.ap, r=[v_.t])
    wkw = load_w(CKI, 72)
    w0_pre = load_w(CSU, 512)
    ra = ring(3, [9, 32], F32, "ra")
    rb = ring(3, [9, 32], F32, "rb")
    qst = ring(3, [9, 64], F32, "qst")
    qr = ring(3, [640], BF16, "qr")
    qiT_c = ring(2, [5, 512], BF16, "qiTc")
    def ip_front(i):
        bq = next_bank()
        tm_block(wqi, 512, i, bq)
        bkw = next_bank()
        tm_block(wkw, 72, i, bkw)
        q_ = qr[i % 3]
        a_, b2_, st_ = ra[i % 3], rb[i % 3], qst[i % 3]
        cp("act", st_.ap[:, 0:8, :], ps[bq][:, :].rearrange("p (h d) -> p h d", h=8), r=[pst[bq]], w=[st_.t])
        cp("act", st_.ap[:, 8, :], ps[bkw][:, 0:64], r=[pst[bkw]], w=[st_.t])
        cosb = cosI.ap[:, i, :].unsqueeze(1).broadcast_to([128, 9, 32])
        sinb = sinI.ap[:, i, :].unsqueeze(1).broadcast_to([128, 9, 32])
        qo_ = q_.ap[:, 0:576].rearrange("p (h d) -> p h d", h=9)
        rd = [st_.t, cosI.t, sinI.t]
        tt("dve", a_.ap, st_.ap[:, :, 0:32], cosb, ALU.mult, r=rd, w=[a_.t])
        tt("dve", b2_.ap, st_.ap[:, :, 32:64], sinb, ALU.mult, r=rd, w=[b2_.t])
        tt("pool", qo_[:, :, 0:32], a_.ap, b2_.ap, ALU.subtract, r=[a_.t, b2_.t], w=[q_.t])
        tt("dve", a_.ap, st_.ap[:, :, 32:64], cosb, ALU.mult, r=rd, w=[a_.t])
        tt("dve", b2_.ap, st_.ap[:, :, 0:32], sinb, ALU.mult, r=rd, w=[b2_.t])
        tt("pool", qo_[:, :, 32:64], a_.ap, b2_.ap, ALU.add, r=[a_.t, b2_.t], w=[q_.t])
        cp("pool", q_.ap[:, 576:640], q_.ap[:, 512:576], r=[q_.t], w=[q_.t])
        cp("act", wi_sb.ap[:, i, :], ps[bkw][:, 64:72], r=[pst[bkw]], w=[wi_sb.t])

    def ip_back(i):
        c, tau = i // 4, i % 4
        q_ = qr[i % 3]
        bt = 4 + (i % 2)
        for jj in range(5):
            tr(psb(bt)[:, jj * 128:(jj + 1) * 128], q_.ap[:, jj * 128:(jj + 1) * 128], ident.ap, r=[q_.t, ident.t], w=[pst[bt]])
        qc = qiT_c[c % 2]
        cp("act", qc.ap[:, :, tau * 128:(tau + 1) * 128], psb(bt)[:, 0:640].rearrange("p (a b) -> p a b", a=5), r=[pst[bt]], w=[qc.t])
        if tau == 3:
            dma("sp", qiT_s[:, :, c * 512:(c + 1) * 512].rearrange("a p t -> p a t"), qc.ap, r=[qc.t])
    for i in range(NT + 1):
        if i < NT:
            ip_front(i)
        if i >= 1:
            ip_back(i - 1)
    K.barrier()
    AR.release(m_b1)
    if stop_after == "1b":
        return finish()
    m_b2 = AR.mark()
    sub_r = ring(3, [512], BF16, "sub")
    su_t = [K.tok(f"suT{c}") for c in range(NCH)]
    w0 = w0_pre
    w1 = load_w(CSU + 512, 512)
    w2 = load_w(CSV, 512)
    nsub = {"n": 0}

    def act_block_out(bk, func, dst, wtok=()):
        o = sub_r[nsub["n"] % 3]
        nsub["n"] += 1
        act(o.ap, ps[bk][:, :], func, r=[pst[bk]], w=[o.t])
        dma("sp", dst, o.ap, r=[o.t], w=list(wtok))

    for blk, wb in enumerate((w0, w1)):
        for c in range(NCH):
            for j in range(4):
                bk = next_bank()
                fm_block(wb, j, c, bk)
                f0 = (blk * 4 + j) * 128
                act_block_out(bk, AF.Gelu_apprx_tanh, suT_s[f0:f0 + 128, c * 512:(c + 1) * 512], wtok=[su_t[c]])
    w3 = load_w(CSV + 512, 512)
    swf = B([8, 128], F32, "swf")
    swb = B([8, 128], BF16, "swb")
    WtT = B([8, 128], BF16, "WtT")
    dma("sp", swf.ap, sw_d.rearrange("g t s -> t g s"), w=[swf.t])
    tt("dve", swf.ap, swf.ap, cm.ap[:, 256:384].unsqueeze(1).broadcast_to([128, 8, 128]), ALU.mult, r=[swf.t, cm.t], w=[swf.t])
    cp("dve", swb.ap, swf.ap, r=[swf.t], w=[swb.t])
    for g in range(8):
        tr(psb(4)[:, g * 128:(g + 1) * 128], swb.ap[:, g, :], ident.ap, r=[swb.t, ident.t], w=[pst[4]])
    cp("act", WtT.ap, psb(4).rearrange("p (a b) -> p a b", a=8), r=[pst[4]], w=[WtT.t])
    bf_ = B([8, 128], F32, "bf")
    bhl = B([8, 128], BF16, "bhl")
    bhf = B([8, 128], F32, "bhf")
    ones2 = B([128], BF16, "ones2")
    mset("pool", ones2.ap, 1.0, w=[ones2.t])
    mset("pool", bf_.ap, 0.0, w=[bf_.t])
    dma("sp", bf_.ap[0:1, :, :], sb_d.rearrange("(o g) t -> o g t", o=1), r=[bf_.t], w=[bf_.t])
    dma("sp", bf_.ap[1:2, :, :], sb_d.rearrange("(o g) t -> o g t", o=1), r=[bf_.t], w=[bf_.t])
    cp("dve", bhl.ap, bf_.ap, r=[bf_.t], w=[bhl.t])
    cp("dve", bhf.ap, bhl.ap, r=[bhl.t], w=[bhf.t])
    tt("dve", bhf.ap, bf_.ap, bhf.ap, ALU.subtract, r=[bf_.t, bhf.t], w=[bhf.t])
    cp("dve", bf_.ap, bhl.ap, r=[bhl.t, bf_.t], w=[bf_.t])
    sel = B([1], F32, "sel")
    mset("pool", sel.ap, 1.0, w=[sel.t])
    K.op("pool", lambda e: e.affine_select(out=sel.ap, in_=sel.ap, pattern=[[0, 1]], compare_op=ALU.is_equal,
                                           fill=0.0, base=0, channel_multiplier=1), r=[sel.t], w=[sel.t])
    tt("dve", bf_.ap, bf_.ap, bhf.ap, ALU.subtract, r=[bf_.t, bhf.t], w=[bf_.t])
    stt(bhf.ap, bf_.ap, sel.ap[:, 0:1], bhf.ap, ALU.mult, ALU.add, r=[bf_.t, sel.t, bhf.t], w=[bhf.t])
    cp("dve", bhl.ap, bhf.ap, r=[bhf.t], w=[bhl.t])

    gsb = B([D], F32, "gsb")
    dma("sp", gsb.ap, gs_d.partition_broadcast(128), w=[gsb.t])
    gv = ring(3, [D], F32, "gv")
    vln = ring(3, [D], BF16, "vln")
    bst = B([NT, 12], F32, "bst")
    mv = B([NT, 2], F32, "mv")
    rsd = B([NT], F32, "rsd")
    bst_t = [K.tok() for _ in range(NT)]
    mv_t = [K.tok() for _ in range(NT)]
    rsd_t = [K.tok() for _ in range(NT)]
    suc = ring(2, [8, 512], BF16, "suc")
    ysg = ring(2, [8, 512], BF16, "ysg")
    wg_pre = load_w(CGA, 512)
    for c in range(NCH):
        su_c = suc[c % 2]
        ys_c = ysg[c % 2]
        dma("sp", su_c.ap, suT_s[:, c * 512:(c + 1) * 512].rearrange("(g p) t -> p g t", p=128), r=[su_t[c]], w=[su_c.t])
        def sv_front(tau, c=c):
            i = 4 * c + tau
            g_ = gv[i % 3]
            vl = vln[i % 3]
            sb0 = 6
            for half, wb in enumerate((w2, w3)):
                bk = next_bank()
                tm_block(wb, 512, i, bk)
                act(g_.ap[:, half * 512:(half + 1) * 512], ps[bk][:, :], AF.Gelu_apprx_tanh, r=[pst[bk]], w=[g_.t])
            K.op("dve", lambda e, i=i, g_=g_: e.bn_stats(out=bst.ap[:, i, 0:6], in_=g_.ap[:, 0:512]), r=[g_.t], w=[bst_t[i]])
            K.op("dve", lambda e, i=i, g_=g_: e.bn_stats(out=bst.ap[:, i, 6:12], in_=g_.ap[:, 512:1024]), r=[g_.t], w=[bst_t[i]])
            K.op("dve", lambda e, i=i: e.bn_aggr(out=mv.ap[:, i, :], in_=bst.ap[:, i, :]), r=[bst_t[i]], w=[mv_t[i]])
            ts("pool", rsd.ap[:, i:i + 1], mv.ap[:, i, 1:2], EPS, ALU.add, r=[mv_t[i]], w=[rsd_t[i]])
            tt("pool", rsd.ap[:, i:i + 1], rsd.ap[:, i:i + 1], col(cst, C_NH), ALU.pow, r=[rsd_t[i], cst.t], w=[rsd_t[i]])
            ts("dve", g_.ap, g_.ap, mv.ap[:, i, 0:1], ALU.subtract, rsd.ap[:, i:i + 1], ALU.mult, r=[g_.t, mv_t[i], rsd_t[i]], w=[g_.t])
            tt("pool", vl.ap, g_.ap, gsb.ap, ALU.mult, r=[g_.t, gsb.t], w=[vl.t])

        def sv_back(tau, c=c, su_c=su_c, ys_c=ys_c):
            i = 4 * c + tau
            vl = vln[i % 3]
            sb0 = 6
            for g in range(8):
                bk = sb0 + g // 4
                o_ = ps[bk][:, (g % 4) * 128:(g % 4 + 1) * 128]
                mm(o_, vl.ap[:, g * 128:(g + 1) * 128], WtT.ap[:, g, :], True, False, r=[vl.t, WtT.t], w=[pst[bk]])
                mm(o_, ones2.ap[0:2, :], bhl.ap[0:2, g, :], False, True, r=[ones2.t, bhl.t], w=[pst[bk]])
            for b_ in range(2):
                tt("dve", ys_c.ap[:, 4 * b_:4 * b_ + 4, tau * 128:(tau + 1) * 128],
                   ps[sb0 + b_][:, :].rearrange("p (a b) -> p a b", a=4),
                   su_c.ap[:, 4 * b_:4 * b_ + 4, tau * 128:(tau + 1) * 128], ALU.mult, r=[pst[sb0 + b_], su_c.t], w=[ys_c.t])
        for tau in range(5):
            if tau < 4:
                sv_front(tau)
            if tau >= 1:
                sv_back(tau - 1)
        dma("sp", ysgT_s[:, c * 512:(c + 1) * 512].rearrange("(g p) t -> p g t", p=128), ys_c.ap, r=[ys_c.t])
    wg = wg_pre
    for blk in range(4):
        wb = wg
        if blk < 3:
            wg = load_w(CGA + (blk + 1) * 512, 512)
        for c in range(NCH):
            for j in range(4):
                bk = next_bank()
                fm_block(wb, j, c, bk)
                f0 = (blk * 4 + j) * 128
                act_block_out(bk, AF.Sigmoid, sgT_s[f0:f0 + 128, c * 512:(c + 1) * 512])
    K.barrier()
    AR.release(m_0)
    if stop_after == "1":
        return finish()
    SCALE = float(128 ** -0.5)
    vres = AR.alloc([NT, 8, 129], BF16)
    v_t = [K.tok(f"v{i}") for i in range(NT)]
    kiT2 = AR.alloc([L], BF16)
    ki_t = [K.tok(f"ki{i}") for i in range(4)]
    for q4 in range(4):
        dma("sp", kiT2[:, q4 * 1024:(q4 + 1) * 1024], qiT_s[4, :, q4 * 1024:(q4 + 1) * 1024], w=[ki_t[q4]])

    def load_v_all():
        for i in range(NT):
            dma("sp", vres[:, i, :, :], v_s[i].rearrange("p (h d) -> p h d", h=8), w=[v_t[i]])
    nrow_t = (NE * CAP + 128) // 128
    for z0 in range(0, nrow_t, 16):
        zn = min(16, nrow_t - z0)
        dma("sp", xs_s[z0 * 128:(z0 + zn) * 128, :].rearrange("(a p) d -> p a d", p=128),
            zt.ap.unsqueeze(1).broadcast_to([128, zn, D]), r=[zt.t])

    kTr = ring(2, [L], BF16, "kTr")
    qTc = B([8, 512], BF16, "qTc")
    qiTc = B([4, 512], BF16, "qiTc")
    S = B([L], F32, "S")
    maskb = B([L], BF16, "maskb")
    maskb2 = B([L], BF16, "maskb2")
    maskT = B([NT, 512], BF16, "maskT")
    rbuf = ring(3, [512], F32, "rb")
    ebuf = ring(3, [512], BF16, "eb")
    pbuf = ring(3, [512], BF16, "pb")
    S2 = B([L], F32, "S2")

    class _V:
        pass
    ytile = _V()
    ytile.ap = S2.ap[:, 0:2048].bitcast(BF16).rearrange("p (a b) -> p a b", a=4)
    ytile.t = S2.t
    yT = _V()
    yT.ap = S2.ap[:, 2048:4096].bitcast(BF16).rearrange("p (a b) -> p a b", a=8)
    yT.t = S2.t
    Sb = (S, S2)
    Mb2 = (maskb, maskb2)
    sst = []
    for q_ in range(2):
        sst.append({nm: B([sz], F32, f"{nm}{q_}") for nm, sz in (
            ("vmax", 1), ("vmin", 1), ("rngv", 1), ("hw", KI + 2), ("nhw", KI + 2), ("cand", 1), ("cnt", 1), ("msg", 1), ("thr", 1))})
    recr = ring(2, [4], F32, "rec")
    ctr = {"sc": 0, "lg": 0, "tr": 0, "e": 0}

    for c in range(NCH):
        csl = slice(c * 512, (c + 1) * 512)
        dma("sp", qTc.ap, qT_s[:, :, csl].rearrange("h p t -> p h t"), w=[qTc.t])
        dma("sp", qiTc.ap, qiT_s[0:4, :, csl].rearrange("a p t -> p a t"), w=[qiTc.t])
        if c == 0:
            load_v_all()
        def idx_scores(tau, Sx):
            i = 4 * c + tau
            n = 128 * (i + 1)
            nv = 128 * i
            for sc in range((n + 511) // 512):
                w_ = min(512, n - 512 * sc)
                sl = slice(sc * 512, sc * 512 + w_)
                for h in range(8):
                    bk = ctr["sc"] % 2
                    ctr["sc"] += 1
                    pr = slice(64 * (h % 2), 64 * (h % 2) + 64)
                    mm(ps[bk][:, 0:w_], qiTc.ap[pr, h // 2, tau * 128:(tau + 1) * 128], kiT2[pr, sl], True, True,
                       r=[qiTc.t, ki_t[sc // 2]], w=[pst[bk]])
                    rb = rbuf[ctr["sc"] % 3]
                    act(rb.ap[:, 0:w_], ps[bk][:, 0:w_], AF.Relu, r=[pst[bk]], w=[rb.t])
                    if h == 0:
                        ts("dve", Sx.ap[:, sl], rb.ap[:, 0:w_], wi_sb.ap[:, i, 0:1], ALU.mult, r=[rb.t, wi_sb.t], w=[Sx.t])
                    else:
                        stt(Sx.ap[:, sl], rb.ap[:, 0:w_], wi_sb.ap[:, i, h:h + 1], Sx.ap[:, sl], ALU.mult, ALU.add,
                            r=[rb.t, wi_sb.t, Sx.t], w=[Sx.t])
            tt("pool", Sx.ap[:, nv:n], Sx.ap[:, nv:n], cm.ap[:, 0:128], ALU.add, r=[Sx.t, cm.t], w=[Sx.t])

        def search_pre(tau, Sx, mb, st, on_act):
            i = 4 * c + tau
            n = 128 * (i + 1)
            nv = 128 * i
            if i < 2:
                return
            A_ = lambda nm: st[nm].ap
            T2 = lambda nm: st[nm].t
            K.op("dve", lambda e: e.tensor_reduce(out=A_("vmax"), in_=Sx.ap[:, 0:n], axis=AX.X, op=ALU.max), r=[Sx.t], w=[T2("vmax")])
            K.op("dve", lambda e: e.tensor_reduce(out=A_("vmin"), in_=Sx.ap[:, 0:nv], axis=AX.X, op=ALU.min), r=[Sx.t], w=[T2("vmin")])
            tt("dve", A_("rngv"), A_("vmax"), A_("vmin"), ALU.subtract, r=[T2("vmax"), T2("vmin")], w=[T2("rngv")])
            ts("dve", A_("hw"), cst.ap[:, C_PW:C_PW + KI + 2], A_("rngv")[:, 0:1], ALU.mult, r=[cst.t, T2("rngv")], w=[T2("hw")])
            if not on_act:
                tt("dve", A_("cand"), A_("vmin"), A_("hw")[:, 0:1], ALU.add, r=[T2("vmin"), T2("hw")], w=[T2("cand")])
            else:
                ts("dve", A_("nhw"), A_("hw"), -1.0, ALU.mult, r=[T2("hw")], w=[T2("nhw")])
                stt(A_("cand"), A_("vmin"), -1.0, A_("hw")[:, 0:1], ALU.mult, ALU.subtract, r=[T2("vmin"), T2("hw")], w=[T2("cand")])

        def search(tau, Sx, mb, st, on_act):
            i = 4 * c + tau
            n = 128 * (i + 1)
            if i < 2:
                return col(cst, C_NTHR), cst.t
            A_ = lambda nm: st[nm].ap
            T2 = lambda nm: st[nm].t
            if not on_act:
                for k in range(KI):
                    ts("dve", mb.ap[:, 0:n], Sx.ap[:, 0:n], A_("cand")[:, 0:1], ALU.is_ge, None, ALU.add,
                       r=[Sx.t, T2("cand")], w=[mb.t, T2("cnt")], accum=A_("cnt"))
                    ts("dve", A_("msg"), A_("cnt"), 255.5, ALU.is_ge, 0.5, ALU.subtract, r=[T2("cnt")], w=[T2("msg")])
                    stt(A_("cand"), A_("msg"), A_("hw")[:, k:k + 1], A_("cand"), ALU.mult, ALU.add, r=[T2("msg"), T2("hw"), T2("cand")], w=[T2("cand")])
                tt("dve", A_("thr"), A_("cand"), A_("hw")[:, KI:KI + 1], ALU.subtract, r=[T2("cand"), T2("hw")], w=[T2("thr")])
            else:
                for k in range(KI):
                    act(mb.ap[:, 0:n], Sx.ap[:, 0:n], AF.Sign, bias=A_("cand")[:, 0:1], r=[Sx.t, T2("cand")], w=[mb.t, T2("cnt")], accum=A_("cnt"))
                    act(A_("msg"), A_("cnt"), AF.Sign, bias=col(cst, C_NB + i), r=[T2("cnt"), cst.t], w=[T2("msg")])
                    act(A_("cand"), A_("msg"), AF.Identity, bias=A_("cand")[:, 0:1], scale=A_("nhw")[:, k + 1:k + 2],
                        r=[T2("msg"), T2("nhw"), T2("cand")], w=[T2("cand")])
                act(A_("thr"), A_("cand"), AF.Identity, bias=A_("nhw")[:, KI:KI + 1], scale=-1.0, r=[T2("cand"), T2("nhw")], w=[T2("thr")])
            return A_("thr")[:, 0:1], T2("thr")

        def make_mask(tau, Sx, mb, thr_ap, thr_t):
            i = 4 * c + tau
            n = 128 * (i + 1)
            ts("dve", mb.ap[:, 0:n], Sx.ap[:, 0:n], thr_ap, ALU.is_ge, r=[Sx.t, thr_t], w=[mb.t])
            for j0 in range(0, i + 1, 8):
                nb = min(8, i + 1 - j0)
                bk = 2 + ctr["tr"] % 2
                ctr["tr"] += 1
                for jj in range(nb):
                    tr(psb(bk)[:, jj * 128:(jj + 1) * 128], mb.ap[:, (j0 + jj) * 128:(j0 + jj + 1) * 128], ident.ap,
                       r=[mb.t, ident.t], w=[pst[bk]])
                cp("act", maskT.ap[:, j0:j0 + nb, tau * 128:(tau + 1) * 128],
                   psb(bk)[:, 0:nb * 128].rearrange("p (a b) -> p a b", a=nb), r=[pst[bk]], w=[maskT.t])

        for pair in range(2):
            t0, t1_ = 2 * pair, 2 * pair + 1
            idx_scores(t0, Sb[0])
            idx_scores(t1_, Sb[1])
            search_pre(t0, Sb[0], Mb2[0], sst[0], False)
            search_pre(t1_, Sb[1], Mb2[1], sst[1], True)
            th1 = search(t1_, Sb[1], Mb2[1], sst[1], True)
            th0 = search(t0, Sb[0], Mb2[0], sst[0], False)
            make_mask(t0, Sb[0], Mb2[0], *th0)
            make_mask(t1_, Sb[1], Mb2[1], *th1)
        nj = 4 * c + 4
        steps = [(h, j) for h in range(8) for j in range(nj)]
        LB = (1, 2, 3)

        def emit_qk(k):
            h, j = steps[k]
            kt = kTr[h % 2]
            if j == 0:
                dma("sp", kt.ap[:, 0:nj * 128], kT_s[h, :, 0:nj * 128], w=[kt.t])
            r0 = max(0, j - 4 * c)
            N = 512 - 128 * r0
            bk = LB[k % 3]
            mm(ps[bk][:, 0:N], kt.ap[:, j * 128:(j + 1) * 128], qTc.ap[:, h, r0 * 128:512], True, True,
               r=[kt.t, qTc.t], w=[pst[bk]])

        emit_qk(0)
        emit_qk(1)
        for k, (h, j) in enumerate(steps):
            if k + 2 < len(steps):
                emit_qk(k + 2)
            accA = 4 + 2 * (h % 2)
            accB = accA + 1
            r0 = max(0, j - 4 * c)
            N = 512 - 128 * r0
            bk = LB[k % 3]
            e_ = ebuf[k % 3]
            p_ = pbuf[k % 3]
            act(e_.ap[:, 0:N], ps[bk][:, 0:N], AF.Exp, scale=SCALE, r=[pst[bk]], w=[e_.t])
            tt("dve", p_.ap[:, 0:N], e_.ap[:, 0:N], maskT.ap[:, j, r0 * 128:512], ALU.mult, r=[e_.t, maskT.t], w=[p_.t])
            for tau in range(r0, 4):
                if tau < 3:
                    o_, ob = ps[accA][:, tau * 129:(tau + 1) * 129], accA
                else:
                    o_, ob = ps[accB][:, 0:129], accB
                first = (j == 0 and tau in (0, 3))
                mm(o_, p_.ap[:, (tau - r0) * 128:(tau - r0 + 1) * 128], vres[:, j, h, :], first, j == nj - 1,
                   r=[p_.t, v_t[j]], w=[pst[ob]], sgc=True)
            if j == nj - 1:
                rc = recr[h % 2]
                K.op("dve", lambda e, rc=rc, accA=accA: e.reciprocal(
                    out=rc.ap[:, 0:3], in_=ps[accA][:, 0:387].rearrange("p (a b) -> p a b", b=129)[:, :, 128]), r=[pst[accA]], w=[rc.t])
                K.op("dve", lambda e, rc=rc, accB=accB: e.reciprocal(out=rc.ap[:, 3:4], in_=ps[accB][:, 128:129]), r=[pst[accB]], w=[rc.t])
                for tau in range(4):
                    src, sb_ = (ps[accA][:, tau * 129:tau * 129 + 128], accA) if tau < 3 else (ps[accB][:, 0:128], accB)
                    ts("dve", ytile.ap[:, tau, h * 128:(h + 1) * 128], src, rc.ap[:, tau:tau + 1], ALU.mult, r=[pst[sb_], rc.t], w=[ytile.t])
        for tau in range(4):
            bk = 2 + ctr["tr"] % 2
            ctr["tr"] += 1
            for kc in range(8):
                tr(psb(bk)[:, kc * 128:(kc + 1) * 128], ytile.ap[:, tau, kc * 128:(kc + 1) * 128], ident.ap, r=[ytile.t, ident.t], w=[pst[bk]])
            cp("act", yT.ap[:, :, tau * 128:(tau + 1) * 128], psb(bk).rearrange("p (a b) -> p a b", a=8), r=[pst[bk]], w=[yT.t])
        dma("sp", yatT_s[:, csl].rearrange("(g p) t -> p g t", p=128), yT.ap, r=[yT.t])
    K.barrier()
    AR.release(m_0)
    if stop_after == "2":
        return finish()
    m_3 = AR.mark()
    Wa_sb = B([8, D], BF16, "Wa")
    Wb_sb = B([8, D], BF16, "Wb")
    Wo_sb = B([8, D], BF16, "Wo")
    w3stg = ring(2, [2, D], F32, "w3stg")
    for wi3, (wsb, wd) in enumerate(((Wa_sb, wa_d), (Wb_sb, wb_d), (Wo_sb, wo_d))):
        for q4 in range(4):
            sg = w3stg[(wi3 * 4 + q4) % 2]
            dma("sp", sg.ap, wd[q4 * 256:(q4 + 1) * 256, :].rearrange("(kc p) n -> p kc n", p=128), w=[sg.t])
            cp("dve" if q4 % 2 else "act", wsb.ap[:, 2 * q4:2 * q4 + 2, :], sg.ap, r=[sg.t], w=[wsb.t])
    g2b = B([D], F32, "g2b")
    dma("sp", g2b.ap, g2_d.partition_broadcast(128), w=[g2b.t])
    wr = B([8, 36], F32, "wr")
    br = B([36], F32, "br")
    dma("sp", wr.ap[:, :, 0:4], wrg_d.rearrange("(kc p) n -> p kc n", p=128), w=[wr.t])
    dma("sp", wr.ap[:, :, 4:36], wre_d.rearrange("(kc p) n -> p kc n", p=128), w=[wr.t])
    dma("sp", br.ap[:, 0:4], brg_d.partition_broadcast(128), w=[br.t])
    dma("sp", br.ap[:, 4:36], bre_d.partition_broadcast(128), w=[br.t])
    trib = B([128], BF16, "trib")
    onesb = B([128], BF16, "onesb")
    cp("pool", trib.ap, cm.ap[:, 128:256], r=[cm.t], w=[trib.t])
    mset("pool", onesb.ap, 1.0, w=[onesb.t])
    base = B([32], F32, "base")
    capb = B([32], F32, "capb")
    ts("pool", base.ap, cm.ap[:, 384:416], -1.0, ALU.add, r=[cm.t], w=[base.t])
    ts("pool", capb.ap, cm.ap[:, 384:416], float(CAP) - 0.5, ALU.add, r=[cm.t], w=[capb.t])
    inr = [ring(2, [8, 512], BF16, nm) for nm in ("ysgc", "yatc", "sgac", "sgbc")]
    mT = B([8, 512], BF16, "mT")
    t1p = ring(2, [512], F32, "t1p")
    t2p = ring(2, [512], F32, "t2p")
    xtr = ring(2, [D], F32, "xt")
    h2r = ring(2, [D], F32, "h2t")
    ms2 = B([NT], F32, "ms2")
    rs2 = B([NT], F32, "rs2")
    xn2f = B([D], F32, "xn2f")
    xn2b = ring(6, [D], BF16, "xn2b")
    xn2T = B([8, 128], F32, "xn2T")
    lgt4 = B([4, 36], F32, "lgt4")
    rb_ = {nm: B(sz, F32, nm) for nm, sz in (
        ("gmax", [4]), ("ohg", [4, 4]), ("dgl", [4, 4]), ("exg", [4, 4]), ("se", [4]), ("gp", [4]), ("prod", [4, 4, 8]), ("esel", [4, 8]),
        ("m1", [4]), ("oh1", [4, 8]), ("es2", [4, 8]), ("m2", [4]), ("oh2", [4, 8]), ("dlt", [4]), ("ex", [4]), ("den", [4]), ("p1", [4]),
        ("p2", [4]), ("gk", [2, 4]), ("M1", [4, 32]), ("M2", [4, 32]), ("posf", [4, 32]), ("okf", [4, 32]), ("pr32", [4, 32]),
        ("pk", [2, 4]), ("ok", [2, 4]))}
    Mb4 = B([4, 32], BF16, "Mb4")
    sm = {nm: B([sz], F32, nm) for nm, sz in (
        ("gmax", 1), ("negg", 1), ("ohg", 4), ("j4", 4), ("se", 1), ("gp", 1), ("esel", 8), ("m1", 1), ("oh1", 8), ("es2", 8),
        ("m2", 1), ("oh2", 8), ("dlt", 1), ("ex", 1), ("den", 1), ("p1", 1), ("p2", 1), ("M1", 32), ("M2", 32), ("posf", 32),
        ("okf", 32), ("j32", 32), ("pk", 2), ("ok", 2), ("gk", 2))}
    Mb = B([32], BF16, "Mb")

    def S_(nm):
        return sm[nm].ap

    def T_(nm):
        return sm[nm].t

    def load_chunk3(c):
        csl = slice(c * 512, (c + 1) * 512)
        ysg_c, yat_c, sga_c, sgb_c = [rg[c % 2] for rg in inr]
        dma("sp", ysg_c.ap, ysgT_s[:, csl].rearrange("(g p) t -> p g t", p=128), w=[ysg_c.t])
        dma("sp", yat_c.ap, yatT_s[:, csl].rearrange("(g p) t -> p g t", p=128), w=[yat_c.t])
        dma("sp", sga_c.ap, sgT_s[0:D, csl].rearrange("(g p) t -> p g t", p=128), w=[sga_c.t])
        dma("sp", sgb_c.ap, sgT_s[D:2 * D, csl].rearrange("(g p) t -> p g t", p=128), w=[sgb_c.t])

    load_chunk3(0)
    for c in range(NCH):
        csl = slice(c * 512, (c + 1) * 512)
        ysg_c, yat_c, sga_c, sgb_c = [rg[c % 2] for rg in inr]
        if c + 1 < NCH:
            load_chunk3(c + 1)
        for nb in range(8):
            bA = next_bank()
            for kc in range(8):
                mm(ps[bA][:, :], Wa_sb.ap[:, kc, nb * 128:(nb + 1) * 128], ysg_c.ap[:, kc, :], kc == 0, kc == 7, r=[Wa_sb.t, ysg_c.t], w=[pst[bA]])
            bB = next_bank()
            for kc in range(8):
                mm(ps[bB][:, :], Wb_sb.ap[:, kc, nb * 128:(nb + 1) * 128], yat_c.ap[:, kc, :], kc == 0, kc == 7, r=[Wb_sb.t, yat_c.t], w=[pst[bB]])
            t1, t2 = t1p[nb % 2], t2p[nb % 2]
            tt("dve", t1.ap, ps[bA][:, :], sga_c.ap[:, nb, :], ALU.mult, r=[pst[bA], sga_c.t], w=[t1.t])
            tt("dve", t2.ap, ps[bB][:, :], sgb_c.ap[:, nb, :], ALU.mult, r=[pst[bB], sgb_c.t], w=[t2.t])
            tt("pool", mT.ap[:, nb, :], t1.ap, t2.ap, ALU.add, r=[t1.t, t2.t], w=[mT.t])
        for tau in range(4):
            i = 4 * c + tau
            xt, h2t, xb2 = xtr[i % 2], h2r[i % 2], xn2b[i % 6]
            dma("sp", xt.ap, x_d[i * 128:(i + 1) * 128, :], w=[xt.t])
            for half in range(2):
                hs = slice(half * 512, (half + 1) * 512)
                bk = next_bank()
                for kc in range(8):
                    mm(ps[bk][:, :], mT.ap[:, kc, tau * 128:(tau + 1) * 128], Wo_sb.ap[:, kc, hs], kc == 0, kc == 7, r=[mT.t, Wo_sb.t], w=[pst[bk]])
                tt("dve", h2t.ap[:, hs], ps[bk][:, :], xt.ap[:, hs], ALU.add, r=[pst[bk], xt.t], w=[h2t.t])
            dma("sp", h2_s[i * 128:(i + 1) * 128, :], h2t.ap, r=[h2t.t])
            stt(xn2f.ap, h2t.ap, 1.0 / D, h2t.ap, ALU.mult, ALU.mult, r=[h2t.t], w=[xn2f.t, ms2.t], accum=ms2.ap[:, i:i + 1])
            ts("pool", rs2.ap[:, i:i + 1], ms2.ap[:, i:i + 1], EPS, ALU.add, r=[ms2.t], w=[rs2.t])
            tt("pool", rs2.ap[:, i:i + 1], rs2.ap[:, i:i + 1], col(cst, C_NH), ALU.pow, r=[rs2.t, cst.t], w=[rs2.t])
            stt(xn2f.ap, h2t.ap, rs2.ap[:, i:i + 1], g2b.ap, ALU.mult, ALU.mult, r=[h2t.t, rs2.t, g2b.t], w=[xn2f.t])
            cp("pool", xb2.ap, xn2f.ap, r=[xn2f.t], w=[xb2.t])
            for q4 in range(2):
                bt = 4 + q4
                for jj in range(4):
                    kc = q4 * 4 + jj
                    tr(ps[bt][:, jj * 128:(jj + 1) * 128], xn2f.ap[:, kc * 128:(kc + 1) * 128], identf.ap, r=[xn2f.t, identf.t], w=[pst[bt]])
                cp("act", xn2T.ap[:, q4 * 4:(q4 + 1) * 4, :], ps[bt][:, :].rearrange("p (a b) -> p a b", a=4), r=[pst[bt]], w=[xn2T.t])
            for kc in range(8):
                mm(ps[6][:, 0:36], xn2T.ap[:, kc, :], wr.ap[:, kc, :], kc == 0, kc == 7, r=[xn2T.t, wr.t], w=[pst[6]])
            tt("dve", lgt4.ap[:, tau, :], ps[6][:, 0:36], br.ap, ALU.add, r=[pst[6], br.t], w=[lgt4.t])

        gl4 = lgt4.ap[:, :, 0:4]
        el4 = lgt4.ap[:, :, 4:36].rearrange("p t (g j) -> p t g j", g=4)
        R_ = lambda nm: rb_[nm].ap
        Q_ = lambda nm: rb_[nm].t
        b3 = lambda ap, shp: ap.unsqueeze(2).broadcast_to(shp)
        K.op("dve", lambda e: e.tensor_reduce(out=R_("gmax"), in_=gl4, axis=AX.X, op=ALU.max), r=[lgt4.t], w=[Q_("gmax")])
        tt("dve", R_("ohg"), gl4, b3(R_("gmax"), [128, 4, 4]), ALU.is_ge, r=[lgt4.t, Q_("gmax")], w=[Q_("ohg")])
        tt("dve", R_("dgl"), gl4, b3(R_("gmax"), [128, 4, 4]), ALU.subtract, r=[lgt4.t, Q_("gmax")], w=[Q_("dgl")])
        act(R_("exg"), R_("dgl"), AF.Exp, r=[Q_("dgl")], w=[Q_("exg")])
        K.op("dve", lambda e: e.tensor_reduce(out=R_("se"), in_=R_("exg"), axis=AX.X, op=ALU.add), r=[Q_("exg")], w=[Q_("se")])
        K.op("dve", lambda e: e.reciprocal(out=R_("gp"), in_=R_("se")), r=[Q_("se")], w=[Q_("gp")])
        tt("dve", R_("prod"), el4, R_("ohg").unsqueeze(3).broadcast_to([128, 4, 4, 8]), ALU.mult, r=[lgt4.t, Q_("ohg")], w=[Q_("prod")])
        tt("dve", R_("esel"), R_("prod")[:, :, 0, :], R_("prod")[:, :, 1, :], ALU.add, r=[Q_("prod")], w=[Q_("esel")])
        tt("dve", R_("esel"), R_("esel"), R_("prod")[:, :, 2, :], ALU.add, r=[Q_("prod"), Q_("esel")], w=[Q_("esel")])
        tt("dve", R_("esel"), R_("esel"), R_("prod")[:, :, 3, :], ALU.add, r=[Q_("prod"), Q_("esel")], w=[Q_("esel")])
        K.op("dve", lambda e: e.tensor_reduce(out=R_("m1"), in_=R_("esel"), axis=AX.X, op=ALU.max), r=[Q_("esel")], w=[Q_("m1")])
        tt("dve", R_("oh1"), R_("esel"), b3(R_("m1"), [128, 4, 8]), ALU.is_ge, r=[Q_("esel"), Q_("m1")], w=[Q_("oh1")])
        stt(R_("es2"), R_("oh1"), NEG, R_("esel"), ALU.mult, ALU.add, r=[Q_("oh1"), Q_("esel")], w=[Q_("es2")])
        K.op("dve", lambda e: e.tensor_reduce(out=R_("m2"), in_=R_("es2"), axis=AX.X, op=ALU.max), r=[Q_("es2")], w=[Q_("m2")])
        tt("dve", R_("oh2"), R_("es2"), b3(R_("m2"), [128, 4, 8]), ALU.is_ge, r=[Q_("es2"), Q_("m2")], w=[Q_("oh2")])
        tt("dve", R_("dlt"), R_("m2"), R_("m1"), ALU.subtract, r=[Q_("m1"), Q_("m2")], w=[Q_("dlt")])
        act(R_("ex"), R_("dlt"), AF.Exp, r=[Q_("dlt")], w=[Q_("ex")])
        ts("dve", R_("den"), R_("ex"), 1.0, ALU.add, r=[Q_("ex")], w=[Q_("den")])
        K.op("dve", lambda e: e.reciprocal(out=R_("p1"), in_=R_("den")), r=[Q_("den")], w=[Q_("p1")])
        tt("dve", R_("p2"), R_("ex"), R_("p1"), ALU.mult, r=[Q_("ex"), Q_("p1")], w=[Q_("p2")])
        tt("dve", R_("gk")[:, 0, :], R_("p1"), R_("gp"), ALU.mult, r=[Q_("p1"), Q_("gp")], w=[Q_("gk")])
        tt("dve", R_("gk")[:, 1, :], R_("p2"), R_("gp"), ALU.mult, r=[Q_("p2"), Q_("gp")], w=[Q_("gk")])
        ohg4 = R_("ohg").unsqueeze(3).broadcast_to([128, 4, 4, 8])
        for nm, oh in (("M1", "oh1"), ("M2", "oh2")):
            tt("dve", R_(nm).rearrange("p t (g j) -> p t g j", g=4), ohg4, R_(oh).unsqueeze(2).broadcast_to([128, 4, 4, 8]), ALU.mult,
               r=[Q_("ohg"), Q_(oh)], w=[Q_(nm)])
        tt("dve", Mb4.ap, R_("M1"), R_("M2"), ALU.add, r=[Q_("M1"), Q_("M2")], w=[Mb4.t])
        for tau in range(4):
            o_ = ps[7][:, tau * 32:(tau + 1) * 32]
            mm(o_, trib.ap, Mb4.ap[:, tau, :], True, tau == 0, r=[trib.t, Mb4.t], w=[pst[7]], sgc=True)
            for tp_ in range(tau):
                mm(o_, onesb.ap, Mb4.ap[:, tp_, :], False, tp_ == tau - 1, r=[onesb.t, Mb4.t], w=[pst[7]], sgc=True)
        for tau in range(4):
            mm(ps[7][:, 128:160], onesb.ap, Mb4.ap[:, tau, :], False, tau == 3, r=[onesb.t, Mb4.t], w=[pst[7]], sgc=True)
        tt("dve", R_("posf"), ps[7][:, 0:128].rearrange("p (t e) -> p t e", t=4), base.ap.unsqueeze(1).broadcast_to([128, 4, 32]), ALU.add,
           r=[pst[7], base.t], w=[Q_("posf")])
        tt("dve", base.ap, ps[7][:, 128:160], base.ap, ALU.add, r=[pst[7], base.t], w=[base.t])
        tt("dve", R_("okf"), R_("posf"), capb.ap.unsqueeze(1).broadcast_to([128, 4, 32]), ALU.is_lt, r=[Q_("posf"), capb.t], w=[Q_("okf")])
        for k_, nm in enumerate(("M1", "M2")):
            tt("dve", R_("pr32"), R_(nm), R_("posf"), ALU.mult, r=[Q_(nm), Q_("posf")], w=[Q_("pr32")])
            K.op("dve", lambda e, k_=k_: e.tensor_reduce(out=R_("pk")[:, k_, :], in_=R_("pr32"), axis=AX.X, op=ALU.add), r=[Q_("pr32")], w=[Q_("pk")])
            tt("dve", R_("pr32"), R_(nm), R_("okf"), ALU.mult, r=[Q_(nm), Q_("okf")], w=[Q_("pr32")])
            K.op("dve", lambda e, k_=k_: e.tensor_reduce(out=R_("ok")[:, k_, :], in_=R_("pr32"), axis=AX.X, op=ALU.add), r=[Q_("pr32")], w=[Q_("ok")])
        ts("dve", R_("pk"), R_("pk"), col(cst, C_DUM), ALU.subtract, r=[Q_("pk"), cst.t], w=[Q_("pk")])
        tt("dve", R_("pk"), R_("pk"), R_("ok"), ALU.mult, r=[Q_("pk"), Q_("ok")], w=[Q_("pk")])
        ts("dve", R_("pk"), R_("pk"), col(cst, C_DUM), ALU.add, r=[Q_("pk"), cst.t], w=[Q_("pk")])
        cp("dve", rt_pos.ap[:, 8 * c:8 * c + 8].rearrange("p (t k) -> p k t", k=2), R_("pk"), r=[Q_("pk")], w=[rt_pos.t])
        tt("dve", rt_gate.ap[:, 8 * c:8 * c + 8].rearrange("p (t k) -> p k t", k=2), R_("gk"), R_("ok"), ALU.mult, r=[Q_("gk"), Q_("ok")], w=[rt_gate.t])
        for tau in range(4):
            i = 4 * c + tau
            xb2 = xn2b[i % 6]
            for k_ in range(2):
                K.op("pool", lambda e, i=i, k_=k_, xb2=xb2: e.indirect_dma_start(
                    out=xs_s[:, :], out_offset=bass.IndirectOffsetOnAxis(ap=rt_pos.ap[:, 2 * i + k_:2 * i + k_ + 1], axis=0),
                    in_=xb2.ap, in_offset=None), r=[xb2.t, rt_pos.t], dma=True)
    K.barrier()
    AR.release(m_3)
    if stop_after == "3":
        return finish()
    m_4 = AR.mark()
    wi_r = ring(2, [8, 512], BF16, "wie")
    wo_r = ring(3, [2, D], BF16, "woe")
    wis_r = ring(2, [8, 512], F32, "wis")
    wos_r = ring(2, [2, D], F32, "wos")
    xs_r = ring(2, [4, D], BF16, "xs")
    xsT_r = ring(2, [8, 512], BF16, "xsT")
    sg_r = ring(2, [512], F32, "sg")
    aT_r = ring(2, [2, 512], BF16, "aT")
    ys_r = ring(3, [D], BF16, "ysb")
    nys = {"n": 0}
    mset("pool", ys_r[2].ap, 0.0, w=[ys_r[2].t])
    dma("sp", ys_s[NE * CAP:NE * CAP + 128, :], ys_r[2].ap, r=[ys_r[2].t])
    def load_expert(e_i):
        wi_, wo_, xs_ = wi_r[e_i % 2], wo_r[e_i % 3], xs_r[e_i % 2]
        sgi, sgo = wis_r[e_i % 2], wos_r[e_i % 2]
        dma("sp", xs_.ap, xs_s[e_i * CAP:(e_i + 1) * CAP, :].rearrange("(st p) d -> p st d", p=128), w=[xs_.t])
        dma("sp", sgi.ap, wei_d[e_i].rearrange("(kc p) f -> p kc f", p=128), w=[sgi.t])
        dma("sp", sgo.ap, weo_d[e_i].rearrange("(fc p) n -> p fc n", p=128), w=[sgo.t])

    def cast_expert(e_i):
        wi_, wo_ = wi_r[e_i % 2], wo_r[e_i % 3]
        sgi, sgo = wis_r[e_i % 2], wos_r[e_i % 2]
        cp("pool", wi_.ap[:, 0:3, :], sgi.ap[:, 0:3, :], r=[sgi.t], w=[wi_.t])
        cp("dve", wi_.ap[:, 3:8, :], sgi.ap[:, 3:8, :], r=[sgi.t], w=[wi_.t])
        cp("act", wo_.ap, sgo.ap, r=[sgo.t], w=[wo_.t])

    def stage_T(e_i):
        xs_, xsT_ = xs_r[e_i % 2], xsT_r[e_i % 2]
        for st in range(4):
            bk = 4 + st % 2
            for kc in range(8):
                tr(psb(bk)[:, kc * 128:(kc + 1) * 128], xs_.ap[:, st, kc * 128:(kc + 1) * 128], ident.ap, r=[xs_.t, ident.t], w=[pst[bk]])
            cp("act" if st % 2 else "dve", xsT_.ap[:, :, st * 128:(st + 1) * 128], psb(bk).rearrange("p (a b) -> p a b", a=8), r=[pst[bk]], w=[xsT_.t])

    def stage_H(e_i):
        wi_, xsT_, aT_ = wi_r[e_i % 2], xsT_r[e_i % 2], aT_r[e_i % 2]
        for p_ in range(2):
            bG = next_bank()
            for kc in range(8):
                mm(ps[bG][:, :], wi_.ap[:, kc, p_ * 128:(p_ + 1) * 128], xsT_.ap[:, kc, :], kc == 0, kc == 7, r=[wi_.t, xsT_.t], w=[pst[bG]])
            bU = next_bank()
            for kc in range(8):
                mm(ps[bU][:, :], wi_.ap[:, kc, 256 + p_ * 128:256 + (p_ + 1) * 128], xsT_.ap[:, kc, :], kc == 0, kc == 7, r=[wi_.t, xsT_.t], w=[pst[bU]])
            sg_ = sg_r[p_]
            act(sg_.ap, ps[bG][:, :], AF.Silu, r=[pst[bG]], w=[sg_.t])
            tt("dve", aT_.ap[:, p_, :], sg_.ap, ps[bU][:, :], ALU.mult, r=[sg_.t, pst[bU]], w=[aT_.t])

    def stage_Y(e_i):
        wo_, aT_ = wo_r[e_i % 3], aT_r[e_i % 2]
        for st in range(4):
            yb = ys_r[nys["n"] % 3]
            nys["n"] += 1
            for half in range(2):
                hs = slice(half * 512, (half + 1) * 512)
                bk = next_bank()
                for fc in range(2):
                    mm(ps[bk][:, :], aT_.ap[:, fc, st * 128:(st + 1) * 128], wo_.ap[:, fc, hs], fc == 0, fc == 1, r=[aT_.t, wo_.t], w=[pst[bk]])
                cp("act" if half else "dve", yb.ap[:, hs], ps[bk][:, :], r=[pst[bk]], w=[yb.t])
            r0_ = e_i * CAP + st * 128
            dma("sp", ys_s[r0_:r0_ + 128, :], yb.ap, r=[yb.t])

    load_expert(0)
    load_expert(1)
    cast_expert(0)
    cast_expert(1)
    stage_T(0)
    stage_T(1)
    stage_H(0)
    for e_i in range(NE):
        if e_i + 2 < NE:
            load_expert(e_i + 2)
            stage_T(e_i + 2)
        if e_i + 1 < NE:
            stage_H(e_i + 1)
        stage_Y(e_i)
        if e_i + 2 < NE:
            cast_expert(e_i + 2)
    K.barrier()
    AR.release(m_4)
    gfb = B([D], F32, "gfb")
    dma("sp", gfb.ap, gf_d.partition_broadcast(128), w=[gfb.t])
    Y0r = ring(3, [D], BF16, "Y0")
    Y1r = ring(3, [D], BF16, "Y1")
    h2l = ring(3, [D], F32, "h2l")
    h3r = ring(3, [D], F32, "h3")
    outr = ring(2, [D], F32, "outb")
    junk4 = B([D], F32, "junk4")
    ms3 = B([NT], F32, "ms3")
    rs3 = B([NT], F32, "rs3")
    for b_ in Y0r + Y1r:
        mset("pool", b_.ap, 0.0, w=[b_.t])
    ms3_t = [K.tok() for _ in range(NT)]
    rs3_t = [K.tok() for _ in range(NT)]

    def fin_front(i):
        Y0, Y1, h2_, h3 = Y0r[i % 3], Y1r[i % 3], h2l[i % 3], h3r[i % 3]
        for k_, Y in enumerate((Y0, Y1)):
            K.op("pool", lambda e, i=i, k_=k_, Y=Y: e.indirect_dma_start(
                out=Y.ap, out_offset=None, in_=ys_s[:, :], in_offset=bass.IndirectOffsetOnAxis(ap=rt_pos.ap[:, 2 * i + k_:2 * i + k_ + 1], axis=0)),
                r=[rt_pos.t], w=[Y.t], dma=True)
        dma("sp", h2_.ap, h2_s[i * 128:(i + 1) * 128, :], w=[h2_.t])
        stt(h3.ap, Y0.ap, rt_gate.ap[:, 2 * i:2 * i + 1], h2_.ap, ALU.mult, ALU.add, r=[Y0.t, rt_gate.t, h2_.t], w=[h3.t])
        stt(h3.ap, Y1.ap, rt_gate.ap[:, 2 * i + 1:2 * i + 2], h3.ap, ALU.mult, ALU.add, r=[Y1.t, rt_gate.t, h3.t], w=[h3.t])
        stt(junk4.ap, h3.ap, 1.0 / D, h3.ap, ALU.mult, ALU.mult, r=[h3.t], w=[junk4.t, ms3_t[i]], accum=ms3.ap[:, i:i + 1])
        ts("pool", rs3.ap[:, i:i + 1], ms3.ap[:, i:i + 1], EPS, ALU.add, r=[ms3_t[i]], w=[rs3_t[i]])
        tt("pool", rs3.ap[:, i:i + 1], rs3.ap[:, i:i + 1], col(cst, C_NH), ALU.pow, r=[rs3_t[i], cst.t], w=[rs3_t[i]])

    def fin_back(i):
        h3, ob = h3r[i % 3], outr[i % 2]
        stt(ob.ap, h3.ap, rs3.ap[:, i:i + 1], gfb.ap, ALU.mult, ALU.mult, r=[h3.t, rs3_t[i], gfb.t], w=[ob.t])
        dma("sp", out_d[i * 128:(i + 1) * 128, :], ob.ap, r=[ob.t])

    for i in range(NT + 1):
        if i < NT:
            fin_front(i)
        if i >= 1:
            fin_back(i - 1)
    return finish()


def make_in_maps(inp, cores):
    c, m = host_consts()
    maps = []
    for b in cores:
        pos = np.ascontiguousarray(inp["positions"][b]).astype(np.int32)
        maps.append({
            "x": np.ascontiguousarray(inp["x"][b], dtype=np.float32),
            "pos_row": pos.reshape(1, L),
            "pos_col": np.ascontiguousarray(pos.reshape(NT, 128).T),
            "norm1_g": np.ascontiguousarray(inp["norm1_g"][0]).reshape(1, D),
            "w_in": np.ascontiguousarray(inp["w_in"][0]),
            "sgu_norm_g": np.ascontiguousarray(inp["sgu_norm_g"][0]).reshape(1, D),
            "sgu_w": np.ascontiguousarray(inp["sgu_w"][0]),
            "sgu_b": np.ascontiguousarray(inp["sgu_b"][0]),
            "w_branch_a": np.ascontiguousarray(inp["w_branch_a"][0]),
            "w_branch_b": np.ascontiguousarray(inp["w_branch_b"][0]),
            "w_out": np.ascontiguousarray(inp["w_out"][0]),
            "norm2_g": np.ascontiguousarray(inp["norm2_g"][0]).reshape(1, D),
            "w_router_group": np.ascontiguousarray(inp["w_router_group"][0]),
            "b_router_group": np.ascontiguousarray(inp["b_router_group"][0]).reshape(1, 4),
            "w_router_expert": np.ascontiguousarray(inp["w_router_expert"][0]),
            "b_router_expert": np.ascontiguousarray(inp["b_router_expert"][0]).reshape(1, 32),
            "w_expert_in": np.ascontiguousarray(inp["w_expert_in"][0]),
            "w_expert_out": np.ascontiguousarray(inp["w_expert_out"][0]),
            "norm_f_g": np.ascontiguousarray(inp["norm_f_g"]).reshape(1, D),
            "cst": c,
            "cmat": m,
        })
    return maps


def kernel(**inputs):
    P = build_program()
    maps = make_in_maps(inputs, list(range(8)))
    res = run_bass_kernel_spmd(P.nc, maps, core_ids=list(range(8)))
    return np.stack([np.asarray(r["out"], dtype=np.float32) for r in res.results], axis=0)
```

```python
from contextlib import ExitStack
import numpy as np
import concourse.bass as bass
import concourse.mybir as mybir

F32 = mybir.dt.float32
BF16 = mybir.dt.bfloat16
I32 = mybir.dt.int32
U32 = mybir.dt.uint32
AF = mybir.ActivationFunctionType
ALU = mybir.AluOpType
AX = mybir.AxisListType


class Tok:
    __slots__ = ("w", "r", "name")

    def __init__(self, name=""):
        self.w = None
        self.r = []
        self.name = name


class Op:
    __slots__ = ("eng", "fn", "deps", "dma", "sig", "sem", "val", "gidx", "slotwait")

    def __init__(self, eng, fn, dma, gidx):
        self.eng = eng
        self.fn = fn
        self.dma = dma
        self.deps = []
        self.sig = False
        self.sem = None
        self.val = 0
        self.gidx = gidx
        self.slotwait = None


class Kern:
    ENGS = ("pe", "act", "dve", "pool", "sp")
    NSLOT = {"sp": 20, "act": 6, "pool": 12}
    ROLL = 30000

    def __init__(self, nc):
        self.nc = nc
        self.ops = {e: [] for e in self.ENGS}
        self.n = 0
        self.toks = []
        self.es = ExitStack()
        self.nsem = 0

    def tok(self, name=""):
        t = Tok(name)
        self.toks.append(t)
        return t

    def toks_n(self, n, name=""):
        return [self.tok(f"{name}{i}") for i in range(n)]

    def op(self, eng, fn, r=(), w=(), dma=False):
        o = Op(eng, fn, dma, self.n)
        self.n += 1
        deps = {}
        for t in r:
            if t.w is not None:
                deps[id(t.w)] = t.w
        for t in w:
            if t.w is not None:
                deps[id(t.w)] = t.w
            for q in t.r:
                deps[id(q)] = q
        for d in deps.values():
            if d is o:
                continue
            if d.eng == eng and not d.dma and not dma:
                if eng == "pe":
                    continue
            o.deps.append(d)
        for t in r:
            t.r.append(o)
        for t in w:
            t.w = o
            t.r = []
        self.ops[eng].append(o)
        return o

    def barrier(self):
        deps = {}
        for t in self.toks:
            if t.w is not None:
                deps[id(t.w)] = t.w
            for q in t.r:
                deps[id(q)] = q
        dl = list(deps.values())
        for e in self.ENGS:
            o = Op(e, None, False, self.n)
            self.n += 1
            o.deps = [d for d in dl if d.fn is not None]
            self.ops[e].append(o)
        for t in self.toks:
            t.r = []
            t.w = None

    def _newsem(self, name):
        self.nsem += 1
        return self.es.enter_context(self.nc.semaphore(f"{name}_{self.nsem}"))

    def emit(self):
        nc = self.nc
        for e in self.ENGS:
            for o in self.ops[e]:
                for d in o.deps:
                    d.sig = True
        for e in self.ENGS:
            cur = None
            cnt = 0
            slots = None
            slot_uses = None
            slot_last = None
            k = 0
            for o in self.ops[e]:
                if o.fn is None:
                    continue
                if o.dma:
                    if slots is None:
                        ns = self.NSLOT[e]
                        slots = [self._newsem(f"d{e}") for _ in range(ns)]
                        slot_uses = [0] * ns
                        slot_last = [None] * ns
                    s = k % len(slots)
                    k += 1
                    o.slotwait = slot_last[s]
                    slot_uses[s] += 1
                    o.sem = slots[s]
                    o.val = 16 * slot_uses[s]
                    o.sig = True
                    slot_last[s] = o
                elif o.sig:
                    if cur is None or cnt >= self.ROLL:
                        cur = self._newsem(f"c{e}")
                        cnt = 0
                    cnt += 1
                    o.sem = cur
                    o.val = cnt
        with nc.Block() as block:
            def run(e, eng):
                waited = {}
                for o in self.ops[e]:
                    need = {}
                    dl = list(o.deps)
                    if o.slotwait is not None:
                        dl.append(o.slotwait)
                    for d in dl:
                        key = id(d.sem)
                        if waited.get(key, 0) >= d.val:
                            continue
                        if key not in need or need[key][1] < d.val:
                            need[key] = (d.sem, d.val)
                    for key, (sem, val) in need.items():
                        eng.wait_ge(sem, val)
                        waited[key] = val
                    if o.fn is None:
                        continue
                    ins = o.fn(eng)
                    if o.sig:
                        ins.then_inc(o.sem, 16 if o.dma else 1)

            @block.tensor
            def _(eng):
                run("pe", eng)

            @block.scalar
            def _(eng):
                run("act", eng)

            @block.vector
            def _(eng):
                run("dve", eng)

            @block.gpsimd
            def _(eng):
                run("pool", eng)

            @block.sync
            def _(eng):
                run("sp", eng)
        self.es.close()


U8 = mybir.dt.uint8
DTSZ = {F32: 4, BF16: 2, I32: 4, U32: 4}


class Arena:
    def __init__(self, nc, nbytes):
        self.t = nc.alloc_sbuf_tensor("arena", [128, nbytes], U8)
        self.n = nbytes
        self.off = 0
        self.peak = 0

    def alloc(self, shape, dt):
        n = int(np.prod(shape)) * DTSZ[dt]
        n = (n + 63) // 64 * 64
        assert self.off + n <= self.n, f"arena overflow {self.off}+{n}>{self.n}"
        v = self.t[:, self.off:self.off + n].bitcast(dt)
        self.off += n
        self.peak = max(self.peak, self.off)
        tot = int(np.prod(shape))
        v = v[:, 0:tot]
        if len(shape) == 2:
            v = v.rearrange("p (a b) -> p a b", a=shape[0])
        elif len(shape) == 3:
            v = v.rearrange("p (a b c) -> p a b c", a=shape[0], b=shape[1])
        return v

    def mark(self):
        return self.off

    def release(self, m):
        self.off = m

from concourse.bass_utils import run_bass_kernel_spmd

L = 4096
D = 1024
NT = 32
NCH = 8
DIN = 7752
CQ, CK, CV, CQI, CKI, CSU, CSV, CGA, CGB = 0, 1024, 2048, 3072, 3584, 3656, 4680, 5704, 6728
NE = 32
CAP = 512
DUMMY = NE * CAP
KI = 20
EPS = 1e-6
PI = float(np.pi)
MAGIC = 12582912.0
C1 = 6.28125
C2 = 2 * PI - C1
NEG = -1.0e30
ARENA = 206 * 1024

C_INV, C_SGN, C_INVI, C_NH, C_HPI, C_NTHR, C_EPS, C_ONE, C_DUM, C_PW, C_NB = 0, 1, 2, 34, 35, 36, 37, 38, 39, 40, 64
NCST = 96


def host_consts():
    c = np.zeros((128, NCST), np.float32)
    inv128 = (np.float32(10000.0) ** (-np.arange(0, 128, 2, dtype=np.float32) / np.float32(128))).astype(np.float32)
    inv64 = (np.float32(10000.0) ** (-np.arange(0, 64, 2, dtype=np.float32) / np.float32(64))).astype(np.float32)
    p = np.arange(128)
    c[:, C_INV] = inv128[p % 64]
    c[:, C_SGN] = np.where(p < 64, -1.0, 1.0)
    c[:, C_INVI:C_INVI + 32] = inv64[None, :]
    c[:, C_NH] = -0.5
    c[:, C_HPI] = PI / 2
    c[:, C_NTHR] = -1.0e29
    c[:, C_EPS] = EPS
    c[:, C_ONE] = 1.0
    c[:, C_DUM] = NE * CAP + p
    c[:, C_PW:C_PW + KI + 2] = (2.0 ** -(np.arange(KI + 2) + 1.0))[None, :]
    c[:, C_NB:C_NB + NT] = (128.0 * (np.arange(NT) + 1.0) - 511.0)[None, :]
    m = np.zeros((128, 128 * 3 + 64), np.float32)
    t = np.arange(128)[:, None]
    s = np.arange(128)[None, :]
    m[:, 0:128] = np.where(s <= t, 0.0, NEG)
    m[:, 128:256] = np.where(s >= t, 1.0, 0.0)
    m[:, 256:384] = np.where(s <= t, 1.0, 0.0)
    m[:, 384:416] = (np.arange(32) * CAP)[None, :]
    m[:, 416:448] = 1.0
    return c, m


class Prog:
    pass


def build_program(stop_after=None, dbg=False):
    nc = bass.Bass("TRN2", target_bir_lowering=False)
    K = Kern(nc)
    AR = Arena(nc, ARENA)
    P = Prog()
    P.nc = nc

    def din(name, shape, dt=F32):
        return nc.dram_tensor(name, shape, dt, kind="ExternalInput").ap()

    def dscr(name, shape, dt, out=False):
        return nc.dram_tensor(name, shape, dt, kind="ExternalOutput" if (out and dbg) else "Internal").ap()

    x_d = din("x", [L, D])
    posr_d = din("pos_row", [1, L], I32)
    posc_d = din("pos_col", [128, NT], I32)
    g1_d = din("norm1_g", [1, D])
    win_d = din("w_in", [D, DIN])
    gs_d = din("sgu_norm_g", [1, D])
    sw_d = din("sgu_w", [8, 128, 128])
    sb_d = din("sgu_b", [8, 128])
    wa_d = din("w_branch_a", [D, D])
    wb_d = din("w_branch_b", [D, D])
    wo_d = din("w_out", [D, D])
    g2_d = din("norm2_g", [1, D])
    wrg_d = din("w_router_group", [D, 4])
    brg_d = din("b_router_group", [1, 4])
    wre_d = din("w_router_expert", [D, 32])
    bre_d = din("b_router_expert", [1, 32])
    wei_d = din("w_expert_in", [NE, D, 512])
    weo_d = din("w_expert_out", [NE, 256, D])
    gf_d = din("norm_f_g", [1, D])
    cst_d = din("cst", [128, NCST])
    cm_d = din("cmat", [128, 448])
    out_d = nc.dram_tensor("out", [L, D], F32, kind="ExternalOutput").ap()

    qT_s = dscr("qT_s", [8, 128, L], BF16, True)
    kT_s = dscr("kT_s", [8, 128, L], BF16, True)
    v_s = dscr("v_s", [NT, 128, 8 * 129], BF16, True)
    qiT_s = dscr("qiT_s", [5, 128, L], BF16, True)
    suT_s = dscr("suT_s", [D, L], BF16)
    ysgT_s = dscr("ysgT_s", [D, L], BF16, True)
    sgT_s = dscr("sgT_s", [2 * D, L], BF16, True)
    yatT_s = dscr("yatT_s", [D, L], BF16, True)
    h2_s = dscr("h2_s", [L, D], F32, True)
    xs_s = dscr("xs_s", [NE * CAP + 128, D], BF16)
    ys_s = dscr("ys_s", [NE * CAP + 128, D], BF16)

    ps = [nc.alloc_psum_tensor(f"ps{i}", [128, 512], F32) for i in range(8)]
    pst = [K.tok(f"ps{i}") for i in range(8)]

    def psb(i):
        return ps[i][:, :].bitcast(BF16)

    def dma(eng, out, in_, r=(), w=()):
        return K.op(eng, lambda e: e.dma_start(out=out, in_=in_), r=r, w=w, dma=True)

    def mm(out, lhsT, rhs, start, stop, r=(), w=(), sgc=False):
        return K.op("pe", lambda e: e.matmul(out, lhsT=lhsT, rhs=rhs, start=start, stop=stop, skip_group_check=sgc), r=r, w=w)

    def tr(out, in_, ident, r=(), w=()):
        return K.op("pe", lambda e: e.transpose(out, in_, ident), r=r, w=w)

    def act(out, in_, func, r=(), w=(), bias=None, scale=1.0, accum=None):
        def f(e):
            kw = {}
            if bias is not None:
                kw["bias"] = bias
            if accum is not None:
                kw["accum_out"] = accum
            return e.activation(out=out, in_=in_, func=func, scale=scale, **kw)
        return K.op("act", f, r=r, w=w)

    def ts(eng, out, in0, s1, op0, s2=None, op1=None, r=(), w=(), accum=None):
        def f(e):
            kw = {}
            if op1 is not None:
                kw["op1"] = op1
            if accum is not None:
                kw["accum_out"] = accum
            return e.tensor_scalar(out=out, in0=in0, scalar1=s1, scalar2=s2, op0=op0, **kw)
        return K.op(eng, f, r=r, w=w)

    def tt(eng, out, in0, in1, op, r=(), w=()):
        return K.op(eng, lambda e: e.tensor_tensor(out=out, in0=in0, in1=in1, op=op), r=r, w=w)

    def stt(out, in0, scalar, in1, op0, op1, r=(), w=(), accum=None):
        def f(e):
            kw = {}
            if accum is not None:
                kw["accum_out"] = accum
            return e.scalar_tensor_tensor(out=out, in0=in0, scalar=scalar, in1=in1, op0=op0, op1=op1, **kw)
        return K.op("dve", f, r=r, w=w)

    def cp(eng, out, in_, r=(), w=()):
        if eng == "act":
            return K.op("act", lambda e: e.activation(out=out, in_=in_, func=AF.Copy), r=r, w=w)
        return K.op(eng, lambda e: e.tensor_copy(out, in_), r=r, w=w)

    def mset(eng, out, val, r=(), w=()):
        return K.op(eng, lambda e: e.memset(out, val), r=r, w=w)

    class B:
        def __init__(self, shape, dt, name=""):
            self.ap = AR.alloc(shape, dt)
            self.t = K.tok(name)

    def ring(n, shape, dt, name=""):
        return [B(shape, dt, f"{name}{i}") for i in range(n)]

    cst = B([NCST], F32, "cst")
    cm = B([448], F32, "cm")
    ident = B([128], BF16, "ident")
    identf = B([128], F32, "identf")
    wi_sb = B([NT, 8], F32, "wi")
    rt_gate = B([NT * 2], F32, "gate")
    rt_pos = B([NT * 2], I32, "pos")
    dma("sp", cst.ap, cst_d, w=[cst.t])
    dma("sp", cm.ap, cm_d, w=[cm.t])
    mset("pool", identf.ap, 0.0, w=[identf.t])
    K.op("pool", lambda e: e.affine_select(out=identf.ap, in_=identf.ap, pattern=[[-1, 128]], compare_op=ALU.not_equal,
                                           fill=1.0, base=0, channel_multiplier=1), r=[identf.t], w=[identf.t])
    cp("pool", ident.ap, identf.ap, r=[identf.t], w=[ident.t])

    zt = B([D], BF16, "zt")
    mset("pool", zt.ap, 0.0, w=[zt.t])

    def col(b, j, n=1):
        return b.ap[:, j:j + n]

    def sincos(ang, n, kk, sin_out, cos_out, r, w):
        tk = K.tok()
        ts("dve", kk, ang, 1.0 / (2 * PI), ALU.mult, MAGIC, ALU.add, r=r, w=[tk])
        ts("dve", kk, kk, MAGIC, ALU.subtract, r=[tk], w=[tk])
        stt(ang, kk, -C1, ang, ALU.mult, ALU.add, r=r + [tk], w=r)
        stt(ang, kk, -C2, ang, ALU.mult, ALU.add, r=r + [tk], w=r)
        ts("dve", ang, ang, PI, ALU.min, -PI, ALU.max, r=r, w=r)
        act(sin_out, ang, AF.Sin, r=r, w=w)
        ts("dve", kk, ang, -1.0, ALU.mult, r=r, w=[tk])
        tt("dve", kk, kk, ang, ALU.max, r=r + [tk], w=[tk])
        act(cos_out, kk, AF.Sin, bias=col(cst, C_HPI), scale=-1.0, r=[tk, cst.t], w=w)


    def finish():
        K.barrier()
        K.emit()
        P.__dict__.update(dict(K=K, AR=AR))
        return P

    m_0 = AR.mark()
    xnT = AR.alloc([8, L], BF16)
    xnT_t = [K.tok(f"xnT{i}") for i in range(NT)]
    m_p1 = AR.mark()

    xbuf = ring(3, [D], F32, "xb")
    junk = B([D], F32, "junk")
    g1b = B([D], F32, "g1b")
    xnb = ring(2, [D], BF16, "xnb")
    ms1 = B([NT], F32, "ms1")
    rs1 = B([NT], F32, "rs1")
    ms1_t = [K.tok() for _ in range(NT)]
    rs1_t = [K.tok() for _ in range(NT)]
    dma("sp", g1b.ap, g1_d.partition_broadcast(128), w=[g1b.t])

    def a_front(i):
        xb = xbuf[i % 3]
        dma("sp", xb.ap, x_d[i * 128:(i + 1) * 128, :], w=[xb.t])
        act(junk.ap, xb.ap, AF.Square, r=[xb.t], w=[junk.t, ms1_t[i]], accum=ms1.ap[:, i:i + 1])
        ts("pool", rs1.ap[:, i:i + 1], ms1.ap[:, i:i + 1], 1.0 / D, ALU.mult, EPS, ALU.add, r=[ms1_t[i]], w=[rs1_t[i]])
        tt("pool", rs1.ap[:, i:i + 1], rs1.ap[:, i:i + 1], col(cst, C_NH), ALU.pow, r=[rs1_t[i], cst.t], w=[rs1_t[i]])

    def a_back(i):
        xb = xbuf[i % 3]
        nb = xnb[i % 2]
        stt(nb.ap, xb.ap, rs1.ap[:, i:i + 1], g1b.ap, ALU.mult, ALU.mult, r=[xb.t, rs1_t[i], g1b.t], w=[nb.t])
        bk = 4 + (i % 2)
        for kc in range(8):
            tr(psb(bk)[:, kc * 128:(kc + 1) * 128], nb.ap[:, kc * 128:(kc + 1) * 128], ident.ap, r=[nb.t, ident.t], w=[pst[bk]])
        cp("act", xnT[:, :, i * 128:(i + 1) * 128], psb(bk).rearrange("p (a b) -> p a b", a=8), r=[pst[bk]], w=[xnT_t[i]])

    for i in range(NT + 1):
        if i < NT:
            a_front(i)
        if i >= 1:
            a_back(i - 1)
    K.barrier()
    AR.release(m_p1)

    wbuf = ring(3, [8, 512], BF16, "wb")
    wstg = ring(2, [4, 512], F32, "wstg")
    wstate = {"n": 0}

    def load_w(c0, ncols, ceng="pool"):
        b = wbuf[wstate["n"] % 3]
        wstate["n"] += 1
        for hf in range(2):
            sg = wstg[hf]
            dma("sp", sg.ap[:, :, 0:ncols], win_d[hf * 512:(hf + 1) * 512, c0:c0 + ncols].rearrange("(kc p) c -> p kc c", p=128), w=[sg.t])
            cp(ceng, b.ap[:, 4 * hf:4 * hf + 4, 0:ncols], sg.ap[:, :, 0:ncols], r=[sg.t], w=[b.t])
        return b

    bank_rr = {"n": 0}

    def next_bank():
        bk = bank_rr["n"] % 4
        bank_rr["n"] += 1
        return bk

    def fm_block(wb, j, c, bk):
        for kc in range(8):
            mm(ps[bk][:, :], wb.ap[:, kc, j * 128:(j + 1) * 128], xnT[:, kc, c * 512:(c + 1) * 512], kc == 0, kc == 7,
               r=[wb.t] + xnT_t[4 * c:4 * c + 4], w=[pst[bk]])

    def tm_block(wb, ncols, i, bk, col0=0):
        for kc in range(8):
            mm(ps[bk][:, 0:ncols], xnT[:, kc, i * 128:(i + 1) * 128], wb.ap[:, kc, col0:col0 + ncols], kc == 0, kc == 7,
               r=[wb.t, xnT_t[i]], w=[pst[bk]])

    m_b1 = AR.mark()
    cosT = B([L], F32, "cosT")
    sinT = B([L], F32, "sinT")
    posi = B([1024], I32, "posi")
    angw = B([1024], F32, "angw")
    kkw = B([1024], F32, "kkw")
    for cc in range(4):
        sl = slice(cc * 1024, (cc + 1) * 1024)
        dma("sp", posi.ap, posr_d[:, sl].partition_broadcast(128), w=[posi.t])
        cp("dve", angw.ap, posi.ap, r=[posi.t], w=[angw.t])
        ts("dve", angw.ap, angw.ap, col(cst, C_INV), ALU.mult, r=[angw.t, cst.t], w=[angw.t])
        sincos(angw.ap, 1024, kkw.ap, sinT.ap[:, sl], cosT.ap[:, sl], r=[angw.t], w=[sinT.t, cosT.t])
        ts("dve", sinT.ap[:, sl], sinT.ap[:, sl], col(cst, C_SGN), ALU.mult, r=[sinT.t, cst.t], w=[sinT.t])
    posc = B([NT], I32, "posc")
    poscf = B([NT], F32, "poscf")
    sinI = B([NT, 32], F32, "sinI")
    cosI = B([NT, 32], F32, "cosI")
    dma("sp", posc.ap, posc_d, w=[posc.t])
    cp("dve", poscf.ap, posc.ap, r=[posc.t], w=[poscf.t])
    angI = angw.ap.rearrange("p (a b) -> p a b", a=NT)
    tt("dve", angI, poscf.ap.unsqueeze(2).broadcast_to([128, NT, 32]),
       cst.ap[:, C_INVI:C_INVI + 32].unsqueeze(1).broadcast_to([128, NT, 32]), ALU.mult, r=[poscf.t, cst.t, angw.t], w=[angw.t])
    sincos(angw.ap, 1024, kkw.ap, sinI.ap.rearrange("p a b -> p (a b)"), cosI.ap.rearrange("p a b -> p (a b)"),
           r=[angw.t], w=[sinI.t, cosI.t])

    t1r = ring(2, [512], F32, "t1")
    t2r = ring(2, [512], F32, "t2")
    qor = ring(3, [512], BF16, "qo")
    n_rope = {"n": 0}

    def rope_fm(bk, c, dst):
        k_ = n_rope["n"]
        n_rope["n"] += 1
        t1 = t1r[k_ % 2]
        t2 = t2r[k_ % 2]
        qo = qor[k_ % 3]
        sl = slice(c * 512, (c + 1) * 512)
        tt("dve", t1.ap, ps[bk][:, :], cosT.ap[:, sl], ALU.mult, r=[pst[bk], cosT.t], w=[t1.t])
        tt("dve", t2.ap[0:64, :], ps[bk][64:128, :], sinT.ap[0:64, sl], ALU.mult, r=[pst[bk], sinT.t], w=[t2.t])
        tt("dve", t2.ap[64:128, :], ps[bk][0:64, :], sinT.ap[64:128, sl], ALU.mult, r=[pst[bk], sinT.t], w=[t2.t])
        tt("pool", qo.ap, t1.ap, t2.ap, ALU.add, r=[t1.t, t2.t], w=[qo.t])
        dma("sp", dst, qo.ap, r=[qo.t])

    qk_blocks = [(CQ, qT_s, 0), (CQ + 512, qT_s, 4), (CK, kT_s, 0), (CK + 512, kT_s, 4)]
    wnext = load_w(qk_blocks[0][0], 512)
    for bi, (c0, dst_s, h0) in enumerate(qk_blocks):
        wb = wnext
        wnext = load_w(qk_blocks[bi + 1][0], 512, "act") if bi + 1 < 4 else load_w(CV, 512, "act")
        for c in range(NCH):
            for j in range(4):
                bk = next_bank()
                fm_block(wb, j, c, bk)
                rope_fm(bk, c, dst_s[h0 + j, :, c * 512:(c + 1) * 512])
    wv0 = wnext
    wv1 = load_w(CV + 512, 512)
    wqi = load_w(CQI, 512)
    vt = ring(2, [8, 129], BF16, "vt")
    for b_ in vt:
        mset("pool", b_.ap[:, :, 128:129], 1.0, w=[b_.t])
    for i in range(NT):
        v_ = vt[i % 2]
        for half, wb in enumerate((wv0, wv1)):
            bk = next_bank()
            tm_block(wb, 512, i, bk)
            cp("act", v_.ap[:, half * 4:(half + 1) * 4, 0:128], ps[bk][:, :].rearrange("p (a b) -> p a b", a=4), r=[pst[bk]], w=[v_.t])
        dma("sp", v_s[i].rearrange("p (h d) -> p h d", h=8), v_.ap, r=[v_.t])
    wkw = load_w(CKI, 72)
    w0_pre = load_w(CSU, 512)
    ra = ring(3, [9, 32], F32, "ra")
    rb = ring(3, [9, 32], F32, "rb")
    qst = ring(3, [9, 64], F32, "qst")
    qr = ring(3, [640], BF16, "qr")
    qiT_c = ring(2, [5, 512], BF16, "qiTc")
    def ip_front(i):
        bq = next_bank()
        tm_block(wqi, 512, i, bq)
        bkw = next_bank()
        tm_block(wkw, 72, i, bkw)
        q_ = qr[i % 3]
        a_, b2_, st_ = ra[i % 3], rb[i % 3], qst[i % 3]
        cp("act", st_.ap[:, 0:8, :], ps[bq][:, :].rearrange("p (h d) -> p h d", h=8), r=[pst[bq]], w=[st_.t])
        cp("act", st_.ap[:, 8, :], ps[bkw][:, 0:64], r=[pst[bkw]], w=[st_.t])
        cosb = cosI.ap[:, i, :].unsqueeze(1).broadcast_to([128, 9, 32])
        sinb = sinI.ap[:, i, :].unsqueeze(1).broadcast_to([128, 9, 32])
        qo_ = q_.ap[:, 0:576].rearrange("p (h d) -> p h d", h=9)
        rd = [st_.t, cosI.t, sinI.t]
        tt("dve", a_.ap, st_.ap[:, :, 0:32], cosb, ALU.mult, r=rd, w=[a_.t])
        tt("dve", b2_.ap, st_.ap[:, :, 32:64], sinb, ALU.mult, r=rd, w=[b2_.t])
        tt("pool", qo_[:, :, 0:32], a_.ap, b2_.ap, ALU.subtract, r=[a_.t, b2_.t], w=[q_.t])
        tt("dve", a_.ap, st_.ap[:, :, 32:64], cosb, ALU.mult, r=rd, w=[a_.t])
        tt("dve", b2_.ap, st_.ap[:, :, 0:32], sinb, ALU.mult, r=rd, w=[b2_.t])
        tt("pool", qo_[:, :, 32:64], a_.ap, b2_.ap, ALU.add, r=[a_.t, b2_.t], w=[q_.t])
        cp("pool", q_.ap[:, 576:640], q_.ap[:, 512:576], r=[q_.t], w=[q_.t])
        cp("act", wi_sb.ap[:, i, :], ps[bkw][:, 64:72], r=[pst[bkw]], w=[wi_sb.t])

    def ip_back(i):
        c, tau = i // 4, i % 4
        q_ = qr[i % 3]
        bt = 4 + (i % 2)
        for jj in range(5):
            tr(psb(bt)[:, jj * 128:(jj + 1) * 128], q_.ap[:, jj * 128:(jj + 1) * 128], ident.ap, r=[q_.t, ident.t], w=[pst[bt]])
        qc = qiT_c[c % 2]
        cp("act", qc.ap[:, :, tau * 128:(tau + 1) * 128], psb(bt)[:, 0:640].rearrange("p (a b) -> p a b", a=5), r=[pst[bt]], w=[qc.t])
        if tau == 3:
            dma("sp", qiT_s[:, :, c * 512:(c + 1) * 512].rearrange("a p t -> p a t"), qc.ap, r=[qc.t])
    for i in range(NT + 1):
        if i < NT:
            ip_front(i)
        if i >= 1:
            ip_back(i - 1)
    K.barrier()
    AR.release(m_b1)
    if stop_after == "1b":
        return finish()
    m_b2 = AR.mark()
    sub_r = ring(3, [512], BF16, "sub")
    su_t = [K.tok(f"suT{c}") for c in range(NCH)]
    w0 = w0_pre
    w1 = load_w(CSU + 512, 512)
    w2 = load_w(CSV, 512)
    nsub = {"n": 0}

    def act_block_out(bk, func, dst, wtok=()):
        o = sub_r[nsub["n"] % 3]
        nsub["n"] += 1
        act(o.ap, ps[bk][:, :], func, r=[pst[bk]], w=[o.t])
        dma("sp", dst, o.ap, r=[o.t], w=list(wtok))

    for blk, wb in enumerate((w0, w1)):
        for c in range(NCH):
            for j in range(4):
                bk = next_bank()
                fm_block(wb, j, c, bk)
                f0 = (blk * 4 + j) * 128
                act_block_out(bk, AF.Gelu_apprx_tanh, suT_s[f0:f0 + 128, c * 512:(c + 1) * 512], wtok=[su_t[c]])
    w3 = load_w(CSV + 512, 512)
    swf = B([8, 128], F32, "swf")
    swb = B([8, 128], BF16, "swb")
    WtT = B([8, 128], BF16, "WtT")
    dma("sp", swf.ap, sw_d.rearrange("g t s -> t g s"), w=[swf.t])
    tt("dve", swf.ap, swf.ap, cm.ap[:, 256:384].unsqueeze(1).broadcast_to([128, 8, 128]), ALU.mult, r=[swf.t, cm.t], w=[swf.t])
    cp("dve", swb.ap, swf.ap, r=[swf.t], w=[swb.t])
    for g in range(8):
        tr(psb(4)[:, g * 128:(g + 1) * 128], swb.ap[:, g, :], ident.ap, r=[swb.t, ident.t], w=[pst[4]])
    cp("act", WtT.ap, psb(4).rearrange("p (a b) -> p a b", a=8), r=[pst[4]], w=[WtT.t])
    bf_ = B([8, 128], F32, "bf")
    bhl = B([8, 128], BF16, "bhl")
    bhf = B([8, 128], F32, "bhf")
    ones2 = B([128], BF16, "ones2")
    mset("pool", ones2.ap, 1.0, w=[ones2.t])
    mset("pool", bf_.ap, 0.0, w=[bf_.t])
    dma("sp", bf_.ap[0:1, :, :], sb_d.rearrange("(o g) t -> o g t", o=1), r=[bf_.t], w=[bf_.t])
    dma("sp", bf_.ap[1:2, :, :], sb_d.rearrange("(o g) t -> o g t", o=1), r=[bf_.t], w=[bf_.t])
    cp("dve", bhl.ap, bf_.ap, r=[bf_.t], w=[bhl.t])
    cp("dve", bhf.ap, bhl.ap, r=[bhl.t], w=[bhf.t])
    tt("dve", bhf.ap, bf_.ap, bhf.ap, ALU.subtract, r=[bf_.t, bhf.t], w=[bhf.t])
    cp("dve", bf_.ap, bhl.ap, r=[bhl.t, bf_.t], w=[bf_.t])
    sel = B([1], F32, "sel")
    mset("pool", sel.ap, 1.0, w=[sel.t])
    K.op("pool", lambda e: e.affine_select(out=sel.ap, in_=sel.ap, pattern=[[0, 1]], compare_op=ALU.is_equal,
                                           fill=0.0, base=0, channel_multiplier=1), r=[sel.t], w=[sel.t])
    tt("dve", bf_.ap, bf_.ap, bhf.ap, ALU.subtract, r=[bf_.t, bhf.t], w=[bf_.t])
    stt(bhf.ap, bf_.ap, sel.ap[:, 0:1], bhf.ap, ALU.mult, ALU.add, r=[bf_.t, sel.t, bhf.t], w=[bhf.t])
    cp("dve", bhl.ap, bhf.ap, r=[bhf.t], w=[bhl.t])

    gsb = B([D], F32, "gsb")
    dma("sp", gsb.ap, gs_d.partition_broadcast(128), w=[gsb.t])
    gv = ring(3, [D], F32, "gv")
    vln = ring(3, [D], BF16, "vln")
    bst = B([NT, 12], F32, "bst")
    mv = B([NT, 2], F32, "mv")
    rsd = B([NT], F32, "rsd")
    bst_t = [K.tok() for _ in range(NT)]
    mv_t = [K.tok() for _ in range(NT)]
    rsd_t = [K.tok() for _ in range(NT)]
    suc = ring(2, [8, 512], BF16, "suc")
    ysg = ring(2, [8, 512], BF16, "ysg")
    wg_pre = load_w(CGA, 512)
    for c in range(NCH):
        su_c = suc[c % 2]
        ys_c = ysg[c % 2]
        dma("sp", su_c.ap, suT_s[:, c * 512:(c + 1) * 512].rearrange("(g p) t -> p g t", p=128), r=[su_t[c]], w=[su_c.t])
        def sv_front(tau, c=c):
            i = 4 * c + tau
            g_ = gv[i % 3]
            vl = vln[i % 3]
            sb0 = 6
            for half, wb in enumerate((w2, w3)):
                bk = next_bank()
                tm_block(wb, 512, i, bk)
                act(g_.ap[:, half * 512:(half + 1) * 512], ps[bk][:, :], AF.Gelu_apprx_tanh, r=[pst[bk]], w=[g_.t])
            K.op("dve", lambda e, i=i, g_=g_: e.bn_stats(out=bst.ap[:, i, 0:6], in_=g_.ap[:, 0:512]), r=[g_.t], w=[bst_t[i]])
            K.op("dve", lambda e, i=i, g_=g_: e.bn_stats(out=bst.ap[:, i, 6:12], in_=g_.ap[:, 512:1024]), r=[g_.t], w=[bst_t[i]])
            K.op("dve", lambda e, i=i: e.bn_aggr(out=mv.ap[:, i, :], in_=bst.ap[:, i, :]), r=[bst_t[i]], w=[mv_t[i]])
            ts("pool", rsd.ap[:, i:i + 1], mv.ap[:, i, 1:2], EPS, ALU.add, r=[mv_t[i]], w=[rsd_t[i]])
            tt("pool", rsd.ap[:, i:i + 1], rsd.ap[:, i:i + 1], col(cst, C_NH), ALU.pow, r=[rsd_t[i], cst.t], w=[rsd_t[i]])
            ts("dve", g_.ap, g_.ap, mv.ap[:, i, 0:1], ALU.subtract, rsd.ap[:, i:i + 1], ALU.mult, r=[g_.t, mv_t[i], rsd_t[i]], w=[g_.t])
            tt("pool", vl.ap, g_.ap, gsb.ap, ALU.mult, r=[g_.t, gsb.t], w=[vl.t])

        def sv_back(tau, c=c, su_c=su_c, ys_c=ys_c):
            i = 4 * c + tau
            vl = vln[i % 3]
            sb0 = 6
            for g in range(8):
                bk = sb0 + g // 4
                o_ = ps[bk][:, (g % 4) * 128:(g % 4 + 1) * 128]
                mm(o_, vl.ap[:, g * 128:(g + 1) * 128], WtT.ap[:, g, :], True, False, r=[vl.t, WtT.t], w=[pst[bk]])
                mm(o_, ones2.ap[0:2, :], bhl.ap[0:2, g, :], False, True, r=[ones2.t, bhl.t], w=[pst[bk]])
            for b_ in range(2):
                tt("dve", ys_c.ap[:, 4 * b_:4 * b_ + 4, tau * 128:(tau + 1) * 128],
                   ps[sb0 + b_][:, :].rearrange("p (a b) -> p a b", a=4),
                   su_c.ap[:, 4 * b_:4 * b_ + 4, tau * 128:(tau + 1) * 128], ALU.mult, r=[pst[sb0 + b_], su_c.t], w=[ys_c.t])
        for tau in range(5):
            if tau < 4:
                sv_front(tau)
            if tau >= 1:
                sv_back(tau - 1)
        dma("sp", ysgT_s[:, c * 512:(c + 1) * 512].rearrange("(g p) t -> p g t", p=128), ys_c.ap, r=[ys_c.t])
    wg = wg_pre
    for blk in range(4):
        wb = wg
        if blk < 3:
            wg = load_w(CGA + (blk + 1) * 512, 512)
        for c in range(NCH):
            for j in range(4):
                bk = next_bank()
                fm_block(wb, j, c, bk)
                f0 = (blk * 4 + j) * 128
                act_block_out(bk, AF.Sigmoid, sgT_s[f0:f0 + 128, c * 512:(c + 1) * 512])
    K.barrier()
    AR.release(m_0)
    if stop_after == "1":
        return finish()
    SCALE = float(128 ** -0.5)
    vres = AR.alloc([NT, 8, 129], BF16)
    v_t = [K.tok(f"v{i}") for i in range(NT)]
    kiT2 = AR.alloc([L], BF16)
    ki_t = [K.tok(f"ki{i}") for i in range(4)]
    for q4 in range(4):
        dma("sp", kiT2[:, q4 * 1024:(q4 + 1) * 1024], qiT_s[4, :, q4 * 1024:(q4 + 1) * 1024], w=[ki_t[q4]])

    def load_v_all():
        for i in range(NT):
            dma("sp", vres[:, i, :, :], v_s[i].rearrange("p (h d) -> p h d", h=8), w=[v_t[i]])
    nrow_t = (NE * CAP + 128) // 128
    for z0 in range(0, nrow_t, 16):
        zn = min(16, nrow_t - z0)
        dma("sp", xs_s[z0 * 128:(z0 + zn) * 128, :].rearrange("(a p) d -> p a d", p=128),
            zt.ap.unsqueeze(1).broadcast_to([128, zn, D]), r=[zt.t])

    kTr = ring(2, [L], BF16, "kTr")
    qTc = B([8, 512], BF16, "qTc")
    qiTc = B([4, 512], BF16, "qiTc")
    S = B([L], F32, "S")
    maskb = B([L], BF16, "maskb")
    maskb2 = B([L], BF16, "maskb2")
    maskT = B([NT, 512], BF16, "maskT")
    rbuf = ring(3, [512], F32, "rb")
    ebuf = ring(3, [512], BF16, "eb")
    pbuf = ring(3, [512], BF16, "pb")
    S2 = B([L], F32, "S2")

    class _V:
        pass
    ytile = _V()
    ytile.ap = S2.ap[:, 0:2048].bitcast(BF16).rearrange("p (a b) -> p a b", a=4)
    ytile.t = S2.t
    yT = _V()
    yT.ap = S2.ap[:, 2048:4096].bitcast(BF16).rearrange("p (a b) -> p a b", a=8)
    yT.t = S2.t
    Sb = (S, S2)
    Mb2 = (maskb, maskb2)
    sst = []
    for q_ in range(2):
        sst.append({nm: B([sz], F32, f"{nm}{q_}") for nm, sz in (
            ("vmax", 1), ("vmin", 1), ("rngv", 1), ("hw", KI + 2), ("nhw", KI + 2), ("cand", 1), ("cnt", 1), ("msg", 1), ("thr", 1))})
    recr = ring(2, [4], F32, "rec")
    ctr = {"sc": 0, "lg": 0, "tr": 0, "e": 0}

    for c in range(NCH):
        csl = slice(c * 512, (c + 1) * 512)
        dma("sp", qTc.ap, qT_s[:, :, csl].rearrange("h p t -> p h t"), w=[qTc.t])
        dma("sp", qiTc.ap, qiT_s[0:4, :, csl].rearrange("a p t -> p a t"), w=[qiTc.t])
        if c == 0:
            load_v_all()
        def idx_scores(tau, Sx):
            i = 4 * c + tau
            n = 128 * (i + 1)
            nv = 128 * i
            for sc in range((n + 511) // 512):
                w_ = min(512, n - 512 * sc)
                sl = slice(sc * 512, sc * 512 + w_)
                for h in range(8):
                    bk = ctr["sc"] % 2
                    ctr["sc"] += 1
                    pr = slice(64 * (h % 2), 64 * (h % 2) + 64)
                    mm(ps[bk][:, 0:w_], qiTc.ap[pr, h // 2, tau * 128:(tau + 1) * 128], kiT2[pr, sl], True, True,
                       r=[qiTc.t, ki_t[sc // 2]], w=[pst[bk]])
                    rb = rbuf[ctr["sc"] % 3]
                    act(rb.ap[:, 0:w_], ps[bk][:, 0:w_], AF.Relu, r=[pst[bk]], w=[rb.t])
                    if h == 0:
                        ts("dve", Sx.ap[:, sl], rb.ap[:, 0:w_], wi_sb.ap[:, i, 0:1], ALU.mult, r=[rb.t, wi_sb.t], w=[Sx.t])
                    else:
                        stt(Sx.ap[:, sl], rb.ap[:, 0:w_], wi_sb.ap[:, i, h:h + 1], Sx.ap[:, sl], ALU.mult, ALU.add,
                            r=[rb.t, wi_sb.t, Sx.t], w=[Sx.t])
            tt("pool", Sx.ap[:, nv:n], Sx.ap[:, nv:n], cm.ap[:, 0:128], ALU.add, r=[Sx.t, cm.t], w=[Sx.t])

        def search_pre(tau, Sx, mb, st, on_act):
            i = 4 * c + tau
            n = 128 * (i + 1)
            nv = 128 * i
            if i < 2:
                return
            A_ = lambda nm: st[nm].ap
            T2 = lambda nm: st[nm].t
            K.op("dve", lambda e: e.tensor_reduce(out=A_("vmax"), in_=Sx.ap[:, 0:n], axis=AX.X, op=ALU.max), r=[Sx.t], w=[T2("vmax")])
            K.op("dve", lambda e: e.tensor_reduce(out=A_("vmin"), in_=Sx.ap[:, 0:256], axis=AX.X, op=ALU.min), r=[Sx.t], w=[T2("vmin")])
            tt("dve", A_("rngv"), A_("vmax"), A_("vmin"), ALU.subtract, r=[T2("vmax"), T2("vmin")], w=[T2("rngv")])
            ts("dve", A_("hw"), cst.ap[:, C_PW:C_PW + KI + 2], A_("rngv")[:, 0:1], ALU.mult, r=[cst.t, T2("rngv")], w=[T2("hw")])
            if not on_act:
                tt("dve", A_("cand"), A_("vmin"), A_("hw")[:, 0:1], ALU.add, r=[T2("vmin"), T2("hw")], w=[T2("cand")])
            else:
                ts("dve", A_("nhw"), A_("hw"), -1.0, ALU.mult, r=[T2("hw")], w=[T2("nhw")])
                stt(A_("cand"), A_("vmin"), -1.0, A_("hw")[:, 0:1], ALU.mult, ALU.subtract, r=[T2("vmin"), T2("hw")], w=[T2("cand")])

        def search(tau, Sx, mb, st, on_act):
            i = 4 * c + tau
            n = 128 * (i + 1)
            if i < 2:
                return col(cst, C_NTHR), cst.t
            A_ = lambda nm: st[nm].ap
            T2 = lambda nm: st[nm].t
            if not on_act:
                for k in range(KI):
                    ts("dve", mb.ap[:, 0:n], Sx.ap[:, 0:n], A_("cand")[:, 0:1], ALU.is_ge, None, ALU.add,
                       r=[Sx.t, T2("cand")], w=[mb.t, T2("cnt")], accum=A_("cnt"))
                    ts("dve", A_("msg"), A_("cnt"), 255.5, ALU.is_ge, 0.5, ALU.subtract, r=[T2("cnt")], w=[T2("msg")])
                    stt(A_("cand"), A_("msg"), A_("hw")[:, k:k + 1], A_("cand"), ALU.mult, ALU.add, r=[T2("msg"), T2("hw"), T2("cand")], w=[T2("cand")])
                tt("dve", A_("thr"), A_("cand"), A_("hw")[:, KI:KI + 1], ALU.subtract, r=[T2("cand"), T2("hw")], w=[T2("thr")])
            else:
                for k in range(KI):
                    act(mb.ap[:, 0:n], Sx.ap[:, 0:n], AF.Sign, bias=A_("cand")[:, 0:1], r=[Sx.t, T2("cand")], w=[mb.t, T2("cnt")], accum=A_("cnt"))
                    act(A_("msg"), A_("cnt"), AF.Sign, bias=col(cst, C_NB + i), r=[T2("cnt"), cst.t], w=[T2("msg")])
                    act(A_("cand"), A_("msg"), AF.Identity, bias=A_("cand")[:, 0:1], scale=A_("nhw")[:, k + 1:k + 2],
                        r=[T2("msg"), T2("nhw"), T2("cand")], w=[T2("cand")])
                act(A_("thr"), A_("cand"), AF.Identity, bias=A_("nhw")[:, KI:KI + 1], scale=-1.0, r=[T2("cand"), T2("nhw")], w=[T2("thr")])
            return A_("thr")[:, 0:1], T2("thr")

        def make_mask(tau, Sx, mb, thr_ap, thr_t):
            i = 4 * c + tau
            n = 128 * (i + 1)
            ts("dve", mb.ap[:, 0:n], Sx.ap[:, 0:n], thr_ap, ALU.is_ge, r=[Sx.t, thr_t], w=[mb.t])
            for j0 in range(0, i + 1, 8):
                nb = min(8, i + 1 - j0)
                bk = 2 + ctr["tr"] % 2
                ctr["tr"] += 1
                for jj in range(nb):
                    tr(psb(bk)[:, jj * 128:(jj + 1) * 128], mb.ap[:, (j0 + jj) * 128:(j0 + jj + 1) * 128], ident.ap,
                       r=[mb.t, ident.t], w=[pst[bk]])
                cp("act", maskT.ap[:, j0:j0 + nb, tau * 128:(tau + 1) * 128],
                   psb(bk)[:, 0:nb * 128].rearrange("p (a b) -> p a b", a=nb), r=[pst[bk]], w=[maskT.t])

        for pair in range(2):
            t0, t1_ = 2 * pair, 2 * pair + 1
            idx_scores(t0, Sb[0])
            idx_scores(t1_, Sb[1])
            search_pre(t0, Sb[0], Mb2[0], sst[0], False)
            search_pre(t1_, Sb[1], Mb2[1], sst[1], True)
            th1 = search(t1_, Sb[1], Mb2[1], sst[1], True)
            th0 = search(t0, Sb[0], Mb2[0], sst[0], False)
            make_mask(t0, Sb[0], Mb2[0], *th0)
            make_mask(t1_, Sb[1], Mb2[1], *th1)
        nj = 4 * c + 4
        steps = [(h, j) for h in range(8) for j in range(nj)]
        LB = (0, 1, 2, 3)

        def emit_qk(k):
            h, j = steps[k]
            kt = kTr[h % 2]
            if j == 0:
                dma("sp", kt.ap[:, 0:nj * 128], kT_s[h, :, 0:nj * 128], w=[kt.t])
            r0 = max(0, j - 4 * c)
            N = 512 - 128 * r0
            bk = LB[k % 4]
            mm(ps[bk][:, 0:N], kt.ap[:, j * 128:(j + 1) * 128], qTc.ap[:, h, r0 * 128:512], True, True,
               r=[kt.t, qTc.t], w=[pst[bk]])

        emit_qk(0)
        emit_qk(1)
        emit_qk(2)
        for k, (h, j) in enumerate(steps):
            if k + 3 < len(steps):
                emit_qk(k + 3)
            accA = 4 + 2 * (h % 2)
            accB = accA + 1
            r0 = max(0, j - 4 * c)
            N = 512 - 128 * r0
            bk = LB[k % 4]
            e_ = ebuf[k % 3]
            p_ = pbuf[k % 3]
            act(e_.ap[:, 0:N], ps[bk][:, 0:N], AF.Exp, scale=SCALE, r=[pst[bk]], w=[e_.t])
            tt("dve", p_.ap[:, 0:N], e_.ap[:, 0:N], maskT.ap[:, j, r0 * 128:512], ALU.mult, r=[e_.t, maskT.t], w=[p_.t])
            for tau in range(r0, 4):
                if tau < 3:
                    o_, ob = ps[accA][:, tau * 129:(tau + 1) * 129], accA
                else:
                    o_, ob = ps[accB][:, 0:129], accB
                first = (j == 0 and tau in (0, 3))
                mm(o_, p_.ap[:, (tau - r0) * 128:(tau - r0 + 1) * 128], vres[:, j, h, :], first, j == nj - 1,
                   r=[p_.t, v_t[j]], w=[pst[ob]], sgc=True)
            if j == nj - 1:
                rc = recr[h % 2]
                K.op("dve", lambda e, rc=rc, accA=accA: e.reciprocal(
                    out=rc.ap[:, 0:3], in_=ps[accA][:, 0:387].rearrange("p (a b) -> p a b", b=129)[:, :, 128]), r=[pst[accA]], w=[rc.t])
                K.op("dve", lambda e, rc=rc, accB=accB: e.reciprocal(out=rc.ap[:, 3:4], in_=ps[accB][:, 128:129]), r=[pst[accB]], w=[rc.t])
                for tau in range(4):
                    src, sb_ = (ps[accA][:, tau * 129:tau * 129 + 128], accA) if tau < 3 else (ps[accB][:, 0:128], accB)
                    ts("dve", ytile.ap[:, tau, h * 128:(h + 1) * 128], src, rc.ap[:, tau:tau + 1], ALU.mult, r=[pst[sb_], rc.t], w=[ytile.t])
        for tau in range(4):
            bk = 2 + ctr["tr"] % 2
            ctr["tr"] += 1
            for kc in range(8):
                tr(psb(bk)[:, kc * 128:(kc + 1) * 128], ytile.ap[:, tau, kc * 128:(kc + 1) * 128], ident.ap, r=[ytile.t, ident.t], w=[pst[bk]])
            cp("act", yT.ap[:, :, tau * 128:(tau + 1) * 128], psb(bk).rearrange("p (a b) -> p a b", a=8), r=[pst[bk]], w=[yT.t])
        dma("sp", yatT_s[:, csl].rearrange("(g p) t -> p g t", p=128), yT.ap, r=[yT.t])
    K.barrier()
    AR.release(m_0)
    if stop_after == "2":
        return finish()
    m_3 = AR.mark()
    Wa_sb = B([8, D], BF16, "Wa")
    Wb_sb = B([8, D], BF16, "Wb")
    Wo_sb = B([8, D], BF16, "Wo")
    w3stg = ring(2, [2, D], F32, "w3stg")
    for wi3, (wsb, wd) in enumerate(((Wa_sb, wa_d), (Wb_sb, wb_d), (Wo_sb, wo_d))):
        for q4 in range(4):
            sg = w3stg[(wi3 * 4 + q4) % 2]
            dma("sp", sg.ap, wd[q4 * 256:(q4 + 1) * 256, :].rearrange("(kc p) n -> p kc n", p=128), w=[sg.t])
            cp("dve" if q4 % 2 else "act", wsb.ap[:, 2 * q4:2 * q4 + 2, :], sg.ap, r=[sg.t], w=[wsb.t])
    g2b = B([D], F32, "g2b")
    dma("sp", g2b.ap, g2_d.partition_broadcast(128), w=[g2b.t])
    wr = B([8, 36], F32, "wr")
    br = B([36], F32, "br")
    dma("sp", wr.ap[:, :, 0:4], wrg_d.rearrange("(kc p) n -> p kc n", p=128), w=[wr.t])
    dma("sp", wr.ap[:, :, 4:36], wre_d.rearrange("(kc p) n -> p kc n", p=128), w=[wr.t])
    dma("sp", br.ap[:, 0:4], brg_d.partition_broadcast(128), w=[br.t])
    dma("sp", br.ap[:, 4:36], bre_d.partition_broadcast(128), w=[br.t])
    trib = B([128], BF16, "trib")
    onesb = B([128], BF16, "onesb")
    cp("pool", trib.ap, cm.ap[:, 128:256], r=[cm.t], w=[trib.t])
    mset("pool", onesb.ap, 1.0, w=[onesb.t])
    base = B([32], F32, "base")
    capb = B([32], F32, "capb")
    ts("pool", base.ap, cm.ap[:, 384:416], -1.0, ALU.add, r=[cm.t], w=[base.t])
    ts("pool", capb.ap, cm.ap[:, 384:416], float(CAP) - 0.5, ALU.add, r=[cm.t], w=[capb.t])
    inr = [ring(2, [8, 512], BF16, nm) for nm in ("ysgc", "yatc", "sgac", "sgbc")]
    mT = B([8, 512], BF16, "mT")
    t1p = ring(2, [512], F32, "t1p")
    t2p = ring(2, [512], F32, "t2p")
    xtr = ring(2, [D], F32, "xt")
    h2r = ring(2, [D], F32, "h2t")
    ms2 = B([NT], F32, "ms2")
    rs2 = B([NT], F32, "rs2")
    xn2f = B([D], F32, "xn2f")
    xn2b = ring(6, [D], BF16, "xn2b")
    xn2T = B([8, 128], F32, "xn2T")
    lgt4 = B([4, 36], F32, "lgt4")
    rb_ = {nm: B(sz, F32, nm) for nm, sz in (
        ("gmax", [4]), ("ohg", [4, 4]), ("dgl", [4, 4]), ("exg", [4, 4]), ("se", [4]), ("gp", [4]), ("prod", [4, 4, 8]), ("esel", [4, 8]),
        ("m1", [4]), ("oh1", [4, 8]), ("es2", [4, 8]), ("m2", [4]), ("oh2", [4, 8]), ("dlt", [4]), ("ex", [4]), ("den", [4]), ("p1", [4]),
        ("p2", [4]), ("gk", [2, 4]), ("M1", [4, 32]), ("M2", [4, 32]), ("posf", [4, 32]), ("okf", [4, 32]), ("pr32", [4, 32]),
        ("pk", [2, 4]), ("ok", [2, 4]))}
    Mb4 = B([4, 32], BF16, "Mb4")
    sm = {nm: B([sz], F32, nm) for nm, sz in (
        ("gmax", 1), ("negg", 1), ("ohg", 4), ("j4", 4), ("se", 1), ("gp", 1), ("esel", 8), ("m1", 1), ("oh1", 8), ("es2", 8),
        ("m2", 1), ("oh2", 8), ("dlt", 1), ("ex", 1), ("den", 1), ("p1", 1), ("p2", 1), ("M1", 32), ("M2", 32), ("posf", 32),
        ("okf", 32), ("j32", 32), ("pk", 2), ("ok", 2), ("gk", 2))}
    Mb = B([32], BF16, "Mb")

    def S_(nm):
        return sm[nm].ap

    def T_(nm):
        return sm[nm].t

    def load_chunk3(c):
        csl = slice(c * 512, (c + 1) * 512)
        ysg_c, yat_c, sga_c, sgb_c = [rg[c % 2] for rg in inr]
        dma("sp", ysg_c.ap, ysgT_s[:, csl].rearrange("(g p) t -> p g t", p=128), w=[ysg_c.t])
        dma("sp", yat_c.ap, yatT_s[:, csl].rearrange("(g p) t -> p g t", p=128), w=[yat_c.t])
        dma("sp", sga_c.ap, sgT_s[0:D, csl].rearrange("(g p) t -> p g t", p=128), w=[sga_c.t])
        dma("sp", sgb_c.ap, sgT_s[D:2 * D, csl].rearrange("(g p) t -> p g t", p=128), w=[sgb_c.t])

    load_chunk3(0)
    for c in range(NCH):
        csl = slice(c * 512, (c + 1) * 512)
        ysg_c, yat_c, sga_c, sgb_c = [rg[c % 2] for rg in inr]
        if c + 1 < NCH:
            load_chunk3(c + 1)
        for nb in range(8):
            bA = next_bank()
            for kc in range(8):
                mm(ps[bA][:, :], Wa_sb.ap[:, kc, nb * 128:(nb + 1) * 128], ysg_c.ap[:, kc, :], kc == 0, kc == 7, r=[Wa_sb.t, ysg_c.t], w=[pst[bA]])
            bB = next_bank()
            for kc in range(8):
                mm(ps[bB][:, :], Wb_sb.ap[:, kc, nb * 128:(nb + 1) * 128], yat_c.ap[:, kc, :], kc == 0, kc == 7, r=[Wb_sb.t, yat_c.t], w=[pst[bB]])
            t1, t2 = t1p[nb % 2], t2p[nb % 2]
            tt("dve", t1.ap, ps[bA][:, :], sga_c.ap[:, nb, :], ALU.mult, r=[pst[bA], sga_c.t], w=[t1.t])
            tt("dve", t2.ap, ps[bB][:, :], sgb_c.ap[:, nb, :], ALU.mult, r=[pst[bB], sgb_c.t], w=[t2.t])
            tt("pool", mT.ap[:, nb, :], t1.ap, t2.ap, ALU.add, r=[t1.t, t2.t], w=[mT.t])
        for tau in range(4):
            i = 4 * c + tau
            xt, h2t, xb2 = xtr[i % 2], h2r[i % 2], xn2b[i % 6]
            dma("sp", xt.ap, x_d[i * 128:(i + 1) * 128, :], w=[xt.t])
            for half in range(2):
                hs = slice(half * 512, (half + 1) * 512)
                bk = next_bank()
                for kc in range(8):
                    mm(ps[bk][:, :], mT.ap[:, kc, tau * 128:(tau + 1) * 128], Wo_sb.ap[:, kc, hs], kc == 0, kc == 7, r=[mT.t, Wo_sb.t], w=[pst[bk]])
                tt("dve", h2t.ap[:, hs], ps[bk][:, :], xt.ap[:, hs], ALU.add, r=[pst[bk], xt.t], w=[h2t.t])
            dma("sp", h2_s[i * 128:(i + 1) * 128, :], h2t.ap, r=[h2t.t])
            stt(xn2f.ap, h2t.ap, 1.0 / D, h2t.ap, ALU.mult, ALU.mult, r=[h2t.t], w=[xn2f.t, ms2.t], accum=ms2.ap[:, i:i + 1])
            ts("pool", rs2.ap[:, i:i + 1], ms2.ap[:, i:i + 1], EPS, ALU.add, r=[ms2.t], w=[rs2.t])
            tt("pool", rs2.ap[:, i:i + 1], rs2.ap[:, i:i + 1], col(cst, C_NH), ALU.pow, r=[rs2.t, cst.t], w=[rs2.t])
            stt(xn2f.ap, h2t.ap, rs2.ap[:, i:i + 1], g2b.ap, ALU.mult, ALU.mult, r=[h2t.t, rs2.t, g2b.t], w=[xn2f.t])
            cp("pool", xb2.ap, xn2f.ap, r=[xn2f.t], w=[xb2.t])
            for q4 in range(2):
                bt = 4 + q4
                for jj in range(4):
                    kc = q4 * 4 + jj
                    tr(ps[bt][:, jj * 128:(jj + 1) * 128], xn2f.ap[:, kc * 128:(kc + 1) * 128], identf.ap, r=[xn2f.t, identf.t], w=[pst[bt]])
                cp("act", xn2T.ap[:, q4 * 4:(q4 + 1) * 4, :], ps[bt][:, :].rearrange("p (a b) -> p a b", a=4), r=[pst[bt]], w=[xn2T.t])
            for kc in range(8):
                mm(ps[6][:, 0:36], xn2T.ap[:, kc, :], wr.ap[:, kc, :], kc == 0, kc == 7, r=[xn2T.t, wr.t], w=[pst[6]])
            tt("dve", lgt4.ap[:, tau, :], ps[6][:, 0:36], br.ap, ALU.add, r=[pst[6], br.t], w=[lgt4.t])

        gl4 = lgt4.ap[:, :, 0:4]
        el4 = lgt4.ap[:, :, 4:36].rearrange("p t (g j) -> p t g j", g=4)
        R_ = lambda nm: rb_[nm].ap
        Q_ = lambda nm: rb_[nm].t
        b3 = lambda ap, shp: ap.unsqueeze(2).broadcast_to(shp)
        K.op("dve", lambda e: e.tensor_reduce(out=R_("gmax"), in_=gl4, axis=AX.X, op=ALU.max), r=[lgt4.t], w=[Q_("gmax")])
        tt("dve", R_("ohg"), gl4, b3(R_("gmax"), [128, 4, 4]), ALU.is_ge, r=[lgt4.t, Q_("gmax")], w=[Q_("ohg")])
        tt("dve", R_("dgl"), gl4, b3(R_("gmax"), [128, 4, 4]), ALU.subtract, r=[lgt4.t, Q_("gmax")], w=[Q_("dgl")])
        act(R_("exg"), R_("dgl"), AF.Exp, r=[Q_("dgl")], w=[Q_("exg")])
        K.op("dve", lambda e: e.tensor_reduce(out=R_("se"), in_=R_("exg"), axis=AX.X, op=ALU.add), r=[Q_("exg")], w=[Q_("se")])
        K.op("dve", lambda e: e.reciprocal(out=R_("gp"), in_=R_("se")), r=[Q_("se")], w=[Q_("gp")])
        tt("dve", R_("prod"), el4, R_("ohg").unsqueeze(3).broadcast_to([128, 4, 4, 8]), ALU.mult, r=[lgt4.t, Q_("ohg")], w=[Q_("prod")])
        tt("dve", R_("esel"), R_("prod")[:, :, 0, :], R_("prod")[:, :, 1, :], ALU.add, r=[Q_("prod")], w=[Q_("esel")])
        tt("dve", R_("esel"), R_("esel"), R_("prod")[:, :, 2, :], ALU.add, r=[Q_("prod"), Q_("esel")], w=[Q_("esel")])
        tt("dve", R_("esel"), R_("esel"), R_("prod")[:, :, 3, :], ALU.add, r=[Q_("prod"), Q_("esel")], w=[Q_("esel")])
        K.op("dve", lambda e: e.tensor_reduce(out=R_("m1"), in_=R_("esel"), axis=AX.X, op=ALU.max), r=[Q_("esel")], w=[Q_("m1")])
        tt("dve", R_("oh1"), R_("esel"), b3(R_("m1"), [128, 4, 8]), ALU.is_ge, r=[Q_("esel"), Q_("m1")], w=[Q_("oh1")])
        stt(R_("es2"), R_("oh1"), NEG, R_("esel"), ALU.mult, ALU.add, r=[Q_("oh1"), Q_("esel")], w=[Q_("es2")])
        K.op("dve", lambda e: e.tensor_reduce(out=R_("m2"), in_=R_("es2"), axis=AX.X, op=ALU.max), r=[Q_("es2")], w=[Q_("m2")])
        tt("dve", R_("oh2"), R_("es2"), b3(R_("m2"), [128, 4, 8]), ALU.is_ge, r=[Q_("es2"), Q_("m2")], w=[Q_("oh2")])
        tt("dve", R_("dlt"), R_("m2"), R_("m1"), ALU.subtract, r=[Q_("m1"), Q_("m2")], w=[Q_("dlt")])
        act(R_("ex"), R_("dlt"), AF.Exp, r=[Q_("dlt")], w=[Q_("ex")])
        ts("dve", R_("den"), R_("ex"), 1.0, ALU.add, r=[Q_("ex")], w=[Q_("den")])
        K.op("dve", lambda e: e.reciprocal(out=R_("p1"), in_=R_("den")), r=[Q_("den")], w=[Q_("p1")])
        tt("dve", R_("p2"), R_("ex"), R_("p1"), ALU.mult, r=[Q_("ex"), Q_("p1")], w=[Q_("p2")])
        tt("dve", R_("gk")[:, 0, :], R_("p1"), R_("gp"), ALU.mult, r=[Q_("p1"), Q_("gp")], w=[Q_("gk")])
        tt("dve", R_("gk")[:, 1, :], R_("p2"), R_("gp"), ALU.mult, r=[Q_("p2"), Q_("gp")], w=[Q_("gk")])
        ohg4 = R_("ohg").unsqueeze(3).broadcast_to([128, 4, 4, 8])
        for nm, oh in (("M1", "oh1"), ("M2", "oh2")):
            tt("dve", R_(nm).rearrange("p t (g j) -> p t g j", g=4), ohg4, R_(oh).unsqueeze(2).broadcast_to([128, 4, 4, 8]), ALU.mult,
               r=[Q_("ohg"), Q_(oh)], w=[Q_(nm)])
        tt("dve", Mb4.ap, R_("M1"), R_("M2"), ALU.add, r=[Q_("M1"), Q_("M2")], w=[Mb4.t])
        for tau in range(4):
            o_ = ps[7][:, tau * 32:(tau + 1) * 32]
            mm(o_, trib.ap, Mb4.ap[:, tau, :], True, tau == 0, r=[trib.t, Mb4.t], w=[pst[7]], sgc=True)
            for tp_ in range(tau):
                mm(o_, onesb.ap, Mb4.ap[:, tp_, :], False, tp_ == tau - 1, r=[onesb.t, Mb4.t], w=[pst[7]], sgc=True)
        for tau in range(4):
            mm(ps[7][:, 128:160], onesb.ap, Mb4.ap[:, tau, :], False, tau == 3, r=[onesb.t, Mb4.t], w=[pst[7]], sgc=True)
        tt("dve", R_("posf"), ps[7][:, 0:128].rearrange("p (t e) -> p t e", t=4), base.ap.unsqueeze(1).broadcast_to([128, 4, 32]), ALU.add,
           r=[pst[7], base.t], w=[Q_("posf")])
        tt("dve", base.ap, ps[7][:, 128:160], base.ap, ALU.add, r=[pst[7], base.t], w=[base.t])
        tt("dve", R_("okf"), R_("posf"), capb.ap.unsqueeze(1).broadcast_to([128, 4, 32]), ALU.is_lt, r=[Q_("posf"), capb.t], w=[Q_("okf")])
        for k_, nm in enumerate(("M1", "M2")):
            tt("dve", R_("pr32"), R_(nm), R_("posf"), ALU.mult, r=[Q_(nm), Q_("posf")], w=[Q_("pr32")])
            K.op("dve", lambda e, k_=k_: e.tensor_reduce(out=R_("pk")[:, k_, :], in_=R_("pr32"), axis=AX.X, op=ALU.add), r=[Q_("pr32")], w=[Q_("pk")])
            tt("dve", R_("pr32"), R_(nm), R_("okf"), ALU.mult, r=[Q_(nm), Q_("okf")], w=[Q_("pr32")])
            K.op("dve", lambda e, k_=k_: e.tensor_reduce(out=R_("ok")[:, k_, :], in_=R_("pr32"), axis=AX.X, op=ALU.add), r=[Q_("pr32")], w=[Q_("ok")])
        ts("dve", R_("pk"), R_("pk"), col(cst, C_DUM), ALU.subtract, r=[Q_("pk"), cst.t], w=[Q_("pk")])
        tt("dve", R_("pk"), R_("pk"), R_("ok"), ALU.mult, r=[Q_("pk"), Q_("ok")], w=[Q_("pk")])
        ts("dve", R_("pk"), R_("pk"), col(cst, C_DUM), ALU.add, r=[Q_("pk"), cst.t], w=[Q_("pk")])
        cp("dve", rt_pos.ap[:, 8 * c:8 * c + 8].rearrange("p (t k) -> p k t", k=2), R_("pk"), r=[Q_("pk")], w=[rt_pos.t])
        tt("dve", rt_gate.ap[:, 8 * c:8 * c + 8].rearrange("p (t k) -> p k t", k=2), R_("gk"), R_("ok"), ALU.mult, r=[Q_("gk"), Q_("ok")], w=[rt_gate.t])
        for tau in range(4):
            i = 4 * c + tau
            xb2 = xn2b[i % 6]
            for k_ in range(2):
                K.op("pool", lambda e, i=i, k_=k_, xb2=xb2: e.indirect_dma_start(
                    out=xs_s[:, :], out_offset=bass.IndirectOffsetOnAxis(ap=rt_pos.ap[:, 2 * i + k_:2 * i + k_ + 1], axis=0),
                    in_=xb2.ap, in_offset=None), r=[xb2.t, rt_pos.t], dma=True)
    K.barrier()
    AR.release(m_3)
    if stop_after == "3":
        return finish()
    m_4 = AR.mark()
    wi_r = ring(2, [8, 512], BF16, "wie")
    wo_r = ring(3, [2, D], BF16, "woe")
    wis_r = ring(2, [8, 512], F32, "wis")
    wos_r = ring(2, [2, D], F32, "wos")
    xs_r = ring(2, [4, D], BF16, "xs")
    xsT_r = ring(2, [8, 512], BF16, "xsT")
    sg_r = ring(2, [512], F32, "sg")
    aT_r = ring(2, [2, 512], BF16, "aT")
    ys_r = ring(3, [D], BF16, "ysb")
    nys = {"n": 0}
    mset("pool", ys_r[2].ap, 0.0, w=[ys_r[2].t])
    dma("sp", ys_s[NE * CAP:NE * CAP + 128, :], ys_r[2].ap, r=[ys_r[2].t])
    def load_expert(e_i):
        wi_, wo_, xs_ = wi_r[e_i % 2], wo_r[e_i % 3], xs_r[e_i % 2]
        sgi, sgo = wis_r[e_i % 2], wos_r[e_i % 2]
        dma("sp", xs_.ap, xs_s[e_i * CAP:(e_i + 1) * CAP, :].rearrange("(st p) d -> p st d", p=128), w=[xs_.t])
        dma("sp", sgi.ap, wei_d[e_i].rearrange("(kc p) f -> p kc f", p=128), w=[sgi.t])
        dma("sp", sgo.ap, weo_d[e_i].rearrange("(fc p) n -> p fc n", p=128), w=[sgo.t])

    def cast_expert(e_i):
        wi_, wo_ = wi_r[e_i % 2], wo_r[e_i % 3]
        sgi, sgo = wis_r[e_i % 2], wos_r[e_i % 2]
        cp("pool", wi_.ap[:, 0:3, :], sgi.ap[:, 0:3, :], r=[sgi.t], w=[wi_.t])
        cp("dve", wi_.ap[:, 3:8, :], sgi.ap[:, 3:8, :], r=[sgi.t], w=[wi_.t])
        cp("act", wo_.ap, sgo.ap, r=[sgo.t], w=[wo_.t])

    def stage_T(e_i):
        xs_, xsT_ = xs_r[e_i % 2], xsT_r[e_i % 2]
        for st in range(4):
            bk = 4 + st % 2
            for kc in range(8):
                tr(psb(bk)[:, kc * 128:(kc + 1) * 128], xs_.ap[:, st, kc * 128:(kc + 1) * 128], ident.ap, r=[xs_.t, ident.t], w=[pst[bk]])
            cp("act" if st % 2 else "dve", xsT_.ap[:, :, st * 128:(st + 1) * 128], psb(bk).rearrange("p (a b) -> p a b", a=8), r=[pst[bk]], w=[xsT_.t])

    def stage_H(e_i):
        wi_, xsT_, aT_ = wi_r[e_i % 2], xsT_r[e_i % 2], aT_r[e_i % 2]
        for p_ in range(2):
            bG = next_bank()
            for kc in range(8):
                mm(ps[bG][:, :], wi_.ap[:, kc, p_ * 128:(p_ + 1) * 128], xsT_.ap[:, kc, :], kc == 0, kc == 7, r=[wi_.t, xsT_.t], w=[pst[bG]])
            bU = next_bank()
            for kc in range(8):
                mm(ps[bU][:, :], wi_.ap[:, kc, 256 + p_ * 128:256 + (p_ + 1) * 128], xsT_.ap[:, kc, :], kc == 0, kc == 7, r=[wi_.t, xsT_.t], w=[pst[bU]])
            sg_ = sg_r[p_]
            act(sg_.ap, ps[bG][:, :], AF.Silu, r=[pst[bG]], w=[sg_.t])
            tt("dve", aT_.ap[:, p_, :], sg_.ap, ps[bU][:, :], ALU.mult, r=[sg_.t, pst[bU]], w=[aT_.t])

    def stage_Y(e_i):
        wo_, aT_ = wo_r[e_i % 3], aT_r[e_i % 2]
        for st in range(4):
            yb = ys_r[nys["n"] % 3]
            nys["n"] += 1
            for half in range(2):
                hs = slice(half * 512, (half + 1) * 512)
                bk = next_bank()
                for fc in range(2):
                    mm(ps[bk][:, :], aT_.ap[:, fc, st * 128:(st + 1) * 128], wo_.ap[:, fc, hs], fc == 0, fc == 1, r=[aT_.t, wo_.t], w=[pst[bk]])
                cp("act" if half else "dve", yb.ap[:, hs], ps[bk][:, :], r=[pst[bk]], w=[yb.t])
            r0_ = e_i * CAP + st * 128
            dma("sp", ys_s[r0_:r0_ + 128, :], yb.ap, r=[yb.t])

    load_expert(0)
    load_expert(1)
    cast_expert(0)
    cast_expert(1)
    stage_T(0)
    stage_T(1)
    stage_H(0)
    for e_i in range(NE):
        if e_i + 2 < NE:
            load_expert(e_i + 2)
            stage_T(e_i + 2)
        if e_i + 1 < NE:
            stage_H(e_i + 1)
        stage_Y(e_i)
        if e_i + 2 < NE:
            cast_expert(e_i + 2)
    K.barrier()
    AR.release(m_4)
    gfb = B([D], F32, "gfb")
    dma("sp", gfb.ap, gf_d.partition_broadcast(128), w=[gfb.t])
    Y0r = ring(3, [D], BF16, "Y0")
    Y1r = ring(3, [D], BF16, "Y1")
    h2l = ring(3, [D], F32, "h2l")
    h3r = ring(3, [D], F32, "h3")
    outr = ring(2, [D], F32, "outb")
    junk4 = B([D], F32, "junk4")
    ms3 = B([NT], F32, "ms3")
    rs3 = B([NT], F32, "rs3")
    for b_ in Y0r + Y1r:
        mset("pool", b_.ap, 0.0, w=[b_.t])
    ms3_t = [K.tok() for _ in range(NT)]
    rs3_t = [K.tok() for _ in range(NT)]

    def fin_front(i):
        Y0, Y1, h2_, h3 = Y0r[i % 3], Y1r[i % 3], h2l[i % 3], h3r[i % 3]
        for k_, Y in enumerate((Y0, Y1)):
            K.op("pool", lambda e, i=i, k_=k_, Y=Y: e.indirect_dma_start(
                out=Y.ap, out_offset=None, in_=ys_s[:, :], in_offset=bass.IndirectOffsetOnAxis(ap=rt_pos.ap[:, 2 * i + k_:2 * i + k_ + 1], axis=0)),
                r=[rt_pos.t], w=[Y.t], dma=True)
        dma("sp", h2_.ap, h2_s[i * 128:(i + 1) * 128, :], w=[h2_.t])
        stt(h3.ap, Y0.ap, rt_gate.ap[:, 2 * i:2 * i + 1], h2_.ap, ALU.mult, ALU.add, r=[Y0.t, rt_gate.t, h2_.t], w=[h3.t])
        stt(h3.ap, Y1.ap, rt_gate.ap[:, 2 * i + 1:2 * i + 2], h3.ap, ALU.mult, ALU.add, r=[Y1.t, rt_gate.t, h3.t], w=[h3.t])
        stt(junk4.ap, h3.ap, 1.0 / D, h3.ap, ALU.mult, ALU.mult, r=[h3.t], w=[junk4.t, ms3_t[i]], accum=ms3.ap[:, i:i + 1])
        ts("pool", rs3.ap[:, i:i + 1], ms3.ap[:, i:i + 1], EPS, ALU.add, r=[ms3_t[i]], w=[rs3_t[i]])
        tt("pool", rs3.ap[:, i:i + 1], rs3.ap[:, i:i + 1], col(cst, C_NH), ALU.pow, r=[rs3_t[i], cst.t], w=[rs3_t[i]])

    def fin_back(i):
        h3, ob = h3r[i % 3], outr[i % 2]
        stt(ob.ap, h3.ap, rs3.ap[:, i:i + 1], gfb.ap, ALU.mult, ALU.mult, r=[h3.t, rs3_t[i], gfb.t], w=[ob.t])
        dma("sp", out_d[i * 128:(i + 1) * 128, :], ob.ap, r=[ob.t])

    for i in range(NT + 1):
        if i < NT:
            fin_front(i)
        if i >= 1:
            fin_back(i - 1)
    return finish()


def make_in_maps(inp, cores):
    c, m = host_consts()
    maps = []
    for b in cores:
        pos = np.ascontiguousarray(inp["positions"][b]).astype(np.int32)
        maps.append({
            "x": np.ascontiguousarray(inp["x"][b], dtype=np.float32),
            "pos_row": pos.reshape(1, L),
            "pos_col": np.ascontiguousarray(pos.reshape(NT, 128).T),
            "norm1_g": np.ascontiguousarray(inp["norm1_g"][0]).reshape(1, D),
            "w_in": np.ascontiguousarray(inp["w_in"][0]),
            "sgu_norm_g": np.ascontiguousarray(inp["sgu_norm_g"][0]).reshape(1, D),
            "sgu_w": np.ascontiguousarray(inp["sgu_w"][0]),
            "sgu_b": np.ascontiguousarray(inp["sgu_b"][0]),
            "w_branch_a": np.ascontiguousarray(inp["w_branch_a"][0]),
            "w_branch_b": np.ascontiguousarray(inp["w_branch_b"][0]),
            "w_out": np.ascontiguousarray(inp["w_out"][0]),
            "norm2_g": np.ascontiguousarray(inp["norm2_g"][0]).reshape(1, D),
            "w_router_group": np.ascontiguousarray(inp["w_router_group"][0]),
            "b_router_group": np.ascontiguousarray(inp["b_router_group"][0]).reshape(1, 4),
            "w_router_expert": np.ascontiguousarray(inp["w_router_expert"][0]),
            "b_router_expert": np.ascontiguousarray(inp["b_router_expert"][0]).reshape(1, 32),
            "w_expert_in": np.ascontiguousarray(inp["w_expert_in"][0]),
            "w_expert_out": np.ascontiguousarray(inp["w_expert_out"][0]),
            "norm_f_g": np.ascontiguousarray(inp["norm_f_g"]).reshape(1, D),
            "cst": c,
            "cmat": m,
        })
    return maps


def kernel(**inputs):
    P = build_program()
    maps = make_in_maps(inputs, list(range(8)))
    res = run_bass_kernel_spmd(P.nc, maps, core_ids=list(range(8)))
    return np.stack([np.asarray(r["out"], dtype=np.float32) for r in res.results], axis=0)
```

```python
from contextlib import ExitStack
import numpy as np
import concourse.bass as bass
import concourse.mybir as mybir

F32 = mybir.dt.float32
BF16 = mybir.dt.bfloat16
I32 = mybir.dt.int32
U32 = mybir.dt.uint32
AF = mybir.ActivationFunctionType
ALU = mybir.AluOpType
AX = mybir.AxisListType


class Tok:
    __slots__ = ("w", "r", "name")

    def __init__(self, name=""):
        self.w = None
        self.r = []
        self.name = name


class Op:
    __slots__ = ("eng", "fn", "deps", "dma", "sig", "sem", "val", "gidx", "slotwait")

    def __init__(self, eng, fn, dma, gidx):
        self.eng = eng
        self.fn = fn
        self.dma = dma
        self.deps = []
        self.sig = False
        self.sem = None
        self.val = 0
        self.gidx = gidx
        self.slotwait = None


class Kern:
    ENGS = ("pe", "act", "dve", "pool", "sp")
    NSLOT = {"sp": 20, "act": 6, "pool": 12}
    ROLL = 30000

    def __init__(self, nc):
        self.nc = nc
        self.ops = {e: [] for e in self.ENGS}
        self.n = 0
        self.toks = []
        self.es = ExitStack()
        self.nsem = 0

    def tok(self, name=""):
        t = Tok(name)
        self.toks.append(t)
        return t

    def toks_n(self, n, name=""):
        return [self.tok(f"{name}{i}") for i in range(n)]

    def op(self, eng, fn, r=(), w=(), dma=False):
        o = Op(eng, fn, dma, self.n)
        self.n += 1
        deps = {}
        for t in r:
            if t.w is not None:
                deps[id(t.w)] = t.w
        for t in w:
            if t.w is not None:
                deps[id(t.w)] = t.w
            for q in t.r:
                deps[id(q)] = q
        for d in deps.values():
            if d is o:
                continue
            if d.eng == eng and not d.dma and not dma:
                if eng == "pe":
                    continue
            o.deps.append(d)
        for t in r:
            t.r.append(o)
        for t in w:
            t.w = o
            t.r = []
        self.ops[eng].append(o)
        return o

    def barrier(self):
        deps = {}
        for t in self.toks:
            if t.w is not None:
                deps[id(t.w)] = t.w
            for q in t.r:
                deps[id(q)] = q
        dl = list(deps.values())
        for e in self.ENGS:
            o = Op(e, None, False, self.n)
            self.n += 1
            o.deps = [d for d in dl if d.fn is not None]
            self.ops[e].append(o)
        for t in self.toks:
            t.r = []
            t.w = None

    def _newsem(self, name):
        self.nsem += 1
        return self.es.enter_context(self.nc.semaphore(f"{name}_{self.nsem}"))

    def emit(self):
        nc = self.nc
        for e in self.ENGS:
            for o in self.ops[e]:
                for d in o.deps:
                    d.sig = True
        for e in self.ENGS:
            cur = None
            cnt = 0
            slots = None
            slot_uses = None
            slot_last = None
            k = 0
            for o in self.ops[e]:
                if o.fn is None:
                    continue
                if o.dma:
                    if slots is None:
                        ns = self.NSLOT[e]
                        slots = [self._newsem(f"d{e}") for _ in range(ns)]
                        slot_uses = [0] * ns
                        slot_last = [None] * ns
                    s = k % len(slots)
                    k += 1
                    o.slotwait = slot_last[s]
                    slot_uses[s] += 1
                    o.sem = slots[s]
                    o.val = 16 * slot_uses[s]
                    o.sig = True
                    slot_last[s] = o
                elif o.sig:
                    if cur is None or cnt >= self.ROLL:
                        cur = self._newsem(f"c{e}")
                        cnt = 0
                    cnt += 1
                    o.sem = cur
                    o.val = cnt
        with nc.Block() as block:
            def run(e, eng):
                waited = {}
                for o in self.ops[e]:
                    need = {}
                    dl = list(o.deps)
                    if o.slotwait is not None:
                        dl.append(o.slotwait)
                    for d in dl:
                        key = id(d.sem)
                        if waited.get(key, 0) >= d.val:
                            continue
                        if key not in need or need[key][1] < d.val:
                            need[key] = (d.sem, d.val)
                    for key, (sem, val) in need.items():
                        eng.wait_ge(sem, val)
                        waited[key] = val
                    if o.fn is None:
                        continue
                    ins = o.fn(eng)
                    if o.sig:
                        ins.then_inc(o.sem, 16 if o.dma else 1)

            @block.tensor
            def _(eng):
                run("pe", eng)

            @block.scalar
            def _(eng):
                run("act", eng)

            @block.vector
            def _(eng):
                run("dve", eng)

            @block.gpsimd
            def _(eng):
                run("pool", eng)

            @block.sync
            def _(eng):
                run("sp", eng)
        self.es.close()


U8 = mybir.dt.uint8
DTSZ = {F32: 4, BF16: 2, I32: 4, U32: 4}


class Arena:
    def __init__(self, nc, nbytes):
        self.t = nc.alloc_sbuf_tensor("arena", [128, nbytes], U8)
        self.n = nbytes
        self.off = 0
        self.peak = 0

    def alloc(self, shape, dt):
        n = int(np.prod(shape)) * DTSZ[dt]
        n = (n + 63) // 64 * 64
        assert self.off + n <= self.n, f"arena overflow {self.off}+{n}>{self.n}"
        v = self.t[:, self.off:self.off + n].bitcast(dt)
        self.off += n
        self.peak = max(self.peak, self.off)
        tot = int(np.prod(shape))
        v = v[:, 0:tot]
        if len(shape) == 2:
            v = v.rearrange("p (a b) -> p a b", a=shape[0])
        elif len(shape) == 3:
            v = v.rearrange("p (a b c) -> p a b c", a=shape[0], b=shape[1])
        return v

    def mark(self):
        return self.off

    def release(self, m):
        self.off = m

from concourse.bass_utils import run_bass_kernel_spmd

L = 4096
D = 1024
NT = 32
NCH = 8
DIN = 7752
CQ, CK, CV, CQI, CKI, CSU, CSV, CGA, CGB = 0, 1024, 2048, 3072, 3584, 3656, 4680, 5704, 6728
NE = 32
CAP = 512
DUMMY = NE * CAP
KI = 20
EPS = 1e-6
PI = float(np.pi)
MAGIC = 12582912.0
C1 = 6.28125
C2 = 2 * PI - C1
NEG = -1.0e30
ARENA = 206 * 1024

C_INV, C_SGN, C_INVI, C_NH, C_HPI, C_NTHR, C_EPS, C_ONE, C_DUM, C_PW, C_NB = 0, 1, 2, 34, 35, 36, 37, 38, 39, 40, 64
NCST = 96


def host_consts():
    c = np.zeros((128, NCST), np.float32)
    inv128 = (np.float32(10000.0) ** (-np.arange(0, 128, 2, dtype=np.float32) / np.float32(128))).astype(np.float32)
    inv64 = (np.float32(10000.0) ** (-np.arange(0, 64, 2, dtype=np.float32) / np.float32(64))).astype(np.float32)
    p = np.arange(128)
    c[:, C_INV] = inv128[p % 64]
    c[:, C_SGN] = np.where(p < 64, -1.0, 1.0)
    c[:, C_INVI:C_INVI + 32] = inv64[None, :]
    c[:, C_NH] = -0.5
    c[:, C_HPI] = PI / 2
    c[:, C_NTHR] = -1.0e29
    c[:, C_EPS] = EPS
    c[:, C_ONE] = 1.0
    c[:, C_DUM] = NE * CAP + p
    c[:, C_PW:C_PW + KI + 2] = (2.0 ** -(np.arange(KI + 2) + 1.0))[None, :]
    c[:, C_NB:C_NB + NT] = (128.0 * (np.arange(NT) + 1.0) - 511.0)[None, :]
    m = np.zeros((128, 128 * 3 + 64), np.float32)
    t = np.arange(128)[:, None]
    s = np.arange(128)[None, :]
    m[:, 0:128] = np.where(s <= t, 0.0, NEG)
    m[:, 128:256] = np.where(s >= t, 1.0, 0.0)
    m[:, 256:384] = np.where(s <= t, 1.0, 0.0)
    m[:, 384:416] = (np.arange(32) * CAP)[None, :]
    m[:, 416:448] = 1.0
    return c, m


class Prog:
    pass


def build_program(stop_after=None, dbg=False):
    nc = bass.Bass("TRN2", target_bir_lowering=False)
    K = Kern(nc)
    AR = Arena(nc, ARENA)
    P = Prog()
    P.nc = nc

    def din(name, shape, dt=F32):
        return nc.dram_tensor(name, shape, dt, kind="ExternalInput").ap()

    def dscr(name, shape, dt, out=False):
        return nc.dram_tensor(name, shape, dt, kind="ExternalOutput" if (out and dbg) else "Internal").ap()

    x_d = din("x", [L, D])
    posr_d = din("pos_row", [1, L], I32)
    posc_d = din("pos_col", [128, NT], I32)
    g1_d = din("norm1_g", [1, D])
    win_d = din("w_in", [D, DIN])
    gs_d = din("sgu_norm_g", [1, D])
    sw_d = din("sgu_w", [8, 128, 128])
    sb_d = din("sgu_b", [8, 128])
    wa_d = din("w_branch_a", [D, D])
    wb_d = din("w_branch_b", [D, D])
    wo_d = din("w_out", [D, D])
    g2_d = din("norm2_g", [1, D])
    wrg_d = din("w_router_group", [D, 4])
    brg_d = din("b_router_group", [1, 4])
    wre_d = din("w_router_expert", [D, 32])
    bre_d = din("b_router_expert", [1, 32])
    wei_d = din("w_expert_in", [NE, D, 512])
    weo_d = din("w_expert_out", [NE, 256, D])
    gf_d = din("norm_f_g", [1, D])
    cst_d = din("cst", [128, NCST])
    cm_d = din("cmat", [128, 448])
    out_d = nc.dram_tensor("out", [L, D], F32, kind="ExternalOutput").ap()

    qT_s = dscr("qT_s", [8, 128, L], BF16, True)
    kT_s = dscr("kT_s", [8, 128, L], BF16, True)
    v_s = dscr("v_s", [NT, 128, 8 * 129], BF16, True)
    qiT_s = dscr("qiT_s", [5, 128, L], BF16, True)
    suT_s = dscr("suT_s", [D, L], BF16)
    ysgT_s = dscr("ysgT_s", [D, L], BF16, True)
    sgT_s = dscr("sgT_s", [2 * D, L], BF16, True)
    yatT_s = dscr("yatT_s", [D, L], BF16, True)
    h2_s = dscr("h2_s", [L, D], F32, True)
    xs_s = dscr("xs_s", [NE * CAP + 128, D], BF16)
    ys_s = dscr("ys_s", [NE * CAP + 128, D], BF16)

    ps = [nc.alloc_psum_tensor(f"ps{i}", [128, 512], F32) for i in range(8)]
    pst = [K.tok(f"ps{i}") for i in range(8)]

    def psb(i):
        return ps[i][:, :].bitcast(BF16)

    def dma(eng, out, in_, r=(), w=()):
        return K.op(eng, lambda e: e.dma_start(out=out, in_=in_), r=r, w=w, dma=True)

    def mm(out, lhsT, rhs, start, stop, r=(), w=(), sgc=False):
        return K.op("pe", lambda e: e.matmul(out, lhsT=lhsT, rhs=rhs, start=start, stop=stop, skip_group_check=sgc), r=r, w=w)

    def tr(out, in_, ident, r=(), w=()):
        return K.op("pe", lambda e: e.transpose(out, in_, ident), r=r, w=w)

    def act(out, in_, func, r=(), w=(), bias=None, scale=1.0, accum=None):
        def f(e):
            kw = {}
            if bias is not None:
                kw["bias"] = bias
            if accum is not None:
                kw["accum_out"] = accum
            return e.activation(out=out, in_=in_, func=func, scale=scale, **kw)
        return K.op("act", f, r=r, w=w)

    def ts(eng, out, in0, s1, op0, s2=None, op1=None, r=(), w=(), accum=None):
        def f(e):
            kw = {}
            if op1 is not None:
                kw["op1"] = op1
            if accum is not None:
                kw["accum_out"] = accum
            return e.tensor_scalar(out=out, in0=in0, scalar1=s1, scalar2=s2, op0=op0, **kw)
        return K.op(eng, f, r=r, w=w)

    def tt(eng, out, in0, in1, op, r=(), w=()):
        return K.op(eng, lambda e: e.tensor_tensor(out=out, in0=in0, in1=in1, op=op), r=r, w=w)

    def stt(out, in0, scalar, in1, op0, op1, r=(), w=(), accum=None):
        def f(e):
            kw = {}
            if accum is not None:
                kw["accum_out"] = accum
            return e.scalar_tensor_tensor(out=out, in0=in0, scalar=scalar, in1=in1, op0=op0, op1=op1, **kw)
        return K.op("dve", f, r=r, w=w)

    def cp(eng, out, in_, r=(), w=()):
        if eng == "act":
            return K.op("act", lambda e: e.activation(out=out, in_=in_, func=AF.Copy), r=r, w=w)
        return K.op(eng, lambda e: e.tensor_copy(out, in_), r=r, w=w)

    def mset(eng, out, val, r=(), w=()):
        return K.op(eng, lambda e: e.memset(out, val), r=r, w=w)

    class B:
        def __init__(self, shape, dt, name=""):
            self.ap = AR.alloc(shape, dt)
            self.t = K.tok(name)

    def ring(n, shape, dt, name=""):
        return [B(shape, dt, f"{name}{i}") for i in range(n)]

    cst = B([NCST], F32, "cst")
    cm = B([448], F32, "cm")
    ident = B([128], BF16, "ident")
    identf = B([128], F32, "identf")
    wi_sb = B([NT, 8], F32, "wi")
    rt_gate = B([NT * 2], F32, "gate")
    rt_pos = B([NT * 2], I32, "pos")
    dma("sp", cst.ap, cst_d, w=[cst.t])
    dma("sp", cm.ap, cm_d, w=[cm.t])
    mset("pool", identf.ap, 0.0, w=[identf.t])
    K.op("pool", lambda e: e.affine_select(out=identf.ap, in_=identf.ap, pattern=[[-1, 128]], compare_op=ALU.not_equal,
                                           fill=1.0, base=0, channel_multiplier=1), r=[identf.t], w=[identf.t])
    cp("pool", ident.ap, identf.ap, r=[identf.t], w=[ident.t])

    zt = B([D], BF16, "zt")
    mset("pool", zt.ap, 0.0, w=[zt.t])

    def col(b, j, n=1):
        return b.ap[:, j:j + n]

    def sincos(ang, n, kk, sin_out, cos_out, r, w):
        tk = K.tok()
        ts("dve", kk, ang, 1.0 / (2 * PI), ALU.mult, MAGIC, ALU.add, r=r, w=[tk])
        ts("dve", kk, kk, MAGIC, ALU.subtract, r=[tk], w=[tk])
        stt(ang, kk, -C1, ang, ALU.mult, ALU.add, r=r + [tk], w=r)
        stt(ang, kk, -C2, ang, ALU.mult, ALU.add, r=r + [tk], w=r)
        ts("dve", ang, ang, PI, ALU.min, -PI, ALU.max, r=r, w=r)
        act(sin_out, ang, AF.Sin, r=r, w=w)
        ts("dve", kk, ang, -1.0, ALU.mult, r=r, w=[tk])
        tt("dve", kk, kk, ang, ALU.max, r=r + [tk], w=[tk])
        act(cos_out, kk, AF.Sin, bias=col(cst, C_HPI), scale=-1.0, r=[tk, cst.t], w=w)


    def finish():
        K.barrier()
        K.emit()
        P.__dict__.update(dict(K=K, AR=AR))
        return P

    m_0 = AR.mark()
    xnT = AR.alloc([8, L], BF16)
    xnT_t = [K.tok(f"xnT{i}") for i in range(NT)]
    m_p1 = AR.mark()

    xbuf = ring(3, [D], F32, "xb")
    junk = B([D], F32, "junk")
    g1b = B([D], F32, "g1b")
    xnb = ring(2, [D], BF16, "xnb")
    ms1 = B([NT], F32, "ms1")
    rs1 = B([NT], F32, "rs1")
    ms1_t = [K.tok() for _ in range(NT)]
    rs1_t = [K.tok() for _ in range(NT)]
    dma("sp", g1b.ap, g1_d.partition_broadcast(128), w=[g1b.t])

    def a_front(i):
        xb = xbuf[i % 3]
        dma("sp", xb.ap, x_d[i * 128:(i + 1) * 128, :], w=[xb.t])
        act(junk.ap, xb.ap, AF.Square, r=[xb.t], w=[junk.t, ms1_t[i]], accum=ms1.ap[:, i:i + 1])
        ts("pool", rs1.ap[:, i:i + 1], ms1.ap[:, i:i + 1], 1.0 / D, ALU.mult, EPS, ALU.add, r=[ms1_t[i]], w=[rs1_t[i]])
        tt("pool", rs1.ap[:, i:i + 1], rs1.ap[:, i:i + 1], col(cst, C_NH), ALU.pow, r=[rs1_t[i], cst.t], w=[rs1_t[i]])

    def a_back(i):
        xb = xbuf[i % 3]
        nb = xnb[i % 2]
        stt(nb.ap, xb.ap, rs1.ap[:, i:i + 1], g1b.ap, ALU.mult, ALU.mult, r=[xb.t, rs1_t[i], g1b.t], w=[nb.t])
        bk = 4 + (i % 2)
        for kc in range(8):
            tr(psb(bk)[:, kc * 128:(kc + 1) * 128], nb.ap[:, kc * 128:(kc + 1) * 128], ident.ap, r=[nb.t, ident.t], w=[pst[bk]])
        cp("act", xnT[:, :, i * 128:(i + 1) * 128], psb(bk).rearrange("p (a b) -> p a b", a=8), r=[pst[bk]], w=[xnT_t[i]])

    for i in range(NT + 1):
        if i < NT:
            a_front(i)
        if i >= 1:
            a_back(i - 1)
    K.barrier()
    AR.release(m_p1)

    wbuf = ring(3, [8, 512], BF16, "wb")
    wstg = ring(2, [4, 512], F32, "wstg")
    wstate = {"n": 0}

    def load_w(c0, ncols, ceng="pool"):
        b = wbuf[wstate["n"] % 3]
        wstate["n"] += 1
        for hf in range(2):
            sg = wstg[hf]
            dma("sp", sg.ap[:, :, 0:ncols], win_d[hf * 512:(hf + 1) * 512, c0:c0 + ncols].rearrange("(kc p) c -> p kc c", p=128), w=[sg.t])
            cp(ceng, b.ap[:, 4 * hf:4 * hf + 4, 0:ncols], sg.ap[:, :, 0:ncols], r=[sg.t], w=[b.t])
        return b

    bank_rr = {"n": 0}

    def next_bank():
        bk = bank_rr["n"] % 4
        bank_rr["n"] += 1
        return bk

    def fm_block(wb, j, c, bk):
        for kc in range(8):
            mm(ps[bk][:, :], wb.ap[:, kc, j * 128:(j + 1) * 128], xnT[:, kc, c * 512:(c + 1) * 512], kc == 0, kc == 7,
               r=[wb.t] + xnT_t[4 * c:4 * c + 4], w=[pst[bk]])

    def tm_block(wb, ncols, i, bk, col0=0):
        for kc in range(8):
            mm(ps[bk][:, 0:ncols], xnT[:, kc, i * 128:(i + 1) * 128], wb.ap[:, kc, col0:col0 + ncols], kc == 0, kc == 7,
               r=[wb.t, xnT_t[i]], w=[pst[bk]])

    m_b1 = AR.mark()
    cosT = B([L], F32, "cosT")
    sinT = B([L], F32, "sinT")
    posi = B([1024], I32, "posi")
    angw = B([1024], F32, "angw")
    kkw = B([1024], F32, "kkw")
    for cc in range(4):
        sl = slice(cc * 1024, (cc + 1) * 1024)
        dma("sp", posi.ap, posr_d[:, sl].partition_broadcast(128), w=[posi.t])
        cp("dve", angw.ap, posi.ap, r=[posi.t], w=[angw.t])
        ts("dve", angw.ap, angw.ap, col(cst, C_INV), ALU.mult, r=[angw.t, cst.t], w=[angw.t])
        sincos(angw.ap, 1024, kkw.ap, sinT.ap[:, sl], cosT.ap[:, sl], r=[angw.t], w=[sinT.t, cosT.t])
        ts("dve", sinT.ap[:, sl], sinT.ap[:, sl], col(cst, C_SGN), ALU.mult, r=[sinT.t, cst.t], w=[sinT.t])
    posc = B([NT], I32, "posc")
    poscf = B([NT], F32, "poscf")
    sinI = B([NT, 32], F32, "sinI")
    cosI = B([NT, 32], F32, "cosI")
    dma("sp", posc.ap, posc_d, w=[posc.t])
    cp("dve", poscf.ap, posc.ap, r=[posc.t], w=[poscf.t])
    angI = angw.ap.rearrange("p (a b) -> p a b", a=NT)
    tt("dve", angI, poscf.ap.unsqueeze(2).broadcast_to([128, NT, 32]),
       cst.ap[:, C_INVI:C_INVI + 32].unsqueeze(1).broadcast_to([128, NT, 32]), ALU.mult, r=[poscf.t, cst.t, angw.t], w=[angw.t])
    sincos(angw.ap, 1024, kkw.ap, sinI.ap.rearrange("p a b -> p (a b)"), cosI.ap.rearrange("p a b -> p (a b)"),
           r=[angw.t], w=[sinI.t, cosI.t])

    t1r = ring(2, [512], F32, "t1")
    t2r = ring(2, [512], F32, "t2")
    qor = ring(3, [512], BF16, "qo")
    n_rope = {"n": 0}

    def rope_fm(bk, c, dst):
        k_ = n_rope["n"]
        n_rope["n"] += 1
        t1 = t1r[k_ % 2]
        t2 = t2r[k_ % 2]
        qo = qor[k_ % 3]
        sl = slice(c * 512, (c + 1) * 512)
        tt("dve", t1.ap, ps[bk][:, :], cosT.ap[:, sl], ALU.mult, r=[pst[bk], cosT.t], w=[t1.t])
        tt("dve", t2.ap[0:64, :], ps[bk][64:128, :], sinT.ap[0:64, sl], ALU.mult, r=[pst[bk], sinT.t], w=[t2.t])
        tt("dve", t2.ap[64:128, :], ps[bk][0:64, :], sinT.ap[64:128, sl], ALU.mult, r=[pst[bk], sinT.t], w=[t2.t])
        tt("pool", qo.ap, t1.ap, t2.ap, ALU.add, r=[t1.t, t2.t], w=[qo.t])
        dma("sp", dst, qo.ap, r=[qo.t])

    qk_blocks = [(CQ, qT_s, 0), (CQ + 512, qT_s, 4), (CK, kT_s, 0), (CK + 512, kT_s, 4)]
    wnext = load_w(qk_blocks[0][0], 512)
    for bi, (c0, dst_s, h0) in enumerate(qk_blocks):
        wb = wnext
        wnext = load_w(qk_blocks[bi + 1][0], 512, "act") if bi + 1 < 4 else load_w(CV, 512, "act")
        for c in range(NCH):
            for j in range(4):
                bk = next_bank()
                fm_block(wb, j, c, bk)
                rope_fm(bk, c, dst_s[h0 + j, :, c * 512:(c + 1) * 512])
    wv0 = wnext
    wv1 = load_w(CV + 512, 512)
    wqi = load_w(CQI, 512)
    vt = ring(2, [8, 129], BF16, "vt")
    for b_ in vt:
        mset("pool", b_.ap[:, :, 128:129], 1.0, w=[b_.t])
    for i in range(NT):
        v_ = vt[i % 2]
        for half, wb in enumerate((wv0, wv1)):
            bk = next_bank()
            tm_block(wb, 512, i, bk)
            cp("act", v_.ap[:, half * 4:(half + 1) * 4, 0:128], ps[bk][:, :].rearrange("p (a b) -> p a b", a=4), r=[pst[bk]], w=[v_.t])
        dma("sp", v_s[i].rearrange("p (h d) -> p h d", h=8), v_.ap, r=[v_.t])
    wkw = load_w(CKI, 72)
    w0_pre = load_w(CSU, 512)
    ra = ring(3, [9, 32], F32, "ra")
    rb = ring(3, [9, 32], F32, "rb")
    qst = ring(3, [9, 64], F32, "qst")
    qr = ring(3, [640], BF16, "qr")
    qiT_c = ring(2, [5, 512], BF16, "qiTc")
    def ip_front(i):
        bq = next_bank()
        tm_block(wqi, 512, i, bq)
        bkw = next_bank()
        tm_block(wkw, 72, i, bkw)
        q_ = qr[i % 3]
        a_, b2_, st_ = ra[i % 3], rb[i % 3], qst[i % 3]
        cp("act", st_.ap[:, 0:8, :], ps[bq][:, :].rearrange("p (h d) -> p h d", h=8), r=[pst[bq]], w=[st_.t])
        cp("act", st_.ap[:, 8, :], ps[bkw][:, 0:64], r=[pst[bkw]], w=[st_.t])
        cosb = cosI.ap[:, i, :].unsqueeze(1).broadcast_to([128, 9, 32])
        sinb = sinI.ap[:, i, :].unsqueeze(1).broadcast_to([128, 9, 32])
        qo_ = q_.ap[:, 0:576].rearrange("p (h d) -> p h d", h=9)
        rd = [st_.t, cosI.t, sinI.t]
        tt("dve", a_.ap, st_.ap[:, :, 0:32], cosb, ALU.mult, r=rd, w=[a_.t])
        tt("dve", b2_.ap, st_.ap[:, :, 32:64], sinb, ALU.mult, r=rd, w=[b2_.t])
        tt("pool", qo_[:, :, 0:32], a_.ap, b2_.ap, ALU.subtract, r=[a_.t, b2_.t], w=[q_.t])
        tt("dve", a_.ap, st_.ap[:, :, 32:64], cosb, ALU.mult, r=rd, w=[a_.t])
        tt("dve", b2_.ap, st_.ap[:, :, 0:32], sinb, ALU.mult, r=rd, w=[b2_.t])
        tt("pool", qo_[:, :, 32:64], a_.ap, b2_.ap, ALU.add, r=[a_.t, b2_.t], w=[q_.t])
        cp("pool", q_.ap[:, 576:640], q_.ap[:, 512:576], r=[q_.t], w=[q_.t])
        cp("act", wi_sb.ap[:, i, :], ps[bkw][:, 64:72], r=[pst[bkw]], w=[wi_sb.t])

    def ip_back(i):
        c, tau = i // 4, i % 4
        q_ = qr[i % 3]
        bt = 4 + (i % 2)
        for jj in range(5):
            tr(psb(bt)[:, jj * 128:(jj + 1) * 128], q_.ap[:, jj * 128:(jj + 1) * 128], ident.ap, r=[q_.t, ident.t], w=[pst[bt]])
        qc = qiT_c[c % 2]
        cp("act", qc.ap[:, :, tau * 128:(tau + 1) * 128], psb(bt)[:, 0:640].rearrange("p (a b) -> p a b", a=5), r=[pst[bt]], w=[qc.t])
        if tau == 3:
            dma("sp", qiT_s[:, :, c * 512:(c + 1) * 512].rearrange("a p t -> p a t"), qc.ap, r=[qc.t])
    for i in range(NT + 1):
        if i < NT:
            ip_front(i)
        if i >= 1:
            ip_back(i - 1)
    K.barrier()
    AR.release(m_b1)
    if stop_after == "1b":
        return finish()
    m_b2 = AR.mark()
    sub_r = ring(3, [512], BF16, "sub")
    su_t = [K.tok(f"suT{c}") for c in range(NCH)]
    w0 = w0_pre
    w1 = load_w(CSU + 512, 512)
    w2 = load_w(CSV, 512)
    nsub = {"n": 0}

    def act_block_out(bk, func, dst, wtok=()):
        o = sub_r[nsub["n"] % 3]
        nsub["n"] += 1
        act(o.ap, ps[bk][:, :], func, r=[pst[bk]], w=[o.t])
        dma("sp", dst, o.ap, r=[o.t], w=list(wtok))

    for blk, wb in enumerate((w0, w1)):
        for c in range(NCH):
            for j in range(4):
                bk = next_bank()
                fm_block(wb, j, c, bk)
                f0 = (blk * 4 + j) * 128
                act_block_out(bk, AF.Gelu_apprx_tanh, suT_s[f0:f0 + 128, c * 512:(c + 1) * 512], wtok=[su_t[c]])
    w3 = load_w(CSV + 512, 512)
    swf = B([8, 128], F32, "swf")
    swb = B([8, 128], BF16, "swb")
    WtT = B([8, 128], BF16, "WtT")
    dma("sp", swf.ap, sw_d.rearrange("g t s -> t g s"), w=[swf.t])
    tt("dve", swf.ap, swf.ap, cm.ap[:, 256:384].unsqueeze(1).broadcast_to([128, 8, 128]), ALU.mult, r=[swf.t, cm.t], w=[swf.t])
    cp("dve", swb.ap, swf.ap, r=[swf.t], w=[swb.t])
    for g in range(8):
        tr(psb(4)[:, g * 128:(g + 1) * 128], swb.ap[:, g, :], ident.ap, r=[swb.t, ident.t], w=[pst[4]])
    cp("act", WtT.ap, psb(4).rearrange("p (a b) -> p a b", a=8), r=[pst[4]], w=[WtT.t])
    bf_ = B([8, 128], F32, "bf")
    bhl = B([8, 128], BF16, "bhl")
    bhf = B([8, 128], F32, "bhf")
    ones2 = B([128], BF16, "ones2")
    mset("pool", ones2.ap, 1.0, w=[ones2.t])
    mset("pool", bf_.ap, 0.0, w=[bf_.t])
    dma("sp", bf_.ap[0:1, :, :], sb_d.rearrange("(o g) t -> o g t", o=1), r=[bf_.t], w=[bf_.t])
    dma("sp", bf_.ap[1:2, :, :], sb_d.rearrange("(o g) t -> o g t", o=1), r=[bf_.t], w=[bf_.t])
    cp("dve", bhl.ap, bf_.ap, r=[bf_.t], w=[bhl.t])
    cp("dve", bhf.ap, bhl.ap, r=[bhl.t], w=[bhf.t])
    tt("dve", bhf.ap, bf_.ap, bhf.ap, ALU.subtract, r=[bf_.t, bhf.t], w=[bhf.t])
    cp("dve", bf_.ap, bhl.ap, r=[bhl.t, bf_.t], w=[bf_.t])
    sel = B([1], F32, "sel")
    mset("pool", sel.ap, 1.0, w=[sel.t])
    K.op("pool", lambda e: e.affine_select(out=sel.ap, in_=sel.ap, pattern=[[0, 1]], compare_op=ALU.is_equal,
                                           fill=0.0, base=0, channel_multiplier=1), r=[sel.t], w=[sel.t])
    tt("dve", bf_.ap, bf_.ap, bhf.ap, ALU.subtract, r=[bf_.t, bhf.t], w=[bf_.t])
    stt(bhf.ap, bf_.ap, sel.ap[:, 0:1], bhf.ap, ALU.mult, ALU.add, r=[bf_.t, sel.t, bhf.t], w=[bhf.t])
    cp("dve", bhl.ap, bhf.ap, r=[bhf.t], w=[bhl.t])

    gsb = B([D], F32, "gsb")
    dma("sp", gsb.ap, gs_d.partition_broadcast(128), w=[gsb.t])
    gv = ring(3, [D], F32, "gv")
    vln = ring(3, [D], BF16, "vln")
    bst = B([NT, 12], F32, "bst")
    mv = B([NT, 2], F32, "mv")
    rsd = B([NT], F32, "rsd")
    bst_t = [K.tok() for _ in range(NT)]
    mv_t = [K.tok() for _ in range(NT)]
    rsd_t = [K.tok() for _ in range(NT)]
    suc = ring(2, [8, 512], BF16, "suc")
    ysg = ring(2, [8, 512], BF16, "ysg")
    wg_pre = load_w(CGA, 512)
    for c in range(NCH):
        su_c = suc[c % 2]
        ys_c = ysg[c % 2]
        dma("sp", su_c.ap, suT_s[:, c * 512:(c + 1) * 512].rearrange("(g p) t -> p g t", p=128), r=[su_t[c]], w=[su_c.t])
        def sv_front(tau, c=c):
            i = 4 * c + tau
            g_ = gv[i % 3]
            vl = vln[i % 3]
            sb0 = 6
            for half, wb in enumerate((w2, w3)):
                bk = next_bank()
                tm_block(wb, 512, i, bk)
                act(g_.ap[:, half * 512:(half + 1) * 512], ps[bk][:, :], AF.Gelu_apprx_tanh, r=[pst[bk]], w=[g_.t])
            K.op("dve", lambda e, i=i, g_=g_: e.bn_stats(out=bst.ap[:, i, 0:6], in_=g_.ap[:, 0:512]), r=[g_.t], w=[bst_t[i]])
            K.op("dve", lambda e, i=i, g_=g_: e.bn_stats(out=bst.ap[:, i, 6:12], in_=g_.ap[:, 512:1024]), r=[g_.t], w=[bst_t[i]])
            K.op("dve", lambda e, i=i: e.bn_aggr(out=mv.ap[:, i, :], in_=bst.ap[:, i, :]), r=[bst_t[i]], w=[mv_t[i]])
            ts("pool", rsd.ap[:, i:i + 1], mv.ap[:, i, 1:2], EPS, ALU.add, r=[mv_t[i]], w=[rsd_t[i]])
            tt("pool", rsd.ap[:, i:i + 1], rsd.ap[:, i:i + 1], col(cst, C_NH), ALU.pow, r=[rsd_t[i], cst.t], w=[rsd_t[i]])
            ts("dve", g_.ap, g_.ap, mv.ap[:, i, 0:1], ALU.subtract, rsd.ap[:, i:i + 1], ALU.mult, r=[g_.t, mv_t[i], rsd_t[i]], w=[g_.t])
            tt("pool", vl.ap, g_.ap, gsb.ap, ALU.mult, r=[g_.t, gsb.t], w=[vl.t])

        def sv_back(tau, c=c, su_c=su_c, ys_c=ys_c):
            i = 4 * c + tau
            vl = vln[i % 3]
            sb0 = 6
            for g in range(8):
                bk = sb0 + g // 4
                o_ = ps[bk][:, (g % 4) * 128:(g % 4 + 1) * 128]
                mm(o_, vl.ap[:, g * 128:(g + 1) * 128], WtT.ap[:, g, :], True, False, r=[vl.t, WtT.t], w=[pst[bk]])
                mm(o_, ones2.ap[0:2, :], bhl.ap[0:2, g, :], False, True, r=[ones2.t, bhl.t], w=[pst[bk]])
            for b_ in range(2):
                tt("dve", ys_c.ap[:, 4 * b_:4 * b_ + 4, tau * 128:(tau + 1) * 128],
                   ps[sb0 + b_][:, :].rearrange("p (a b) -> p a b", a=4),
                   su_c.ap[:, 4 * b_:4 * b_ + 4, tau * 128:(tau + 1) * 128], ALU.mult, r=[pst[sb0 + b_], su_c.t], w=[ys_c.t])
        for tau in range(5):
            if tau < 4:
                sv_front(tau)
            if tau >= 1:
                sv_back(tau - 1)
        dma("sp", ysgT_s[:, c * 512:(c + 1) * 512].rearrange("(g p) t -> p g t", p=128), ys_c.ap, r=[ys_c.t])
    wg = wg_pre
    for blk in range(4):
        wb = wg
        if blk < 3:
            wg = load_w(CGA + (blk + 1) * 512, 512)
        for c in range(NCH):
            for j in range(4):
                bk = next_bank()
                fm_block(wb, j, c, bk)
                f0 = (blk * 4 + j) * 128
                act_block_out(bk, AF.Sigmoid, sgT_s[f0:f0 + 128, c * 512:(c + 1) * 512])
    K.barrier()
    AR.release(m_0)
    if stop_after == "1":
        return finish()
    SCALE = float(128 ** -0.5)
    vres = AR.alloc([NT, 8, 129], BF16)
    v_t = [K.tok(f"v{i}") for i in range(NT)]
    kiT2 = AR.alloc([L], BF16)
    ki_t = [K.tok(f"ki{i}") for i in range(4)]
    for q4 in range(4):
        dma("sp", kiT2[:, q4 * 1024:(q4 + 1) * 1024], qiT_s[4, :, q4 * 1024:(q4 + 1) * 1024], w=[ki_t[q4]])

    def load_v_all():
        for i in range(NT):
            dma("sp", vres[:, i, :, :], v_s[i].rearrange("p (h d) -> p h d", h=8), w=[v_t[i]])
    nrow_t = (NE * CAP + 128) // 128
    for z0 in range(0, nrow_t, 16):
        zn = min(16, nrow_t - z0)
        dma("sp", xs_s[z0 * 128:(z0 + zn) * 128, :].rearrange("(a p) d -> p a d", p=128),
            zt.ap.unsqueeze(1).broadcast_to([128, zn, D]), r=[zt.t])

    kTr = ring(2, [L], BF16, "kTr")
    qTc = B([8, 512], BF16, "qTc")
    qiTc = B([4, 512], BF16, "qiTc")
    S = B([L], F32, "S")
    maskb = B([L], BF16, "maskb")
    maskb2 = B([L], BF16, "maskb2")
    maskT = B([NT, 512], BF16, "maskT")
    rbuf = ring(3, [512], F32, "rb")
    ebuf = ring(3, [512], BF16, "eb")
    pbuf = ring(3, [512], BF16, "pb")
    S2 = B([L], F32, "S2")

    class _V:
        pass
    ytile = _V()
    ytile.ap = S2.ap[:, 0:2048].bitcast(BF16).rearrange("p (a b) -> p a b", a=4)
    ytile.t = S2.t
    yT = _V()
    yT.ap = S2.ap[:, 2048:4096].bitcast(BF16).rearrange("p (a b) -> p a b", a=8)
    yT.t = S2.t
    Sb = (S, S2)
    Mb2 = (maskb, maskb2)
    sst = []
    for q_ in range(2):
        sst.append({nm: B([sz], F32, f"{nm}{q_}") for nm, sz in (
            ("vmax", 1), ("vmin", 1), ("rngv", 1), ("hw", KI + 2), ("nhw", KI + 2), ("cand", 1), ("cnt", 1), ("msg", 1), ("thr", 1))})
    recr = ring(2, [4], F32, "rec")
    ctr = {"sc": 0, "lg": 0, "tr": 0, "e": 0}

    for c in range(NCH):
        csl = slice(c * 512, (c + 1) * 512)
        dma("sp", qTc.ap, qT_s[:, :, csl].rearrange("h p t -> p h t"), w=[qTc.t])
        dma("sp", qiTc.ap, qiT_s[0:4, :, csl].rearrange("a p t -> p a t"), w=[qiTc.t])
        if c == 0:
            load_v_all()
        def idx_scores(tau, Sx):
            i = 4 * c + tau
            n = 128 * (i + 1)
            nv = 128 * i
            for sc in range((n + 511) // 512):
                w_ = min(512, n - 512 * sc)
                sl = slice(sc * 512, sc * 512 + w_)
                for h in range(8):
                    bk = ctr["sc"] % 2
                    ctr["sc"] += 1
                    pr = slice(64 * (h % 2), 64 * (h % 2) + 64)
                    mm(ps[bk][:, 0:w_], qiTc.ap[pr, h // 2, tau * 128:(tau + 1) * 128], kiT2[pr, sl], True, True,
                       r=[qiTc.t, ki_t[sc // 2]], w=[pst[bk]])
                    rb = rbuf[ctr["sc"] % 3]
                    act(rb.ap[:, 0:w_], ps[bk][:, 0:w_], AF.Relu, r=[pst[bk]], w=[rb.t])
                    if h == 0:
                        ts("dve", Sx.ap[:, sl], rb.ap[:, 0:w_], wi_sb.ap[:, i, 0:1], ALU.mult, r=[rb.t, wi_sb.t], w=[Sx.t])
                    else:
                        stt(Sx.ap[:, sl], rb.ap[:, 0:w_], wi_sb.ap[:, i, h:h + 1], Sx.ap[:, sl], ALU.mult, ALU.add,
                            r=[rb.t, wi_sb.t, Sx.t], w=[Sx.t])
            tt("pool", Sx.ap[:, nv:n], Sx.ap[:, nv:n], cm.ap[:, 0:128], ALU.add, r=[Sx.t, cm.t], w=[Sx.t])

        def search_pre(tau, Sx, mb, st, on_act):
            i = 4 * c + tau
            n = 128 * (i + 1)
            nv = 128 * i
            if i < 2:
                return
            A_ = lambda nm: st[nm].ap
            T2 = lambda nm: st[nm].t
            K.op("dve", lambda e: e.tensor_reduce(out=A_("vmax"), in_=Sx.ap[:, 0:n], axis=AX.X, op=ALU.max), r=[Sx.t], w=[T2("vmax")])
            K.op("dve", lambda e: e.tensor_reduce(out=A_("vmin"), in_=Sx.ap[:, 0:256], axis=AX.X, op=ALU.min), r=[Sx.t], w=[T2("vmin")])
            tt("dve", A_("rngv"), A_("vmax"), A_("vmin"), ALU.subtract, r=[T2("vmax"), T2("vmin")], w=[T2("rngv")])
            ts("dve", A_("hw"), cst.ap[:, C_PW:C_PW + KI + 2], A_("rngv")[:, 0:1], ALU.mult, r=[cst.t, T2("rngv")], w=[T2("hw")])
            if not on_act:
                tt("dve", A_("cand"), A_("vmin"), A_("hw")[:, 0:1], ALU.add, r=[T2("vmin"), T2("hw")], w=[T2("cand")])
            else:
                ts("dve", A_("nhw"), A_("hw"), -1.0, ALU.mult, r=[T2("hw")], w=[T2("nhw")])
                stt(A_("cand"), A_("vmin"), -1.0, A_("hw")[:, 0:1], ALU.mult, ALU.subtract, r=[T2("vmin"), T2("hw")], w=[T2("cand")])

        def search(tau, Sx, mb, st, on_act):
            i = 4 * c + tau
            n = 128 * (i + 1)
            if i < 2:
                return col(cst, C_NTHR), cst.t
            A_ = lambda nm: st[nm].ap
            T2 = lambda nm: st[nm].t
            if not on_act:
                for k in range(KI):
                    ts("dve", mb.ap[:, 0:n], Sx.ap[:, 0:n], A_("cand")[:, 0:1], ALU.is_ge, None, ALU.add,
                       r=[Sx.t, T2("cand")], w=[mb.t, T2("cnt")], accum=A_("cnt"))
                    ts("dve", A_("msg"), A_("cnt"), 255.5, ALU.is_ge, 0.5, ALU.subtract, r=[T2("cnt")], w=[T2("msg")])
                    stt(A_("cand"), A_("msg"), A_("hw")[:, k:k + 1], A_("cand"), ALU.mult, ALU.add, r=[T2("msg"), T2("hw"), T2("cand")], w=[T2("cand")])
                tt("dve", A_("thr"), A_("cand"), A_("hw")[:, KI:KI + 1], ALU.subtract, r=[T2("cand"), T2("hw")], w=[T2("thr")])
            else:
                for k in range(KI):
                    act(mb.ap[:, 0:n], Sx.ap[:, 0:n], AF.Sign, bias=A_("cand")[:, 0:1], r=[Sx.t, T2("cand")], w=[mb.t, T2("cnt")], accum=A_("cnt"))
                    act(A_("msg"), A_("cnt"), AF.Sign, bias=col(cst, C_NB + i), r=[T2("cnt"), cst.t], w=[T2("msg")])
                    act(A_("cand"), A_("msg"), AF.Identity, bias=A_("cand")[:, 0:1], scale=A_("nhw")[:, k + 1:k + 2],
                        r=[T2("msg"), T2("nhw"), T2("cand")], w=[T2("cand")])
                act(A_("thr"), A_("cand"), AF.Identity, bias=A_("nhw")[:, KI:KI + 1], scale=-1.0, r=[T2("cand"), T2("nhw")], w=[T2("thr")])
            return A_("thr")[:, 0:1], T2("thr")

        def make_mask(tau, Sx, mb, thr_ap, thr_t):
            i = 4 * c + tau
            n = 128 * (i + 1)
            ts("dve", mb.ap[:, 0:n], Sx.ap[:, 0:n], thr_ap, ALU.is_ge, r=[Sx.t, thr_t], w=[mb.t])
            for j0 in range(0, i + 1, 8):
                nb = min(8, i + 1 - j0)
                bk = 2 + ctr["tr"] % 2
                ctr["tr"] += 1
                for jj in range(nb):
                    tr(psb(bk)[:, jj * 128:(jj + 1) * 128], mb.ap[:, (j0 + jj) * 128:(j0 + jj + 1) * 128], ident.ap,
                       r=[mb.t, ident.t], w=[pst[bk]])
                cp("act", maskT.ap[:, j0:j0 + nb, tau * 128:(tau + 1) * 128],
                   psb(bk)[:, 0:nb * 128].rearrange("p (a b) -> p a b", a=nb), r=[pst[bk]], w=[maskT.t])

        for pair in range(2):
            t0, t1_ = 2 * pair, 2 * pair + 1
            idx_scores(t0, Sb[0])
            idx_scores(t1_, Sb[1])
            search_pre(t0, Sb[0], Mb2[0], sst[0], False)
            search_pre(t1_, Sb[1], Mb2[1], sst[1], True)
            th1 = search(t1_, Sb[1], Mb2[1], sst[1], True)
            th0 = search(t0, Sb[0], Mb2[0], sst[0], False)
            make_mask(t0, Sb[0], Mb2[0], *th0)
            make_mask(t1_, Sb[1], Mb2[1], *th1)
        nj = 4 * c + 4
        steps = [(h, j) for h in range(8) for j in range(nj)]
        LB = (0, 1, 2, 3)

        def emit_qk(k):
            h, j = steps[k]
            kt = kTr[h % 2]
            if j == 0:
                dma("sp", kt.ap[:, 0:nj * 128], kT_s[h, :, 0:nj * 128], w=[kt.t])
            r0 = max(0, j - 4 * c)
            N = 512 - 128 * r0
            bk = LB[k % 4]
            mm(ps[bk][:, 0:N], kt.ap[:, j * 128:(j + 1) * 128], qTc.ap[:, h, r0 * 128:512], True, True,
               r=[kt.t, qTc.t], w=[pst[bk]])

        emit_qk(0)
        emit_qk(1)
        emit_qk(2)
        for k, (h, j) in enumerate(steps):
            if k + 3 < len(steps):
                emit_qk(k + 3)
            accA = 4 + 2 * (h % 2)
            accB = accA + 1
            r0 = max(0, j - 4 * c)
            N = 512 - 128 * r0
            bk = LB[k % 4]
            e_ = ebuf[k % 3]
            p_ = pbuf[k % 3]
            act(e_.ap[:, 0:N], ps[bk][:, 0:N], AF.Exp, scale=SCALE, r=[pst[bk]], w=[e_.t])
            tt("dve", p_.ap[:, 0:N], e_.ap[:, 0:N], maskT.ap[:, j, r0 * 128:512], ALU.mult, r=[e_.t, maskT.t], w=[p_.t])
            for tau in range(r0, 4):
                if tau < 3:
                    o_, ob = ps[accA][:, tau * 129:(tau + 1) * 129], accA
                else:
                    o_, ob = ps[accB][:, 0:129], accB
                first = (j == 0 and tau in (0, 3))
                mm(o_, p_.ap[:, (tau - r0) * 128:(tau - r0 + 1) * 128], vres[:, j, h, :], first, j == nj - 1,
                   r=[p_.t, v_t[j]], w=[pst[ob]], sgc=True)
            if j == nj - 1:
                rc = recr[h % 2]
                K.op("dve", lambda e, rc=rc, accA=accA: e.reciprocal(
                    out=rc.ap[:, 0:3], in_=ps[accA][:, 0:387].rearrange("p (a b) -> p a b", b=129)[:, :, 128]), r=[pst[accA]], w=[rc.t])
                K.op("dve", lambda e, rc=rc, accB=accB: e.reciprocal(out=rc.ap[:, 3:4], in_=ps[accB][:, 128:129]), r=[pst[accB]], w=[rc.t])
                for tau in range(4):
                    src, sb_ = (ps[accA][:, tau * 129:tau * 129 + 128], accA) if tau < 3 else (ps[accB][:, 0:128], accB)
                    ts("dve", ytile.ap[:, tau, h * 128:(h + 1) * 128], src, rc.ap[:, tau:tau + 1], ALU.mult, r=[pst[sb_], rc.t], w=[ytile.t])
        for tau in range(4):
            bk = 2 + ctr["tr"] % 2
            ctr["tr"] += 1
            for kc in range(8):
                tr(psb(bk)[:, kc * 128:(kc + 1) * 128], ytile.ap[:, tau, kc * 128:(kc + 1) * 128], ident.ap, r=[ytile.t, ident.t], w=[pst[bk]])
            cp("act", yT.ap[:, :, tau * 128:(tau + 1) * 128], psb(bk).rearrange("p (a b) -> p a b", a=8), r=[pst[bk]], w=[yT.t])
        dma("sp", yatT_s[:, csl].rearrange("(g p) t -> p g t", p=128), yT.ap, r=[yT.t])
    K.barrier()
    AR.release(m_0)
    if stop_after == "2":
        return finish()
    m_3 = AR.mark()
    Wa_sb = B([8, D], BF16, "Wa")
    Wb_sb = B([8, D], BF16, "Wb")
    Wo_sb = B([8, D], BF16, "Wo")
    w3stg = ring(2, [2, D], F32, "w3stg")
    for wi3, (wsb, wd) in enumerate(((Wa_sb, wa_d), (Wb_sb, wb_d), (Wo_sb, wo_d))):
        for q4 in range(4):
            sg = w3stg[(wi3 * 4 + q4) % 2]
            dma("sp", sg.ap, wd[q4 * 256:(q4 + 1) * 256, :].rearrange("(kc p) n -> p kc n", p=128), w=[sg.t])
            cp("dve" if q4 % 2 else "act", wsb.ap[:, 2 * q4:2 * q4 + 2, :], sg.ap, r=[sg.t], w=[wsb.t])
    g2b = B([D], F32, "g2b")
    dma("sp", g2b.ap, g2_d.partition_broadcast(128), w=[g2b.t])
    wr = B([8, 36], F32, "wr")
    br = B([36], F32, "br")
    dma("sp", wr.ap[:, :, 0:4], wrg_d.rearrange("(kc p) n -> p kc n", p=128), w=[wr.t])
    dma("sp", wr.ap[:, :, 4:36], wre_d.rearrange("(kc p) n -> p kc n", p=128), w=[wr.t])
    dma("sp", br.ap[:, 0:4], brg_d.partition_broadcast(128), w=[br.t])
    dma("sp", br.ap[:, 4:36], bre_d.partition_broadcast(128), w=[br.t])
    trib = B([128], BF16, "trib")
    onesb = B([128], BF16, "onesb")
    cp("pool", trib.ap, cm.ap[:, 128:256], r=[cm.t], w=[trib.t])
    mset("pool", onesb.ap, 1.0, w=[onesb.t])
    base = B([32], F32, "base")
    capb = B([32], F32, "capb")
    ts("pool", base.ap, cm.ap[:, 384:416], -1.0, ALU.add, r=[cm.t], w=[base.t])
    ts("pool", capb.ap, cm.ap[:, 384:416], float(CAP) - 0.5, ALU.add, r=[cm.t], w=[capb.t])
    inr = [ring(2, [8, 512], BF16, nm) for nm in ("ysgc", "yatc", "sgac", "sgbc")]
    mT = B([8, 512], BF16, "mT")
    t1p = ring(2, [512], F32, "t1p")
    t2p = ring(2, [512], F32, "t2p")
    xtr = ring(2, [D], F32, "xt")
    h2r = ring(2, [D], F32, "h2t")
    ms2 = B([NT], F32, "ms2")
    rs2 = B([NT], F32, "rs2")
    xn2f = B([D], F32, "xn2f")
    xn2b = ring(6, [D], BF16, "xn2b")
    xn2T = B([8, 128], F32, "xn2T")
    lgt4 = B([4, 36], F32, "lgt4")
    rb_ = {nm: B(sz, F32, nm) for nm, sz in (
        ("gmax", [4]), ("ohg", [4, 4]), ("dgl", [4, 4]), ("exg", [4, 4]), ("se", [4]), ("gp", [4]), ("prod", [4, 4, 8]), ("esel", [4, 8]),
        ("m1", [4]), ("oh1", [4, 8]), ("es2", [4, 8]), ("m2", [4]), ("oh2", [4, 8]), ("dlt", [4]), ("ex", [4]), ("den", [4]), ("p1", [4]),
        ("p2", [4]), ("gk", [2, 4]), ("M1", [4, 32]), ("M2", [4, 32]), ("posf", [4, 32]), ("okf", [4, 32]), ("pr32", [4, 32]),
        ("pk", [2, 4]), ("ok", [2, 4]))}
    Mb4 = B([4, 32], BF16, "Mb4")
    sm = {nm: B([sz], F32, nm) for nm, sz in (
        ("gmax", 1), ("negg", 1), ("ohg", 4), ("j4", 4), ("se", 1), ("gp", 1), ("esel", 8), ("m1", 1), ("oh1", 8), ("es2", 8),
        ("m2", 1), ("oh2", 8), ("dlt", 1), ("ex", 1), ("den", 1), ("p1", 1), ("p2", 1), ("M1", 32), ("M2", 32), ("posf", 32),
        ("okf", 32), ("j32", 32), ("pk", 2), ("ok", 2), ("gk", 2))}
    Mb = B([32], BF16, "Mb")

    def S_(nm):
        return sm[nm].ap

    def T_(nm):
        return sm[nm].t

    def load_chunk3(c):
        csl = slice(c * 512, (c + 1) * 512)
        ysg_c, yat_c, sga_c, sgb_c = [rg[c % 2] for rg in inr]
        dma("sp", ysg_c.ap, ysgT_s[:, csl].rearrange("(g p) t -> p g t", p=128), w=[ysg_c.t])
        dma("sp", yat_c.ap, yatT_s[:, csl].rearrange("(g p) t -> p g t", p=128), w=[yat_c.t])
        dma("sp", sga_c.ap, sgT_s[0:D, csl].rearrange("(g p) t -> p g t", p=128), w=[sga_c.t])
        dma("sp", sgb_c.ap, sgT_s[D:2 * D, csl].rearrange("(g p) t -> p g t", p=128), w=[sgb_c.t])

    load_chunk3(0)
    for c in range(NCH):
        csl = slice(c * 512, (c + 1) * 512)
        ysg_c, yat_c, sga_c, sgb_c = [rg[c % 2] for rg in inr]
        if c + 1 < NCH:
            load_chunk3(c + 1)
        for nb in range(8):
            bA = next_bank()
            for kc in range(8):
                mm(ps[bA][:, :], Wa_sb.ap[:, kc, nb * 128:(nb + 1) * 128], ysg_c.ap[:, kc, :], kc == 0, kc == 7, r=[Wa_sb.t, ysg_c.t], w=[pst[bA]])
            bB = next_bank()
            for kc in range(8):
                mm(ps[bB][:, :], Wb_sb.ap[:, kc, nb * 128:(nb + 1) * 128], yat_c.ap[:, kc, :], kc == 0, kc == 7, r=[Wb_sb.t, yat_c.t], w=[pst[bB]])
            t1, t2 = t1p[nb % 2], t2p[nb % 2]
            tt("dve", t1.ap, ps[bA][:, :], sga_c.ap[:, nb, :], ALU.mult, r=[pst[bA], sga_c.t], w=[t1.t])
            tt("dve", t2.ap, ps[bB][:, :], sgb_c.ap[:, nb, :], ALU.mult, r=[pst[bB], sgb_c.t], w=[t2.t])
            tt("pool", mT.ap[:, nb, :], t1.ap, t2.ap, ALU.add, r=[t1.t, t2.t], w=[mT.t])
        for tau in range(4):
            i = 4 * c + tau
            xt, h2t, xb2 = xtr[i % 2], h2r[i % 2], xn2b[i % 6]
            dma("sp", xt.ap, x_d[i * 128:(i + 1) * 128, :], w=[xt.t])
            for half in range(2):
                hs = slice(half * 512, (half + 1) * 512)
                bk = next_bank()
                for kc in range(8):
                    mm(ps[bk][:, :], mT.ap[:, kc, tau * 128:(tau + 1) * 128], Wo_sb.ap[:, kc, hs], kc == 0, kc == 7, r=[mT.t, Wo_sb.t], w=[pst[bk]])
                tt("dve", h2t.ap[:, hs], ps[bk][:, :], xt.ap[:, hs], ALU.add, r=[pst[bk], xt.t], w=[h2t.t])
            dma("sp", h2_s[i * 128:(i + 1) * 128, :], h2t.ap, r=[h2t.t])
            stt(xn2f.ap, h2t.ap, 1.0 / D, h2t.ap, ALU.mult, ALU.mult, r=[h2t.t], w=[xn2f.t, ms2.t], accum=ms2.ap[:, i:i + 1])
            ts("pool", rs2.ap[:, i:i + 1], ms2.ap[:, i:i + 1], EPS, ALU.add, r=[ms2.t], w=[rs2.t])
            tt("pool", rs2.ap[:, i:i + 1], rs2.ap[:, i:i + 1], col(cst, C_NH), ALU.pow, r=[rs2.t, cst.t], w=[rs2.t])
            stt(xn2f.ap, h2t.ap, rs2.ap[:, i:i + 1], g2b.ap, ALU.mult, ALU.mult, r=[h2t.t, rs2.t, g2b.t], w=[xn2f.t])
            cp("pool", xb2.ap, xn2f.ap, r=[xn2f.t], w=[xb2.t])
            for q4 in range(2):
                bt = 4 + q4
                for jj in range(4):
                    kc = q4 * 4 + jj
                    tr(ps[bt][:, jj * 128:(jj + 1) * 128], xn2f.ap[:, kc * 128:(kc + 1) * 128], identf.ap, r=[xn2f.t, identf.t], w=[pst[bt]])
                cp("act", xn2T.ap[:, q4 * 4:(q4 + 1) * 4, :], ps[bt][:, :].rearrange("p (a b) -> p a b", a=4), r=[pst[bt]], w=[xn2T.t])
            for kc in range(8):
                mm(ps[6][:, 0:36], xn2T.ap[:, kc, :], wr.ap[:, kc, :], kc == 0, kc == 7, r=[xn2T.t, wr.t], w=[pst[6]])
            tt("dve", lgt4.ap[:, tau, :], ps[6][:, 0:36], br.ap, ALU.add, r=[pst[6], br.t], w=[lgt4.t])

        gl4 = lgt4.ap[:, :, 0:4]
        el4 = lgt4.ap[:, :, 4:36].rearrange("p t (g j) -> p t g j", g=4)
        R_ = lambda nm: rb_[nm].ap
        Q_ = lambda nm: rb_[nm].t
        b3 = lambda ap, shp: ap.unsqueeze(2).broadcast_to(shp)
        K.op("dve", lambda e: e.tensor_reduce(out=R_("gmax"), in_=gl4, axis=AX.X, op=ALU.max), r=[lgt4.t], w=[Q_("gmax")])
        tt("dve", R_("ohg"), gl4, b3(R_("gmax"), [128, 4, 4]), ALU.is_ge, r=[lgt4.t, Q_("gmax")], w=[Q_("ohg")])
        tt("dve", R_("dgl"), gl4, b3(R_("gmax"), [128, 4, 4]), ALU.subtract, r=[lgt4.t, Q_("gmax")], w=[Q_("dgl")])
        act(R_("exg"), R_("dgl"), AF.Exp, r=[Q_("dgl")], w=[Q_("exg")])
        K.op("dve", lambda e: e.tensor_reduce(out=R_("se"), in_=R_("exg"), axis=AX.X, op=ALU.add), r=[Q_("exg")], w=[Q_("se")])
        K.op("dve", lambda e: e.reciprocal(out=R_("gp"), in_=R_("se")), r=[Q_("se")], w=[Q_("gp")])
        tt("dve", R_("prod"), el4, R_("ohg").unsqueeze(3).broadcast_to([128, 4, 4, 8]), ALU.mult, r=[lgt4.t, Q_("ohg")], w=[Q_("prod")])
        tt("dve", R_("esel"), R_("prod")[:, :, 0, :], R_("prod")[:, :, 1, :], ALU.add, r=[Q_("prod")], w=[Q_("esel")])
        tt("dve", R_("esel"), R_("esel"), R_("prod")[:, :, 2, :], ALU.add, r=[Q_("prod"), Q_("esel")], w=[Q_("esel")])
        tt("dve", R_("esel"), R_("esel"), R_("prod")[:, :, 3, :], ALU.add, r=[Q_("prod"), Q_("esel")], w=[Q_("esel")])
        K.op("dve", lambda e: e.tensor_reduce(out=R_("m1"), in_=R_("esel"), axis=AX.X, op=ALU.max), r=[Q_("esel")], w=[Q_("m1")])
        tt("dve", R_("oh1"), R_("esel"), b3(R_("m1"), [128, 4, 8]), ALU.is_ge, r=[Q_("esel"), Q_("m1")], w=[Q_("oh1")])
        stt(R_("es2"), R_("oh1"), NEG, R_("esel"), ALU.mult, ALU.add, r=[Q_("oh1"), Q_("esel")], w=[Q_("es2")])
        K.op("dve", lambda e: e.tensor_reduce(out=R_("m2"), in_=R_("es2"), axis=AX.X, op=ALU.max), r=[Q_("es2")], w=[Q_("m2")])
        tt("dve", R_("oh2"), R_("es2"), b3(R_("m2"), [128, 4, 8]), ALU.is_ge, r=[Q_("es2"), Q_("m2")], w=[Q_("oh2")])
        tt("dve", R_("dlt"), R_("m2"), R_("m1"), ALU.subtract, r=[Q_("m1"), Q_("m2")], w=[Q_("dlt")])
        act(R_("ex"), R_("dlt"), AF.Exp, r=[Q_("dlt")], w=[Q_("ex")])
        ts("dve", R_("den"), R_("ex"), 1.0, ALU.add, r=[Q_("ex")], w=[Q_("den")])
        K.op("dve", lambda e: e.reciprocal(out=R_("p1"), in_=R_("den")), r=[Q_("den")], w=[Q_("p1")])
        tt("dve", R_("p2"), R_("ex"), R_("p1"), ALU.mult, r=[Q_("ex"), Q_("p1")], w=[Q_("p2")])
        tt("dve", R_("gk")[:, 0, :], R_("p1"), R_("gp"), ALU.mult, r=[Q_("p1"), Q_("gp")], w=[Q_("gk")])
        tt("dve", R_("gk")[:, 1, :], R_("p2"), R_("gp"), ALU.mult, r=[Q_("p2"), Q_("gp")], w=[Q_("gk")])
        ohg4 = R_("ohg").unsqueeze(3).broadcast_to([128, 4, 4, 8])
        for nm, oh in (("M1", "oh1"), ("M2", "oh2")):
            tt("dve", R_(nm).rearrange("p t (g j) -> p t g j", g=4), ohg4, R_(oh).unsqueeze(2).broadcast_to([128, 4, 4, 8]), ALU.mult,
               r=[Q_("ohg"), Q_(oh)], w=[Q_(nm)])
        tt("dve", Mb4.ap, R_("M1"), R_("M2"), ALU.add, r=[Q_("M1"), Q_("M2")], w=[Mb4.t])
        for tau in range(4):
            o_ = ps[7][:, tau * 32:(tau + 1) * 32]
            mm(o_, trib.ap, Mb4.ap[:, tau, :], True, tau == 0, r=[trib.t, Mb4.t], w=[pst[7]], sgc=True)
            for tp_ in range(tau):
                mm(o_, onesb.ap, Mb4.ap[:, tp_, :], False, tp_ == tau - 1, r=[onesb.t, Mb4.t], w=[pst[7]], sgc=True)
        for tau in range(4):
            mm(ps[7][:, 128:160], onesb.ap, Mb4.ap[:, tau, :], False, tau == 3, r=[onesb.t, Mb4.t], w=[pst[7]], sgc=True)
        tt("dve", R_("posf"), ps[7][:, 0:128].rearrange("p (t e) -> p t e", t=4), base.ap.unsqueeze(1).broadcast_to([128, 4, 32]), ALU.add,
           r=[pst[7], base.t], w=[Q_("posf")])
        tt("dve", base.ap, ps[7][:, 128:160], base.ap, ALU.add, r=[pst[7], base.t], w=[base.t])
        tt("dve", R_("okf"), R_("posf"), capb.ap.unsqueeze(1).broadcast_to([128, 4, 32]), ALU.is_lt, r=[Q_("posf"), capb.t], w=[Q_("okf")])
        for k_, nm in enumerate(("M1", "M2")):
            tt("dve", R_("pr32"), R_(nm), R_("posf"), ALU.mult, r=[Q_(nm), Q_("posf")], w=[Q_("pr32")])
            K.op("dve", lambda e, k_=k_: e.tensor_reduce(out=R_("pk")[:, k_, :], in_=R_("pr32"), axis=AX.X, op=ALU.add), r=[Q_("pr32")], w=[Q_("pk")])
            tt("dve", R_("pr32"), R_(nm), R_("okf"), ALU.mult, r=[Q_(nm), Q_("okf")], w=[Q_("pr32")])
            K.op("dve", lambda e, k_=k_: e.tensor_reduce(out=R_("ok")[:, k_, :], in_=R_("pr32"), axis=AX.X, op=ALU.add), r=[Q_("pr32")], w=[Q_("ok")])
        ts("dve", R_("pk"), R_("pk"), col(cst, C_DUM), ALU.subtract, r=[Q_("pk"), cst.t], w=[Q_("pk")])
        tt("dve", R_("pk"), R_("pk"), R_("ok"), ALU.mult, r=[Q_("pk"), Q_("ok")], w=[Q_("pk")])
        ts("dve", R_("pk"), R_("pk"), col(cst, C_DUM), ALU.add, r=[Q_("pk"), cst.t], w=[Q_("pk")])
        cp("dve", rt_pos.ap[:, 8 * c:8 * c + 8].rearrange("p (t k) -> p k t", k=2), R_("pk"), r=[Q_("pk")], w=[rt_pos.t])
        tt("dve", rt_gate.ap[:, 8 * c:8 * c + 8].rearrange("p (t k) -> p k t", k=2), R_("gk"), R_("ok"), ALU.mult, r=[Q_("gk"), Q_("ok")], w=[rt_gate.t])
        for tau in range(4):
            i = 4 * c + tau
            xb2 = xn2b[i % 6]
            for k_ in range(2):
                K.op("pool", lambda e, i=i, k_=k_, xb2=xb2: e.indirect_dma_start(
                    out=xs_s[:, :], out_offset=bass.IndirectOffsetOnAxis(ap=rt_pos.ap[:, 2 * i + k_:2 * i + k_ + 1], axis=0),
                    in_=xb2.ap, in_offset=None), r=[xb2.t, rt_pos.t], dma=True)
    K.barrier()
    AR.release(m_3)
    if stop_after == "3":
        return finish()
    m_4 = AR.mark()
    bank4 = {"n": 0}

    def next_bank4():
        bk = (0, 1, 2, 3, 6, 7)[bank4["n"] % 6]
        bank4["n"] += 1
        return bk

    wi_r = ring(2, [8, 512], BF16, "wie")
    wo_r = ring(3, [2, D], BF16, "woe")
    wis_r = ring(2, [8, 512], F32, "wis")
    wos_r = ring(2, [2, D], F32, "wos")
    xs_r = ring(2, [4, D], BF16, "xs")
    xsT_r = ring(2, [8, 512], BF16, "xsT")
    sg_r = ring(2, [512], F32, "sg")
    aT_r = ring(2, [2, 512], BF16, "aT")
    ys_r = ring(3, [D], BF16, "ysb")
    nys = {"n": 0}
    mset("pool", ys_r[2].ap, 0.0, w=[ys_r[2].t])
    dma("sp", ys_s[NE * CAP:NE * CAP + 128, :], ys_r[2].ap, r=[ys_r[2].t])
    def load_expert(e_i):
        wi_, wo_, xs_ = wi_r[e_i % 2], wo_r[e_i % 3], xs_r[e_i % 2]
        sgi, sgo = wis_r[e_i % 2], wos_r[e_i % 2]
        dma("sp", xs_.ap, xs_s[e_i * CAP:(e_i + 1) * CAP, :].rearrange("(st p) d -> p st d", p=128), w=[xs_.t])
        dma("sp", sgi.ap, wei_d[e_i].rearrange("(kc p) f -> p kc f", p=128), w=[sgi.t])
        dma("sp", sgo.ap, weo_d[e_i].rearrange("(fc p) n -> p fc n", p=128), w=[sgo.t])

    def cast_expert(e_i):
        wi_, wo_ = wi_r[e_i % 2], wo_r[e_i % 3]
        sgi, sgo = wis_r[e_i % 2], wos_r[e_i % 2]
        cp("pool", wi_.ap[:, 0:3, :], sgi.ap[:, 0:3, :], r=[sgi.t], w=[wi_.t])
        cp("dve", wi_.ap[:, 3:8, :], sgi.ap[:, 3:8, :], r=[sgi.t], w=[wi_.t])
        cp("act", wo_.ap, sgo.ap, r=[sgo.t], w=[wo_.t])

    def stage_T(e_i):
        xs_, xsT_ = xs_r[e_i % 2], xsT_r[e_i % 2]
        for st in range(4):
            bk = 4 + st % 2
            for kc in range(8):
                tr(psb(bk)[:, kc * 128:(kc + 1) * 128], xs_.ap[:, st, kc * 128:(kc + 1) * 128], ident.ap, r=[xs_.t, ident.t], w=[pst[bk]])
            cp("act" if st % 2 else "dve", xsT_.ap[:, :, st * 128:(st + 1) * 128], psb(bk).rearrange("p (a b) -> p a b", a=8), r=[pst[bk]], w=[xsT_.t])

    def stage_H(e_i):
        wi_, xsT_, aT_ = wi_r[e_i % 2], xsT_r[e_i % 2], aT_r[e_i % 2]
        for p_ in range(2):
            bG = next_bank4()
            for kc in range(8):
                mm(ps[bG][:, :], wi_.ap[:, kc, p_ * 128:(p_ + 1) * 128], xsT_.ap[:, kc, :], kc == 0, kc == 7, r=[wi_.t, xsT_.t], w=[pst[bG]])
            bU = next_bank4()
            for kc in range(8):
                mm(ps[bU][:, :], wi_.ap[:, kc, 256 + p_ * 128:256 + (p_ + 1) * 128], xsT_.ap[:, kc, :], kc == 0, kc == 7, r=[wi_.t, xsT_.t], w=[pst[bU]])
            sg_ = sg_r[p_]
            act(sg_.ap, ps[bG][:, :], AF.Silu, r=[pst[bG]], w=[sg_.t])
            tt("dve", aT_.ap[:, p_, :], sg_.ap, ps[bU][:, :], ALU.mult, r=[sg_.t, pst[bU]], w=[aT_.t])

    def stage_Y(e_i):
        wo_, aT_ = wo_r[e_i % 3], aT_r[e_i % 2]
        for st in range(4):
            yb = ys_r[nys["n"] % 3]
            nys["n"] += 1
            for half in range(2):
                hs = slice(half * 512, (half + 1) * 512)
                bk = next_bank4()
                for fc in range(2):
                    mm(ps[bk][:, :], aT_.ap[:, fc, st * 128:(st + 1) * 128], wo_.ap[:, fc, hs], fc == 0, fc == 1, r=[aT_.t, wo_.t], w=[pst[bk]])
                cp("act" if half else "dve", yb.ap[:, hs], ps[bk][:, :], r=[pst[bk]], w=[yb.t])
            r0_ = e_i * CAP + st * 128
            dma("sp", ys_s[r0_:r0_ + 128, :], yb.ap, r=[yb.t])

    load_expert(0)
    load_expert(1)
    cast_expert(0)
    cast_expert(1)
    stage_T(0)
    stage_T(1)
    stage_H(0)
    for e_i in range(NE):
        if e_i + 2 < NE:
            load_expert(e_i + 2)
            stage_T(e_i + 2)
        if e_i + 1 < NE:
            stage_H(e_i + 1)
        stage_Y(e_i)
        if e_i + 2 < NE:
            cast_expert(e_i + 2)
    K.barrier()
    AR.release(m_4)
    gfb = B([D], F32, "gfb")
    dma("sp", gfb.ap, gf_d.partition_broadcast(128), w=[gfb.t])
    Y0r = ring(3, [D], BF16, "Y0")
    Y1r = ring(3, [D], BF16, "Y1")
    h2l = ring(3, [D], F32, "h2l")
    h3r = ring(3, [D], F32, "h3")
    outr = ring(2, [D], F32, "outb")
    junk4 = B([D], F32, "junk4")
    ms3 = B([NT], F32, "ms3")
    rs3 = B([NT], F32, "rs3")
    for b_ in Y0r + Y1r:
        mset("pool", b_.ap, 0.0, w=[b_.t])
    ms3_t = [K.tok() for _ in range(NT)]
    rs3_t = [K.tok() for _ in range(NT)]

    def fin_front(i):
        Y0, Y1, h2_, h3 = Y0r[i % 3], Y1r[i % 3], h2l[i % 3], h3r[i % 3]
        for k_, Y in enumerate((Y0, Y1)):
            K.op("pool", lambda e, i=i, k_=k_, Y=Y: e.indirect_dma_start(
                out=Y.ap, out_offset=None, in_=ys_s[:, :], in_offset=bass.IndirectOffsetOnAxis(ap=rt_pos.ap[:, 2 * i + k_:2 * i + k_ + 1], axis=0)),
                r=[rt_pos.t], w=[Y.t], dma=True)
        dma("sp", h2_.ap, h2_s[i * 128:(i + 1) * 128, :], w=[h2_.t])
        stt(h3.ap, Y0.ap, rt_gate.ap[:, 2 * i:2 * i + 1], h2_.ap, ALU.mult, ALU.add, r=[Y0.t, rt_gate.t, h2_.t], w=[h3.t])
        stt(h3.ap, Y1.ap, rt_gate.ap[:, 2 * i + 1:2 * i + 2], h3.ap, ALU.mult, ALU.add, r=[Y1.t, rt_gate.t, h3.t], w=[h3.t])
        stt(junk4.ap, h3.ap, 1.0 / D, h3.ap, ALU.mult, ALU.mult, r=[h3.t], w=[junk4.t, ms3_t[i]], accum=ms3.ap[:, i:i + 1])
        ts("pool", rs3.ap[:, i:i + 1], ms3.ap[:, i:i + 1], EPS, ALU.add, r=[ms3_t[i]], w=[rs3_t[i]])
        tt("pool", rs3.ap[:, i:i + 1], rs3.ap[:, i:i + 1], col(cst, C_NH), ALU.pow, r=[rs3_t[i], cst.t], w=[rs3_t[i]])

    def fin_back(i):
        h3, ob = h3r[i % 3], outr[i % 2]
        stt(ob.ap, h3.ap, rs3.ap[:, i:i + 1], gfb.ap, ALU.mult, ALU.mult, r=[h3.t, rs3_t[i], gfb.t], w=[ob.t])
        dma("sp", out_d[i * 128:(i + 1) * 128, :], ob.ap, r=[ob.t])

    for i in range(NT + 1):
        if i < NT:
            fin_front(i)
        if i >= 1:
            fin_back(i - 1)
    return finish()


def make_in_maps(inp, cores):
    c, m = host_consts()
    maps = []
    for b in cores:
        pos = np.ascontiguousarray(inp["positions"][b]).astype(np.int32)
        maps.append({
            "x": np.ascontiguousarray(inp["x"][b], dtype=np.float32),
            "pos_row": pos.reshape(1, L),
            "pos_col": np.ascontiguousarray(pos.reshape(NT, 128).T),
            "norm1_g": np.ascontiguousarray(inp["norm1_g"][0]).reshape(1, D),
            "w_in": np.ascontiguousarray(inp["w_in"][0]),
            "sgu_norm_g": np.ascontiguousarray(inp["sgu_norm_g"][0]).reshape(1, D),
            "sgu_w": np.ascontiguousarray(inp["sgu_w"][0]),
            "sgu_b": np.ascontiguousarray(inp["sgu_b"][0]),
            "w_branch_a": np.ascontiguousarray(inp["w_branch_a"][0]),
            "w_branch_b": np.ascontiguousarray(inp["w_branch_b"][0]),
            "w_out": np.ascontiguousarray(inp["w_out"][0]),
            "norm2_g": np.ascontiguousarray(inp["norm2_g"][0]).reshape(1, D),
            "w_router_group": np.ascontiguousarray(inp["w_router_group"][0]),
            "b_router_group": np.ascontiguousarray(inp["b_router_group"][0]).reshape(1, 4),
            "w_router_expert": np.ascontiguousarray(inp["w_router_expert"][0]),
            "b_router_expert": np.ascontiguousarray(inp["b_router_expert"][0]).reshape(1, 32),
            "w_expert_in": np.ascontiguousarray(inp["w_expert_in"][0]),
            "w_expert_out": np.ascontiguousarray(inp["w_expert_out"][0]),
            "norm_f_g": np.ascontiguousarray(inp["norm_f_g"]).reshape(1, D),
            "cst": c,
            "cmat": m,
        })
    return maps


def kernel(**inputs):
    P = build_program()
    maps = make_in_maps(inputs, list(range(8)))
    res = run_bass_kernel_spmd(P.nc, maps, core_ids=list(range(8)))
    return np.stack([np.asarray(r["out"], dtype=np.float32) for r in res.results], axis=0)
```
